# Optimizing a Trainium2 kernel written in Bass

```python
import math
import jax, jax.numpy as jnp
from jax import lax
import numpy as np

D_MODEL = 1024
BATCH = 8
SEQ = 4096
DEPTH = 4

CTX_LEN = 256
GRID_W = 64
HEAD_DIM = 64
ROPE_BASE = 10000.0
EPS = 1e-6
BLOCK = 128
DIFF_HEADS = D_MODEL // (2 * HEAD_DIM)
DIFF_V_DIM = 2 * HEAD_DIM
HY_WIDTH = D_MODEL
HY_BANDS = 16
HY_EMB_DIM = 1 + 2 * HY_BANDS
HY_FF = 64
HY_FAST_DECAY = 0.3
HY_SLOW_DECAY = 1.5
HY_TARGET = 1e-2
SHORT_CONV = 3
SWA_Q_HEADS = D_MODEL // HEAD_DIM
SWA_KV_HEADS = 4
SWA_GROUP = SWA_Q_HEADS // SWA_KV_HEADS
WINDOW = 128
N_BRANCH = 3
N_EXPERTS = 16
D_EXPERT = 1024
CAPACITY_FACTOR = 2
IN_WIDTHS = (DIFF_HEADS * 2 * HEAD_DIM, DIFF_HEADS * 2 * HEAD_DIM, DIFF_HEADS * DIFF_V_DIM,
             3 * HY_WIDTH, SWA_Q_HEADS * HEAD_DIM, SWA_KV_HEADS * HEAD_DIM, SWA_KV_HEADS * HEAD_DIM,
             N_BRANCH * D_MODEL)
D_IN = sum(IN_WIDTHS)

kernel_name = "hybrid_diffattn_hyena_swa_ecmoe_dit"


def rmsnorm(x, g):
    x32 = x.astype(jnp.float32)
    y = x32 * lax.rsqrt(jnp.mean(x32 * x32, axis=-1, keepdims=True) + EPS)
    return y.astype(x.dtype) * g


def modulate(h, shift, scale):
    return h * (1 + scale) + shift


def split_in(proj):
    offs, acc = [], 0
    for w in IN_WIDTHS[:-1]:
        acc += w
        offs.append(acc)
    return jnp.split(proj, offs, axis=-1)


def axial_rope(n_tok, dtype):
    rows = n_tok // GRID_W
    row = jnp.broadcast_to(jnp.arange(rows, dtype=jnp.float32)[:, None], (rows, GRID_W)).reshape(-1)
    col = jnp.broadcast_to(jnp.arange(GRID_W, dtype=jnp.float32)[None, :], (rows, GRID_W)).reshape(-1)
    n_freq = HEAD_DIM // 4
    inv = ROPE_BASE ** (-jnp.arange(n_freq, dtype=jnp.float32) / n_freq)
    ang = jnp.stack([row[:, None] * inv, col[:, None] * inv], axis=1)
    return jnp.cos(ang).astype(dtype), jnp.sin(ang).astype(dtype)


def apply_rope(x, cos, sin):
    shape = x.shape
    xr = x.reshape(shape[:-1] + (2, 2, HEAD_DIM // 4))
    bshape = (shape[1],) + (1,) * (x.ndim - 3) + (2, HEAD_DIM // 4)
    c, s = cos.reshape(bshape), sin.reshape(bshape)
    x1, x2 = xr[..., 0, :], xr[..., 1, :]
    return jnp.stack([x1 * c - x2 * s, x2 * c + x1 * s], axis=-2).reshape(shape)


def diff_attend(q, k, v, lam):
    s = jnp.einsum('bqhmd,bkhmd->bhmqk', q, k).astype(jnp.float32) * HEAD_DIM ** -0.5
    a = jax.nn.softmax(s, axis=-1)
    w = a[:, :, 0] - lam * a[:, :, 1]
    return jnp.einsum('bhqk,bkhe->bqhe', w.astype(v.dtype), v)


def diff_latent(q, k_all, v_all, lam):
    B, L = q.shape[:2]
    nb = L // BLOCK
    qb = jnp.moveaxis(q.reshape((B, nb, BLOCK) + q.shape[2:]), 1, 0)
    o = lax.map(lambda qi: diff_attend(qi, k_all, v_all, lam), qb)
    return jnp.moveaxis(o, 0, 1).reshape(B, L, DIFF_HEADS, DIFF_V_DIM)


def diff_post(o, g, lambda_init):
    return (rmsnorm(o, g) * (1.0 - lambda_init)).reshape(o.shape[0], o.shape[1], -1)


def short_conv(u, w, b):
    L = u.shape[1]
    up = jnp.pad(u, ((0, 0), (SHORT_CONV // 2, SHORT_CONV // 2), (0, 0)))
    return sum(up[:, j:j + L] * w[j] for j in range(SHORT_CONV)) + b


def hyena_filter(L, p):
    f32 = jnp.float32
    t = jnp.linspace(0.0, 1.0, L, dtype=f32)[:, None]
    w = 2.0 * math.pi * jnp.arange(L, dtype=f32)[:, None] / L
    f = jnp.linspace(1e-4, HY_BANDS - 1, HY_BANDS, dtype=f32)[None, :]
    emb = jnp.concatenate([t, jnp.cos(f * w), -jnp.sin(f * w)], axis=-1)
    freq = p['hy_sin_freq'].astype(f32)
    z = jnp.sin(freq * (emb @ p['hy_ff_w1'].astype(f32) + p['hy_ff_b1'].astype(f32)))
    z = jnp.sin(freq * (z @ p['hy_ff_w2'].astype(f32) + p['hy_ff_b2'].astype(f32)))
    h = (z @ p['hy_ff_w3'].astype(f32)).reshape(L, 2, HY_WIDTH)
    deltas = jnp.abs(jnp.linspace(math.log(HY_TARGET) / HY_FAST_DECAY,
                                  math.log(HY_TARGET) / HY_SLOW_DECAY, HY_WIDTH, dtype=f32))
    h = h * jnp.exp(-t * deltas)[:, None, :]
    return h / (jnp.sum(jnp.abs(h), axis=(0, 1), keepdims=True) + EPS)


def fft_long_conv(u, h, bias):
    L = u.shape[1]
    k = jnp.concatenate([h[:, 0], jnp.zeros((1, HY_WIDTH), jnp.float32), h[:0:-1, 1]], axis=0)
    u32 = u.astype(jnp.float32)
    uf = jnp.fft.rfft(u32, n=2 * L, axis=1)
    kf = jnp.fft.rfft(k, n=2 * L, axis=0)
    y = jnp.fft.irfft(uf * kf[None], n=2 * L, axis=1)[:, :L]
    return (y + u32 * bias.astype(jnp.float32)).astype(u.dtype)


def hyena(u_in, p):
    L = u_in.shape[1]
    u = short_conv(u_in, p['hy_conv_w'], p['hy_conv_b'])
    x0, x1, v = jnp.split(u, 3, axis=-1)
    return x0 * fft_long_conv(x1 * v, hyena_filter(L, p), p['hy_bias'])


def attend_sink(q, ks, vs, masks, sink):
    B, Lq = q.shape[:2]
    parts = []
    for k, m in zip(ks, masks):
        s = jnp.einsum('bqhgd,bkhd->bhgqk', q, k).astype(jnp.float32) * HEAD_DIM ** -0.5
        parts.append(s if m is None else jnp.where(m, s, -jnp.inf))
    sink_col = jnp.broadcast_to(sink.astype(jnp.float32)[None, :, :, None, None],
                                (B, SWA_KV_HEADS, SWA_GROUP, Lq, 1))
    pr = jax.nn.softmax(jnp.concatenate(parts + [sink_col], axis=-1), axis=-1)
    out, off = 0, 0
    for v in vs:
        n = v.shape[1]
        out = out + jnp.einsum('bhgqk,bkhd->bqhgd', pr[..., off:off + n].astype(v.dtype), v)
        off += n
    return out


def swa_latent(q, k, v, kc, vc, sink):
    B, L = q.shape[:2]
    nb = L // BLOCK
    pad = ((0, 0), (BLOCK, BLOCK), (0, 0), (0, 0))
    kp, vp = jnp.pad(k, pad), jnp.pad(v, pad)

    def block(i):
        start = i * BLOCK
        qb = lax.dynamic_slice_in_dim(q, start, BLOCK, axis=1)
        kb = lax.dynamic_slice_in_dim(kp, start, 3 * BLOCK, axis=1)
        vb = lax.dynamic_slice_in_dim(vp, start, 3 * BLOCK, axis=1)
        q_pos = start + jnp.arange(BLOCK)
        k_pos = start - BLOCK + jnp.arange(3 * BLOCK)
        valid = ((jnp.abs(k_pos[None, :] - q_pos[:, None]) <= WINDOW)
                 & (k_pos >= 0)[None, :] & (k_pos < L)[None, :])
        return attend_sink(qb, [kb, kc], [vb, vc], [valid, None], sink)

    o = lax.map(block, jnp.arange(nb))
    return jnp.moveaxis(o, 0, 1).reshape(B, L, SWA_Q_HEADS * HEAD_DIM)


def merge(ys, gate_pre, p):
    gates = jax.nn.sigmoid(gate_pre.astype(jnp.float32)).astype(gate_pre.dtype)
    gs = jnp.split(gates, N_BRANCH, axis=-1)
    merged = sum(gs[b] * (ys[b] @ p['w_branch'][b]) for b in range(N_BRANCH))
    return merged @ p['w_out']


def token_mixer(h, hc, p, lambda_init, ctx_out):
    B, L, _ = h.shape
    Lc = hc.shape[1]
    dq, dk, dv, hy, sq, sk, sv, gt = split_in(h @ p['w_in'])
    dqc, dkc, dvc, hyc, sqc, skc, svc, gtc = split_in(hc @ p['w_in'])
    cos, sin = axial_rope(L, h.dtype)
    lp = p['diff_lambda'].astype(jnp.float32)
    lam = jnp.exp(jnp.sum(lp[0] * lp[1])) - jnp.exp(jnp.sum(lp[2] * lp[3])) + lambda_init
    qk_shape = (DIFF_HEADS, 2, HEAD_DIM)
    q = apply_rope(dq.reshape((B, L) + qk_shape), cos, sin)
    k = apply_rope(dk.reshape((B, L) + qk_shape), cos, sin)
    v = dv.reshape(B, L, DIFF_HEADS, DIFF_V_DIM)
    kc = dkc.reshape((B, Lc) + qk_shape)
    vc = dvc.reshape(B, Lc, DIFF_HEADS, DIFF_V_DIM)
    y_diff = diff_post(diff_latent(q, jnp.concatenate([k, kc], axis=1), jnp.concatenate([v, vc], axis=1), lam),
                       p['diff_subln_g'], lambda_init)
    y_hy = hyena(hy, p)
    sink = p['swa_sink'].reshape(SWA_KV_HEADS, SWA_GROUP)
    s_q = apply_rope(sq.reshape(B, L, SWA_KV_HEADS, SWA_GROUP, HEAD_DIM), cos, sin)
    s_k = apply_rope(sk.reshape(B, L, SWA_KV_HEADS, HEAD_DIM), cos, sin)
    s_v = sv.reshape(B, L, SWA_KV_HEADS, HEAD_DIM)
    s_kc = skc.reshape(B, Lc, SWA_KV_HEADS, HEAD_DIM)
    s_vc = svc.reshape(B, Lc, SWA_KV_HEADS, HEAD_DIM)
    y_swa = swa_latent(s_q, s_k, s_v, s_kc, s_vc, sink)
    y = merge([y_diff, y_hy, y_swa], gt, p)
    if not ctx_out:
        return y, None
    qc = dqc.reshape((B, Lc) + qk_shape)
    yc_diff = diff_post(diff_attend(qc, kc, vc, lam), p['diff_subln_g'], lambda_init)
    yc_hy = hyena(hyc, p)
    yc_swa = attend_sink(sqc.reshape(B, Lc, SWA_KV_HEADS, SWA_GROUP, HEAD_DIM), [s_kc], [s_vc], [None],
                         sink).reshape(B, Lc, SWA_Q_HEADS * HEAD_DIM)
    yc = merge([yc_diff, yc_hy, yc_swa], gtc, p)
    return y, yc


def ec_moe(h, p):
    B, N, D = h.shape
    cap = CAPACITY_FACTOR * N // N_EXPERTS
    logits = jnp.einsum('bnd,de->bne', h, p['router_w']).astype(jnp.float32)
    aff = jax.nn.softmax(logits, axis=-1)
    g, idx = lax.top_k(jnp.swapaxes(aff, 1, 2), cap)
    flat = idx.reshape(B, N_EXPERTS * cap)
    xg = jnp.take_along_axis(h, flat[:, :, None], axis=1).reshape(B, N_EXPERTS, cap, D)
    a = jnp.einsum('becd,edf->becf', xg, p['moe_w1'])
    b = jnp.einsum('becd,edf->becf', xg, p['moe_w3'])
    y = jnp.einsum('becf,efd->becd', jax.nn.silu(a) * b, p['moe_w2']) * g[..., None].astype(h.dtype)
    return jnp.zeros_like(h).at[jnp.arange(B)[:, None], flat].add(y.reshape(B, N_EXPERTS * cap, D))


def _layer(x, xc, sc, sc_ctx, p, lambda_init, ctx_out):
    mod = jnp.split((sc @ p['w_mod'] + p['b_mod'])[:, None, :], 6, axis=-1)
    modc = jnp.split(sc_ctx @ p['w_mod'] + p['b_mod'], 6, axis=-1)
    h = modulate(rmsnorm(x, p['norm1_g']), mod[0], mod[1])
    hc = modulate(rmsnorm(xc, p['norm1_g']), modc[0], modc[1])
    y, yc = token_mixer(h, hc, p, lambda_init, ctx_out)
    x = x + mod[2] * y
    h2 = modulate(rmsnorm(x, p['norm2_g']), mod[3], mod[4])
    x = x + mod[5] * ec_moe(h2, p)
    if ctx_out:
        xc = xc + modc[2] * yc
        h2c = modulate(rmsnorm(xc, p['norm2_g']), modc[3], modc[4])
        xc = xc + modc[5] * ec_moe(h2c, p)
    return x, xc


def setup_inputs(seed: int = 0) -> dict:
    key = jax.random.key(seed)
    ks = jax.random.split(key, 28)
    D, L = D_MODEL, DEPTH

    def nrm(k, shape, s):
        return jax.random.normal(k, shape, jnp.float32) * s

    return {
        'x': nrm(ks[0], (BATCH, SEQ, D), 1.0),
        'c': nrm(ks[1], (BATCH, D), 1.0),
        'ctx': nrm(ks[2], (BATCH, CTX_LEN, D), 1.0),
        'c_ctx': nrm(ks[3], (D,), 1.0),
        'w_mod': nrm(ks[4], (L, D, 6 * D), 0.5 * D ** -0.5),
        'b_mod': nrm(ks[5], (L, 6 * D), 0.02),
        'norm1_g': 1.0 + nrm(ks[6], (L, D), 0.02),
        'norm2_g': 1.0 + nrm(ks[7], (L, D), 0.02),
        'w_in': nrm(ks[8], (L, D, D_IN), D ** -0.5),
        'diff_lambda': nrm(ks[9], (L, 4, HEAD_DIM), 0.1),
        'diff_subln_g': 1.0 + nrm(ks[10], (L, DIFF_V_DIM), 0.02),
        'hy_conv_w': nrm(ks[11], (L, SHORT_CONV, 3 * HY_WIDTH), SHORT_CONV ** -0.5),
        'hy_conv_b': nrm(ks[12], (L, 3 * HY_WIDTH), 0.02),
        'hy_ff_w1': nrm(ks[13], (L, HY_EMB_DIM, HY_FF), HY_EMB_DIM ** -0.5),
        'hy_ff_b1': nrm(ks[14], (L, HY_FF), 0.02),
        'hy_ff_w2': nrm(ks[15], (L, HY_FF, HY_FF), HY_FF ** -0.5),
        'hy_ff_b2': nrm(ks[16], (L, HY_FF), 0.02),
        'hy_ff_w3': nrm(ks[17], (L, HY_FF, 2 * HY_WIDTH), HY_FF ** -0.5),
        'hy_sin_freq': 1.0 + nrm(ks[18], (L, HY_FF), 0.02),
        'hy_bias': nrm(ks[19], (L, HY_WIDTH), 1.0),
        'swa_sink': nrm(ks[20], (L, SWA_Q_HEADS), 0.5),
        'w_branch': nrm(ks[21], (L, N_BRANCH, D, D), D ** -0.5),
        'w_out': nrm(ks[22], (L, D, D), D ** -0.5),
        'router_w': nrm(ks[23], (L, D, N_EXPERTS), D ** -0.5),
        'moe_w1': nrm(ks[24], (L, N_EXPERTS, D, D_EXPERT), D ** -0.5),
        'moe_w3': nrm(ks[25], (L, N_EXPERTS, D, D_EXPERT), D ** -0.5),
        'moe_w2': nrm(ks[26], (L, N_EXPERTS, D_EXPERT, D), D_EXPERT ** -0.5),
        'final_g': 1.0 + nrm(ks[27], (D,), 0.02),
    }


def reference(x, c, ctx, c_ctx, w_mod, b_mod, norm1_g, norm2_g, w_in, diff_lambda, diff_subln_g,
              hy_conv_w, hy_conv_b, hy_ff_w1, hy_ff_b1, hy_ff_w2, hy_ff_b2, hy_ff_w3, hy_sin_freq,
              hy_bias, swa_sink, w_branch, w_out, router_w, moe_w1, moe_w3, moe_w2, final_g):
    sc = jax.nn.silu(c)
    sc_ctx = jax.nn.silu(c_ctx)
    xc = ctx
    for l in range(DEPTH):
        p = {'w_mod': w_mod[l], 'b_mod': b_mod[l], 'norm1_g': norm1_g[l], 'norm2_g': norm2_g[l],
             'w_in': w_in[l], 'diff_lambda': diff_lambda[l], 'diff_subln_g': diff_subln_g[l],
             'hy_conv_w': hy_conv_w[l], 'hy_conv_b': hy_conv_b[l], 'hy_ff_w1': hy_ff_w1[l],
             'hy_ff_b1': hy_ff_b1[l], 'hy_ff_w2': hy_ff_w2[l], 'hy_ff_b2': hy_ff_b2[l],
             'hy_ff_w3': hy_ff_w3[l], 'hy_sin_freq': hy_sin_freq[l], 'hy_bias': hy_bias[l],
             'swa_sink': swa_sink[l], 'w_branch': w_branch[l], 'w_out': w_out[l],
             'router_w': router_w[l], 'moe_w1': moe_w1[l], 'moe_w3': moe_w3[l], 'moe_w2': moe_w2[l]}
        lambda_init = 0.8 - 0.6 * math.exp(-0.3 * l)
        x, xc = _layer(x, xc, sc, sc_ctx, p, lambda_init, l < DEPTH - 1)
    return rmsnorm(x, final_g)
```

```python
import contextlib
import math
import numpy as np
import ml_dtypes
import concourse.bass as bass
import concourse.mybir as mybir
from concourse.bass_utils import run_bass_kernel_spmd

F32 = mybir.dt.float32
BF16 = mybir.dt.bfloat16
AF = mybir.ActivationFunctionType
ALU = mybir.AluOpType
AX = mybir.AxisListType

D = 1024
L = 4096
LC = 256
T = L + LC
DEPTH = 4
D_IN = 10752
NE = 16
CAP = 512
CAPC = 32
EPS = 1e-6
NF = 4224
NFC = 384
TCH = [(i * 512, 512) for i in range(8)] + [(L, LC)]


class Buf:
    __slots__ = ("name", "w", "r", "sem", "cnt")

    def __init__(self, name):
        self.name = name
        self.w = {}
        self.r = {}
        self.sem = None
        self.cnt = 0


class Sched:
    def __init__(self, nc, es):
        self.nc, self.es = nc, es
        self.E = {}
        for n, h in (("pe", nc.tensor), ("dve", nc.vector), ("act", nc.scalar),
                     ("pool", nc.gpsimd), ("sp", nc.sync)):
            self.E[n] = dict(h=h, sem=es.enter_context(nc.semaphore("s_" + n)), cnt=0, seen={})
        self.dma_bufs = []
        self.nbuf = 0
        self.persist = True
        self.pool = []

    def buf(self, name):
        self.nbuf += 1
        b = Buf("%s_%d" % (name, self.nbuf))
        b.w["_persist"] = self.persist
        return b

    @staticmethod
    def _add(evs, d):
        for k, sv in d.items():
            if k == "_persist":
                continue
            sem, v = sv
            if k not in evs or evs[k][1] < v:
                evs[k] = (sem, v)

    def _waits(self, e, evs):
        E = self.E[e]
        for name, (sem, val) in evs.items():
            if E["seen"].get(name, 0) < val:
                E["h"].wait_ge(sem, val)
                E["seen"][name] = val

    def op(self, e, fn, reads=(), writes=(), skip_self=False):
        evs = {}
        for b in reads:
            self._add(evs, b.w)
        for b in writes:
            self._add(evs, b.w)
            self._add(evs, b.r)
        if skip_self:
            evs.pop("s_" + e, None)
        self._waits(e, evs)
        E = self.E[e]
        ins = fn(E["h"])
        E["cnt"] += 1
        ins.then_inc(E["sem"], 1)
        key, ev = "s_" + e, (E["sem"], E["cnt"])
        for b in reads:
            b.r[key] = ev
        for b in writes:
            b.w[key] = ev

    def dma(self, q, out_ap, in_ap, dst, srcs=(), **kw):
        evs = {}
        self._add(evs, dst.w)
        self._add(evs, dst.r)
        for b in srcs:
            self._add(evs, b.w)
        self._waits(q, evs)
        if dst.sem is None:
            if self.pool and not dst.w["_persist"]:
                dst.name, dst.sem, dst.cnt = self.pool.pop()
            else:
                dst.sem = self.es.enter_context(self.nc.semaphore("d_" + dst.name))
            self.dma_bufs.append(dst)
        ins = self.E[q]["h"].dma_start(out=out_ap, in_=in_ap, **kw)
        dst.cnt += 16
        ins.then_inc(dst.sem, 16)
        key, ev = "d_" + dst.name, (dst.sem, dst.cnt)
        dst.w[key] = ev
        for b in srcs:
            b.r[key] = ev

    def barrier(self):
        evs = {}
        for n, E in self.E.items():
            if E["cnt"]:
                evs["s_" + n] = (E["sem"], E["cnt"])
        for b in self.dma_bufs:
            evs["d_" + b.name] = (b.sem, b.cnt)
        for n in self.E:
            self._waits(n, evs)
        keep = []
        for b in self.dma_bufs:
            if b.w["_persist"]:
                keep.append(b)
            else:
                self.pool.append((b.name, b.sem, b.cnt))
        self.dma_bufs = keep

    def barrier_known(self):
        pass


class Ctx:
    pass


def _mm_group(S, ps, out_ap, pairs, reads):
    def fn(h):
        ins = None
        n = len(pairs)
        for i, (a, b) in enumerate(pairs):
            ins = h.matmul(out_ap, a, b, start=(i == 0), stop=(i == n - 1))
        return ins
    S.op("pe", fn, reads=reads, writes=[ps])


def _mm1(S, ps, out_ap, a, b, reads, start, stop):
    S.op("pe", lambda h: h.matmul(out_ap, a, b, start=start, stop=stop), reads=reads, writes=[ps], skip_self=True)


def build(cfg):
    nl = cfg["nl"]
    debug = cfg.get("debug", ())
    stop_after = cfg.get("stop_after", None)
    nc = bass.Bass("TRN2", target_bir_lowering=False)
    es = contextlib.ExitStack()
    K = Ctx()
    K.nc, K.es, K.cfg = nc, es, cfg
    S = Sched(nc, es)
    K.S = S

    def din(name, shape, dt=F32):
        return nc.dram_tensor(name, list(shape), dt, kind="ExternalInput").ap()

    def scratch(name, shape, dt):
        kind = "ExternalOutput" if name in debug else "Internal"
        return nc.dram_tensor(name, list(shape), dt, kind=kind).ap()

    I = {}
    I["xT"] = din("xT", [D, T])
    I["ccT"] = din("ccT", [128, 8, 2])
    I["w_mod"] = din("w_mod", [nl, D, 6 * D])
    I["b_modT"] = din("b_modT", [nl, 128, 48])
    I["g1T"] = din("g1T", [nl, 128, 8])
    I["g2T"] = din("g2T", [nl, 128, 8])
    I["w_in"] = din("w_in", [nl, D, D_IN])
    I["ropeC"] = din("ropeC", [128, L])
    I["ropeS"] = din("ropeS", [128, L])
    I["ident"] = din("ident", [128, 128])
    I["diff_lambda"] = din("diff_lambda", [nl, 256])
    I["sublnT"] = din("sublnT", [nl, 128, 1])
    I["swa_sink"] = din("swa_sink", [nl, 16])
    I["trilo"] = din("trilo", [128, 128])
    I["triup"] = din("triup", [128, 128])
    I["w_branch"] = din("w_branch", [nl, 3, D, D])
    I["w_out"] = din("w_out", [nl, D, D])
    I["fgT"] = din("fgT", [128, 8])
    if "moe" in cfg.get("phases", ()):
        I["router_w"] = din("router_w", [nl, D, NE])
        I["moe_w1"] = din("moe_w1", [nl, NE, D, D])
        I["moe_w3"] = din("moe_w3", [nl, NE, D, D])
        I["moe_w2"] = din("moe_w2", [nl, NE, D, D])
        I["selm"] = din("selm", [16, 16 * 128])
        I["slotidx"] = din("slotidx", [128, 5])
        I["iota512"] = din("iota512", [128, 512])
        I["ustrict"] = din("ustrict", [128, 128])
    if "hy" in cfg.get("phases", ()):
        I["cwT"] = din("cwT", [nl, 128, 24, 3])
        I["cbT"] = din("cbT", [nl, 128, 24])
        I["hbT"] = din("hbT", [nl, 128, 8])
        I["hy_w1"] = din("hy_w1", [nl, 33, 64])
        I["hy_w2"] = din("hy_w2", [nl, 64, 64])
        I["hy_w3"] = din("hy_w3", [nl, 64, 2048])
        I["hy_b1"] = din("hy_b1", [nl, 64, 1])
        I["hy_b2"] = din("hy_b2", [nl, 64, 1])
        I["hy_fr"] = din("hy_fr", [nl, 64, 1])
        I["embT"] = din("embT", [33, L])
        I["embTc"] = din("embTc", [33, LC])
        I["negt"] = din("negt", [128, 32])
        I["negtc"] = din("negtc", [128, 2])
        I["deltas"] = din("deltas", [1, D])
        I["dftC"] = din("dftC", [NF, NF], BF16)
        I["dftS"] = din("dftS", [NF, NF], BF16)
        I["dftCc"] = din("dftCc", [NFC, NFC], BF16)
        I["dftSc"] = din("dftSc", [NFC, NFC], BF16)
        I["wf"] = din("wf", [128, NF // 128])
        I["wfc"] = din("wfc", [128, NFC // 128])
    K.I = I
    K.outT = nc.dram_tensor("outT", [D, L], F32, kind="ExternalOutput").ap()
    K.outT_b = S.buf("outT")

    R = {}
    R["XT"] = (scratch("XT", [D, T], F32), S.buf("XT"))
    R["QdT"] = (scratch("QdT", [D, T], BF16), S.buf("QdT"))
    R["KdT"] = (scratch("KdT", [D, T], BF16), S.buf("KdT"))
    R["Vd"] = (scratch("Vd", [T, D], BF16), S.buf("Vd"))
    R["HyT"] = (scratch("HyT", [3 * D, T], BF16), S.buf("HyT"))
    R["QsT"] = (scratch("QsT", [D, T], BF16), S.buf("QsT"))
    R["KsT"] = (scratch("KsT", [256, T], BF16), S.buf("KsT"))
    R["Vs"] = (scratch("Vs", [T, 256], BF16), S.buf("Vs"))
    R["GT"] = (scratch("GT", [3 * D, T], BF16), S.buf("GT"))
    R["YdT"] = (scratch("YdT", [D, T], BF16), S.buf("YdT"))
    R["YhT"] = (scratch("YhT", [D, T], BF16), S.buf("YhT"))
    R["YsT"] = (scratch("YsT", [D, T], BF16), S.buf("YsT"))
    R["X0T"] = (scratch("X0T", [D, T], BF16), S.buf("X0T"))
    R["UT"] = (scratch("UT", [D, T], BF16), S.buf("UT"))
    R["KF"] = (scratch("KF", [2, NF, D], F32), S.buf("KF"))
    R["KFc"] = (scratch("KFc", [2, NFC, D], F32), S.buf("KFc"))
    R["YF"] = (scratch("YF", [2, NF, D], BF16), S.buf("YF"))
    R["YFc"] = (scratch("YFc", [2, NFC, D], BF16), S.buf("YFc"))
    R["YE"] = (scratch("YE", [NE, CAP + CAPC, D], BF16), S.buf("YE"))
    R["XT1"] = (scratch("XT1", [D, T], F32), S.buf("XT1"))
    R["PT"] = (scratch("PT", [2, 16, T], F32), S.buf("PT"))
    R["LG"] = (scratch("LG", [128, T // 128, NE], F32), S.buf("LG"))
    R["H2d"] = (scratch("H2d", [128, T // 128, D], BF16), S.buf("H2d"))
    K.R = R

    uid = [0]

    def sb(name, shape, dt, stack=es):
        uid[0] += 1
        t = stack.enter_context(nc.sbuf_tensor("sb%d_%s" % (uid[0], name), list(shape), dt))
        return t, S.buf(name)

    def pst(name, shape, dt, stack):
        uid[0] += 1
        t = stack.enter_context(nc.psum_tensor("ps%d_%s" % (uid[0], name), list(shape), dt))
        return t, S.buf(name)
    K.sb, K.pst = sb, pst

    ident_f, ident_f_b = sb("ident_f", [128, 128], F32)
    ident_b, ident_b_b = sb("ident_b", [128, 128], BF16)
    ones_f, ones_f_b = sb("ones_f", [128, 128], F32)
    ones_b, ones_b_b = sb("ones_b", [128, 128], BF16)
    S.dma("sp", ident_f[:], I["ident"][:, :], ident_f_b)
    S.dma("pool", ident_b[:], I["ident"][:, :], ident_b_b)
    S.op("dve", lambda h: h.memset(ones_f[:], 1.0), writes=[ones_f_b])
    S.op("dve", lambda h: h.memset(ones_b[:], 1.0), writes=[ones_b_b])
    K.ident_f, K.ident_b, K.ones_f, K.ones_b = (ident_f, ident_f_b), (ident_b, ident_b_b), (ones_f, ones_f_b), (ones_b, ones_b_b)

    sc, sc_b = sb("sc", [128, 8, 2], F32)
    S.dma("sp", sc[:], I["ccT"][:, :, :], sc_b)
    S.op("act", lambda h: h.activation(out=sc[:], in_=sc[:], func=AF.Silu), reads=[sc_b], writes=[sc_b])
    K.sc = (sc, sc_b)
    modT, modT_b = sb("modT", [128, 48, 2], F32)
    K.modT = (modT, modT_b)
    A1, A1_b = sb("A1", [128, 8, 2], F32)
    A2, A2_b = sb("A2", [128, 8, 2], F32)
    K.A1, K.A2 = (A1, A1_b), (A2, A2_b)

    with contextlib.ExitStack() as ph:
        xc, xc_b = sb("xcp", [128, 8, 512], F32, ph)
        for (t0, tn) in TCH:
            S.dma("sp", xc[:, :, :tn], I["xT"].rearrange("(k p) t -> p k t", p=128)[:, :, t0:t0 + tn], xc_b)
            S.dma("sp", R["XT"][0].rearrange("(k p) t -> p k t", p=128)[:, :, t0:t0 + tn], xc[:, :, :tn], R["XT"][1], [xc_b])
        S.barrier()

    S.persist = False
    phases = cfg.get("phases", ("diff", "swa", "merge"))
    if "swa" not in phases:
        with contextlib.ExitStack() as ph:
            zt, zt_b = sb("zt2", [128, T], BF16, ph)
            S.op("dve", lambda h: h.memset(zt[:], 0.0), writes=[zt_b])
            for k in range(8):
                S.dma("sp", R["YsT"][0][k * 128:(k + 1) * 128, :], zt[:], R["YsT"][1], [zt_b])
            S.barrier()
    if "hy" not in phases:
        with contextlib.ExitStack() as ph:
            zt, zt_b = sb("zt", [128, T], BF16, ph)
            S.op("dve", lambda h: h.memset(zt[:], 0.0), writes=[zt_b])
            for k in range(8):
                S.dma("sp", R["YhT"][0][k * 128:(k + 1) * 128, :], zt[:], R["YhT"][1], [zt_b])
            S.barrier()
    for l in range(nl):
        ctx_out = l < DEPTH - 1
        phase_mod(K, l)
        with contextlib.ExitStack() as ph:
            hT, hT_b = sb("hT", [128, 8, T], BF16, ph)
            phase_norm(K, ph, R["XT"], K.A1, 0, (hT, hT_b))
            if stop_after == "norm1":
                dbg = scratch("dbg_hT", [D, T], BF16)
                S.dma("sp", dbg.rearrange("(k p) t -> p k t", p=128), hT[:], S.buf("dbg"), [hT_b])
                S.barrier()
                break
            phase_inproj(K, ph, l, (hT, hT_b))
            S.barrier()
        if stop_after == "inproj":
            break
        if "diff" in phases:
            phase_diff(K, l, ctx_out)
        if stop_after == "diff":
            break
        if "swa" in phases:
            phase_swa(K, l, ctx_out)
        if "hy" in phases:
            phase_hyena(K, l, ctx_out)
        if stop_after == "hy":
            break
        if stop_after == "swa":
            break
        if "merge" in phases:
            phase_merge(K, l)
        if stop_after == "merge":
            break
        if "moe" in phases:
            phase_moe(K, l, ctx_out)
        if stop_after == "moe":
            break
    if stop_after is None:
        phase_final(K)

    S.barrier()
    es.close()
    return nc


def phase_mod(K, l):
    nc, S, I = K.nc, K.S, K.I
    modT, modT_b = K.modT
    sc, sc_b = K.sc
    with contextlib.ExitStack() as ph:
        wts = [K.sb("wmod%d" % i, [128, 8, 512], F32, ph) for i in range(2)]
        bm, bm_b = K.sb("bmodT", [128, 48], F32, ph)
        g1, g1_b = K.sb("g1T", [128, 8], F32, ph)
        g2, g2_b = K.sb("g2T", [128, 8], F32, ph)
        ps, ps_b = K.pst("ps_mod", [128, 512], F32, ph)
        S.dma("sp", bm[:], I["b_modT"][l], bm_b)
        S.dma("sp", g1[:], I["g1T"][l], g1_b)
        S.dma("sp", g2[:], I["g2T"][l], g2_b)
        for g in range(12):
            wt, wt_b = wts[g % 2]
            S.dma("sp", wt[:], I["w_mod"][l].rearrange("(k p) n -> p k n", p=128)[:, :, g * 512:(g + 1) * 512], wt_b)
            for m in range(4):
                mc = g * 4 + m
                _mm_group(S, ps_b, ps[:, 0:2],
                          [(wt[:, k, m * 128:(m + 1) * 128], sc[:, k, :]) for k in range(8)],
                          [wt_b, sc_b])
                S.op("dve", lambda h, mc=mc: h.tensor_scalar(out=modT[:, mc, :], in0=ps[:, 0:2], scalar1=bm[:, mc:mc + 1],
                                                            scalar2=None, op0=ALU.add),
                     reads=[ps_b, bm_b], writes=[modT_b])
        for (A, A_b), (g, g_b), j in ((K.A1, (g1, g1_b), 1), (K.A2, (g2, g2_b), 4)):
            for r in range(2):
                S.op("dve", lambda h, A=A, g=g, j=j, r=r: h.scalar_tensor_tensor(
                    out=A[:, :, r], in0=modT[:, j * 8:(j + 1) * 8, r], scalar=1.0, in1=g[:], op0=ALU.add, op1=ALU.mult),
                    reads=[modT_b, g_b], writes=[A_b])
        S.barrier()


def phase_norm(K, ph, X, Acoef, shift_j, out, out_f32_cb=None):
    nc, S = K.nc, K.S
    XT, XT_b = X
    A, A_b = Acoef
    modT, modT_b = K.modT
    hT, hT_b = out
    ones_f, ones_f_b = K.ones_f
    with contextlib.ExitStack() as st:
        xs = [K.sb("nx%d" % i, [128, 8, 512], F32, st) for i in range(2)]
        sq, sq_b = K.sb("nsq", [128, 8, 512], F32, st)
        rstd, rstd_b = K.sb("nrstd", [128, 512], F32, st)
        tmp, tmp_b = K.sb("ntmp", [128, 512], F32, st)
        ps, ps_b = K.pst("ps_norm", [128, 512], F32, st)
        for ci, (t0, tn) in enumerate(TCH):
            r = 0 if t0 < L else 1
            x, x_b = xs[ci % 2]
            S.dma("sp", x[:, :, :tn], XT.rearrange("(k p) t -> p k t", p=128)[:, :, t0:t0 + tn], x_b, [XT_b])
            S.op("act", lambda h: h.activation(out=sq[:, :, :tn], in_=x[:, :, :tn], func=AF.Square), reads=[x_b], writes=[sq_b])
            _mm_group(S, ps_b, ps[:, :tn], [(ones_f[:], sq[:, k, :tn]) for k in range(8)], [ones_f_b, sq_b])
            S.op("dve", lambda h: h.tensor_scalar(out=rstd[:, :tn], in0=ps[:, :tn], scalar1=1.0 / D, scalar2=EPS,
                                                  op0=ALU.mult, op1=ALU.add), reads=[ps_b], writes=[rstd_b])
            S.op("act", lambda h: h.activation(out=rstd[:, :tn], in_=rstd[:, :tn], func=AF.Sqrt), reads=[rstd_b], writes=[rstd_b])
            S.op("dve", lambda h: h.reciprocal(out=rstd[:, :tn], in_=rstd[:, :tn]), reads=[rstd_b], writes=[rstd_b])
            for k in range(8):
                S.op("dve", lambda h, k=k: h.tensor_tensor(out=tmp[:, :tn], in0=x[:, k, :tn], in1=rstd[:, :tn], op=ALU.mult),
                     reads=[x_b, rstd_b], writes=[tmp_b])
                S.op("act", lambda h, k=k: h.activation(out=hT[:, k, t0:t0 + tn], in_=tmp[:, :tn], func=AF.Identity,
                                                        scale=A[:, k, r:r + 1], bias=modT[:, shift_j * 8 + k, r:r + 1]),
                     reads=[tmp_b, A_b, modT_b], writes=[hT_b])
                if out_f32_cb is not None:
                    out_f32_cb(ci, k, t0, tn, r, tmp, tmp_b)
        S.barrier()


def _inproj_groups():
    g = []
    g += [("qk", "QdT", 0), ("qk", "QdT", 512), ("qk", "KdT", 0), ("qk", "KdT", 512)]
    g += [("v", "Vd", 0), ("v", "Vd", 512)]
    g += [("plain", "HyT", i * 512) for i in range(6)]
    g += [("qk", "QsT", 0), ("qk", "QsT", 512)]
    g += [("kv", None, 0)]
    g += [("gate", "GT", i * 512) for i in range(6)]
    return g


def phase_inproj(K, ph, l, hTb):
    nc, S, I, R = K.nc, K.S, K.I, K.R
    hT, hT_b = hTb
    with contextlib.ExitStack() as st:
        wts = [K.sb("wi%d" % i, [128, 8, 512], BF16, st) for i in range(2)]
        wsw, wsw_b = K.sb("wsw", [128, 8, 512], BF16, st)
        rC, rC_b = K.sb("ropeC", [128, L], F32, st)
        rS, rS_b = K.sb("ropeS", [128, L], F32, st)
        stg = [K.sb("stg%d" % i, [128, T], BF16, st) for i in range(2)]
        vst = [K.sb("vst%d" % i, [128, 512], BF16, st) for i in range(2)]
        t1, t1_b = K.sb("rt1", [128, 512], F32, st)
        t2, t2_b = K.sb("rt2", [128, 512], F32, st)
        psA = [K.pst("psA%d" % i, [128, 512], F32, st) for i in range(2)]
        psB = [K.pst("psB%d" % i, [128, 512], F32, st) for i in range(2)]
        S.dma("sp", rC[:], I["ropeC"][:, :], rC_b)
        S.dma("sp", rS[:], I["ropeS"][:, :], rS_b)
        win = I["w_in"][l].rearrange("(k p) n -> p k n", p=128)
        cnt = dict(s=0, p=0, v=0)

        def fm_chunk(wt, wt_b, m, kind, dst, row0):
            sg, sg_b = stg[cnt["s"] % 2]
            cnt["s"] += 1
            for (t0, tn) in TCH:
                pa, pa_b = psA[cnt["p"] % 2]
                pb, pb_b = psB[cnt["p"] % 2]
                cnt["p"] += 1
                _mm_group(S, pa_b, pa[:, :tn], [(wt[:, k, m * 128:(m + 1) * 128], hT[:, k, t0:t0 + tn]) for k in range(8)],
                          [wt_b, hT_b])
                if kind == "qk" and t0 < L:
                    _mm_group(S, pb_b, pb[:, :tn], [(wsw[:, k, m * 128:(m + 1) * 128], hT[:, k, t0:t0 + tn]) for k in range(8)],
                              [wsw_b, hT_b])
                    S.op("dve", lambda h: h.tensor_tensor(out=t1[:, :tn], in0=pa[:, :tn], in1=rC[:, t0:t0 + tn], op=ALU.mult),
                         reads=[pa_b, rC_b], writes=[t1_b])
                    S.op("dve", lambda h: h.tensor_tensor(out=t2[:, :tn], in0=pb[:, :tn], in1=rS[:, t0:t0 + tn], op=ALU.mult),
                         reads=[pb_b, rS_b], writes=[t2_b])
                    S.op("pool", lambda h: h.tensor_tensor(out=sg[:, t0:t0 + tn], in0=t1[:, :tn], in1=t2[:, :tn], op=ALU.add),
                         reads=[t1_b, t2_b], writes=[sg_b])
                elif kind == "gate":
                    S.op("act", lambda h: h.activation(out=sg[:, t0:t0 + tn], in_=pa[:, :tn], func=AF.Sigmoid),
                         reads=[pa_b], writes=[sg_b])
                else:
                    S.op("act", lambda h: h.activation(out=sg[:, t0:t0 + tn], in_=pa[:, :tn], func=AF.Copy),
                         reads=[pa_b], writes=[sg_b])
            S.dma("sp", R[dst][0][row0:row0 + 128, :], sg[:], R[dst][1], [sg_b])

        def tm_cols(wt, wt_b, c0, cn, dst, col0):
            for tt in range(T // 128):
                pa, pa_b = psA[cnt["p"] % 2]
                cnt["p"] += 1
                vs, vs_b = vst[cnt["v"] % 2]
                cnt["v"] += 1
                _mm_group(S, pa_b, pa[:, :cn], [(hT[:, k, tt * 128:(tt + 1) * 128], wt[:, k, c0:c0 + cn]) for k in range(8)],
                          [wt_b, hT_b])
                S.op("act", lambda h: h.activation(out=vs[:, :cn], in_=pa[:, :cn], func=AF.Copy), reads=[pa_b], writes=[vs_b])
                S.dma("sp", R[dst][0][tt * 128:(tt + 1) * 128, col0:col0 + cn], vs[:, :cn], R[dst][1], [vs_b])

        def make_swapped(wt, wt_b, ncols):
            src = wt[:, :, :ncols].rearrange("p k (q s f) -> p k q s f", s=2, f=16)
            dstv = wsw[:, :, :ncols].rearrange("p k (q s f) -> p k q s f", s=2, f=16)
            for k in range(8):
                S.op("pool", lambda h, k=k: h.tensor_copy(out=dstv[:, k, :, 0, :], in_=src[:, k, :, 1, :]), reads=[wt_b], writes=[wsw_b])
                S.op("pool", lambda h, k=k: h.tensor_copy(out=dstv[:, k, :, 1, :], in_=src[:, k, :, 0, :]), reads=[wt_b], writes=[wsw_b])

        for gi, (kind, dst, row0) in enumerate(_inproj_groups()):
            wt, wt_b = wts[gi % 2]
            S.dma("pool", wt[:], win[:, :, gi * 512:(gi + 1) * 512], wt_b)
            if kind == "qk":
                make_swapped(wt, wt_b, 512)
                for m in range(4):
                    fm_chunk(wt, wt_b, m, "qk", dst, row0 + m * 128)
            elif kind == "v":
                tm_cols(wt, wt_b, 0, 512, dst, row0)
            elif kind == "kv":
                make_swapped(wt, wt_b, 256)
                for m in range(2):
                    fm_chunk(wt, wt_b, m, "qk", "KsT", m * 128)
                tm_cols(wt, wt_b, 256, 256, "Vs", 0)
            else:
                for m in range(4):
                    fm_chunk(wt, wt_b, m, kind, dst, row0 + m * 128)
        S.barrier()


def _bcast_rows(ap, n):
    return bass.AP(ap.tensor, ap.offset, [[0, 128], [1, n]])


def phase_diff(K, l, ctx_out):
    nc, S, I, R = K.nc, K.S, K.I, K.R
    ones_f, ones_f_b = K.ones_f
    ones_b, ones_b_b = K.ones_b
    lambda_init = 0.8 - 0.6 * math.exp(-0.3 * l)
    with contextlib.ExitStack() as st:
        lp, lp_b = K.sb("lp", [128, 256], F32, st)
        lsc, lsc_b = K.sb("lsc", [128, 4], F32, st)
        gsc, gsc_b = K.sb("gsc", [128, 1], F32, st)
        S.dma("sp", lp[:], _bcast_rows(I["diff_lambda"][l], 256), lp_b)
        S.dma("sp", gsc[:], I["sublnT"][l], gsc_b)
        S.op("dve", lambda h: h.tensor_tensor(out=lp[:, 0:64], in0=lp[:, 0:64], in1=lp[:, 64:128], op=ALU.mult), reads=[lp_b], writes=[lp_b])
        S.op("dve", lambda h: h.tensor_tensor(out=lp[:, 128:192], in0=lp[:, 128:192], in1=lp[:, 192:256], op=ALU.mult), reads=[lp_b], writes=[lp_b])
        S.op("dve", lambda h: h.reduce_sum(out=lsc[:, 0:1], in_=lp[:, 0:64], axis=AX.X), reads=[lp_b], writes=[lsc_b])
        S.op("dve", lambda h: h.reduce_sum(out=lsc[:, 1:2], in_=lp[:, 128:192], axis=AX.X), reads=[lp_b], writes=[lsc_b])
        S.op("act", lambda h: h.activation(out=lsc[:, 0:2], in_=lsc[:, 0:2], func=AF.Exp), reads=[lsc_b], writes=[lsc_b])
        S.op("dve", lambda h: h.scalar_tensor_tensor(out=lsc[:, 2:3], in0=lsc[:, 1:2], scalar=-lambda_init, in1=lsc[:, 0:1],
                                                     op0=ALU.add, op1=ALU.subtract), reads=[lsc_b], writes=[lsc_b])
        S.op("dve", lambda h: h.tensor_scalar(out=gsc[:], in0=gsc[:], scalar1=1.0 - lambda_init, scalar2=None, op0=ALU.mult),
             reads=[gsc_b], writes=[gsc_b])

        QT = [K.sb("dQT%d" % i, [128, T], BF16, st) for i in range(2)]
        KT = [K.sb("dKT%d" % i, [128, T], BF16, st) for i in range(2)]
        VV = [K.sb("dV%d" % i, [128, T // 128, 128], BF16, st) for i in range(2)]
        pt = [[K.sb("dP%d%d" % (m, i), [128, 512], BF16, st) for i in range(2)] for m in range(2)]
        psS = [K.pst("dpsS%d" % m, [128, 512], F32, st) for m in range(2)]
        psO = [K.pst("dpsO%d" % m, [128, 512], F32, st) for m in range(2)]
        psZ = [K.pst("dpsZ%d" % m, [128, 512], F32, st) for m in range(2)]
        rz = [K.sb("drz%d" % m, [128, 512], F32, st) for m in range(2)]
        oo = [K.sb("doo%d" % m, [128, 512], F32, st) for m in range(2)]
        of, of_b = K.sb("dof", [128, 512], F32, st)
        sqf, sqf_b = K.sb("dsq", [128, 512], F32, st)
        rs, rs_b = K.sb("drs", [128, 512], F32, st)
        ost = [K.sb("dost%d" % i, [128, 512], BF16, st) for i in range(2)]
        nch = 0
        for hd in range(8):
            q, q_b = QT[hd % 2]
            k, k_b = KT[hd % 2]
            v, v_b = VV[hd % 2]
            S.dma("sp", q[:], R["QdT"][0][hd * 128:(hd + 1) * 128, :], q_b, [R["QdT"][1]])
            S.dma("sp", k[:], R["KdT"][0][hd * 128:(hd + 1) * 128, :], k_b, [R["KdT"][1]])
            S.dma("sp", v[:], R["Vd"][0][:, hd * 128:(hd + 1) * 128].rearrange("(t p) e -> p t e", p=128), v_b, [R["Vd"][1]])
            for (q0, qn) in TCH:
                if q0 >= L and not ctx_out:
                    continue
                kts = list(range(T // 128)) if q0 < L else [32, 33]
                for i, kt in enumerate(kts):
                    first, last = i == 0, i == len(kts) - 1
                    for m in range(2):
                        ps, ps_b = psS[m]
                        p, p_b = pt[m][i % 2]
                        _mm_group(S, ps_b, ps[:, :qn], [(k[m * 64:(m + 1) * 64, kt * 128:(kt + 1) * 128], q[m * 64:(m + 1) * 64, q0:q0 + qn])],
                                  [k_b, q_b])
                        S.op("act", lambda h, ps=ps, p=p: h.activation(out=p[:, :qn], in_=ps[:, :qn], func=AF.Exp, scale=0.125),
                             reads=[ps_b], writes=[p_b])
                        _mm1(S, psO[m][1], psO[m][0][:, :qn], v[:, kt, :], p[:, :qn], [v_b, p_b], first, last)
                        _mm1(S, psZ[m][1], psZ[m][0][:, :qn], ones_b[:], p[:, :qn], [ones_b_b, p_b], first, last)
                for m in range(2):
                    S.op("dve", lambda h, m=m: h.reciprocal(out=rz[m][0][:, :qn], in_=psZ[m][0][:, :qn]), reads=[psZ[m][1]], writes=[rz[m][1]])
                    S.op("dve", lambda h, m=m: h.tensor_tensor(out=oo[m][0][:, :qn], in0=psO[m][0][:, :qn], in1=rz[m][0][:, :qn], op=ALU.mult),
                         reads=[psO[m][1], rz[m][1]], writes=[oo[m][1]])
                S.op("dve", lambda h: h.scalar_tensor_tensor(out=of[:, :qn], in0=oo[1][0][:, :qn], scalar=lsc[:, 2:3], in1=oo[0][0][:, :qn],
                                                              op0=ALU.mult, op1=ALU.add), reads=[oo[0][1], oo[1][1], lsc_b], writes=[of_b])
                S.op("pool", lambda h: h.tensor_tensor(out=sqf[:, :qn], in0=of[:, :qn], in1=of[:, :qn], op=ALU.mult), reads=[of_b], writes=[sqf_b])
                ps, ps_b = psS[0]
                _mm_group(S, ps_b, ps[:, :qn], [(ones_f[:], sqf[:, :qn])], [ones_f_b, sqf_b])
                S.op("dve", lambda h: h.tensor_scalar(out=rs[:, :qn], in0=ps[:, :qn], scalar1=1.0 / 128, scalar2=EPS, op0=ALU.mult, op1=ALU.add),
                     reads=[ps_b], writes=[rs_b])
                S.op("act", lambda h: h.activation(out=rs[:, :qn], in_=rs[:, :qn], func=AF.Sqrt), reads=[rs_b], writes=[rs_b])
                S.op("dve", lambda h: h.reciprocal(out=rs[:, :qn], in_=rs[:, :qn]), reads=[rs_b], writes=[rs_b])
                S.op("dve", lambda h: h.tensor_tensor(out=of[:, :qn], in0=of[:, :qn], in1=rs[:, :qn], op=ALU.mult), reads=[of_b, rs_b], writes=[of_b])
                o, o_b = ost[nch % 2]
                nch += 1
                S.op("act", lambda h, o=o: h.activation(out=o[:, :qn], in_=of[:, :qn], func=AF.Copy, scale=gsc[:, 0:1]), reads=[of_b, gsc_b], writes=[o_b])
                S.dma("sp", R["YdT"][0][hd * 128:(hd + 1) * 128, q0:q0 + qn], o[:, :qn], R["YdT"][1], [o_b])
        S.barrier()


def phase_swa(K, l, ctx_out):
    nc, S, I, R = K.nc, K.S, K.I, K.R
    ones_b, ones_b_b = K.ones_b
    with contextlib.ExitStack() as st:
        es_, es_b = K.sb("esink", [128, 16], F32, st)
        S.dma("sp", es_[:], _bcast_rows(I["swa_sink"][l], 16), es_b)
        S.op("act", lambda h: h.activation(out=es_[:], in_=es_[:], func=AF.Exp), reads=[es_b], writes=[es_b])
        mlo, mlo_b = K.sb("mlo", [128, 128], BF16, st)
        mup, mup_b = K.sb("mup", [128, 128], BF16, st)
        S.dma("pool", mlo[:], I["trilo"][:, :], mlo_b)
        S.dma("pool", mup[:], I["triup"][:, :], mup_b)
        QC = [[K.sb("sQ%d%d" % (c, i), [128, T], BF16, st) for i in range(2)] for c in range(2)]
        KT = [K.sb("sKT%d" % i, [128, T], BF16, st) for i in range(2)]
        VV = [K.sb("sV%d" % i, [128, T // 128, 64], BF16, st) for i in range(2)]
        pt = [K.sb("sP%d" % i, [128, 512], BF16, st) for i in range(2)]
        psS = [K.pst("spsS%d" % i, [128, 512], F32, st) for i in range(2)]
        psO = [K.pst("spsO%d" % i, [128, 512], F32, st) for i in range(2)]
        psZ = [K.pst("spsZ%d" % i, [128, 512], F32, st) for i in range(2)]
        zz, zz_b = K.sb("szz", [64, 512], F32, st)
        ost = [K.sb("sost%d" % i, [64, 512], BF16, st) for i in range(2)]
        nblk = 0
        npt = 0
        for kh in range(4):
            k, k_b = KT[kh % 2]
            v, v_b = VV[kh % 2]
            for half in range(2):
                S.dma("sp", k[half * 64:(half + 1) * 64, :], R["KsT"][0][kh * 64:(kh + 1) * 64, :], k_b, [R["KsT"][1]])
            S.dma("sp", v[:], R["Vs"][0][:, kh * 64:(kh + 1) * 64].rearrange("(t p) e -> p t e", p=128), v_b, [R["Vs"][1]])
            qc = []
            for c in range(2):
                qq, qq_b = QC[c][kh % 2]
                S.dma("sp", qq[:], R["QsT"][0][kh * 256 + c * 128: kh * 256 + (c + 1) * 128, :], qq_b, [R["QsT"][1]])
                qc.append((qq, qq_b))
            nqb = T // 128 if ctx_out else L // 128
            dbgl = K.cfg.get("swa_dbg", 9)
            if dbgl < 9:
                nqb = 2 if kh == 0 else 0
            for qb in range(nqb):
                if qb < 32:
                    kts = [(kk, mk) for kk, mk in ((qb - 1, "lo"), (qb, None), (qb + 1, "up")) if 0 <= kk < 32] + [(32, None), (33, None)]
                else:
                    kts = [(32, None), (33, None)]
                po, po_b = psO[nblk % 2]
                pz, pz_b = psZ[nblk % 2]
                for i, (kt, mk) in enumerate(kts):
                    first, last = i == 0, i == len(kts) - 1
                    ps, ps_b = psS[npt % 2]
                    p, p_b = pt[npt % 2]
                    npt += 1
                    for g in range(4):
                        r0 = (g % 2) * 64
                        qq, qq_b = qc[g // 2]
                        _mm_group(S, ps_b, ps[:, g * 128:(g + 1) * 128],
                                  [(k[r0:r0 + 64, kt * 128:(kt + 1) * 128], qq[r0:r0 + 64, qb * 128:(qb + 1) * 128])], [k_b, qq_b])
                    S.op("act", lambda h: h.activation(out=p[:], in_=ps[:], func=AF.Exp, scale=0.125),
                         reads=[ps_b], writes=[p_b])
                    if mk is not None and dbgl >= 3:
                        mt, mt_b = (mlo, mlo_b) if mk == "lo" else (mup, mup_b)
                        for g in range(4):
                            S.op("pool", lambda h, g=g, mt=mt: h.tensor_tensor(out=p[:, g * 128:(g + 1) * 128], in0=p[:, g * 128:(g + 1) * 128], in1=mt[:], op=ALU.mult),
                                 reads=[p_b, mt_b], writes=[p_b])
                    pf = p[:]
                    if dbgl < 4:
                        continue
                    _mm1(S, po_b, po[0:64, :], v[:, kt, :], pf, [v_b, p_b], first, last)
                    _mm1(S, pz_b, pz[0:64, :], ones_b[:, 0:64], pf, [ones_b_b, p_b], first, last)
                if dbgl < 5:
                    continue
                for g in range(4):
                    S.op("dve", lambda h, g=g: h.tensor_scalar(out=zz[:, g * 128:(g + 1) * 128], in0=pz[0:64, g * 128:(g + 1) * 128],
                                                               scalar1=es_[0:64, kh * 4 + g:kh * 4 + g + 1], scalar2=None, op0=ALU.add),
                         reads=[pz_b, es_b], writes=[zz_b])
                S.op("dve", lambda h: h.reciprocal(out=zz[:], in_=zz[:]), reads=[zz_b], writes=[zz_b])
                o, o_b = ost[nblk % 2]
                nblk += 1
                S.op("dve", lambda h, o=o: h.tensor_tensor(out=o[:], in0=po[0:64, :], in1=zz[:], op=ALU.mult),
                     reads=[po_b, zz_b], writes=[o_b])
                S.dma("sp", R["YsT"][0][kh * 256:(kh + 1) * 256, qb * 128:(qb + 1) * 128].rearrange("(g d) q -> d g q", g=4),
                      o[:].rearrange("p (g q) -> p g q", g=4), R["YsT"][1], [o_b])
        S.barrier()


def phase_merge(K, l):
    nc, S, I, R = K.nc, K.S, K.I, K.R
    modT, modT_b = K.modT
    with contextlib.ExitStack() as st:
        wb, wb_b = K.sb("wbr", [128, 24, D], BF16, st)
        wo, wo_b = K.sb("wout", [128, 8, D], BF16, st)
        for b in range(3):
            S.dma("pool", wb[:, b * 8:(b + 1) * 8, :], I["w_branch"][l, b].rearrange("(k p) n -> p k n", p=128), wb_b)
        S.dma("pool", wo[:], I["w_out"][l].rearrange("(k p) n -> p k n", p=128), wo_b)
        yb = [K.sb("my%d" % b, [128, 8, 512], BF16, st) for b in range(3)]
        gt, gt_b = K.sb("mgt", [128, 24, 512], BF16, st)
        mg, mg_b = K.sb("mmg", [128, 8, 512], F32, st)
        mgb, mgb_b = K.sb("mmgb", [128, 8, 512], BF16, st)
        tmp = [K.sb("mtmp%d" % i, [128, 512], F32, st) for i in range(2)]
        xx, xx_b = K.sb("mx", [128, 8, 512], F32, st)
        pss = [K.pst("mps%d" % i, [128, 512], F32, st) for i in range(4)]
        npz = 0
        ntmp = 0
        XTv = R["XT"][0].rearrange("(k p) t -> p k t", p=128)
        for (t0, tn) in TCH:
            r = 0 if t0 < L else 1
            for b, nm in enumerate(("YdT", "YhT", "YsT")):
                S.dma("sp", yb[b][0][:, :, :tn], R[nm][0].rearrange("(k p) t -> p k t", p=128)[:, :, t0:t0 + tn], yb[b][1], [R[nm][1]])
            S.dma("sp", gt[:, :, :tn], R["GT"][0].rearrange("(k p) t -> p k t", p=128)[:, :, t0:t0 + tn], gt_b, [R["GT"][1]])
            S.dma("sp", xx[:, :, :tn], XTv[:, :, t0:t0 + tn], xx_b, [R["XT"][1]])
            for m in range(8):
                for b in range(3):
                    ps, ps_b = pss[npz % 4]
                    npz += 1
                    _mm_group(S, ps_b, ps[:, :tn], [(wb[:, b * 8 + k, m * 128:(m + 1) * 128], yb[b][0][:, k, :tn]) for k in range(8)],
                              [wb_b, yb[b][1]])
                    if b == 0:
                        S.op("dve", lambda h, m=m, ps=ps: h.tensor_tensor(out=mg[:, m, :tn], in0=ps[:, :tn], in1=gt[:, m, :tn], op=ALU.mult),
                             reads=[ps_b, gt_b], writes=[mg_b])
                    else:
                        tp, tp_b = tmp[ntmp % 2]
                        ntmp += 1
                        S.op("dve", lambda h, m=m, b=b, ps=ps, tp=tp: h.tensor_tensor(out=tp[:, :tn], in0=ps[:, :tn], in1=gt[:, b * 8 + m, :tn], op=ALU.mult),
                             reads=[ps_b, gt_b], writes=[tp_b])
                        S.op("pool", lambda h, m=m, tp=tp: h.tensor_tensor(out=mg[:, m, :tn], in0=mg[:, m, :tn], in1=tp[:, :tn], op=ALU.add),
                             reads=[tp_b, mg_b], writes=[mg_b])
                S.op("act", lambda h, m=m: h.activation(out=mgb[:, m, :tn], in_=mg[:, m, :tn], func=AF.Copy), reads=[mg_b], writes=[mgb_b])
            for m in range(8):
                ps, ps_b = pss[npz % 4]
                npz += 1
                _mm_group(S, ps_b, ps[:, :tn], [(wo[:, k, m * 128:(m + 1) * 128], mgb[:, k, :tn]) for k in range(8)], [wo_b, mgb_b])
                S.op("dve", lambda h, m=m, ps=ps: h.scalar_tensor_tensor(out=xx[:, m, :tn], in0=ps[:, :tn], scalar=modT[:, 16 + m, r:r + 1],
                                                                         in1=xx[:, m, :tn], op0=ALU.mult, op1=ALU.add),
                     reads=[ps_b, modT_b, xx_b], writes=[xx_b])
            S.dma("sp", XTv[:, :, t0:t0 + tn], xx[:, :, :tn], R["XT"][1], [xx_b])
        S.barrier()


PI = math.pi


def _hy_filter(K, l, Lx, embT_ap, negt_ap, Cm, Sm, wf_ap, KFs):
    nc, S, I, R = K.nc, K.S, K.I, K.R
    ones_b, ones_b_b = K.ones_b
    nlt = Lx // 128
    nft = (Lx + 128) // 128
    nch = max(1, Lx // 512)
    cw = min(512, Lx)
    with contextlib.ExitStack() as st:
        w1, w1_b = K.sb("fw1", [33, 64], F32, st)
        w2, w2_b = K.sb("fw2", [64, 64], F32, st)
        w3, w3_b = K.sb("fw3", [64, 2048], F32, st)
        b1, b1_b = K.sb("fb1", [64, 1], F32, st)
        b2, b2_b = K.sb("fb2", [64, 1], F32, st)
        fr, fr_b = K.sb("ffr", [64, 1], F32, st)
        emb, emb_b = K.sb("femb", [33, Lx], F32, st)
        ngt, ngt_b = K.sb("fngt", [128, nlt], F32, st)
        dl, dl_b = K.sb("fdl", [128, D], F32, st)
        wf, wf_b = K.sb("fwf", [128, nft], F32, st)
        z1, z1_b = K.sb("fz1", [64, Lx], F32, st)
        z2, z2_b = K.sb("fz2", [64, Lx], F32, st)
        rn, rn_b = K.sb("frn", [128, D], F32, st)
        dec, dec_b = K.sb("fdec", [128, D], F32, st)
        hd = [K.sb("fhd%d" % i, [128, D], F32, st) for i in range(2)]
        ab, ab_b = K.sb("fab", [128, 512], BF16, st)
        gsel, gsel_b = K.sb("fgsel", [64, 512], F32, st)
        AT, AT_b = K.sb("fAT", [128, nlt, D], BF16, st)
        for t_, src in ((w1, I["hy_w1"][l]), (w2, I["hy_w2"][l]), (w3, I["hy_w3"][l]), (b1, I["hy_b1"][l]), (b2, I["hy_b2"][l]),
                        (fr, I["hy_fr"][l]), (emb, embT_ap), (ngt, negt_ap), (wf, wf_ap)):
            pass
        S.dma("sp", w1[:], I["hy_w1"][l], w1_b)
        S.dma("sp", w2[:], I["hy_w2"][l], w2_b)
        S.dma("sp", w3[:], I["hy_w3"][l], w3_b)
        S.dma("sp", b1[:], I["hy_b1"][l], b1_b)
        S.dma("sp", b2[:], I["hy_b2"][l], b2_b)
        S.dma("sp", fr[:], I["hy_fr"][l], fr_b)
        S.dma("sp", emb[:], embT_ap, emb_b)
        S.dma("sp", ngt[:], negt_ap, ngt_b)
        S.dma("sp", wf[:], wf_ap, wf_b)
        S.dma("sp", dl[:], _bcast_rows(I["deltas"][0], D), dl_b)
        psm = [K.pst("fps%d" % i, [128, 512], F32, st) for i in range(4)]
        psN = [K.pst("fpsN%d" % i, [128, 512], F32, st) for i in range(2)]
        for (wm, wm_b, bb, bb_b, src, src_b, dst, dst_b) in ((w1, w1_b, b1, b1_b, emb, emb_b, z1, z1_b), (w2, w2_b, b2, b2_b, z1, z1_b, z2, z2_b)):
            for ci in range(nch):
                ps, ps_b = psm[ci % 4]
                sl = slice(ci * cw, (ci + 1) * cw)
                _mm_group(S, ps_b, ps[0:64, :cw], [(wm[:], src[:, sl])], [wm_b, src_b])
                S.op("dve", lambda h, ps=ps, sl=sl, bb=bb, dst=dst: h.tensor_scalar(out=dst[:, sl], in0=ps[0:64, :cw], scalar1=bb[:, 0:1], scalar2=fr[:, 0:1],
                                                                                 op0=ALU.add, op1=ALU.mult), reads=[ps_b, bb_b, fr_b], writes=[dst_b])
                for _ in range(2):
                    for cop, sh in ((ALU.is_gt, -2.0 * PI), (ALU.is_lt, 2.0 * PI)):
                        thr = PI if cop == ALU.is_gt else -PI
                        S.op("dve", lambda h, sl=sl, dst=dst, cop=cop, thr=thr: h.tensor_scalar(out=gsel[:, :cw], in0=dst[:, sl], scalar1=thr, scalar2=None, op0=cop),
                             reads=[dst_b], writes=[gsel_b])
                        S.op("dve", lambda h, sl=sl, dst=dst, sh=sh: h.scalar_tensor_tensor(out=dst[:, sl], in0=gsel[:, :cw], scalar=sh, in1=dst[:, sl],
                                                                                          op0=ALU.mult, op1=ALU.add), reads=[gsel_b, dst_b], writes=[dst_b])
                S.op("act", lambda h, sl=sl, dst=dst: h.activation(out=dst[:, sl], in_=dst[:, sl], func=AF.Sin), reads=[dst_b], writes=[dst_b])

        def htile(lt, cb):
            S.op("act", lambda h: h.activation(out=dec[:], in_=dl[:], func=AF.Exp, scale=ngt[:, lt:lt + 1]), reads=[dl_b, ngt_b], writes=[dec_b])
            for j in range(4):
                ps, ps_b = psm[j]
                _mm_group(S, ps_b, ps[:], [(z2[:, lt * 128:(lt + 1) * 128], w3[:, j * 512:(j + 1) * 512])], [z2_b, w3_b])
            cb(lt)

        for lt in range(nlt):
            def cb1(lt):
                for j in range(4):
                    ps, ps_b = psm[j]
                    half = j % 2
                    tf, tf_b = hd[j // 2]
                    S.op("dve", lambda h, ps=ps, half=half, tf=tf: h.tensor_tensor(out=tf[:, half * 512:(half + 1) * 512], in0=ps[:],
                                                                                   in1=dec[:, half * 512:(half + 1) * 512], op=ALU.mult),
                         reads=[ps_b, dec_b], writes=[tf_b])
                    S.op("act", lambda h, half=half, tf=tf: h.activation(out=ab[:], in_=tf[:, half * 512:(half + 1) * 512], func=AF.Abs),
                         reads=[tf_b], writes=[ab_b])
                    first = (lt == 0 and j < 2)
                    last = (lt == nlt - 1 and j >= 2)
                    _mm1(S, psN[half][1], psN[half][0][:], ones_b[:], ab[:], [ones_b_b, ab_b], first, last)
            htile(lt, cb1)
        for half in range(2):
            S.op("dve", lambda h, half=half: h.tensor_scalar(out=rn[:, half * 512:(half + 1) * 512], in0=psN[half][0][:], scalar1=EPS, scalar2=None, op0=ALU.add),
                 reads=[psN[half][1]], writes=[rn_b])
        S.op("dve", lambda h: h.reciprocal(out=rn[:], in_=rn[:]), reads=[rn_b], writes=[rn_b])
        for plane, op2 in (("C", ALU.add), ("S", ALU.subtract)):
            for lt in range(nlt):
                def cb2(lt):
                    for j in range(4):
                        ps, ps_b = psm[j]
                        half, dr = j % 2, j // 2
                        S.op("dve", lambda h, ps=ps, half=half, dr=dr: h.tensor_tensor(out=hd[dr][0][:, half * 512:(half + 1) * 512], in0=ps[:],
                                                                                      in1=dec[:, half * 512:(half + 1) * 512], op=ALU.mult),
                             reads=[ps_b, dec_b], writes=[hd[dr][1]])
                    if lt == 0:
                        S.op("dve", lambda h: h.memset(hd[1][0][0:1, :], 0.0), writes=[hd[1][1]])
                    S.op("pool", lambda h: h.tensor_tensor(out=hd[0][0][:], in0=hd[0][0][:], in1=hd[1][0][:], op=op2), reads=[hd[0][1], hd[1][1]], writes=[hd[0][1]])
                    S.op("pool", lambda h: h.tensor_tensor(out=AT[:, lt, :], in0=hd[0][0][:], in1=rn[:], op=ALU.mult), reads=[hd[0][1], rn_b], writes=[AT_b])
                htile(lt, cb2)
            pi = 0 if plane == "C" else 1

            def evac(ft, pc, ps_, pi=pi):
                src = pc if pi == 0 else ps_
                for hh in range(2):
                    o, o_b = hd[hh]
                    S.op("dve", lambda h, hh=hh, o=o: h.tensor_scalar(out=o[:, 0:512], in0=src[hh][0][:], scalar1=wf[:, ft:ft + 1], scalar2=None, op0=ALU.mult),
                         reads=[src[hh][1], wf_b], writes=[o_b])
                    S.dma("sp", KFs[0][pi, ft * 128:(ft + 1) * 128, hh * 512:(hh + 1) * 512], o[:, 0:512], KFs[1], [o_b])
            _fwd_dft(K, st, Cm, Sm, nlt, nft, AT, AT_b, 0, (plane,), evac, psm)
        S.barrier()


def _fwd_dft(K, st, Cm, Sm, ntt, nft, rhs, rhs_b, tt0, planes, evac, pspool):
    S = K.S
    with contextlib.ExitStack() as s2:
        blk = {p: [K.sb("dblk%s%d" % (p, i), [128, ntt, 128], BF16, s2) for i in range(2)] for p in planes}
        for ft in range(nft):
            pss = {}
            for pi, p in enumerate(("C", "S")):
                if p not in planes:
                    pss[p] = None
                    continue
                M = Cm if p == "C" else Sm
                b, b_b = blk[p][ft % 2]
                S.dma("sp", b[:], M.rearrange("(t p) f -> p t f", p=128)[:, 0:ntt, ft * 128:(ft + 1) * 128], b_b)
                pss[p] = [pspool[pi * 2 + hh] for hh in range(2)]
                for hh in range(2):
                    ps, ps_b = pss[p][hh]
                    _mm_group(S, ps_b, ps[:], [(b[:, t, :], rhs[:, tt0 + t, hh * 512:(hh + 1) * 512]) for t in range(ntt)], [b_b, rhs_b])
            evac(ft, pss["C"], pss["S"])


def phase_hyena(K, l, ctx_out):
    nc, S, I, R = K.nc, K.S, K.I, K.R
    ident_b, ident_b_b = K.ident_b
    _hy_filter(K, l, L, I["embT"][:, :], I["negt"][:, :], I["dftC"], I["dftS"], I["wf"][:, :], R["KF"])
    if ctx_out:
        _hy_filter(K, l, LC, I["embTc"][:, :], I["negtc"][:, :], I["dftCc"], I["dftSc"], I["wfc"][:, :], R["KFc"])
    segs = [(0, L)] + ([(L, LC)] if ctx_out else [])
    TT = T if ctx_out else L
    with contextlib.ExitStack() as st:
        utok, utok_b = K.sb("hutok", [128, T // 128, D], BF16, st)
        with contextlib.ExitStack() as s2:
            cw, cw_b = K.sb("hcw", [128, 24, 3], F32, s2)
            cb, cb_b = K.sb("hcb", [128, 24], F32, s2)
            S.dma("sp", cw[:], I["cwT"][l], cw_b)
            S.dma("sp", cb[:], I["cbT"][l], cb_b)
            xin = [K.sb("hxin%d" % i, [128, T], BF16, s2) for i in range(3)]
            yc = [K.sb("hyc%d" % i, [128, T], F32, s2) for i in range(3)]
            x0b, x0b_b = K.sb("hx0b", [128, T], BF16, s2)
            ub, ub_b = K.sb("hub", [128, T], BF16, s2)
            ptr = [K.pst("hptr%d" % i, [128, 1024], BF16, s2) for i in range(2)]
            ntr = 0
            for c in range(8):
                for s_ in range(3):
                    j = s_ * 8 + c
                    xi, xi_b = xin[s_]
                    y, y_b = yc[s_]
                    S.dma("sp", xi[:, :TT], R["HyT"][0][j * 128:(j + 1) * 128, 0:TT], xi_b, [R["HyT"][1]])
                    for (a0, n) in segs:
                        S.op("dve", lambda h, j=j, xi=xi, y=y: h.tensor_scalar(out=y[:, a0:a0 + n], in0=xi[:, a0:a0 + n], scalar1=cw[:, j, 1:2], scalar2=cb[:, j:j + 1],
                                                                            op0=ALU.mult, op1=ALU.add), reads=[xi_b, cw_b, cb_b], writes=[y_b])
                        S.op("dve", lambda h, j=j, xi=xi, y=y: h.scalar_tensor_tensor(out=y[:, a0 + 1:a0 + n], in0=xi[:, a0:a0 + n - 1], scalar=cw[:, j, 0:1],
                                                                                   in1=y[:, a0 + 1:a0 + n], op0=ALU.mult, op1=ALU.add),
                             reads=[xi_b, cw_b, y_b], writes=[y_b])
                        S.op("dve", lambda h, j=j, xi=xi, y=y: h.scalar_tensor_tensor(out=y[:, a0:a0 + n - 1], in0=xi[:, a0 + 1:a0 + n], scalar=cw[:, j, 2:3],
                                                                                   in1=y[:, a0:a0 + n - 1], op0=ALU.mult, op1=ALU.add),
                             reads=[xi_b, cw_b, y_b], writes=[y_b])
                S.op("act", lambda h: h.activation(out=x0b[:, :TT], in_=yc[0][0][:, :TT], func=AF.Copy), reads=[yc[0][1]], writes=[x0b_b])
                S.op("pool", lambda h: h.tensor_tensor(out=ub[:, :TT], in0=yc[1][0][:, :TT], in1=yc[2][0][:, :TT], op=ALU.mult),
                     reads=[yc[1][1], yc[2][1]], writes=[ub_b])
                S.dma("sp", R["X0T"][0][c * 128:(c + 1) * 128, 0:TT], x0b[:, :TT], R["X0T"][1], [x0b_b])
                S.dma("sp", R["UT"][0][c * 128:(c + 1) * 128, 0:TT], ub[:, :TT], R["UT"][1], [ub_b])
                for t8 in range(0, TT // 128, 8):
                    n8 = min(8, TT // 128 - t8)
                    pt_, pt_b = ptr[ntr % 2]
                    ntr += 1

                    def trf(h, t8=t8, n8=n8, pt_=pt_):
                        ins = None
                        for q in range(n8):
                            ins = h.transpose(pt_[:, q * 128:(q + 1) * 128], ub[:, (t8 + q) * 128:(t8 + q + 1) * 128], ident_b[:])
                        return ins
                    S.op("pe", trf, reads=[ub_b, ident_b_b], writes=[pt_b])
                    S.op("act", lambda h, t8=t8, n8=n8, pt_=pt_, c=c: h.activation(out=utok[:, t8:t8 + n8, c * 128:(c + 1) * 128],
                                                                                   in_=pt_[:, :n8 * 128].rearrange("p (q f) -> p q f", f=128), func=AF.Copy),
                         reads=[pt_b], writes=[utok_b])
            S.barrier()
        for si, (a0, n) in enumerate(segs):
            Cm, Sm = (I["dftC"], I["dftS"]) if si == 0 else (I["dftCc"], I["dftSc"])
            KFs = R["KF"] if si == 0 else R["KFc"]
            YFs = R["YF"] if si == 0 else R["YFc"]
            ntt = n // 128
            nft = (n + 128) // 128
            with contextlib.ExitStack() as s2:
                kf = [K.sb("hkf%d" % i, [128, 2, D], F32, s2) for i in range(2)]
                tq = [K.sb("htq%d" % i, [128, 512], F32, s2) for i in range(4)]
                yo = [K.sb("hyo%d" % i, [128, 2, D], BF16, s2) for i in range(2)]
                pspool = [K.pst("hps%d" % i, [128, 512], F32, s2) for i in range(4)]

                def evac(ft, pc, ps_, KFs=KFs, YFs=YFs):
                    k_, k_b = kf[ft % 2]
                    o, o_b = yo[ft % 2]
                    S.dma("sp", k_[:], KFs[0][:, ft * 128:(ft + 1) * 128, :].rearrange("a p c -> p a c"), k_b, [KFs[1]])
                    for hh in range(2):
                        cs = slice(hh * 512, (hh + 1) * 512)
                        ur, ur_b = pc[hh]
                        ui, ui_b = ps_[hh]
                        S.op("dve", lambda h: h.tensor_tensor(out=tq[0][0][:], in0=ur[:], in1=k_[:, 0, cs], op=ALU.mult), reads=[ur_b, k_b], writes=[tq[0][1]])
                        S.op("dve", lambda h: h.tensor_tensor(out=tq[1][0][:], in0=ui[:], in1=k_[:, 1, cs], op=ALU.mult), reads=[ui_b, k_b], writes=[tq[1][1]])
                        S.op("pool", lambda h: h.tensor_tensor(out=o[:, 0, cs], in0=tq[0][0][:], in1=tq[1][0][:], op=ALU.subtract),
                             reads=[tq[0][1], tq[1][1]], writes=[o_b])
                        S.op("dve", lambda h: h.tensor_tensor(out=tq[2][0][:], in0=ur[:], in1=k_[:, 1, cs], op=ALU.mult), reads=[ur_b, k_b], writes=[tq[2][1]])
                        S.op("dve", lambda h: h.tensor_tensor(out=tq[3][0][:], in0=ui[:], in1=k_[:, 0, cs], op=ALU.mult), reads=[ui_b, k_b], writes=[tq[3][1]])
                        S.op("pool", lambda h: h.tensor_tensor(out=o[:, 1, cs], in0=tq[2][0][:], in1=tq[3][0][:], op=ALU.add),
                             reads=[tq[2][1], tq[3][1]], writes=[o_b])
                    S.dma("sp", YFs[0][:, ft * 128:(ft + 1) * 128, :].rearrange("a p c -> p a c"), o[:], YFs[1], [o_b])
                _fwd_dft(K, s2, Cm, Sm, ntt, nft, utok, utok_b, a0 // 128, ("C", "S"), evac, pspool)
                S.barrier()
    for si, (a0, n) in enumerate(segs):
        Cm, Sm = (I["dftC"], I["dftS"]) if si == 0 else (I["dftCc"], I["dftSc"])
        YFs = R["YF"] if si == 0 else R["YFc"]
        nft = (n + 128) // 128
        with contextlib.ExitStack() as s2:
            hb, hb_b = K.sb("ihb", [128, 8], F32, s2)
            S.dma("sp", hb[:], I["hbT"][l], hb_b)
            Yh = [K.sb("iY%d" % i, [128, nft, 512], BF16, s2) for i in range(2)]
            Cr = [K.sb("iC%d" % i, [128, nft, 256], BF16, s2) for i in range(2)]
            Sr = [K.sb("iS%d" % i, [128, nft, 256], BF16, s2) for i in range(2)]
            x0c = [K.sb("ix0%d" % i, [128, 4, 256], BF16, s2) for i in range(2)]
            uc = [K.sb("iu%d" % i, [128, 4, 256], BF16, s2) for i in range(2)]
            tmp, tmp_b = K.sb("itmp", [128, 256], F32, s2)
            og = [K.sb("iog%d" % i, [128, 4, 256], BF16, s2) for i in range(2)]
            psI = [K.pst("ips%d" % i, [128, 512], F32, s2) for i in range(2)]
            nk = 0
            npi = 0
            for half in range(2):
                for pl in range(2):
                    S.dma("sp", Yh[pl][0][:], YFs[0][pl, 0:nft * 128, half * 512:(half + 1) * 512].rearrange("(f p) c -> p f c", p=128), Yh[pl][1], [YFs[1]])
                for t0 in range(0, n, 256):
                    cr, cr_b = Cr[nk % 2]
                    sr, sr_b = Sr[nk % 2]
                    xx, xx_b = x0c[nk % 2]
                    uu, uu_b = uc[nk % 2]
                    oo, oo_b = og[nk % 2]
                    nk += 1
                    S.dma("sp", cr[:], Cm.rearrange("(f p) t -> p f t", p=128)[:, 0:nft, t0:t0 + 256], cr_b)
                    S.dma("sp", sr[:], Sm.rearrange("(f p) t -> p f t", p=128)[:, 0:nft, t0:t0 + 256], sr_b)
                    rows = slice(half * 512, (half + 1) * 512)
                    S.dma("sp", xx[:], R["X0T"][0][rows, a0 + t0:a0 + t0 + 256].rearrange("(c p) t -> p c t", p=128), xx_b, [R["X0T"][1]])
                    S.dma("sp", uu[:], R["UT"][0][rows, a0 + t0:a0 + t0 + 256].rearrange("(c p) t -> p c t", p=128), uu_b, [R["UT"][1]])
                    for ci in range(4):
                        ps, ps_b = psI[npi % 2]
                        npi += 1
                        pairs = [(Yh[0][0][:, f, ci * 128:(ci + 1) * 128], cr[:, f, :]) for f in range(nft)] + \
                                [(Yh[1][0][:, f, ci * 128:(ci + 1) * 128], sr[:, f, :]) for f in range(nft)]
                        _mm_group(S, ps_b, ps[:, 0:256], pairs, [Yh[0][1], Yh[1][1], cr_b, sr_b])
                        cidx = half * 4 + ci
                        S.op("dve", lambda h, ci=ci, cidx=cidx, ps=ps, uu=uu: h.scalar_tensor_tensor(out=tmp[:], in0=uu[:, ci, :], scalar=hb[:, cidx:cidx + 1],
                                                                                                    in1=ps[:, 0:256], op0=ALU.mult, op1=ALU.add),
                             reads=[uu_b, hb_b, ps_b], writes=[tmp_b])
                        S.op("pool", lambda h, ci=ci, oo=oo, xx=xx: h.tensor_tensor(out=oo[:, ci, :], in0=tmp[:], in1=xx[:, ci, :], op=ALU.mult),
                             reads=[tmp_b, xx_b], writes=[oo_b])
                    S.dma("sp", R["YhT"][0][rows, a0 + t0:a0 + t0 + 256].rearrange("(c p) t -> p c t", p=128), oo[:], R["YhT"][1], [oo_b])
            S.barrier()
    if not ctx_out:
        pass


def _bc_mid(ap2d, n):
    a = ap2d.ap
    return bass.AP(ap2d.tensor, ap2d.offset, [list(a[0]), [0, n], list(a[1])])


def phase_moe(K, l, ctx_out):
    nc, S, I, R = K.nc, K.S, K.I, K.R
    modT, modT_b = K.modT
    A2, A2_b = K.A2
    ones_f, ones_f_b = K.ones_f
    ones_b, ones_b_b = K.ones_b
    ident_f, ident_f_b = K.ident_f
    ident_b, ident_b_b = K.ident_b
    debug = K.cfg.get("debug", ())
    XTv = R["XT"][0].rearrange("(k p) t -> p k t", p=128)
    chunks = TCH if ctx_out else TCH[:8]
    ntt = 34 if ctx_out else 32
    groups = [(0, 32, CAP)] + ([(32, 34, CAPC)] if ctx_out else [])
    with contextlib.ExitStack() as st:
        lg, lg_b = K.sb("lg", [128, 34, NE], F32, st)
        aff, aff_b = K.sb("aff", [128, 34, NE], F32, st)
        psel, psel_b = K.sb("psel", [128, 34, NE], F32, st)
        wr, wr_b = K.sb("wr", [128, 8, NE], F32, st)
        S.dma("sp", wr[:], I["router_w"][l].rearrange("(k p) e -> p k e", p=128), wr_b)
        sH = contextlib.ExitStack()
        H2, H2_b = K.sb("H2", [128, 34, D], BF16, sH)
        if ctx_out is False:
            S.op("dve", lambda h: h.memset(lg[:, 32:34, :], 0.0), writes=[lg_b])
        with contextlib.ExitStack() as s2:
            xs = [K.sb("qx%d" % i, [128, 8, 512], F32, s2) for i in range(2)]
            sq, sq_b = K.sb("qsq", [128, 8, 512], F32, s2)
            rstd, rstd_b = K.sb("qrstd", [128, 512], F32, s2)
            tmp, tmp_b = K.sb("qtmp", [128, 512], F32, s2)
            h2f, h2f_b = K.sb("qh2f", [128, 8, 512], F32, s2)
            hTc, hTc_b = K.sb("qhTc", [128, 8, 512], BF16, s2)
            ps, ps_b = K.pst("qps", [128, 512], F32, s2)
            psl, psl_b = K.pst("qpsl", [128, 64], F32, s2)
            pstr = [K.pst("qpst%d" % i, [128, 1024], BF16, s2) for i in range(2)]
            ntr = 0
            for ci, (t0, tn) in enumerate(chunks):
                r = 0 if t0 < L else 1
                x, x_b = xs[ci % 2]
                S.dma("sp", x[:, :, :tn], XTv[:, :, t0:t0 + tn], x_b, [R["XT"][1]])
                if "XT1" in debug:
                    S.dma("sp", R["XT1"][0].rearrange("(k p) t -> p k t", p=128)[:, :, t0:t0 + tn], x[:, :, :tn], R["XT1"][1], [x_b])
                S.op("act", lambda h: h.activation(out=sq[:, :, :tn], in_=x[:, :, :tn], func=AF.Square), reads=[x_b], writes=[sq_b])
                _mm_group(S, ps_b, ps[:, :tn], [(ones_f[:], sq[:, k, :tn]) for k in range(8)], [ones_f_b, sq_b])
                S.op("dve", lambda h: h.tensor_scalar(out=rstd[:, :tn], in0=ps[:, :tn], scalar1=1.0 / D, scalar2=EPS,
                                                      op0=ALU.mult, op1=ALU.add), reads=[ps_b], writes=[rstd_b])
                S.op("act", lambda h: h.activation(out=rstd[:, :tn], in_=rstd[:, :tn], func=AF.Sqrt), reads=[rstd_b], writes=[rstd_b])
                S.op("dve", lambda h: h.reciprocal(out=rstd[:, :tn], in_=rstd[:, :tn]), reads=[rstd_b], writes=[rstd_b])
                for k in range(8):
                    S.op("dve", lambda h, k=k: h.tensor_tensor(out=tmp[:, :tn], in0=x[:, k, :tn], in1=rstd[:, :tn], op=ALU.mult),
                         reads=[x_b, rstd_b], writes=[tmp_b])
                    S.op("act", lambda h, k=k: h.activation(out=h2f[:, k, :tn], in_=tmp[:, :tn], func=AF.Identity,
                                                            scale=A2[:, k, r:r + 1], bias=modT[:, 24 + k, r:r + 1]),
                         reads=[tmp_b, A2_b, modT_b], writes=[h2f_b])
                    S.op("pool", lambda h, k=k: h.tensor_copy(out=hTc[:, k, :tn], in_=h2f[:, k, :tn]), reads=[h2f_b], writes=[hTc_b])
                nj = tn // 128
                for j in range(nj):
                    tt = t0 // 128 + j
                    _mm_group(S, psl_b, psl[:, j * 16:(j + 1) * 16],
                              [(h2f[:, k, j * 128:(j + 1) * 128], wr[:, k, :]) for k in range(8)], [h2f_b, wr_b])
                    pt_, pt_b = pstr[ntr % 2]
                    ntr += 1

                    def trf(h, j=j, pt_=pt_):
                        ins = None
                        for k in range(8):
                            ins = h.transpose(pt_[:, k * 128:(k + 1) * 128], hTc[:, k, j * 128:(j + 1) * 128], ident_b[:])
                        return ins
                    S.op("pe", trf, reads=[hTc_b, ident_b_b], writes=[pt_b])
                    S.op("act", lambda h, tt=tt, pt_=pt_: h.activation(out=H2[:, tt, :], in_=pt_[:], func=AF.Copy), reads=[pt_b], writes=[H2_b])
                S.op("dve", lambda h: h.tensor_copy(out=lg[:, t0 // 128:t0 // 128 + nj, :].rearrange("p t e -> p (t e)"),
                                                    in_=psl[:, :nj * 16]), reads=[psl_b], writes=[lg_b])
            S.barrier()
        if "LG" in debug:
            S.dma("sp", R["LG"][0], lg[:], R["LG"][1], [lg_b])
            S.dma("sp", R["H2d"][0], H2[:], R["H2d"][1], [H2_b])
        with contextlib.ExitStack() as s2:
            se, se_b = K.sb("rse", [128, 34], F32, s2)
            lo, lo_b = K.sb("rlo", [128, NE], F32, s2)
            mid, mid_b = K.sb("rmid", [128, NE], F32, s2)
            cmpt, cmp_b = K.sb("rcmp", [128, 32, NE], F32, s2)
            cntp, cntp_b = K.sb("rcntp", [128, NE], F32, s2)
            ge, ge_b = K.sb("rge", [128, NE], F32, s2)
            mk, mk_b = K.sb("rmk", [128, 34, NE], F32, s2)
            mkb, mkb_b = K.sb("rmkb", [128, 34, NE], BF16, s2)
            tot, tot_b = K.sb("rtot", [128, 34, NE], F32, s2)
            base, base_b = K.sb("rbase", [128, 34, NE], F32, s2)
            posT, posT_b = K.sb("posT", [16, T], F32, s2)
            affT, affT_b = K.sb("affT", [16, T], F32, s2)
            us, us_b = K.sb("rus", [128, 128], BF16, s2)
            S.dma("pool", us[:], I["ustrict"][:, :], us_b)
            psc, psc_b = K.pst("rpsc", [128, 512], F32, s2)
            psw, psw_b = K.pst("rpsw", [128, 512], F32, s2)
            pst_, pst_b = K.pst("rpst", [128, 512], F32, s2)
            S.op("act", lambda h: h.activation(out=aff[:].rearrange("p t e -> p (t e)"), in_=lg[:].rearrange("p t e -> p (t e)"), func=AF.Exp),
                 reads=[lg_b], writes=[aff_b])
            S.op("dve", lambda h: h.reduce_sum(out=se[:], in_=aff[:], axis=AX.X), reads=[aff_b], writes=[se_b])
            S.op("dve", lambda h: h.reciprocal(out=se[:], in_=se[:]), reads=[se_b], writes=[se_b])
            for tt in range(34):
                S.op("dve", lambda h, tt=tt: h.tensor_scalar(out=aff[:, tt, :], in0=aff[:, tt, :], scalar1=se[:, tt:tt + 1], scalar2=None, op0=ALU.mult),
                     reads=[aff_b, se_b], writes=[aff_b])
            S.op("dve", lambda h: h.memset(mk[:], 0.0), writes=[mk_b])
            S.op("dve", lambda h: h.memset(base[:], 0.0), writes=[base_b])
            for (ta, tb, cap) in groups:
                nt = tb - ta
                S.op("dve", lambda h: h.memset(lo[:], 0.0), writes=[lo_b])
                for it in range(30):
                    w = 0.5 ** (it + 1)
                    S.op("dve", lambda h: h.tensor_scalar(out=mid[:], in0=lo[:], scalar1=w, scalar2=None, op0=ALU.add), reads=[lo_b], writes=[mid_b])
                    S.op("dve", lambda h: h.tensor_tensor(out=cmpt[:, :nt, :], in0=aff[:, ta:tb, :], in1=_bc_mid(mid[:], nt), op=ALU.is_ge),
                         reads=[aff_b, mid_b], writes=[cmp_b])
                    S.op("dve", lambda h: h.reduce_sum(out=cntp[:], in_=cmpt[:, :nt, :].rearrange("p t e -> p e t"), axis=AX.X),
                         reads=[cmp_b], writes=[cntp_b])
                    _mm_group(S, psc_b, psc[:, 0:NE], [(ones_f[:], cntp[:])], [ones_f_b, cntp_b])
                    S.op("dve", lambda h: h.tensor_scalar(out=ge[:], in0=psc[:, 0:NE], scalar1=cap - 0.5, scalar2=None, op0=ALU.is_ge),
                         reads=[psc_b], writes=[ge_b])
                    S.op("dve", lambda h: h.scalar_tensor_tensor(out=lo[:], in0=ge[:], scalar=w, in1=lo[:], op0=ALU.mult, op1=ALU.add),
                         reads=[ge_b, lo_b], writes=[lo_b])
                S.op("dve", lambda h: h.tensor_tensor(out=mk[:, ta:tb, :], in0=aff[:, ta:tb, :], in1=_bc_mid(lo[:], nt), op=ALU.is_ge),
                     reads=[aff_b, lo_b], writes=[mk_b])
                S.op("dve", lambda h: h.tensor_copy(out=mkb[:, ta:tb, :], in_=mk[:, ta:tb, :]), reads=[mk_b], writes=[mkb_b])
                mflat = mkb[:, ta:tb, :].rearrange("p t e -> p (t e)")
                _mm_group(S, psw_b, psw[:, :nt * NE], [(us[:], mflat)], [us_b, mkb_b])
                _mm_group(S, pst_b, pst_[:, :nt * NE], [(ones_b[:], mflat)], [ones_b_b, mkb_b])
                S.op("dve", lambda h: h.tensor_copy(out=tot[:, ta:tb, :].rearrange("p t e -> p (t e)"), in_=pst_[:, :nt * NE]),
                     reads=[pst_b], writes=[tot_b])
                for t in range(ta + 1, tb):
                    S.op("dve", lambda h, t=t: h.tensor_tensor(out=base[:, t, :], in0=base[:, t - 1, :], in1=tot[:, t - 1, :], op=ALU.add),
                         reads=[base_b, tot_b], writes=[base_b])
                S.op("dve", lambda h: h.tensor_tensor(out=psel[:, ta:tb, :].rearrange("p t e -> p (t e)"), in0=psw[:, :nt * NE],
                                                      in1=base[:, ta:tb, :].rearrange("p t e -> p (t e)"), op=ALU.add),
                     reads=[psw_b, base_b], writes=[psel_b])
                S.op("dve", lambda h: h.scalar_tensor_tensor(out=psel[:, ta:tb, :], in0=psel[:, ta:tb, :], scalar=1.0, in1=mk[:, ta:tb, :],
                                                             op0=ALU.add, op1=ALU.mult), reads=[psel_b, mk_b], writes=[psel_b])
                S.op("dve", lambda h: h.tensor_scalar(out=psel[:, ta:tb, :], in0=psel[:, ta:tb, :], scalar1=-1.0, scalar2=None, op0=ALU.add),
                     reads=[psel_b], writes=[psel_b])
            S.op("dve", lambda h: h.tensor_tensor(out=aff[:], in0=aff[:], in1=mk[:], op=ALU.mult), reads=[aff_b, mk_b], writes=[aff_b])
            for src, src_b, dstT, dstT_b in ((psel, psel_b, posT, posT_b), (aff, aff_b, affT, affT_b)):
                for t4 in range(0, ntt, 4):
                    n4 = min(4, ntt - t4)

                    def trf(h, t4=t4, n4=n4, src=src):
                        ins = None
                        for j in range(n4):
                            ins = h.transpose(psc[0:16, j * 128:(j + 1) * 128], src[:, t4 + j, :], ident_f[:])
                        return ins
                    S.op("pe", trf, reads=[src_b, ident_f_b], writes=[psc_b])
                    S.op("act", lambda h, t4=t4, n4=n4, dstT=dstT: h.activation(out=dstT[:, t4 * 128:(t4 + n4) * 128], in_=psc[0:16, :n4 * 128], func=AF.Copy),
                         reads=[psc_b], writes=[dstT_b])
            S.dma("sp", R["PT"][0][0, :, 0:ntt * 128], posT[:, 0:ntt * 128], R["PT"][1], [posT_b])
            S.dma("sp", R["PT"][0][1, :, 0:ntt * 128], affT[:, 0:ntt * 128], R["PT"][1], [affT_b])
            S.barrier()
        NS = CAP + CAPC
        with contextlib.ExitStack() as s3:
            w1, w1_b = K.sb("ew1", [128, 8, D], BF16, s3)
            w3, w3_b = K.sb("ew3", [128, 8, D], BF16, s3)
            w2, w2_b = K.sb("ew2", [128, 8, D], BF16, s3)
            Se, Se_b = K.sb("eS", [128, 32, 512], BF16, s3)
            Sc, Sc_b = K.sb("eSc", [128, 2, CAPC], BF16, s3)
            io, io_b = K.sb("eio", [128, 512], F32, s3)
            S.dma("sp", io[:], I["iota512"][:, :], io_b)
            xg, xg_b = K.sb("exg", [128, 8, NS], BF16, s3)
            gT, gT_b = K.sb("egT", [128, 8, NS], BF16, s3)
            sil, sil_b = K.sb("esil", [128, NS], F32, s3)
            yst = [K.sb("eyst%d" % i, [128, 512], BF16, s3) for i in range(2)]
            psG = [K.pst("epsG%d" % i, [128, 512], F32, s3) for i in range(2)]
            psA, psA_b = K.pst("epsA", [128, 512], F32, s3)
            psB, psB_b = K.pst("epsB", [128, 512], F32, s3)
            psC, psC_b = K.pst("epsC", [128, 512], F32, s3)
            psY = [K.pst("epsY%d" % i, [128, 512], F32, s3) for i in range(2)]
            ng = 0
            ny = 0
            for e in range(NE):
                S.dma("pool", w1[:], I["moe_w1"][l, e].rearrange("(k p) n -> p k n", p=128), w1_b)
                S.dma("pool", w3[:], I["moe_w3"][l, e].rearrange("(k p) n -> p k n", p=128), w3_b)
                S.dma("pool", w2[:], I["moe_w2"][l, e].rearrange("(k p) n -> p k n", p=128), w2_b)
                for tt in range(32):
                    S.op("dve", lambda h, tt=tt: h.tensor_scalar(out=Se[:, tt, :], in0=io[:], scalar1=psel[:, tt, e:e + 1], scalar2=None, op0=ALU.is_equal),
                         reads=[io_b, psel_b], writes=[Se_b])
                if ctx_out:
                    for tt in range(32, 34):
                        S.op("dve", lambda h, tt=tt: h.tensor_scalar(out=Sc[:, tt - 32, :], in0=io[:, 0:CAPC], scalar1=psel[:, tt, e:e + 1], scalar2=None,
                                                                     op0=ALU.is_equal), reads=[io_b, psel_b], writes=[Sc_b])
                for dk in range(8):
                    pg, pg_b = psG[ng % 2]
                    ng += 1
                    _mm_group(S, pg_b, pg[:], [(H2[:, tt, dk * 128:(dk + 1) * 128], Se[:, tt, :]) for tt in range(32)], [H2_b, Se_b])
                    S.op("act", lambda h, dk=dk, pg=pg: h.activation(out=xg[:, dk, 0:CAP], in_=pg[:], func=AF.Copy), reads=[pg_b], writes=[xg_b])
                    if ctx_out:
                        _mm_group(S, psC_b, psC[:, 0:CAPC], [(H2[:, tt, dk * 128:(dk + 1) * 128], Sc[:, tt - 32, :]) for tt in (32, 33)], [H2_b, Sc_b])
                        S.op("act", lambda h, dk=dk: h.activation(out=xg[:, dk, CAP:NS], in_=psC[:, 0:CAPC], func=AF.Copy), reads=[psC_b], writes=[xg_b])
                for m in range(8):
                    _mm_group(S, psA_b, psA[:], [(w1[:, k, m * 128:(m + 1) * 128], xg[:, k, 0:CAP]) for k in range(8)], [w1_b, xg_b])
                    _mm_group(S, psB_b, psB[:], [(w3[:, k, m * 128:(m + 1) * 128], xg[:, k, 0:CAP]) for k in range(8)], [w3_b, xg_b])
                    S.op("act", lambda h: h.activation(out=sil[:, 0:CAP], in_=psA[:], func=AF.Silu), reads=[psA_b], writes=[sil_b])
                    S.op("dve", lambda h, m=m: h.tensor_tensor(out=gT[:, m, 0:CAP], in0=psB[:], in1=sil[:, 0:CAP], op=ALU.mult),
                         reads=[psB_b, sil_b], writes=[gT_b])
                    if ctx_out:
                        _mm_group(S, psC_b, psC[:, 0:CAPC], [(w1[:, k, m * 128:(m + 1) * 128], xg[:, k, CAP:NS]) for k in range(8)], [w1_b, xg_b])
                        S.op("act", lambda h: h.activation(out=sil[:, CAP:NS], in_=psC[:, 0:CAPC], func=AF.Silu), reads=[psC_b], writes=[sil_b])
                        _mm_group(S, psC_b, psC[:, 0:CAPC], [(w3[:, k, m * 128:(m + 1) * 128], xg[:, k, CAP:NS]) for k in range(8)], [w3_b, xg_b])
                        S.op("dve", lambda h, m=m: h.tensor_tensor(out=gT[:, m, CAP:NS], in0=psC[:, 0:CAPC], in1=sil[:, CAP:NS], op=ALU.mult),
                             reads=[psC_b, sil_b], writes=[gT_b])
                for c in range(5 if ctx_out else 4):
                    rows = 128 if c < 4 else CAPC
                    for dh in range(2):
                        py, py_b = psY[ny % 2]
                        ys, ys_b = yst[ny % 2]
                        ny += 1
                        _mm_group(S, py_b, py[0:rows, :], [(gT[:, f, c * 128:c * 128 + rows], w2[:, f, dh * 512:(dh + 1) * 512]) for f in range(8)],
                                  [gT_b, w2_b])
                        S.op("act", lambda h, py=py, ys=ys, rows=rows: h.activation(out=ys[0:rows, :], in_=py[0:rows, :], func=AF.Copy),
                             reads=[py_b], writes=[ys_b])
                        S.dma("sp", R["YE"][0][e, c * 128:c * 128 + rows, dh * 512:(dh + 1) * 512], ys[0:rows, :], R["YE"][1], [ys_b])
            S.barrier()
        sH.close()
        with contextlib.ExitStack() as s4:
            YEs, YEs_b = K.sb("cYE", [128, NE, 5, 512], BF16, s4)
            ST, ST_b = K.sb("cST", [128, NE, 4, 512], BF16, s4)
            abc = [K.sb("cabc%d" % i, [128, 512], F32, s4) for i in range(2)]
            xh, xh_b = K.sb("cxh", [128, 4, 512], F32, s4)
            selm, selm_b = K.sb("cselm", [16, NE, 128], F32, s4)
            sidx, sidx_b = K.sb("csidx", [128, 5], F32, s4)
            posT, posT_b = K.sb("cposT", [16, T], F32, s4)
            affT, affT_b = K.sb("caffT", [16, T], F32, s4)
            S.dma("sp", posT[:, 0:ntt * 128], R["PT"][0][0, :, 0:ntt * 128], posT_b, [R["PT"][1]])
            S.dma("sp", affT[:, 0:ntt * 128], R["PT"][0][1, :, 0:ntt * 128], affT_b, [R["PT"][1]])
            S.dma("sp", selm[:], I["selm"].rearrange("k (e m) -> k e m", e=NE), selm_b)
            S.dma("sp", sidx[:], I["slotidx"][:, :], sidx_b)
            psP = [K.pst("cpsP%d" % i, [128, 512], F32, s4) for i in range(2)]
            psQ = [K.pst("cpsQ%d" % i, [128, 512], F32, s4) for i in range(2)]
            psO = [K.pst("cpsO%d" % i, [128, 512], F32, s4) for i in range(2)]
            nb_ = 0
            no = 0
            for dh in range(2):
                for e in range(NE):
                    S.dma("sp", YEs[:, e, 0:4, :], R["YE"][0][e, 0:CAP, dh * 512:(dh + 1) * 512].rearrange("(c p) d -> p c d", p=128), YEs_b, [R["YE"][1]])
                    if ctx_out:
                        S.dma("sp", YEs[0:CAPC, e, 4, :], R["YE"][0][e, CAP:NS, dh * 512:(dh + 1) * 512], YEs_b, [R["YE"][1]])
                for (t0, tn) in chunks:
                    r = 0 if t0 < L else 1
                    lat = t0 < L
                    for e in range(NE):
                        pp, pp_b = psP[nb_ % 2]
                        pq, pq_b = psQ[nb_ % 2]
                        ab, ab_b = abc[nb_ % 2]
                        nb_ += 1
                        _mm_group(S, pp_b, pp[:, :tn], [(selm[:, e, :], posT[:, t0:t0 + tn])], [selm_b, posT_b])
                        _mm_group(S, pq_b, pq[:, :tn], [(selm[:, e, :], affT[:, t0:t0 + tn])], [selm_b, affT_b])
                        S.op("act", lambda h, ab=ab, pq=pq: h.activation(out=ab[:, :tn], in_=pq[:, :tn], func=AF.Copy), reads=[pq_b], writes=[ab_b])
                        if lat:
                            for c in range(4):
                                S.op("dve", lambda h, c=c, pp=pp, ab=ab: h.scalar_tensor_tensor(out=ST[:, e, c, :tn], in0=pp[:, :tn], scalar=sidx[:, c:c + 1],
                                                                                               in1=ab[:, :tn], op0=ALU.is_equal, op1=ALU.mult),
                                     reads=[pp_b, ab_b, sidx_b], writes=[ST_b])
                        else:
                            S.op("dve", lambda h, pp=pp, ab=ab: h.scalar_tensor_tensor(out=ST[0:CAPC, e, 0, :tn], in0=pp[0:CAPC, :tn], scalar=sidx[0:CAPC, 4:5],
                                                                                      in1=ab[0:CAPC, :tn], op0=ALU.is_equal, op1=ALU.mult),
                                 reads=[pp_b, ab_b, sidx_b], writes=[ST_b])
                    S.dma("sp", xh[:, :, :tn], XTv[:, dh * 4:(dh + 1) * 4, t0:t0 + tn], xh_b, [R["XT"][1]])
                    for m in range(4):
                        po, po_b = psO[no % 2]
                        no += 1
                        if lat:
                            pairs = [(YEs[:, e, c, m * 128:(m + 1) * 128], ST[:, e, c, :tn]) for e in range(NE) for c in range(4)]
                        else:
                            pairs = [(YEs[0:CAPC, e, 4, m * 128:(m + 1) * 128], ST[0:CAPC, e, 0, :tn]) for e in range(NE)]
                        _mm_group(S, po_b, po[:, :tn], pairs, [YEs_b, ST_b])
                        S.op("dve", lambda h, m=m, po=po: h.scalar_tensor_tensor(out=xh[:, m, :tn], in0=po[:, :tn], scalar=modT[:, 40 + dh * 4 + m, r:r + 1],
                                                                                 in1=xh[:, m, :tn], op0=ALU.mult, op1=ALU.add),
                             reads=[po_b, modT_b, xh_b], writes=[xh_b])
                    S.dma("sp", XTv[:, dh * 4:(dh + 1) * 4, t0:t0 + tn], xh[:, :, :tn], R["XT"][1], [xh_b])
            S.barrier()


def phase_final(K):
    nc, S, I, R = K.nc, K.S, K.I, K.R
    ones_f, ones_f_b = K.ones_f
    XTv = R["XT"][0].rearrange("(k p) t -> p k t", p=128)
    with contextlib.ExitStack() as st:
        fg, fg_b = K.sb("fg", [128, 8], F32, st)
        S.dma("sp", fg[:], I["fgT"][:, :], fg_b)
        xs = [K.sb("fx%d" % i, [128, 8, 512], F32, st) for i in range(2)]
        sq, sq_b = K.sb("fsq", [128, 8, 512], F32, st)
        rstd, rstd_b = K.sb("frstd", [128, 512], F32, st)
        ps, ps_b = K.pst("ps_fin", [128, 512], F32, st)
        for ci in range(8):
            t0 = ci * 512
            x, x_b = xs[ci % 2]
            S.dma("sp", x[:], XTv[:, :, t0:t0 + 512], x_b, [R["XT"][1]])
            S.op("act", lambda h: h.activation(out=sq[:], in_=x[:], func=AF.Square), reads=[x_b], writes=[sq_b])
            _mm_group(S, ps_b, ps[:], [(ones_f[:], sq[:, k, :]) for k in range(8)], [ones_f_b, sq_b])
            S.op("dve", lambda h: h.tensor_scalar(out=rstd[:], in0=ps[:], scalar1=1.0 / D, scalar2=EPS, op0=ALU.mult, op1=ALU.add),
                 reads=[ps_b], writes=[rstd_b])
            S.op("act", lambda h: h.activation(out=rstd[:], in_=rstd[:], func=AF.Sqrt), reads=[rstd_b], writes=[rstd_b])
            S.op("dve", lambda h: h.reciprocal(out=rstd[:], in_=rstd[:]), reads=[rstd_b], writes=[rstd_b])
            for k in range(8):
                S.op("dve", lambda h, k=k: h.scalar_tensor_tensor(out=sq[:, k, :], in0=x[:, k, :], scalar=fg[:, k:k + 1], in1=rstd[:],
                                                                  op0=ALU.mult, op1=ALU.mult), reads=[x_b, rstd_b, fg_b], writes=[sq_b])
            S.dma("sp", K.outT.rearrange("(k p) t -> p k t", p=128)[:, :, t0:t0 + 512], sq[:], K.outT_b, [sq_b])
        S.barrier()


def rope_tables():
    rows = L // 64
    row = np.repeat(np.arange(rows, dtype=np.float32), 64)
    col = np.tile(np.arange(64, dtype=np.float32), rows)
    inv = (10000.0 ** (-np.arange(16, dtype=np.float32) / 16)).astype(np.float32)
    C = np.zeros((64, L), np.float32)
    Sg = np.zeros((64, L), np.float32)
    for j in range(64):
        a, hf, f = j // 32, (j % 32) // 16, j % 16
        pos = row if a == 0 else col
        ang = (pos * inv[f]).astype(np.float32)
        C[j] = np.cos(ang)
        Sg[j] = np.sin(ang) * (-1.0 if hf == 0 else 1.0)
    return np.concatenate([C, C], 0), np.concatenate([Sg, Sg], 0)


_HC = {}


def hyena_consts():
    if _HC:
        return _HC
    f32 = np.float32

    def emb_of(Lx):
        t = np.linspace(0.0, 1.0, Lx, dtype=f32)[:, None]
        w = (2.0 * math.pi * np.arange(Lx, dtype=f32)[:, None] / Lx).astype(f32)
        f = np.linspace(1e-4, 15, 16, dtype=f32)[None, :]
        e = np.concatenate([t, np.cos(f * w), -np.sin(f * w)], axis=-1).astype(f32)
        return np.ascontiguousarray(e.T), t[:, 0]
    eT, t = emb_of(L)
    eTc, tc = emb_of(LC)
    _HC["embT"], _HC["embTc"] = eT, eTc
    _HC["negt"] = np.ascontiguousarray((-t).reshape(L // 128, 128).T)
    _HC["negtc"] = np.ascontiguousarray((-tc).reshape(LC // 128, 128).T)
    _HC["deltas"] = np.abs(np.linspace(math.log(1e-2) / 0.3, math.log(1e-2) / 1.5, D, dtype=f32)).reshape(1, D).astype(f32)

    def dft(Lx, npad):
        N = 2 * Lx
        a = np.arange(Lx + 1, dtype=np.int64)
        ph = (a[:, None] * a[None, :]) % N
        ang = ph.astype(np.float64) * (2.0 * math.pi / N)
        C = np.zeros((npad, npad), f32)
        S_ = np.zeros((npad, npad), f32)
        C[:Lx + 1, :Lx + 1] = np.cos(ang)
        S_[:Lx + 1, :Lx + 1] = np.sin(ang)
        wfv = np.zeros(npad, f32)
        wfv[:Lx + 1] = 2.0 / N
        wfv[0] = 1.0 / N
        wfv[Lx] = 1.0 / N
        return C.astype(ml_dtypes.bfloat16), S_.astype(ml_dtypes.bfloat16), np.ascontiguousarray(wfv.reshape(npad // 128, 128).T)
    _HC["dftC"], _HC["dftS"], _HC["wf"] = dft(L, NF)
    _HC["dftCc"], _HC["dftSc"], _HC["wfc"] = dft(LC, NFC)
    return _HC


def host_inputs(inputs, b, nl):
    x, c, ctx, c_ctx = inputs["x"], inputs["c"], inputs["ctx"], inputs["c_ctx"]
    m = {}
    m["xT"] = np.ascontiguousarray(np.concatenate([x[b], ctx[b]], 0).T)
    cc = np.stack([c[b], c_ctx], 0)
    m["ccT"] = np.ascontiguousarray(cc.reshape(2, 8, 128).transpose(2, 1, 0))
    m["w_mod"] = np.ascontiguousarray(inputs["w_mod"][:nl])
    m["b_modT"] = np.ascontiguousarray(inputs["b_mod"][:nl].reshape(nl, 48, 128).transpose(0, 2, 1))
    m["g1T"] = np.ascontiguousarray(inputs["norm1_g"][:nl].reshape(nl, 8, 128).transpose(0, 2, 1))
    m["g2T"] = np.ascontiguousarray(inputs["norm2_g"][:nl].reshape(nl, 8, 128).transpose(0, 2, 1))
    m["w_in"] = np.ascontiguousarray(inputs["w_in"][:nl])
    rc, rs = rope_tables()
    m["ropeC"], m["ropeS"] = rc, rs
    m["ident"] = np.eye(128, dtype=np.float32)
    m["diff_lambda"] = np.ascontiguousarray(inputs["diff_lambda"][:nl].reshape(nl, 256))
    m["sublnT"] = np.ascontiguousarray(inputs["diff_subln_g"][:nl].reshape(nl, 128, 1))
    m["swa_sink"] = np.ascontiguousarray(inputs["swa_sink"][:nl])
    kk, qq = np.meshgrid(np.arange(128), np.arange(128), indexing="ij")
    m["trilo"] = (kk >= qq).astype(np.float32)
    m["triup"] = (kk <= qq).astype(np.float32)
    m["w_branch"] = np.ascontiguousarray(inputs["w_branch"][:nl])
    m["w_out"] = np.ascontiguousarray(inputs["w_out"][:nl])
    m["fgT"] = np.ascontiguousarray(inputs["final_g"].reshape(8, 128).T)
    if "hy_ff_w1" in inputs:
        cwv = inputs["hy_conv_w"][:nl]
        m["cwT"] = np.ascontiguousarray(cwv.reshape(nl, 3, 24, 128).transpose(0, 3, 2, 1))
        m["cbT"] = np.ascontiguousarray(inputs["hy_conv_b"][:nl].reshape(nl, 24, 128).transpose(0, 2, 1))
        m["hbT"] = np.ascontiguousarray(inputs["hy_bias"][:nl].reshape(nl, 8, 128).transpose(0, 2, 1))
        m["hy_w1"] = np.ascontiguousarray(inputs["hy_ff_w1"][:nl])
        m["hy_w2"] = np.ascontiguousarray(inputs["hy_ff_w2"][:nl])
        m["hy_w3"] = np.ascontiguousarray(inputs["hy_ff_w3"][:nl])
        m["hy_b1"] = np.ascontiguousarray(inputs["hy_ff_b1"][:nl].reshape(nl, 64, 1))
        m["hy_b2"] = np.ascontiguousarray(inputs["hy_ff_b2"][:nl].reshape(nl, 64, 1))
        m["hy_fr"] = np.ascontiguousarray(inputs["hy_sin_freq"][:nl].reshape(nl, 64, 1))
        m.update(hyena_consts())
    if "moe_w1" in inputs:
        m["router_w"] = np.ascontiguousarray(inputs["router_w"][:nl])
        for k in ("moe_w1", "moe_w3", "moe_w2"):
            m[k] = np.ascontiguousarray(inputs[k][:nl])
        sel = np.zeros((16, 16, 128), np.float32)
        for e in range(16):
            sel[e, e, :] = 1.0
        m["selm"] = sel.reshape(16, 16 * 128)
        si = np.zeros((128, 5), np.float32)
        for c in range(4):
            si[:, c] = np.arange(128) + 128 * c
        si[:, 4] = np.arange(128)
        m["slotidx"] = si
        m["iota512"] = np.tile(np.arange(512, dtype=np.float32)[None, :], (128, 1))
        m["ustrict"] = (kk < qq).astype(np.float32)
    return m


def kernel(**inputs):
    inputs = {k: np.asarray(v) for k, v in inputs.items()}
    nb = inputs["x"].shape[0]
    nc = build(dict(nl=DEPTH, phases=("diff", "swa", "hy", "merge", "moe")))
    in_maps = [host_inputs(inputs, b, DEPTH) for b in range(nb)]
    res = run_bass_kernel_spmd(nc, in_maps, core_ids=list(range(nb)))
    out = np.stack([np.asarray(res.results[b]["outT"]).T for b in range(nb)], 0)
    return np.ascontiguousarray(out.astype(np.float32))
```

```python
import contextlib
import math
import numpy as np
import ml_dtypes
import concourse.bass as bass
import concourse.mybir as mybir
from concourse.bass_utils import run_bass_kernel_spmd

F32 = mybir.dt.float32
BF16 = mybir.dt.bfloat16
AF = mybir.ActivationFunctionType
ALU = mybir.AluOpType
AX = mybir.AxisListType

D = 1024
L = 4096
LC = 256
T = L + LC
DEPTH = 4
D_IN = 10752
NE = 16
CAP = 512
CAPC = 32
EPS = 1e-6
NF = 4224
NFC = 384
TCH = [(i * 512, 512) for i in range(8)] + [(L, LC)]


class Buf:
    __slots__ = ("name", "w", "r", "sem", "cnt")

    def __init__(self, name):
        self.name = name
        self.w = {}
        self.r = {}
        self.sem = None
        self.cnt = 0


class Sched:
    def __init__(self, nc, es):
        self.nc, self.es = nc, es
        self.E = {}
        for n, h in (("pe", nc.tensor), ("dve", nc.vector), ("act", nc.scalar),
                     ("pool", nc.gpsimd), ("sp", nc.sync)):
            self.E[n] = dict(h=h, sem=es.enter_context(nc.semaphore("s_" + n)), cnt=0, seen={})
        self.dma_bufs = []
        self.nbuf = 0
        self.persist = True
        self.pool = []

    def buf(self, name):
        self.nbuf += 1
        b = Buf("%s_%d" % (name, self.nbuf))
        b.w["_persist"] = self.persist
        return b

    @staticmethod
    def _add(evs, d):
        for k, sv in d.items():
            if k == "_persist":
                continue
            sem, v = sv
            if k not in evs or evs[k][1] < v:
                evs[k] = (sem, v)

    def _waits(self, e, evs):
        E = self.E[e]
        for name, (sem, val) in evs.items():
            if E["seen"].get(name, 0) < val:
                E["h"].wait_ge(sem, val)
                E["seen"][name] = val

    def op(self, e, fn, reads=(), writes=(), skip_self=False, drain_self=False):
        evs = {}
        for b in reads:
            self._add(evs, b.w)
        for b in writes:
            self._add(evs, b.w)
            self._add(evs, b.r)
        if skip_self:
            evs.pop("s_" + e, None)
        if drain_self and self.E[e]["cnt"]:
            evs["s_" + e] = (self.E[e]["sem"], self.E[e]["cnt"])
        self._waits(e, evs)
        E = self.E[e]
        ins = fn(E["h"])
        E["cnt"] += 1
        ins.then_inc(E["sem"], 1)
        key, ev = "s_" + e, (E["sem"], E["cnt"])
        for b in reads:
            b.r[key] = ev
        for b in writes:
            b.w[key] = ev

    def dma(self, q, out_ap, in_ap, dst, srcs=(), **kw):
        evs = {}
        self._add(evs, dst.w)
        self._add(evs, dst.r)
        for b in srcs:
            self._add(evs, b.w)
        self._waits(q, evs)
        if dst.sem is None:
            if self.pool and not dst.w["_persist"]:
                dst.name, dst.sem, dst.cnt = self.pool.pop()
            else:
                dst.sem = self.es.enter_context(self.nc.semaphore("d_" + dst.name))
            self.dma_bufs.append(dst)
        ins = self.E[q]["h"].dma_start(out=out_ap, in_=in_ap, **kw)
        dst.cnt += 16
        ins.then_inc(dst.sem, 16)
        key, ev = "d_" + dst.name, (dst.sem, dst.cnt)
        dst.w[key] = ev
        for b in srcs:
            b.r[key] = ev

    def barrier(self):
        evs = {}
        for n, E in self.E.items():
            if E["cnt"]:
                evs["s_" + n] = (E["sem"], E["cnt"])
        for b in self.dma_bufs:
            evs["d_" + b.name] = (b.sem, b.cnt)
        for n in self.E:
            self._waits(n, evs)
        keep = []
        for b in self.dma_bufs:
            if b.w["_persist"]:
                keep.append(b)
            else:
                self.pool.append((b.name, b.sem, b.cnt))
        self.dma_bufs = keep

    def barrier_known(self):
        pass


class Ctx:
    pass


def _mm_group(S, ps, out_ap, pairs, reads):
    def fn(h):
        ins = None
        n = len(pairs)
        for i, (a, b) in enumerate(pairs):
            ins = h.matmul(out_ap, a, b, start=(i == 0), stop=(i == n - 1))
        return ins
    S.op("pe", fn, reads=reads, writes=[ps])


def _mm1(S, ps, out_ap, a, b, reads, start, stop, drain=False):
    S.op("pe", lambda h: h.matmul(out_ap, a, b, start=start, stop=stop), reads=reads, writes=[ps], skip_self=True, drain_self=drain)


def build(cfg):
    nl = cfg["nl"]
    debug = cfg.get("debug", ())
    stop_after = cfg.get("stop_after", None)
    nc = bass.Bass("TRN2", target_bir_lowering=False)
    es = contextlib.ExitStack()
    K = Ctx()
    K.nc, K.es, K.cfg = nc, es, cfg
    S = Sched(nc, es)
    K.S = S

    def din(name, shape, dt=F32):
        return nc.dram_tensor(name, list(shape), dt, kind="ExternalInput").ap()

    def scratch(name, shape, dt):
        kind = "ExternalOutput" if name in debug else "Internal"
        return nc.dram_tensor(name, list(shape), dt, kind=kind).ap()

    I = {}
    I["xT"] = din("xT", [D, T])
    I["ccT"] = din("ccT", [128, 8, 2])
    I["w_mod"] = din("w_mod", [nl, D, 6 * D])
    I["b_modT"] = din("b_modT", [nl, 128, 48])
    I["g1T"] = din("g1T", [nl, 128, 8])
    I["g2T"] = din("g2T", [nl, 128, 8])
    I["w_in"] = din("w_in", [nl, D, D_IN])
    I["ropeC"] = din("ropeC", [128, L])
    I["ropeS"] = din("ropeS", [128, L])
    I["ident"] = din("ident", [128, 128])
    I["diff_lambda"] = din("diff_lambda", [nl, 256])
    I["sublnT"] = din("sublnT", [nl, 128, 1])
    I["swa_sink"] = din("swa_sink", [nl, 16])
    I["trilo"] = din("trilo", [128, 128])
    I["triup"] = din("triup", [128, 128])
    I["w_branch"] = din("w_branch", [nl, 3, D, D])
    I["w_out"] = din("w_out", [nl, D, D])
    I["fgT"] = din("fgT", [128, 8])
    if "moe" in cfg.get("phases", ()):
        I["router_w"] = din("router_w", [nl, D, NE])
        I["moe_w1"] = din("moe_w1", [nl, NE, D, D])
        I["moe_w3"] = din("moe_w3", [nl, NE, D, D])
        I["moe_w2"] = din("moe_w2", [nl, NE, D, D])
        I["selm"] = din("selm", [16, 16 * 128])
        I["slotidx"] = din("slotidx", [128, 5])
        I["iota512"] = din("iota512", [128, 512])
        I["ustrict"] = din("ustrict", [128, 128])
    if "hy" in cfg.get("phases", ()):
        I["cwT"] = din("cwT", [nl, 128, 24, 3])
        I["cbT"] = din("cbT", [nl, 128, 24])
        I["hbT"] = din("hbT", [nl, 128, 8])
        I["hy_w1"] = din("hy_w1", [nl, 33, 64])
        I["hy_w2"] = din("hy_w2", [nl, 64, 64])
        I["hy_w3"] = din("hy_w3", [nl, 64, 2048])
        I["hy_b1"] = din("hy_b1", [nl, 64, 1])
        I["hy_b2"] = din("hy_b2", [nl, 64, 1])
        I["hy_fr"] = din("hy_fr", [nl, 64, 1])
        I["embT"] = din("embT", [33, L])
        I["embTc"] = din("embTc", [33, LC])
        I["negt"] = din("negt", [128, 32])
        I["negtc"] = din("negtc", [128, 2])
        I["deltas"] = din("deltas", [1, D])
        I["dftC"] = din("dftC", [NF, NF], BF16)
        I["dftS"] = din("dftS", [NF, NF], BF16)
        I["dftCc"] = din("dftCc", [NFC, NFC], BF16)
        I["dftSc"] = din("dftSc", [NFC, NFC], BF16)
        I["wf"] = din("wf", [128, NF // 128])
        I["wfc"] = din("wfc", [128, NFC // 128])
    K.I = I
    K.outT = nc.dram_tensor("outT", [D, L], F32, kind="ExternalOutput").ap()
    K.outT_b = S.buf("outT")

    R = {}
    R["XT"] = (scratch("XT", [D, T], F32), S.buf("XT"))
    R["QdT"] = (scratch("QdT", [D, T], BF16), S.buf("QdT"))
    R["KdT"] = (scratch("KdT", [D, T], BF16), S.buf("KdT"))
    R["Vd"] = (scratch("Vd", [T, D], BF16), S.buf("Vd"))
    R["HyT"] = (scratch("HyT", [3 * D, T], BF16), S.buf("HyT"))
    R["QsT"] = (scratch("QsT", [D, T], BF16), S.buf("QsT"))
    R["KsT"] = (scratch("KsT", [256, T], BF16), S.buf("KsT"))
    R["Vs"] = (scratch("Vs", [T, 256], BF16), S.buf("Vs"))
    R["GT"] = (scratch("GT", [3 * D, T], BF16), S.buf("GT"))
    R["YdT"] = (scratch("YdT", [D, T], BF16), S.buf("YdT"))
    R["YhT"] = (scratch("YhT", [D, T], BF16), S.buf("YhT"))
    R["YsT"] = (scratch("YsT", [D, T], BF16), S.buf("YsT"))
    R["X0T"] = (scratch("X0T", [D, T], BF16), S.buf("X0T"))
    R["UT"] = (scratch("UT", [D, T], BF16), S.buf("UT"))
    R["KF"] = (scratch("KF", [2, NF, D], F32), S.buf("KF"))
    R["KFc"] = (scratch("KFc", [2, NFC, D], F32), S.buf("KFc"))
    R["YF"] = (scratch("YF", [2, NF, D], BF16), S.buf("YF"))
    R["YFc"] = (scratch("YFc", [2, NFC, D], BF16), S.buf("YFc"))
    R["YE"] = (scratch("YE", [NE, CAP + CAPC, D], BF16), S.buf("YE"))
    R["XT1"] = (scratch("XT1", [D, T], F32), S.buf("XT1"))
    R["PT"] = (scratch("PT", [2, 16, T], F32), S.buf("PT"))
    R["LG"] = (scratch("LG", [128, T // 128, NE], F32), S.buf("LG"))
    R["H2d"] = (scratch("H2d", [128, T // 128, D], BF16), S.buf("H2d"))
    K.R = R

    uid = [0]

    def sb(name, shape, dt, stack=es):
        uid[0] += 1
        t = stack.enter_context(nc.sbuf_tensor("sb%d_%s" % (uid[0], name), list(shape), dt))
        return t, S.buf(name)

    def pst(name, shape, dt, stack):
        uid[0] += 1
        t = stack.enter_context(nc.psum_tensor("ps%d_%s" % (uid[0], name), list(shape), dt))
        return t, S.buf(name)
    K.sb, K.pst = sb, pst

    ident_f, ident_f_b = sb("ident_f", [128, 128], F32)
    ident_b, ident_b_b = sb("ident_b", [128, 128], BF16)
    ones_f, ones_f_b = sb("ones_f", [128, 128], F32)
    ones_b, ones_b_b = sb("ones_b", [128, 128], BF16)
    S.dma("sp", ident_f[:], I["ident"][:, :], ident_f_b)
    S.dma("pool", ident_b[:], I["ident"][:, :], ident_b_b)
    S.op("dve", lambda h: h.memset(ones_f[:], 1.0), writes=[ones_f_b])
    S.op("dve", lambda h: h.memset(ones_b[:], 1.0), writes=[ones_b_b])
    K.ident_f, K.ident_b, K.ones_f, K.ones_b = (ident_f, ident_f_b), (ident_b, ident_b_b), (ones_f, ones_f_b), (ones_b, ones_b_b)

    sc, sc_b = sb("sc", [128, 8, 2], F32)
    S.dma("sp", sc[:], I["ccT"][:, :, :], sc_b)
    S.op("act", lambda h: h.activation(out=sc[:], in_=sc[:], func=AF.Silu), reads=[sc_b], writes=[sc_b])
    K.sc = (sc, sc_b)
    modT, modT_b = sb("modT", [128, 48, 2], F32)
    K.modT = (modT, modT_b)
    A1, A1_b = sb("A1", [128, 8, 2], F32)
    A2, A2_b = sb("A2", [128, 8, 2], F32)
    K.A1, K.A2 = (A1, A1_b), (A2, A2_b)

    with contextlib.ExitStack() as ph:
        xc, xc_b = sb("xcp", [128, 8, 512], F32, ph)
        for (t0, tn) in TCH:
            S.dma("sp", xc[:, :, :tn], I["xT"].rearrange("(k p) t -> p k t", p=128)[:, :, t0:t0 + tn], xc_b)
            S.dma("sp", R["XT"][0].rearrange("(k p) t -> p k t", p=128)[:, :, t0:t0 + tn], xc[:, :, :tn], R["XT"][1], [xc_b])
        S.barrier()

    S.persist = False
    phases = cfg.get("phases", ("diff", "swa", "merge"))
    if "swa" not in phases:
        with contextlib.ExitStack() as ph:
            zt, zt_b = sb("zt2", [128, T], BF16, ph)
            S.op("dve", lambda h: h.memset(zt[:], 0.0), writes=[zt_b])
            for k in range(8):
                S.dma("sp", R["YsT"][0][k * 128:(k + 1) * 128, :], zt[:], R["YsT"][1], [zt_b])
            S.barrier()
    if "hy" not in phases:
        with contextlib.ExitStack() as ph:
            zt, zt_b = sb("zt", [128, T], BF16, ph)
            S.op("dve", lambda h: h.memset(zt[:], 0.0), writes=[zt_b])
            for k in range(8):
                S.dma("sp", R["YhT"][0][k * 128:(k + 1) * 128, :], zt[:], R["YhT"][1], [zt_b])
            S.barrier()
    for l in range(nl):
        ctx_out = l < DEPTH - 1
        phase_mod(K, l)
        with contextlib.ExitStack() as ph:
            hT, hT_b = sb("hT", [128, 8, T], BF16, ph)
            phase_norm(K, ph, R["XT"], K.A1, 0, (hT, hT_b))
            if stop_after == "norm1":
                dbg = scratch("dbg_hT", [D, T], BF16)
                S.dma("sp", dbg.rearrange("(k p) t -> p k t", p=128), hT[:], S.buf("dbg"), [hT_b])
                S.barrier()
                break
            phase_inproj(K, ph, l, (hT, hT_b))
            S.barrier()
        if stop_after == "inproj":
            break
        if "diff" in phases:
            phase_diff(K, l, ctx_out)
        if stop_after == "diff":
            break
        if "swa" in phases:
            phase_swa(K, l, ctx_out)
        if "hy" in phases:
            phase_hyena(K, l, ctx_out)
        if stop_after == "hy":
            break
        if stop_after == "swa":
            break
        if "merge" in phases:
            phase_merge(K, l)
        if stop_after == "merge":
            break
        if "moe" in phases:
            phase_moe(K, l, ctx_out)
        if stop_after == "moe":
            break
    if stop_after is None:
        phase_final(K)

    S.barrier()
    es.close()
    return nc


def phase_mod(K, l):
    nc, S, I = K.nc, K.S, K.I
    modT, modT_b = K.modT
    sc, sc_b = K.sc
    with contextlib.ExitStack() as ph:
        wts = [K.sb("wmod%d" % i, [128, 8, 512], F32, ph) for i in range(2)]
        bm, bm_b = K.sb("bmodT", [128, 48], F32, ph)
        g1, g1_b = K.sb("g1T", [128, 8], F32, ph)
        g2, g2_b = K.sb("g2T", [128, 8], F32, ph)
        ps, ps_b = K.pst("ps_mod", [128, 512], F32, ph)
        S.dma("sp", bm[:], I["b_modT"][l], bm_b)
        S.dma("sp", g1[:], I["g1T"][l], g1_b)
        S.dma("sp", g2[:], I["g2T"][l], g2_b)
        for g in range(12):
            wt, wt_b = wts[g % 2]
            S.dma("sp", wt[:], I["w_mod"][l].rearrange("(k p) n -> p k n", p=128)[:, :, g * 512:(g + 1) * 512], wt_b)
            for m in range(4):
                mc = g * 4 + m
                _mm_group(S, ps_b, ps[:, 0:2],
                          [(wt[:, k, m * 128:(m + 1) * 128], sc[:, k, :]) for k in range(8)],
                          [wt_b, sc_b])
                S.op("dve", lambda h, mc=mc: h.tensor_scalar(out=modT[:, mc, :], in0=ps[:, 0:2], scalar1=bm[:, mc:mc + 1],
                                                            scalar2=None, op0=ALU.add),
                     reads=[ps_b, bm_b], writes=[modT_b])
        for (A, A_b), (g, g_b), j in ((K.A1, (g1, g1_b), 1), (K.A2, (g2, g2_b), 4)):
            for r in range(2):
                S.op("dve", lambda h, A=A, g=g, j=j, r=r: h.scalar_tensor_tensor(
                    out=A[:, :, r], in0=modT[:, j * 8:(j + 1) * 8, r], scalar=1.0, in1=g[:], op0=ALU.add, op1=ALU.mult),
                    reads=[modT_b, g_b], writes=[A_b])
        S.barrier()


def phase_norm(K, ph, X, Acoef, shift_j, out, out_f32_cb=None):
    nc, S = K.nc, K.S
    XT, XT_b = X
    A, A_b = Acoef
    modT, modT_b = K.modT
    hT, hT_b = out
    ones_f, ones_f_b = K.ones_f
    with contextlib.ExitStack() as st:
        xs = [K.sb("nx%d" % i, [128, 8, 512], F32, st) for i in range(2)]
        sq, sq_b = K.sb("nsq", [128, 8, 512], F32, st)
        rstd, rstd_b = K.sb("nrstd", [128, 512], F32, st)
        tmp, tmp_b = K.sb("ntmp", [128, 512], F32, st)
        ps, ps_b = K.pst("ps_norm", [128, 512], F32, st)
        for ci, (t0, tn) in enumerate(TCH):
            r = 0 if t0 < L else 1
            x, x_b = xs[ci % 2]
            S.dma("sp", x[:, :, :tn], XT.rearrange("(k p) t -> p k t", p=128)[:, :, t0:t0 + tn], x_b, [XT_b])
            S.op("act", lambda h: h.activation(out=sq[:, :, :tn], in_=x[:, :, :tn], func=AF.Square), reads=[x_b], writes=[sq_b])
            _mm_group(S, ps_b, ps[:, :tn], [(ones_f[:], sq[:, k, :tn]) for k in range(8)], [ones_f_b, sq_b])
            S.op("dve", lambda h: h.tensor_scalar(out=rstd[:, :tn], in0=ps[:, :tn], scalar1=1.0 / D, scalar2=EPS,
                                                  op0=ALU.mult, op1=ALU.add), reads=[ps_b], writes=[rstd_b])
            S.op("act", lambda h: h.activation(out=rstd[:, :tn], in_=rstd[:, :tn], func=AF.Sqrt), reads=[rstd_b], writes=[rstd_b])
            S.op("dve", lambda h: h.reciprocal(out=rstd[:, :tn], in_=rstd[:, :tn]), reads=[rstd_b], writes=[rstd_b])
            for k in range(8):
                S.op("dve", lambda h, k=k: h.tensor_tensor(out=tmp[:, :tn], in0=x[:, k, :tn], in1=rstd[:, :tn], op=ALU.mult),
                     reads=[x_b, rstd_b], writes=[tmp_b])
                S.op("act", lambda h, k=k: h.activation(out=hT[:, k, t0:t0 + tn], in_=tmp[:, :tn], func=AF.Identity,
                                                        scale=A[:, k, r:r + 1], bias=modT[:, shift_j * 8 + k, r:r + 1]),
                     reads=[tmp_b, A_b, modT_b], writes=[hT_b])
                if out_f32_cb is not None:
                    out_f32_cb(ci, k, t0, tn, r, tmp, tmp_b)
        S.barrier()


def _inproj_groups():
    g = []
    g += [("qk", "QdT", 0), ("qk", "QdT", 512), ("qk", "KdT", 0), ("qk", "KdT", 512)]
    g += [("v", "Vd", 0), ("v", "Vd", 512)]
    g += [("plain", "HyT", i * 512) for i in range(6)]
    g += [("qk", "QsT", 0), ("qk", "QsT", 512)]
    g += [("kv", None, 0)]
    g += [("gate", "GT", i * 512) for i in range(6)]
    return g


def phase_inproj(K, ph, l, hTb):
    nc, S, I, R = K.nc, K.S, K.I, K.R
    hT, hT_b = hTb
    with contextlib.ExitStack() as st:
        wts = [K.sb("wi%d" % i, [128, 8, 512], BF16, st) for i in range(2)]
        wsw, wsw_b = K.sb("wsw", [128, 8, 512], BF16, st)
        rC, rC_b = K.sb("ropeC", [128, L], F32, st)
        rS, rS_b = K.sb("ropeS", [128, L], F32, st)
        stg = [K.sb("stg%d" % i, [128, T], BF16, st) for i in range(2)]
        vst = [K.sb("vst%d" % i, [128, 512], BF16, st) for i in range(2)]
        t1, t1_b = K.sb("rt1", [128, 512], F32, st)
        t2, t2_b = K.sb("rt2", [128, 512], F32, st)
        psA = [K.pst("psA%d" % i, [128, 512], F32, st) for i in range(2)]
        psB = [K.pst("psB%d" % i, [128, 512], F32, st) for i in range(2)]
        S.dma("sp", rC[:], I["ropeC"][:, :], rC_b)
        S.dma("sp", rS[:], I["ropeS"][:, :], rS_b)
        win = I["w_in"][l].rearrange("(k p) n -> p k n", p=128)
        cnt = dict(s=0, p=0, v=0)

        def fm_chunk(wt, wt_b, m, kind, dst, row0):
            sg, sg_b = stg[cnt["s"] % 2]
            cnt["s"] += 1
            for (t0, tn) in TCH:
                pa, pa_b = psA[cnt["p"] % 2]
                pb, pb_b = psB[cnt["p"] % 2]
                cnt["p"] += 1
                _mm_group(S, pa_b, pa[:, :tn], [(wt[:, k, m * 128:(m + 1) * 128], hT[:, k, t0:t0 + tn]) for k in range(8)],
                          [wt_b, hT_b])
                if kind == "qk" and t0 < L:
                    _mm_group(S, pb_b, pb[:, :tn], [(wsw[:, k, m * 128:(m + 1) * 128], hT[:, k, t0:t0 + tn]) for k in range(8)],
                              [wsw_b, hT_b])
                    S.op("dve", lambda h: h.tensor_tensor(out=t1[:, :tn], in0=pa[:, :tn], in1=rC[:, t0:t0 + tn], op=ALU.mult),
                         reads=[pa_b, rC_b], writes=[t1_b])
                    S.op("dve", lambda h: h.tensor_tensor(out=t2[:, :tn], in0=pb[:, :tn], in1=rS[:, t0:t0 + tn], op=ALU.mult),
                         reads=[pb_b, rS_b], writes=[t2_b])
                    S.op("pool", lambda h: h.tensor_tensor(out=sg[:, t0:t0 + tn], in0=t1[:, :tn], in1=t2[:, :tn], op=ALU.add),
                         reads=[t1_b, t2_b], writes=[sg_b])
                elif kind == "gate":
                    S.op("act", lambda h: h.activation(out=sg[:, t0:t0 + tn], in_=pa[:, :tn], func=AF.Sigmoid),
                         reads=[pa_b], writes=[sg_b])
                else:
                    S.op("act", lambda h: h.activation(out=sg[:, t0:t0 + tn], in_=pa[:, :tn], func=AF.Copy),
                         reads=[pa_b], writes=[sg_b])
            S.dma("sp", R[dst][0][row0:row0 + 128, :], sg[:], R[dst][1], [sg_b])

        def tm_cols(wt, wt_b, c0, cn, dst, col0):
            for tt in range(T // 128):
                pa, pa_b = psA[cnt["p"] % 2]
                cnt["p"] += 1
                vs, vs_b = vst[cnt["v"] % 2]
                cnt["v"] += 1
                _mm_group(S, pa_b, pa[:, :cn], [(hT[:, k, tt * 128:(tt + 1) * 128], wt[:, k, c0:c0 + cn]) for k in range(8)],
                          [wt_b, hT_b])
                S.op("act", lambda h: h.activation(out=vs[:, :cn], in_=pa[:, :cn], func=AF.Copy), reads=[pa_b], writes=[vs_b])
                S.dma("sp", R[dst][0][tt * 128:(tt + 1) * 128, col0:col0 + cn], vs[:, :cn], R[dst][1], [vs_b])

        def make_swapped(wt, wt_b, ncols):
            src = wt[:, :, :ncols].rearrange("p k (q s f) -> p k q s f", s=2, f=16)
            dstv = wsw[:, :, :ncols].rearrange("p k (q s f) -> p k q s f", s=2, f=16)
            for k in range(8):
                S.op("pool", lambda h, k=k: h.tensor_copy(out=dstv[:, k, :, 0, :], in_=src[:, k, :, 1, :]), reads=[wt_b], writes=[wsw_b])
                S.op("pool", lambda h, k=k: h.tensor_copy(out=dstv[:, k, :, 1, :], in_=src[:, k, :, 0, :]), reads=[wt_b], writes=[wsw_b])

        for gi, (kind, dst, row0) in enumerate(_inproj_groups()):
            wt, wt_b = wts[gi % 2]
            S.dma("pool", wt[:], win[:, :, gi * 512:(gi + 1) * 512], wt_b)
            if kind == "qk":
                make_swapped(wt, wt_b, 512)
                for m in range(4):
                    fm_chunk(wt, wt_b, m, "qk", dst, row0 + m * 128)
            elif kind == "v":
                tm_cols(wt, wt_b, 0, 512, dst, row0)
            elif kind == "kv":
                make_swapped(wt, wt_b, 256)
                for m in range(2):
                    fm_chunk(wt, wt_b, m, "qk", "KsT", m * 128)
                tm_cols(wt, wt_b, 256, 256, "Vs", 0)
            else:
                for m in range(4):
                    fm_chunk(wt, wt_b, m, kind, dst, row0 + m * 128)
        S.barrier()


def _bcast_rows(ap, n):
    return bass.AP(ap.tensor, ap.offset, [[0, 128], [1, n]])


def phase_diff(K, l, ctx_out):
    nc, S, I, R = K.nc, K.S, K.I, K.R
    ones_f, ones_f_b = K.ones_f
    ones_b, ones_b_b = K.ones_b
    lambda_init = 0.8 - 0.6 * math.exp(-0.3 * l)
    with contextlib.ExitStack() as st:
        lp, lp_b = K.sb("lp", [128, 256], F32, st)
        lsc, lsc_b = K.sb("lsc", [128, 4], F32, st)
        gsc, gsc_b = K.sb("gsc", [128, 1], F32, st)
        S.dma("sp", lp[:], _bcast_rows(I["diff_lambda"][l], 256), lp_b)
        S.dma("sp", gsc[:], I["sublnT"][l], gsc_b)
        S.op("dve", lambda h: h.tensor_tensor(out=lp[:, 0:64], in0=lp[:, 0:64], in1=lp[:, 64:128], op=ALU.mult), reads=[lp_b], writes=[lp_b])
        S.op("dve", lambda h: h.tensor_tensor(out=lp[:, 128:192], in0=lp[:, 128:192], in1=lp[:, 192:256], op=ALU.mult), reads=[lp_b], writes=[lp_b])
        S.op("dve", lambda h: h.reduce_sum(out=lsc[:, 0:1], in_=lp[:, 0:64], axis=AX.X), reads=[lp_b], writes=[lsc_b])
        S.op("dve", lambda h: h.reduce_sum(out=lsc[:, 1:2], in_=lp[:, 128:192], axis=AX.X), reads=[lp_b], writes=[lsc_b])
        S.op("act", lambda h: h.activation(out=lsc[:, 0:2], in_=lsc[:, 0:2], func=AF.Exp), reads=[lsc_b], writes=[lsc_b])
        S.op("dve", lambda h: h.scalar_tensor_tensor(out=lsc[:, 2:3], in0=lsc[:, 1:2], scalar=-lambda_init, in1=lsc[:, 0:1],
                                                     op0=ALU.add, op1=ALU.subtract), reads=[lsc_b], writes=[lsc_b])
        S.op("dve", lambda h: h.tensor_scalar(out=gsc[:], in0=gsc[:], scalar1=1.0 - lambda_init, scalar2=None, op0=ALU.mult),
             reads=[gsc_b], writes=[gsc_b])

        QT = [K.sb("dQT%d" % i, [128, T], BF16, st) for i in range(2)]
        KT = [K.sb("dKT%d" % i, [128, T], BF16, st) for i in range(2)]
        VV = [K.sb("dV%d" % i, [128, T // 128, 128], BF16, st) for i in range(2)]
        pt = [[K.sb("dP%d%d" % (m, i), [128, 512], BF16, st) for i in range(2)] for m in range(2)]
        psS = [[K.pst("dpsS%d%d" % (m, i), [128, 512], F32, st) for i in range(2)] for m in range(2)]
        psO = [K.pst("dpsO%d" % m, [128, 512], F32, st) for m in range(2)]
        psZ = [K.pst("dpsZ%d" % m, [128, 512], F32, st) for m in range(2)]
        rz = [K.sb("drz%d" % m, [128, 512], F32, st) for m in range(2)]
        oo = [K.sb("doo%d" % m, [128, 512], F32, st) for m in range(2)]
        of, of_b = K.sb("dof", [128, 512], F32, st)
        sqf, sqf_b = K.sb("dsq", [128, 512], F32, st)
        rs, rs_b = K.sb("drs", [128, 512], F32, st)
        ost = [K.sb("dost%d" % i, [128, 512], BF16, st) for i in range(2)]
        nch = 0
        for hd in range(8):
            q, q_b = QT[hd % 2]
            k, k_b = KT[hd % 2]
            v, v_b = VV[hd % 2]
            S.dma("sp", q[:], R["QdT"][0][hd * 128:(hd + 1) * 128, :], q_b, [R["QdT"][1]])
            S.dma("sp", k[:], R["KdT"][0][hd * 128:(hd + 1) * 128, :], k_b, [R["KdT"][1]])
            S.dma("sp", v[:], R["Vd"][0][:, hd * 128:(hd + 1) * 128].rearrange("(t p) e -> p t e", p=128), v_b, [R["Vd"][1]])
            for (q0, qn) in TCH:
                if q0 >= L and not ctx_out:
                    continue
                kts = list(range(T // 128)) if q0 < L else [32, 33]
                def qk_exp(i, kt, m, drain=False):
                    ps, ps_b = psS[m][i % 2]
                    p, p_b = pt[m][i % 2]
                    _mm1(S, ps_b, ps[:, :qn], k[m * 64:(m + 1) * 64, kt * 128:(kt + 1) * 128], q[m * 64:(m + 1) * 64, q0:q0 + qn],
                         [k_b, q_b], True, True, drain=drain)
                    S.op("act", lambda h, ps=ps, p=p: h.activation(out=p[:, :qn], in_=ps[:, :qn], func=AF.Exp, scale=0.125),
                         reads=[ps_b], writes=[p_b])
                qk_exp(0, kts[0], 0, drain=True)
                qk_exp(0, kts[0], 1, drain=True)
                for i, kt in enumerate(kts):
                    first, last = i == 0, i == len(kts) - 1
                    for m in range(2):
                        if not last:
                            qk_exp(i + 1, kts[i + 1], m, drain=(i == 0 and m == 0))
                        p, p_b = pt[m][i % 2]
                        _mm1(S, psO[m][1], psO[m][0][:, :qn], v[:, kt, :], p[:, :qn], [v_b, p_b], first, last)
                        _mm1(S, psZ[m][1], psZ[m][0][:, :qn], ones_b[:], p[:, :qn], [ones_b_b, p_b], first, last)
                for m in range(2):
                    S.op("dve", lambda h, m=m: h.reciprocal(out=rz[m][0][:, :qn], in_=psZ[m][0][:, :qn]), reads=[psZ[m][1]], writes=[rz[m][1]])
                    S.op("dve", lambda h, m=m: h.tensor_tensor(out=oo[m][0][:, :qn], in0=psO[m][0][:, :qn], in1=rz[m][0][:, :qn], op=ALU.mult),
                         reads=[psO[m][1], rz[m][1]], writes=[oo[m][1]])
                S.op("dve", lambda h: h.scalar_tensor_tensor(out=of[:, :qn], in0=oo[1][0][:, :qn], scalar=lsc[:, 2:3], in1=oo[0][0][:, :qn],
                                                              op0=ALU.mult, op1=ALU.add), reads=[oo[0][1], oo[1][1], lsc_b], writes=[of_b])
                S.op("pool", lambda h: h.tensor_tensor(out=sqf[:, :qn], in0=of[:, :qn], in1=of[:, :qn], op=ALU.mult), reads=[of_b], writes=[sqf_b])
                ps, ps_b = psS[0][0]
                _mm_group(S, ps_b, ps[:, :qn], [(ones_f[:], sqf[:, :qn])], [ones_f_b, sqf_b])
                S.op("dve", lambda h: h.tensor_scalar(out=rs[:, :qn], in0=ps[:, :qn], scalar1=1.0 / 128, scalar2=EPS, op0=ALU.mult, op1=ALU.add),
                     reads=[ps_b], writes=[rs_b])
                S.op("act", lambda h: h.activation(out=rs[:, :qn], in_=rs[:, :qn], func=AF.Sqrt), reads=[rs_b], writes=[rs_b])
                S.op("dve", lambda h: h.reciprocal(out=rs[:, :qn], in_=rs[:, :qn]), reads=[rs_b], writes=[rs_b])
                S.op("dve", lambda h: h.tensor_tensor(out=of[:, :qn], in0=of[:, :qn], in1=rs[:, :qn], op=ALU.mult), reads=[of_b, rs_b], writes=[of_b])
                o, o_b = ost[nch % 2]
                nch += 1
                S.op("act", lambda h, o=o: h.activation(out=o[:, :qn], in_=of[:, :qn], func=AF.Copy, scale=gsc[:, 0:1]), reads=[of_b, gsc_b], writes=[o_b])
                S.dma("sp", R["YdT"][0][hd * 128:(hd + 1) * 128, q0:q0 + qn], o[:, :qn], R["YdT"][1], [o_b])
        S.barrier()


def phase_swa(K, l, ctx_out):
    nc, S, I, R = K.nc, K.S, K.I, K.R
    ones_b, ones_b_b = K.ones_b
    with contextlib.ExitStack() as st:
        es_, es_b = K.sb("esink", [128, 16], F32, st)
        S.dma("sp", es_[:], _bcast_rows(I["swa_sink"][l], 16), es_b)
        S.op("act", lambda h: h.activation(out=es_[:], in_=es_[:], func=AF.Exp), reads=[es_b], writes=[es_b])
        mlo, mlo_b = K.sb("mlo", [128, 128], BF16, st)
        mup, mup_b = K.sb("mup", [128, 128], BF16, st)
        S.dma("pool", mlo[:], I["trilo"][:, :], mlo_b)
        S.dma("pool", mup[:], I["triup"][:, :], mup_b)
        QC = [[K.sb("sQ%d%d" % (c, i), [128, T], BF16, st) for i in range(2)] for c in range(2)]
        KT = [K.sb("sKT%d" % i, [128, T], BF16, st) for i in range(2)]
        VV = [K.sb("sV%d" % i, [128, T // 128, 64], BF16, st) for i in range(2)]
        pt = [K.sb("sP%d" % i, [128, 512], BF16, st) for i in range(2)]
        psS = [K.pst("spsS%d" % i, [128, 512], F32, st) for i in range(2)]
        psO = [K.pst("spsO%d" % i, [128, 512], F32, st) for i in range(2)]
        psZ = [K.pst("spsZ%d" % i, [128, 512], F32, st) for i in range(2)]
        zz, zz_b = K.sb("szz", [64, 512], F32, st)
        ost = [K.sb("sost%d" % i, [64, 512], BF16, st) for i in range(2)]
        nblk = 0
        npt = 0
        for kh in range(4):
            k, k_b = KT[kh % 2]
            v, v_b = VV[kh % 2]
            for half in range(2):
                S.dma("sp", k[half * 64:(half + 1) * 64, :], R["KsT"][0][kh * 64:(kh + 1) * 64, :], k_b, [R["KsT"][1]])
            S.dma("sp", v[:], R["Vs"][0][:, kh * 64:(kh + 1) * 64].rearrange("(t p) e -> p t e", p=128), v_b, [R["Vs"][1]])
            qc = []
            for c in range(2):
                qq, qq_b = QC[c][kh % 2]
                S.dma("sp", qq[:], R["QsT"][0][kh * 256 + c * 128: kh * 256 + (c + 1) * 128, :], qq_b, [R["QsT"][1]])
                qc.append((qq, qq_b))
            nqb = T // 128 if ctx_out else L // 128
            dbgl = K.cfg.get("swa_dbg", 9)
            if dbgl < 9:
                nqb = 2 if kh == 0 else 0
            steps = []
            for qb in range(nqb):
                if qb < 32:
                    kts = [(kk, mk) for kk, mk in ((qb - 1, "lo"), (qb, None), (qb + 1, "up")) if 0 <= kk < 32] + [(32, None), (33, None)]
                else:
                    kts = [(32, None), (33, None)]
                for i, (kt, mk) in enumerate(kts):
                    steps.append((qb, kt, mk, i == 0, i == len(kts) - 1))

            def stage_a(si):
                qb, kt, mk, first, last = steps[si]
                ps, ps_b = psS[si % 2]
                p, p_b = pt[si % 2]
                for gi, g in enumerate((0, 2, 1, 3)):
                    r0 = (g % 2) * 64
                    qq, qq_b = qc[g // 2]
                    _mm1(S, ps_b, ps[:, g * 128:(g + 1) * 128], k[r0:r0 + 64, kt * 128:(kt + 1) * 128], qq[r0:r0 + 64, qb * 128:(qb + 1) * 128],
                         [k_b, qq_b], True, True, drain=(gi == 2 or (gi == 0 and si <= 1)))
                S.op("act", lambda h: h.activation(out=p[:], in_=ps[:], func=AF.Exp, scale=0.125), reads=[ps_b], writes=[p_b])
                if mk is not None:
                    mt, mt_b = (mlo, mlo_b) if mk == "lo" else (mup, mup_b)
                    for g in range(4):
                        S.op("pool", lambda h, g=g, mt=mt: h.tensor_tensor(out=p[:, g * 128:(g + 1) * 128], in0=p[:, g * 128:(g + 1) * 128], in1=mt[:], op=ALU.mult),
                             reads=[p_b, mt_b], writes=[p_b])

            if steps:
                stage_a(0)
            for si, (qb, kt, mk, first, last) in enumerate(steps):
                if si + 1 < len(steps):
                    stage_a(si + 1)
                po, po_b = psO[qb % 2]
                pz, pz_b = psZ[qb % 2]
                p, p_b = pt[si % 2]
                _mm1(S, po_b, po[0:64, :], v[:, kt, :], p[:], [v_b, p_b], first, last)
                _mm1(S, pz_b, pz[0:64, :], ones_b[:, 0:64], p[:], [ones_b_b, p_b], first, last)
                if not last:
                    continue
                for g in range(4):
                    S.op("dve", lambda h, g=g: h.tensor_scalar(out=zz[:, g * 128:(g + 1) * 128], in0=pz[0:64, g * 128:(g + 1) * 128],
                                                               scalar1=es_[0:64, kh * 4 + g:kh * 4 + g + 1], scalar2=None, op0=ALU.add),
                         reads=[pz_b, es_b], writes=[zz_b])
                S.op("dve", lambda h: h.reciprocal(out=zz[:], in_=zz[:]), reads=[zz_b], writes=[zz_b])
                o, o_b = ost[qb % 2]
                S.op("dve", lambda h, o=o: h.tensor_tensor(out=o[:], in0=po[0:64, :], in1=zz[:], op=ALU.mult),
                     reads=[po_b, zz_b], writes=[o_b])
                S.dma("sp", R["YsT"][0][kh * 256:(kh + 1) * 256, qb * 128:(qb + 1) * 128].rearrange("(g d) q -> d g q", g=4),
                      o[:].rearrange("p (g q) -> p g q", g=4), R["YsT"][1], [o_b])
        S.barrier()


def phase_merge(K, l):
    nc, S, I, R = K.nc, K.S, K.I, K.R
    modT, modT_b = K.modT
    with contextlib.ExitStack() as st:
        wb, wb_b = K.sb("wbr", [128, 24, D], BF16, st)
        wo, wo_b = K.sb("wout", [128, 8, D], BF16, st)
        for b in range(3):
            S.dma("pool", wb[:, b * 8:(b + 1) * 8, :], I["w_branch"][l, b].rearrange("(k p) n -> p k n", p=128), wb_b)
        S.dma("pool", wo[:], I["w_out"][l].rearrange("(k p) n -> p k n", p=128), wo_b)
        yb = [K.sb("my%d" % b, [128, 8, 512], BF16, st) for b in range(3)]
        gt, gt_b = K.sb("mgt", [128, 24, 512], BF16, st)
        mg, mg_b = K.sb("mmg", [128, 8, 512], F32, st)
        mgb, mgb_b = K.sb("mmgb", [128, 8, 512], BF16, st)
        tmp = [K.sb("mtmp%d" % i, [128, 512], F32, st) for i in range(2)]
        xx, xx_b = K.sb("mx", [128, 8, 512], F32, st)
        pss = [K.pst("mps%d" % i, [128, 512], F32, st) for i in range(4)]
        npz = 0
        ntmp = 0
        XTv = R["XT"][0].rearrange("(k p) t -> p k t", p=128)
        for (t0, tn) in TCH:
            r = 0 if t0 < L else 1
            for b, nm in enumerate(("YdT", "YhT", "YsT")):
                S.dma("sp", yb[b][0][:, :, :tn], R[nm][0].rearrange("(k p) t -> p k t", p=128)[:, :, t0:t0 + tn], yb[b][1], [R[nm][1]])
            S.dma("sp", gt[:, :, :tn], R["GT"][0].rearrange("(k p) t -> p k t", p=128)[:, :, t0:t0 + tn], gt_b, [R["GT"][1]])
            S.dma("sp", xx[:, :, :tn], XTv[:, :, t0:t0 + tn], xx_b, [R["XT"][1]])
            for m in range(8):
                for b in range(3):
                    ps, ps_b = pss[npz % 4]
                    npz += 1
                    _mm_group(S, ps_b, ps[:, :tn], [(wb[:, b * 8 + k, m * 128:(m + 1) * 128], yb[b][0][:, k, :tn]) for k in range(8)],
                              [wb_b, yb[b][1]])
                    if b == 0:
                        S.op("dve", lambda h, m=m, ps=ps: h.tensor_tensor(out=mg[:, m, :tn], in0=ps[:, :tn], in1=gt[:, m, :tn], op=ALU.mult),
                             reads=[ps_b, gt_b], writes=[mg_b])
                    else:
                        tp, tp_b = tmp[ntmp % 2]
                        ntmp += 1
                        S.op("dve", lambda h, m=m, b=b, ps=ps, tp=tp: h.tensor_tensor(out=tp[:, :tn], in0=ps[:, :tn], in1=gt[:, b * 8 + m, :tn], op=ALU.mult),
                             reads=[ps_b, gt_b], writes=[tp_b])
                        S.op("pool", lambda h, m=m, tp=tp: h.tensor_tensor(out=mg[:, m, :tn], in0=mg[:, m, :tn], in1=tp[:, :tn], op=ALU.add),
                             reads=[tp_b, mg_b], writes=[mg_b])
                S.op("act", lambda h, m=m: h.activation(out=mgb[:, m, :tn], in_=mg[:, m, :tn], func=AF.Copy), reads=[mg_b], writes=[mgb_b])
            for m in range(8):
                ps, ps_b = pss[npz % 4]
                npz += 1
                _mm_group(S, ps_b, ps[:, :tn], [(wo[:, k, m * 128:(m + 1) * 128], mgb[:, k, :tn]) for k in range(8)], [wo_b, mgb_b])
                S.op("dve", lambda h, m=m, ps=ps: h.scalar_tensor_tensor(out=xx[:, m, :tn], in0=ps[:, :tn], scalar=modT[:, 16 + m, r:r + 1],
                                                                         in1=xx[:, m, :tn], op0=ALU.mult, op1=ALU.add),
                     reads=[ps_b, modT_b, xx_b], writes=[xx_b])
            S.dma("sp", XTv[:, :, t0:t0 + tn], xx[:, :, :tn], R["XT"][1], [xx_b])
        S.barrier()


PI = math.pi


def _hy_filter(K, l, Lx, embT_ap, negt_ap, Cm, Sm, wf_ap, KFs):
    nc, S, I, R = K.nc, K.S, K.I, K.R
    ones_b, ones_b_b = K.ones_b
    nlt = Lx // 128
    nft = (Lx + 128) // 128
    nch = max(1, Lx // 512)
    cw = min(512, Lx)
    with contextlib.ExitStack() as st:
        w1, w1_b = K.sb("fw1", [33, 64], F32, st)
        w2, w2_b = K.sb("fw2", [64, 64], F32, st)
        w3, w3_b = K.sb("fw3", [64, 2048], F32, st)
        b1, b1_b = K.sb("fb1", [64, 1], F32, st)
        b2, b2_b = K.sb("fb2", [64, 1], F32, st)
        fr, fr_b = K.sb("ffr", [64, 1], F32, st)
        emb, emb_b = K.sb("femb", [33, Lx], F32, st)
        ngt, ngt_b = K.sb("fngt", [128, nlt], F32, st)
        dl, dl_b = K.sb("fdl", [128, D], F32, st)
        wf, wf_b = K.sb("fwf", [128, nft], F32, st)
        z1, z1_b = K.sb("fz1", [64, Lx], F32, st)
        z2, z2_b = K.sb("fz2", [64, Lx], F32, st)
        rn, rn_b = K.sb("frn", [128, D], F32, st)
        dec, dec_b = K.sb("fdec", [128, D], F32, st)
        hd = [K.sb("fhd%d" % i, [128, D], F32, st) for i in range(2)]
        ab, ab_b = K.sb("fab", [128, 512], BF16, st)
        gsel, gsel_b = K.sb("fgsel", [64, 512], F32, st)
        AT, AT_b = K.sb("fAT", [128, nlt, D], BF16, st)
        for t_, src in ((w1, I["hy_w1"][l]), (w2, I["hy_w2"][l]), (w3, I["hy_w3"][l]), (b1, I["hy_b1"][l]), (b2, I["hy_b2"][l]),
                        (fr, I["hy_fr"][l]), (emb, embT_ap), (ngt, negt_ap), (wf, wf_ap)):
            pass
        S.dma("sp", w1[:], I["hy_w1"][l], w1_b)
        S.dma("sp", w2[:], I["hy_w2"][l], w2_b)
        S.dma("sp", w3[:], I["hy_w3"][l], w3_b)
        S.dma("sp", b1[:], I["hy_b1"][l], b1_b)
        S.dma("sp", b2[:], I["hy_b2"][l], b2_b)
        S.dma("sp", fr[:], I["hy_fr"][l], fr_b)
        S.dma("sp", emb[:], embT_ap, emb_b)
        S.dma("sp", ngt[:], negt_ap, ngt_b)
        S.dma("sp", wf[:], wf_ap, wf_b)
        S.dma("sp", dl[:], _bcast_rows(I["deltas"][0], D), dl_b)
        psm = [K.pst("fps%d" % i, [128, 512], F32, st) for i in range(4)]
        psN = [K.pst("fpsN%d" % i, [128, 512], F32, st) for i in range(2)]
        for (wm, wm_b, bb, bb_b, src, src_b, dst, dst_b) in ((w1, w1_b, b1, b1_b, emb, emb_b, z1, z1_b), (w2, w2_b, b2, b2_b, z1, z1_b, z2, z2_b)):
            for ci in range(nch):
                ps, ps_b = psm[ci % 4]
                sl = slice(ci * cw, (ci + 1) * cw)
                _mm_group(S, ps_b, ps[0:64, :cw], [(wm[:], src[:, sl])], [wm_b, src_b])
                S.op("dve", lambda h, ps=ps, sl=sl, bb=bb, dst=dst: h.tensor_scalar(out=dst[:, sl], in0=ps[0:64, :cw], scalar1=bb[:, 0:1], scalar2=fr[:, 0:1],
                                                                                 op0=ALU.add, op1=ALU.mult), reads=[ps_b, bb_b, fr_b], writes=[dst_b])
                for _ in range(2):
                    for cop, sh in ((ALU.is_gt, -2.0 * PI), (ALU.is_lt, 2.0 * PI)):
                        thr = PI if cop == ALU.is_gt else -PI
                        S.op("dve", lambda h, sl=sl, dst=dst, cop=cop, thr=thr: h.tensor_scalar(out=gsel[:, :cw], in0=dst[:, sl], scalar1=thr, scalar2=None, op0=cop),
                             reads=[dst_b], writes=[gsel_b])
                        S.op("dve", lambda h, sl=sl, dst=dst, sh=sh: h.scalar_tensor_tensor(out=dst[:, sl], in0=gsel[:, :cw], scalar=sh, in1=dst[:, sl],
                                                                                          op0=ALU.mult, op1=ALU.add), reads=[gsel_b, dst_b], writes=[dst_b])
                S.op("act", lambda h, sl=sl, dst=dst: h.activation(out=dst[:, sl], in_=dst[:, sl], func=AF.Sin), reads=[dst_b], writes=[dst_b])

        def htile(lt, cb):
            S.op("act", lambda h: h.activation(out=dec[:], in_=dl[:], func=AF.Exp, scale=ngt[:, lt:lt + 1]), reads=[dl_b, ngt_b], writes=[dec_b])
            for j in range(4):
                ps, ps_b = psm[j]
                _mm_group(S, ps_b, ps[:], [(z2[:, lt * 128:(lt + 1) * 128], w3[:, j * 512:(j + 1) * 512])], [z2_b, w3_b])
            cb(lt)

        for lt in range(nlt):
            def cb1(lt):
                for j in range(4):
                    ps, ps_b = psm[j]
                    half = j % 2
                    tf, tf_b = hd[j // 2]
                    S.op("dve", lambda h, ps=ps, half=half, tf=tf: h.tensor_tensor(out=tf[:, half * 512:(half + 1) * 512], in0=ps[:],
                                                                                   in1=dec[:, half * 512:(half + 1) * 512], op=ALU.mult),
                         reads=[ps_b, dec_b], writes=[tf_b])
                    S.op("act", lambda h, half=half, tf=tf: h.activation(out=ab[:], in_=tf[:, half * 512:(half + 1) * 512], func=AF.Abs),
                         reads=[tf_b], writes=[ab_b])
                    first = (lt == 0 and j < 2)
                    last = (lt == nlt - 1 and j >= 2)
                    _mm1(S, psN[half][1], psN[half][0][:], ones_b[:], ab[:], [ones_b_b, ab_b], first, last)
            htile(lt, cb1)
        for half in range(2):
            S.op("dve", lambda h, half=half: h.tensor_scalar(out=rn[:, half * 512:(half + 1) * 512], in0=psN[half][0][:], scalar1=EPS, scalar2=None, op0=ALU.add),
                 reads=[psN[half][1]], writes=[rn_b])
        S.op("dve", lambda h: h.reciprocal(out=rn[:], in_=rn[:]), reads=[rn_b], writes=[rn_b])
        for plane, op2 in (("C", ALU.add), ("S", ALU.subtract)):
            for lt in range(nlt):
                def cb2(lt):
                    for j in range(4):
                        ps, ps_b = psm[j]
                        half, dr = j % 2, j // 2
                        S.op("dve", lambda h, ps=ps, half=half, dr=dr: h.tensor_tensor(out=hd[dr][0][:, half * 512:(half + 1) * 512], in0=ps[:],
                                                                                      in1=dec[:, half * 512:(half + 1) * 512], op=ALU.mult),
                             reads=[ps_b, dec_b], writes=[hd[dr][1]])
                    if lt == 0:
                        S.op("dve", lambda h: h.memset(hd[1][0][0:1, :], 0.0), writes=[hd[1][1]])
                    S.op("pool", lambda h: h.tensor_tensor(out=hd[0][0][:], in0=hd[0][0][:], in1=hd[1][0][:], op=op2), reads=[hd[0][1], hd[1][1]], writes=[hd[0][1]])
                    S.op("pool", lambda h: h.tensor_tensor(out=AT[:, lt, :], in0=hd[0][0][:], in1=rn[:], op=ALU.mult), reads=[hd[0][1], rn_b], writes=[AT_b])
                htile(lt, cb2)
            pi = 0 if plane == "C" else 1

            def evac(ft, pc, ps_, pi=pi):
                src = pc if pi == 0 else ps_
                for hh in range(2):
                    o, o_b = hd[hh]
                    S.op("dve", lambda h, hh=hh, o=o: h.tensor_scalar(out=o[:, 0:512], in0=src[hh][0][:], scalar1=wf[:, ft:ft + 1], scalar2=None, op0=ALU.mult),
                         reads=[src[hh][1], wf_b], writes=[o_b])
                    S.dma("sp", KFs[0][pi, ft * 128:(ft + 1) * 128, hh * 512:(hh + 1) * 512], o[:, 0:512], KFs[1], [o_b])
            _fwd_dft(K, st, Cm, Sm, nlt, nft, AT, AT_b, 0, (plane,), evac, psm)
        S.barrier()


def _fwd_dft(K, st, Cm, Sm, ntt, nft, rhs, rhs_b, tt0, planes, evac, pspool):
    S = K.S
    with contextlib.ExitStack() as s2:
        blk = {p: [K.sb("dblk%s%d" % (p, i), [128, ntt, 128], BF16, s2) for i in range(2)] for p in planes}
        for ft in range(nft):
            pss = {}
            for pi, p in enumerate(("C", "S")):
                if p not in planes:
                    pss[p] = None
                    continue
                M = Cm if p == "C" else Sm
                b, b_b = blk[p][ft % 2]
                S.dma("sp", b[:], M.rearrange("(t p) f -> p t f", p=128)[:, 0:ntt, ft * 128:(ft + 1) * 128], b_b)
                pss[p] = [pspool[pi * 2 + hh] for hh in range(2)]
                for hh in range(2):
                    ps, ps_b = pss[p][hh]
                    _mm_group(S, ps_b, ps[:], [(b[:, t, :], rhs[:, tt0 + t, hh * 512:(hh + 1) * 512]) for t in range(ntt)], [b_b, rhs_b])
            evac(ft, pss["C"], pss["S"])


def phase_hyena(K, l, ctx_out):
    nc, S, I, R = K.nc, K.S, K.I, K.R
    ident_b, ident_b_b = K.ident_b
    _hy_filter(K, l, L, I["embT"][:, :], I["negt"][:, :], I["dftC"], I["dftS"], I["wf"][:, :], R["KF"])
    if ctx_out:
        _hy_filter(K, l, LC, I["embTc"][:, :], I["negtc"][:, :], I["dftCc"], I["dftSc"], I["wfc"][:, :], R["KFc"])
    segs = [(0, L)] + ([(L, LC)] if ctx_out else [])
    TT = T if ctx_out else L
    with contextlib.ExitStack() as st:
        utok, utok_b = K.sb("hutok", [128, T // 128, D], BF16, st)
        with contextlib.ExitStack() as s2:
            cw, cw_b = K.sb("hcw", [128, 24, 3], F32, s2)
            cb, cb_b = K.sb("hcb", [128, 24], F32, s2)
            S.dma("sp", cw[:], I["cwT"][l], cw_b)
            S.dma("sp", cb[:], I["cbT"][l], cb_b)
            xin = [K.sb("hxin%d" % i, [128, T], BF16, s2) for i in range(3)]
            yc = [K.sb("hyc%d" % i, [128, T], F32, s2) for i in range(3)]
            x0b, x0b_b = K.sb("hx0b", [128, T], BF16, s2)
            ub, ub_b = K.sb("hub", [128, T], BF16, s2)
            ptr = [K.pst("hptr%d" % i, [128, 1024], BF16, s2) for i in range(2)]
            ntr = 0
            for c in range(8):
                for s_ in range(3):
                    j = s_ * 8 + c
                    xi, xi_b = xin[s_]
                    y, y_b = yc[s_]
                    S.dma("sp", xi[:, :TT], R["HyT"][0][j * 128:(j + 1) * 128, 0:TT], xi_b, [R["HyT"][1]])
                    for (a0, n) in segs:
                        S.op("dve", lambda h, j=j, xi=xi, y=y: h.tensor_scalar(out=y[:, a0:a0 + n], in0=xi[:, a0:a0 + n], scalar1=cw[:, j, 1:2], scalar2=cb[:, j:j + 1],
                                                                            op0=ALU.mult, op1=ALU.add), reads=[xi_b, cw_b, cb_b], writes=[y_b])
                        S.op("dve", lambda h, j=j, xi=xi, y=y: h.scalar_tensor_tensor(out=y[:, a0 + 1:a0 + n], in0=xi[:, a0:a0 + n - 1], scalar=cw[:, j, 0:1],
                                                                                   in1=y[:, a0 + 1:a0 + n], op0=ALU.mult, op1=ALU.add),
                             reads=[xi_b, cw_b, y_b], writes=[y_b])
                        S.op("dve", lambda h, j=j, xi=xi, y=y: h.scalar_tensor_tensor(out=y[:, a0:a0 + n - 1], in0=xi[:, a0 + 1:a0 + n], scalar=cw[:, j, 2:3],
                                                                                   in1=y[:, a0:a0 + n - 1], op0=ALU.mult, op1=ALU.add),
                             reads=[xi_b, cw_b, y_b], writes=[y_b])
                S.op("act", lambda h: h.activation(out=x0b[:, :TT], in_=yc[0][0][:, :TT], func=AF.Copy), reads=[yc[0][1]], writes=[x0b_b])
                S.op("pool", lambda h: h.tensor_tensor(out=ub[:, :TT], in0=yc[1][0][:, :TT], in1=yc[2][0][:, :TT], op=ALU.mult),
                     reads=[yc[1][1], yc[2][1]], writes=[ub_b])
                S.dma("sp", R["X0T"][0][c * 128:(c + 1) * 128, 0:TT], x0b[:, :TT], R["X0T"][1], [x0b_b])
                S.dma("sp", R["UT"][0][c * 128:(c + 1) * 128, 0:TT], ub[:, :TT], R["UT"][1], [ub_b])
                for t8 in range(0, TT // 128, 8):
                    n8 = min(8, TT // 128 - t8)
                    pt_, pt_b = ptr[ntr % 2]
                    ntr += 1

                    def trf(h, t8=t8, n8=n8, pt_=pt_):
                        ins = None
                        for q in range(n8):
                            ins = h.transpose(pt_[:, q * 128:(q + 1) * 128], ub[:, (t8 + q) * 128:(t8 + q + 1) * 128], ident_b[:])
                        return ins
                    S.op("pe", trf, reads=[ub_b, ident_b_b], writes=[pt_b])
                    S.op("act", lambda h, t8=t8, n8=n8, pt_=pt_, c=c: h.activation(out=utok[:, t8:t8 + n8, c * 128:(c + 1) * 128],
                                                                                   in_=pt_[:, :n8 * 128].rearrange("p (q f) -> p q f", f=128), func=AF.Copy),
                         reads=[pt_b], writes=[utok_b])
            S.barrier()
        for si, (a0, n) in enumerate(segs):
            Cm, Sm = (I["dftC"], I["dftS"]) if si == 0 else (I["dftCc"], I["dftSc"])
            KFs = R["KF"] if si == 0 else R["KFc"]
            YFs = R["YF"] if si == 0 else R["YFc"]
            ntt = n // 128
            nft = (n + 128) // 128
            with contextlib.ExitStack() as s2:
                kf = [K.sb("hkf%d" % i, [128, 2, D], F32, s2) for i in range(2)]
                tq = [K.sb("htq%d" % i, [128, 512], F32, s2) for i in range(4)]
                yo = [K.sb("hyo%d" % i, [128, 2, D], BF16, s2) for i in range(2)]
                pspool = [K.pst("hps%d" % i, [128, 512], F32, s2) for i in range(4)]

                def evac(ft, pc, ps_, KFs=KFs, YFs=YFs):
                    k_, k_b = kf[ft % 2]
                    o, o_b = yo[ft % 2]
                    S.dma("sp", k_[:], KFs[0][:, ft * 128:(ft + 1) * 128, :].rearrange("a p c -> p a c"), k_b, [KFs[1]])
                    for hh in range(2):
                        cs = slice(hh * 512, (hh + 1) * 512)
                        ur, ur_b = pc[hh]
                        ui, ui_b = ps_[hh]
                        S.op("dve", lambda h: h.tensor_tensor(out=tq[0][0][:], in0=ur[:], in1=k_[:, 0, cs], op=ALU.mult), reads=[ur_b, k_b], writes=[tq[0][1]])
                        S.op("dve", lambda h: h.tensor_tensor(out=tq[1][0][:], in0=ui[:], in1=k_[:, 1, cs], op=ALU.mult), reads=[ui_b, k_b], writes=[tq[1][1]])
                        S.op("pool", lambda h: h.tensor_tensor(out=o[:, 0, cs], in0=tq[0][0][:], in1=tq[1][0][:], op=ALU.subtract),
                             reads=[tq[0][1], tq[1][1]], writes=[o_b])
                        S.op("dve", lambda h: h.tensor_tensor(out=tq[2][0][:], in0=ur[:], in1=k_[:, 1, cs], op=ALU.mult), reads=[ur_b, k_b], writes=[tq[2][1]])
                        S.op("dve", lambda h: h.tensor_tensor(out=tq[3][0][:], in0=ui[:], in1=k_[:, 0, cs], op=ALU.mult), reads=[ui_b, k_b], writes=[tq[3][1]])
                        S.op("pool", lambda h: h.tensor_tensor(out=o[:, 1, cs], in0=tq[2][0][:], in1=tq[3][0][:], op=ALU.add),
                             reads=[tq[2][1], tq[3][1]], writes=[o_b])
                    S.dma("sp", YFs[0][:, ft * 128:(ft + 1) * 128, :].rearrange("a p c -> p a c"), o[:], YFs[1], [o_b])
                _fwd_dft(K, s2, Cm, Sm, ntt, nft, utok, utok_b, a0 // 128, ("C", "S"), evac, pspool)
                S.barrier()
    for si, (a0, n) in enumerate(segs):
        Cm, Sm = (I["dftC"], I["dftS"]) if si == 0 else (I["dftCc"], I["dftSc"])
        YFs = R["YF"] if si == 0 else R["YFc"]
        nft = (n + 128) // 128
        with contextlib.ExitStack() as s2:
            hb, hb_b = K.sb("ihb", [128, 8], F32, s2)
            S.dma("sp", hb[:], I["hbT"][l], hb_b)
            Yh = [K.sb("iY%d" % i, [128, nft, 512], BF16, s2) for i in range(2)]
            Cr = [K.sb("iC%d" % i, [128, nft, 256], BF16, s2) for i in range(2)]
            Sr = [K.sb("iS%d" % i, [128, nft, 256], BF16, s2) for i in range(2)]
            x0c = [K.sb("ix0%d" % i, [128, 4, 256], BF16, s2) for i in range(2)]
            uc = [K.sb("iu%d" % i, [128, 4, 256], BF16, s2) for i in range(2)]
            tmp, tmp_b = K.sb("itmp", [128, 256], F32, s2)
            og = [K.sb("iog%d" % i, [128, 4, 256], BF16, s2) for i in range(2)]
            psI = [K.pst("ips%d" % i, [128, 512], F32, s2) for i in range(2)]
            nk = 0
            npi = 0
            for half in range(2):
                for pl in range(2):
                    S.dma("sp", Yh[pl][0][:], YFs[0][pl, 0:nft * 128, half * 512:(half + 1) * 512].rearrange("(f p) c -> p f c", p=128), Yh[pl][1], [YFs[1]])
                for t0 in range(0, n, 256):
                    cr, cr_b = Cr[nk % 2]
                    sr, sr_b = Sr[nk % 2]
                    xx, xx_b = x0c[nk % 2]
                    uu, uu_b = uc[nk % 2]
                    oo, oo_b = og[nk % 2]
                    nk += 1
                    S.dma("sp", cr[:], Cm.rearrange("(f p) t -> p f t", p=128)[:, 0:nft, t0:t0 + 256], cr_b)
                    S.dma("sp", sr[:], Sm.rearrange("(f p) t -> p f t", p=128)[:, 0:nft, t0:t0 + 256], sr_b)
                    rows = slice(half * 512, (half + 1) * 512)
                    S.dma("sp", xx[:], R["X0T"][0][rows, a0 + t0:a0 + t0 + 256].rearrange("(c p) t -> p c t", p=128), xx_b, [R["X0T"][1]])
                    S.dma("sp", uu[:], R["UT"][0][rows, a0 + t0:a0 + t0 + 256].rearrange("(c p) t -> p c t", p=128), uu_b, [R["UT"][1]])
                    for ci in range(4):
                        ps, ps_b = psI[npi % 2]
                        npi += 1
                        pairs = [(Yh[0][0][:, f, ci * 128:(ci + 1) * 128], cr[:, f, :]) for f in range(nft)] + \
                                [(Yh[1][0][:, f, ci * 128:(ci + 1) * 128], sr[:, f, :]) for f in range(nft)]
                        _mm_group(S, ps_b, ps[:, 0:256], pairs, [Yh[0][1], Yh[1][1], cr_b, sr_b])
                        cidx = half * 4 + ci
                        S.op("dve", lambda h, ci=ci, cidx=cidx, ps=ps, uu=uu: h.scalar_tensor_tensor(out=tmp[:], in0=uu[:, ci, :], scalar=hb[:, cidx:cidx + 1],
                                                                                                    in1=ps[:, 0:256], op0=ALU.mult, op1=ALU.add),
                             reads=[uu_b, hb_b, ps_b], writes=[tmp_b])
                        S.op("pool", lambda h, ci=ci, oo=oo, xx=xx: h.tensor_tensor(out=oo[:, ci, :], in0=tmp[:], in1=xx[:, ci, :], op=ALU.mult),
                             reads=[tmp_b, xx_b], writes=[oo_b])
                    S.dma("sp", R["YhT"][0][rows, a0 + t0:a0 + t0 + 256].rearrange("(c p) t -> p c t", p=128), oo[:], R["YhT"][1], [oo_b])
            S.barrier()
    if not ctx_out:
        pass


def _bc_mid(ap2d, n):
    a = ap2d.ap
    return bass.AP(ap2d.tensor, ap2d.offset, [list(a[0]), [0, n], list(a[1])])


def phase_moe(K, l, ctx_out):
    nc, S, I, R = K.nc, K.S, K.I, K.R
    modT, modT_b = K.modT
    A2, A2_b = K.A2
    ones_f, ones_f_b = K.ones_f
    ones_b, ones_b_b = K.ones_b
    ident_f, ident_f_b = K.ident_f
    ident_b, ident_b_b = K.ident_b
    debug = K.cfg.get("debug", ())
    XTv = R["XT"][0].rearrange("(k p) t -> p k t", p=128)
    chunks = TCH if ctx_out else TCH[:8]
    ntt = 34 if ctx_out else 32
    groups = [(0, 32, CAP)] + ([(32, 34, CAPC)] if ctx_out else [])
    with contextlib.ExitStack() as st:
        lg, lg_b = K.sb("lg", [128, 34, NE], F32, st)
        aff, aff_b = K.sb("aff", [128, 34, NE], F32, st)
        psel, psel_b = K.sb("psel", [128, 34, NE], F32, st)
        wr, wr_b = K.sb("wr", [128, 8, NE], F32, st)
        S.dma("sp", wr[:], I["router_w"][l].rearrange("(k p) e -> p k e", p=128), wr_b)
        sH = contextlib.ExitStack()
        H2, H2_b = K.sb("H2", [128, 34, D], BF16, sH)
        if ctx_out is False:
            S.op("dve", lambda h: h.memset(lg[:, 32:34, :], 0.0), writes=[lg_b])
        with contextlib.ExitStack() as s2:
            xs = [K.sb("qx%d" % i, [128, 8, 512], F32, s2) for i in range(2)]
            sq, sq_b = K.sb("qsq", [128, 8, 512], F32, s2)
            rstd, rstd_b = K.sb("qrstd", [128, 512], F32, s2)
            tmp, tmp_b = K.sb("qtmp", [128, 512], F32, s2)
            h2f, h2f_b = K.sb("qh2f", [128, 8, 512], F32, s2)
            hTc, hTc_b = K.sb("qhTc", [128, 8, 512], BF16, s2)
            ps, ps_b = K.pst("qps", [128, 512], F32, s2)
            psl, psl_b = K.pst("qpsl", [128, 64], F32, s2)
            pstr = [K.pst("qpst%d" % i, [128, 1024], BF16, s2) for i in range(2)]
            ntr = 0
            for ci, (t0, tn) in enumerate(chunks):
                r = 0 if t0 < L else 1
                x, x_b = xs[ci % 2]
                S.dma("sp", x[:, :, :tn], XTv[:, :, t0:t0 + tn], x_b, [R["XT"][1]])
                if "XT1" in debug:
                    S.dma("sp", R["XT1"][0].rearrange("(k p) t -> p k t", p=128)[:, :, t0:t0 + tn], x[:, :, :tn], R["XT1"][1], [x_b])
                S.op("act", lambda h: h.activation(out=sq[:, :, :tn], in_=x[:, :, :tn], func=AF.Square), reads=[x_b], writes=[sq_b])
                _mm_group(S, ps_b, ps[:, :tn], [(ones_f[:], sq[:, k, :tn]) for k in range(8)], [ones_f_b, sq_b])
                S.op("dve", lambda h: h.tensor_scalar(out=rstd[:, :tn], in0=ps[:, :tn], scalar1=1.0 / D, scalar2=EPS,
                                                      op0=ALU.mult, op1=ALU.add), reads=[ps_b], writes=[rstd_b])
                S.op("act", lambda h: h.activation(out=rstd[:, :tn], in_=rstd[:, :tn], func=AF.Sqrt), reads=[rstd_b], writes=[rstd_b])
                S.op("dve", lambda h: h.reciprocal(out=rstd[:, :tn], in_=rstd[:, :tn]), reads=[rstd_b], writes=[rstd_b])
                for k in range(8):
                    S.op("dve", lambda h, k=k: h.tensor_tensor(out=tmp[:, :tn], in0=x[:, k, :tn], in1=rstd[:, :tn], op=ALU.mult),
                         reads=[x_b, rstd_b], writes=[tmp_b])
                    S.op("act", lambda h, k=k: h.activation(out=h2f[:, k, :tn], in_=tmp[:, :tn], func=AF.Identity,
                                                            scale=A2[:, k, r:r + 1], bias=modT[:, 24 + k, r:r + 1]),
                         reads=[tmp_b, A2_b, modT_b], writes=[h2f_b])
                    S.op("pool", lambda h, k=k: h.tensor_copy(out=hTc[:, k, :tn], in_=h2f[:, k, :tn]), reads=[h2f_b], writes=[hTc_b])
                nj = tn // 128
                for j in range(nj):
                    tt = t0 // 128 + j
                    _mm_group(S, psl_b, psl[:, j * 16:(j + 1) * 16],
                              [(h2f[:, k, j * 128:(j + 1) * 128], wr[:, k, :]) for k in range(8)], [h2f_b, wr_b])
                    pt_, pt_b = pstr[ntr % 2]
                    ntr += 1

                    def trf(h, j=j, pt_=pt_):
                        ins = None
                        for k in range(8):
                            ins = h.transpose(pt_[:, k * 128:(k + 1) * 128], hTc[:, k, j * 128:(j + 1) * 128], ident_b[:])
                        return ins
                    S.op("pe", trf, reads=[hTc_b, ident_b_b], writes=[pt_b])
                    S.op("act", lambda h, tt=tt, pt_=pt_: h.activation(out=H2[:, tt, :], in_=pt_[:], func=AF.Copy), reads=[pt_b], writes=[H2_b])
                S.op("dve", lambda h: h.tensor_copy(out=lg[:, t0 // 128:t0 // 128 + nj, :].rearrange("p t e -> p (t e)"),
                                                    in_=psl[:, :nj * 16]), reads=[psl_b], writes=[lg_b])
            S.barrier()
        if "LG" in debug:
            S.dma("sp", R["LG"][0], lg[:], R["LG"][1], [lg_b])
            S.dma("sp", R["H2d"][0], H2[:], R["H2d"][1], [H2_b])
        with contextlib.ExitStack() as s2:
            se, se_b = K.sb("rse", [128, 34], F32, s2)
            lo, lo_b = K.sb("rlo", [128, NE], F32, s2)
            mid, mid_b = K.sb("rmid", [128, NE], F32, s2)
            cmpt, cmp_b = K.sb("rcmp", [128, 32, NE], F32, s2)
            cntp, cntp_b = K.sb("rcntp", [128, NE], F32, s2)
            ge, ge_b = K.sb("rge", [128, NE], F32, s2)
            mk, mk_b = K.sb("rmk", [128, 34, NE], F32, s2)
            mkb, mkb_b = K.sb("rmkb", [128, 34, NE], BF16, s2)
            tot, tot_b = K.sb("rtot", [128, 34, NE], F32, s2)
            base, base_b = K.sb("rbase", [128, 34, NE], F32, s2)
            posT, posT_b = K.sb("posT", [16, T], F32, s2)
            affT, affT_b = K.sb("affT", [16, T], F32, s2)
            us, us_b = K.sb("rus", [128, 128], BF16, s2)
            S.dma("pool", us[:], I["ustrict"][:, :], us_b)
            psc, psc_b = K.pst("rpsc", [128, 512], F32, s2)
            psw, psw_b = K.pst("rpsw", [128, 512], F32, s2)
            pst_, pst_b = K.pst("rpst", [128, 512], F32, s2)
            S.op("act", lambda h: h.activation(out=aff[:].rearrange("p t e -> p (t e)"), in_=lg[:].rearrange("p t e -> p (t e)"), func=AF.Exp),
                 reads=[lg_b], writes=[aff_b])
            S.op("dve", lambda h: h.reduce_sum(out=se[:], in_=aff[:], axis=AX.X), reads=[aff_b], writes=[se_b])
            S.op("dve", lambda h: h.reciprocal(out=se[:], in_=se[:]), reads=[se_b], writes=[se_b])
            for tt in range(34):
                S.op("dve", lambda h, tt=tt: h.tensor_scalar(out=aff[:, tt, :], in0=aff[:, tt, :], scalar1=se[:, tt:tt + 1], scalar2=None, op0=ALU.mult),
                     reads=[aff_b, se_b], writes=[aff_b])
            S.op("dve", lambda h: h.memset(mk[:], 0.0), writes=[mk_b])
            S.op("dve", lambda h: h.memset(base[:], 0.0), writes=[base_b])
            for (ta, tb, cap) in groups:
                nt = tb - ta
                S.op("dve", lambda h: h.memset(lo[:], 0.0), writes=[lo_b])
                for it in range(30):
                    w = 0.5 ** (it + 1)
                    S.op("dve", lambda h: h.tensor_scalar(out=mid[:], in0=lo[:], scalar1=w, scalar2=None, op0=ALU.add), reads=[lo_b], writes=[mid_b])
                    S.op("dve", lambda h: h.tensor_tensor(out=cmpt[:, :nt, :], in0=aff[:, ta:tb, :], in1=_bc_mid(mid[:], nt), op=ALU.is_ge),
                         reads=[aff_b, mid_b], writes=[cmp_b])
                    S.op("dve", lambda h: h.reduce_sum(out=cntp[:], in_=cmpt[:, :nt, :].rearrange("p t e -> p e t"), axis=AX.X),
                         reads=[cmp_b], writes=[cntp_b])
                    _mm_group(S, psc_b, psc[:, 0:NE], [(ones_f[:], cntp[:])], [ones_f_b, cntp_b])
                    S.op("dve", lambda h: h.tensor_scalar(out=ge[:], in0=psc[:, 0:NE], scalar1=cap - 0.5, scalar2=None, op0=ALU.is_ge),
                         reads=[psc_b], writes=[ge_b])
                    S.op("dve", lambda h: h.scalar_tensor_tensor(out=lo[:], in0=ge[:], scalar=w, in1=lo[:], op0=ALU.mult, op1=ALU.add),
                         reads=[ge_b, lo_b], writes=[lo_b])
                S.op("dve", lambda h: h.tensor_tensor(out=mk[:, ta:tb, :], in0=aff[:, ta:tb, :], in1=_bc_mid(lo[:], nt), op=ALU.is_ge),
                     reads=[aff_b, lo_b], writes=[mk_b])
                S.op("dve", lambda h: h.tensor_copy(out=mkb[:, ta:tb, :], in_=mk[:, ta:tb, :]), reads=[mk_b], writes=[mkb_b])
                mflat = mkb[:, ta:tb, :].rearrange("p t e -> p (t e)")
                _mm_group(S, psw_b, psw[:, :nt * NE], [(us[:], mflat)], [us_b, mkb_b])
                _mm_group(S, pst_b, pst_[:, :nt * NE], [(ones_b[:], mflat)], [ones_b_b, mkb_b])
                S.op("dve", lambda h: h.tensor_copy(out=tot[:, ta:tb, :].rearrange("p t e -> p (t e)"), in_=pst_[:, :nt * NE]),
                     reads=[pst_b], writes=[tot_b])
                for t in range(ta + 1, tb):
                    S.op("dve", lambda h, t=t: h.tensor_tensor(out=base[:, t, :], in0=base[:, t - 1, :], in1=tot[:, t - 1, :], op=ALU.add),
                         reads=[base_b, tot_b], writes=[base_b])
                S.op("dve", lambda h: h.tensor_tensor(out=psel[:, ta:tb, :].rearrange("p t e -> p (t e)"), in0=psw[:, :nt * NE],
                                                      in1=base[:, ta:tb, :].rearrange("p t e -> p (t e)"), op=ALU.add),
                     reads=[psw_b, base_b], writes=[psel_b])
                S.op("dve", lambda h: h.scalar_tensor_tensor(out=psel[:, ta:tb, :], in0=psel[:, ta:tb, :], scalar=1.0, in1=mk[:, ta:tb, :],
                                                             op0=ALU.add, op1=ALU.mult), reads=[psel_b, mk_b], writes=[psel_b])
                S.op("dve", lambda h: h.tensor_scalar(out=psel[:, ta:tb, :], in0=psel[:, ta:tb, :], scalar1=-1.0, scalar2=None, op0=ALU.add),
                     reads=[psel_b], writes=[psel_b])
            S.op("dve", lambda h: h.tensor_tensor(out=aff[:], in0=aff[:], in1=mk[:], op=ALU.mult), reads=[aff_b, mk_b], writes=[aff_b])
            for src, src_b, dstT, dstT_b in ((psel, psel_b, posT, posT_b), (aff, aff_b, affT, affT_b)):
                for t4 in range(0, ntt, 4):
                    n4 = min(4, ntt - t4)

                    def trf(h, t4=t4, n4=n4, src=src):
                        ins = None
                        for j in range(n4):
                            ins = h.transpose(psc[0:16, j * 128:(j + 1) * 128], src[:, t4 + j, :], ident_f[:])
                        return ins
                    S.op("pe", trf, reads=[src_b, ident_f_b], writes=[psc_b])
                    S.op("act", lambda h, t4=t4, n4=n4, dstT=dstT: h.activation(out=dstT[:, t4 * 128:(t4 + n4) * 128], in_=psc[0:16, :n4 * 128], func=AF.Copy),
                         reads=[psc_b], writes=[dstT_b])
            S.dma("sp", R["PT"][0][0, :, 0:ntt * 128], posT[:, 0:ntt * 128], R["PT"][1], [posT_b])
            S.dma("sp", R["PT"][0][1, :, 0:ntt * 128], affT[:, 0:ntt * 128], R["PT"][1], [affT_b])
            S.barrier()
        NS = CAP + CAPC
        with contextlib.ExitStack() as s3:
            w1, w1_b = K.sb("ew1", [128, 8, D], BF16, s3)
            w3, w3_b = K.sb("ew3", [128, 8, D], BF16, s3)
            w2, w2_b = K.sb("ew2", [128, 8, D], BF16, s3)
            Se, Se_b = K.sb("eS", [128, 32, 512], BF16, s3)
            Sc, Sc_b = K.sb("eSc", [128, 2, CAPC], BF16, s3)
            io, io_b = K.sb("eio", [128, 512], F32, s3)
            S.dma("sp", io[:], I["iota512"][:, :], io_b)
            xg, xg_b = K.sb("exg", [128, 8, NS], BF16, s3)
            gT, gT_b = K.sb("egT", [128, 8, NS], BF16, s3)
            sil, sil_b = K.sb("esil", [128, NS], F32, s3)
            yst = [K.sb("eyst%d" % i, [128, 512], BF16, s3) for i in range(2)]
            psG = [K.pst("epsG%d" % i, [128, 512], F32, s3) for i in range(2)]
            psA, psA_b = K.pst("epsA", [128, 512], F32, s3)
            psB, psB_b = K.pst("epsB", [128, 512], F32, s3)
            psC, psC_b = K.pst("epsC", [128, 512], F32, s3)
            psY = [K.pst("epsY%d" % i, [128, 512], F32, s3) for i in range(2)]
            ng = 0
            ny = 0
            for e in range(NE):
                S.dma("pool", w1[:], I["moe_w1"][l, e].rearrange("(k p) n -> p k n", p=128), w1_b)
                S.dma("pool", w3[:], I["moe_w3"][l, e].rearrange("(k p) n -> p k n", p=128), w3_b)
                S.dma("pool", w2[:], I["moe_w2"][l, e].rearrange("(k p) n -> p k n", p=128), w2_b)
                for tt in range(32):
                    S.op("dve", lambda h, tt=tt: h.tensor_scalar(out=Se[:, tt, :], in0=io[:], scalar1=psel[:, tt, e:e + 1], scalar2=None, op0=ALU.is_equal),
                         reads=[io_b, psel_b], writes=[Se_b])
                if ctx_out:
                    for tt in range(32, 34):
                        S.op("dve", lambda h, tt=tt: h.tensor_scalar(out=Sc[:, tt - 32, :], in0=io[:, 0:CAPC], scalar1=psel[:, tt, e:e + 1], scalar2=None,
                                                                     op0=ALU.is_equal), reads=[io_b, psel_b], writes=[Sc_b])
                for dk in range(8):
                    pg, pg_b = psG[ng % 2]
                    ng += 1
                    _mm_group(S, pg_b, pg[:], [(H2[:, tt, dk * 128:(dk + 1) * 128], Se[:, tt, :]) for tt in range(32)], [H2_b, Se_b])
                    S.op("act", lambda h, dk=dk, pg=pg: h.activation(out=xg[:, dk, 0:CAP], in_=pg[:], func=AF.Copy), reads=[pg_b], writes=[xg_b])
                    if ctx_out:
                        _mm_group(S, psC_b, psC[:, 0:CAPC], [(H2[:, tt, dk * 128:(dk + 1) * 128], Sc[:, tt - 32, :]) for tt in (32, 33)], [H2_b, Sc_b])
                        S.op("act", lambda h, dk=dk: h.activation(out=xg[:, dk, CAP:NS], in_=psC[:, 0:CAPC], func=AF.Copy), reads=[psC_b], writes=[xg_b])
                for m in range(8):
                    _mm_group(S, psA_b, psA[:], [(w1[:, k, m * 128:(m + 1) * 128], xg[:, k, 0:CAP]) for k in range(8)], [w1_b, xg_b])
                    _mm_group(S, psB_b, psB[:], [(w3[:, k, m * 128:(m + 1) * 128], xg[:, k, 0:CAP]) for k in range(8)], [w3_b, xg_b])
                    S.op("act", lambda h: h.activation(out=sil[:, 0:CAP], in_=psA[:], func=AF.Silu), reads=[psA_b], writes=[sil_b])
                    S.op("dve", lambda h, m=m: h.tensor_tensor(out=gT[:, m, 0:CAP], in0=psB[:], in1=sil[:, 0:CAP], op=ALU.mult),
                         reads=[psB_b, sil_b], writes=[gT_b])
                    if ctx_out:
                        _mm_group(S, psC_b, psC[:, 0:CAPC], [(w1[:, k, m * 128:(m + 1) * 128], xg[:, k, CAP:NS]) for k in range(8)], [w1_b, xg_b])
                        S.op("act", lambda h: h.activation(out=sil[:, CAP:NS], in_=psC[:, 0:CAPC], func=AF.Silu), reads=[psC_b], writes=[sil_b])
                        _mm_group(S, psC_b, psC[:, 0:CAPC], [(w3[:, k, m * 128:(m + 1) * 128], xg[:, k, CAP:NS]) for k in range(8)], [w3_b, xg_b])
                        S.op("dve", lambda h, m=m: h.tensor_tensor(out=gT[:, m, CAP:NS], in0=psC[:, 0:CAPC], in1=sil[:, CAP:NS], op=ALU.mult),
                             reads=[psC_b, sil_b], writes=[gT_b])
                for c in range(5 if ctx_out else 4):
                    rows = 128 if c < 4 else CAPC
                    for dh in range(2):
                        py, py_b = psY[ny % 2]
                        ys, ys_b = yst[ny % 2]
                        ny += 1
                        _mm_group(S, py_b, py[0:rows, :], [(gT[:, f, c * 128:c * 128 + rows], w2[:, f, dh * 512:(dh + 1) * 512]) for f in range(8)],
                                  [gT_b, w2_b])
                        S.op("act", lambda h, py=py, ys=ys, rows=rows: h.activation(out=ys[0:rows, :], in_=py[0:rows, :], func=AF.Copy),
                             reads=[py_b], writes=[ys_b])
                        S.dma("sp", R["YE"][0][e, c * 128:c * 128 + rows, dh * 512:(dh + 1) * 512], ys[0:rows, :], R["YE"][1], [ys_b])
            S.barrier()
        sH.close()
        with contextlib.ExitStack() as s4:
            YEs, YEs_b = K.sb("cYE", [128, NE, 5, 512], BF16, s4)
            ST, ST_b = K.sb("cST", [128, NE, 4, 512], BF16, s4)
            abc = [K.sb("cabc%d" % i, [128, 512], F32, s4) for i in range(2)]
            xh, xh_b = K.sb("cxh", [128, 4, 512], F32, s4)
            selm, selm_b = K.sb("cselm", [16, NE, 128], F32, s4)
            sidx, sidx_b = K.sb("csidx", [128, 5], F32, s4)
            posT, posT_b = K.sb("cposT", [16, T], F32, s4)
            affT, affT_b = K.sb("caffT", [16, T], F32, s4)
            S.dma("sp", posT[:, 0:ntt * 128], R["PT"][0][0, :, 0:ntt * 128], posT_b, [R["PT"][1]])
            S.dma("sp", affT[:, 0:ntt * 128], R["PT"][0][1, :, 0:ntt * 128], affT_b, [R["PT"][1]])
            S.dma("sp", selm[:], I["selm"].rearrange("k (e m) -> k e m", e=NE), selm_b)
            S.dma("sp", sidx[:], I["slotidx"][:, :], sidx_b)
            psP = [K.pst("cpsP%d" % i, [128, 512], F32, s4) for i in range(2)]
            psQ = [K.pst("cpsQ%d" % i, [128, 512], F32, s4) for i in range(2)]
            psO = [K.pst("cpsO%d" % i, [128, 512], F32, s4) for i in range(2)]
            nb_ = 0
            no = 0
            for dh in range(2):
                for e in range(NE):
                    S.dma("sp", YEs[:, e, 0:4, :], R["YE"][0][e, 0:CAP, dh * 512:(dh + 1) * 512].rearrange("(c p) d -> p c d", p=128), YEs_b, [R["YE"][1]])
                    if ctx_out:
                        S.dma("sp", YEs[0:CAPC, e, 4, :], R["YE"][0][e, CAP:NS, dh * 512:(dh + 1) * 512], YEs_b, [R["YE"][1]])
                for (t0, tn) in chunks:
                    r = 0 if t0 < L else 1
                    lat = t0 < L
                    for e in range(NE):
                        pp, pp_b = psP[nb_ % 2]
                        pq, pq_b = psQ[nb_ % 2]
                        ab, ab_b = abc[nb_ % 2]
                        nb_ += 1
                        _mm_group(S, pp_b, pp[:, :tn], [(selm[:, e, :], posT[:, t0:t0 + tn])], [selm_b, posT_b])
                        _mm_group(S, pq_b, pq[:, :tn], [(selm[:, e, :], affT[:, t0:t0 + tn])], [selm_b, affT_b])
                        S.op("act", lambda h, ab=ab, pq=pq: h.activation(out=ab[:, :tn], in_=pq[:, :tn], func=AF.Copy), reads=[pq_b], writes=[ab_b])
                        if lat:
                            for c in range(4):
                                S.op("dve", lambda h, c=c, pp=pp, ab=ab: h.scalar_tensor_tensor(out=ST[:, e, c, :tn], in0=pp[:, :tn], scalar=sidx[:, c:c + 1],
                                                                                               in1=ab[:, :tn], op0=ALU.is_equal, op1=ALU.mult),
                                     reads=[pp_b, ab_b, sidx_b], writes=[ST_b])
                        else:
                            S.op("dve", lambda h, pp=pp, ab=ab: h.scalar_tensor_tensor(out=ST[0:CAPC, e, 0, :tn], in0=pp[0:CAPC, :tn], scalar=sidx[0:CAPC, 4:5],
                                                                                      in1=ab[0:CAPC, :tn], op0=ALU.is_equal, op1=ALU.mult),
                                 reads=[pp_b, ab_b, sidx_b], writes=[ST_b])
                    S.dma("sp", xh[:, :, :tn], XTv[:, dh * 4:(dh + 1) * 4, t0:t0 + tn], xh_b, [R["XT"][1]])
                    for m in range(4):
                        po, po_b = psO[no % 2]
                        no += 1
                        if lat:
                            pairs = [(YEs[:, e, c, m * 128:(m + 1) * 128], ST[:, e, c, :tn]) for e in range(NE) for c in range(4)]
                        else:
                            pairs = [(YEs[0:CAPC, e, 4, m * 128:(m + 1) * 128], ST[0:CAPC, e, 0, :tn]) for e in range(NE)]
                        _mm_group(S, po_b, po[:, :tn], pairs, [YEs_b, ST_b])
                        S.op("dve", lambda h, m=m, po=po: h.scalar_tensor_tensor(out=xh[:, m, :tn], in0=po[:, :tn], scalar=modT[:, 40 + dh * 4 + m, r:r + 1],
                                                                                 in1=xh[:, m, :tn], op0=ALU.mult, op1=ALU.add),
                             reads=[po_b, modT_b, xh_b], writes=[xh_b])
                    S.dma("sp", XTv[:, dh * 4:(dh + 1) * 4, t0:t0 + tn], xh[:, :, :tn], R["XT"][1], [xh_b])
            S.barrier()


def phase_final(K):
    nc, S, I, R = K.nc, K.S, K.I, K.R
    ones_f, ones_f_b = K.ones_f
    XTv = R["XT"][0].rearrange("(k p) t -> p k t", p=128)
    with contextlib.ExitStack() as st:
        fg, fg_b = K.sb("fg", [128, 8], F32, st)
        S.dma("sp", fg[:], I["fgT"][:, :], fg_b)
        xs = [K.sb("fx%d" % i, [128, 8, 512], F32, st) for i in range(2)]
        sq, sq_b = K.sb("fsq", [128, 8, 512], F32, st)
        rstd, rstd_b = K.sb("frstd", [128, 512], F32, st)
        ps, ps_b = K.pst("ps_fin", [128, 512], F32, st)
        for ci in range(8):
            t0 = ci * 512
            x, x_b = xs[ci % 2]
            S.dma("sp", x[:], XTv[:, :, t0:t0 + 512], x_b, [R["XT"][1]])
            S.op("act", lambda h: h.activation(out=sq[:], in_=x[:], func=AF.Square), reads=[x_b], writes=[sq_b])
            _mm_group(S, ps_b, ps[:], [(ones_f[:], sq[:, k, :]) for k in range(8)], [ones_f_b, sq_b])
            S.op("dve", lambda h: h.tensor_scalar(out=rstd[:], in0=ps[:], scalar1=1.0 / D, scalar2=EPS, op0=ALU.mult, op1=ALU.add),
                 reads=[ps_b], writes=[rstd_b])
            S.op("act", lambda h: h.activation(out=rstd[:], in_=rstd[:], func=AF.Sqrt), reads=[rstd_b], writes=[rstd_b])
            S.op("dve", lambda h: h.reciprocal(out=rstd[:], in_=rstd[:]), reads=[rstd_b], writes=[rstd_b])
            for k in range(8):
                S.op("dve", lambda h, k=k: h.scalar_tensor_tensor(out=sq[:, k, :], in0=x[:, k, :], scalar=fg[:, k:k + 1], in1=rstd[:],
                                                                  op0=ALU.mult, op1=ALU.mult), reads=[x_b, rstd_b, fg_b], writes=[sq_b])
            S.dma("sp", K.outT.rearrange("(k p) t -> p k t", p=128)[:, :, t0:t0 + 512], sq[:], K.outT_b, [sq_b])
        S.barrier()


def rope_tables():
    rows = L // 64
    row = np.repeat(np.arange(rows, dtype=np.float32), 64)
    col = np.tile(np.arange(64, dtype=np.float32), rows)
    inv = (10000.0 ** (-np.arange(16, dtype=np.float32) / 16)).astype(np.float32)
    C = np.zeros((64, L), np.float32)
    Sg = np.zeros((64, L), np.float32)
    for j in range(64):
        a, hf, f = j // 32, (j % 32) // 16, j % 16
        pos = row if a == 0 else col
        ang = (pos * inv[f]).astype(np.float32)
        C[j] = np.cos(ang)
        Sg[j] = np.sin(ang) * (-1.0 if hf == 0 else 1.0)
    return np.concatenate([C, C], 0), np.concatenate([Sg, Sg], 0)


_HC = {}


def hyena_consts():
    if _HC:
        return _HC
    f32 = np.float32

    def emb_of(Lx):
        t = np.linspace(0.0, 1.0, Lx, dtype=f32)[:, None]
        w = (2.0 * math.pi * np.arange(Lx, dtype=f32)[:, None] / Lx).astype(f32)
        f = np.linspace(1e-4, 15, 16, dtype=f32)[None, :]
        e = np.concatenate([t, np.cos(f * w), -np.sin(f * w)], axis=-1).astype(f32)
        return np.ascontiguousarray(e.T), t[:, 0]
    eT, t = emb_of(L)
    eTc, tc = emb_of(LC)
    _HC["embT"], _HC["embTc"] = eT, eTc
    _HC["negt"] = np.ascontiguousarray((-t).reshape(L // 128, 128).T)
    _HC["negtc"] = np.ascontiguousarray((-tc).reshape(LC // 128, 128).T)
    _HC["deltas"] = np.abs(np.linspace(math.log(1e-2) / 0.3, math.log(1e-2) / 1.5, D, dtype=f32)).reshape(1, D).astype(f32)

    def dft(Lx, npad):
        N = 2 * Lx
        a = np.arange(Lx + 1, dtype=np.int64)
        ph = (a[:, None] * a[None, :]) % N
        ang = ph.astype(np.float64) * (2.0 * math.pi / N)
        C = np.zeros((npad, npad), f32)
        S_ = np.zeros((npad, npad), f32)
        C[:Lx + 1, :Lx + 1] = np.cos(ang)
        S_[:Lx + 1, :Lx + 1] = np.sin(ang)
        wfv = np.zeros(npad, f32)
        wfv[:Lx + 1] = 2.0 / N
        wfv[0] = 1.0 / N
        wfv[Lx] = 1.0 / N
        return C.astype(ml_dtypes.bfloat16), S_.astype(ml_dtypes.bfloat16), np.ascontiguousarray(wfv.reshape(npad // 128, 128).T)
    _HC["dftC"], _HC["dftS"], _HC["wf"] = dft(L, NF)
    _HC["dftCc"], _HC["dftSc"], _HC["wfc"] = dft(LC, NFC)
    return _HC


def host_inputs(inputs, b, nl):
    x, c, ctx, c_ctx = inputs["x"], inputs["c"], inputs["ctx"], inputs["c_ctx"]
    m = {}
    m["xT"] = np.ascontiguousarray(np.concatenate([x[b], ctx[b]], 0).T)
    cc = np.stack([c[b], c_ctx], 0)
    m["ccT"] = np.ascontiguousarray(cc.reshape(2, 8, 128).transpose(2, 1, 0))
    m["w_mod"] = np.ascontiguousarray(inputs["w_mod"][:nl])
    m["b_modT"] = np.ascontiguousarray(inputs["b_mod"][:nl].reshape(nl, 48, 128).transpose(0, 2, 1))
    m["g1T"] = np.ascontiguousarray(inputs["norm1_g"][:nl].reshape(nl, 8, 128).transpose(0, 2, 1))
    m["g2T"] = np.ascontiguousarray(inputs["norm2_g"][:nl].reshape(nl, 8, 128).transpose(0, 2, 1))
    m["w_in"] = np.ascontiguousarray(inputs["w_in"][:nl])
    rc, rs = rope_tables()
    m["ropeC"], m["ropeS"] = rc, rs
    m["ident"] = np.eye(128, dtype=np.float32)
    m["diff_lambda"] = np.ascontiguousarray(inputs["diff_lambda"][:nl].reshape(nl, 256))
    m["sublnT"] = np.ascontiguousarray(inputs["diff_subln_g"][:nl].reshape(nl, 128, 1))
    m["swa_sink"] = np.ascontiguousarray(inputs["swa_sink"][:nl])
    kk, qq = np.meshgrid(np.arange(128), np.arange(128), indexing="ij")
    m["trilo"] = (kk >= qq).astype(np.float32)
    m["triup"] = (kk <= qq).astype(np.float32)
    m["w_branch"] = np.ascontiguousarray(inputs["w_branch"][:nl])
    m["w_out"] = np.ascontiguousarray(inputs["w_out"][:nl])
    m["fgT"] = np.ascontiguousarray(inputs["final_g"].reshape(8, 128).T)
    if "hy_ff_w1" in inputs:
        cwv = inputs["hy_conv_w"][:nl]
        m["cwT"] = np.ascontiguousarray(cwv.reshape(nl, 3, 24, 128).transpose(0, 3, 2, 1))
        m["cbT"] = np.ascontiguousarray(inputs["hy_conv_b"][:nl].reshape(nl, 24, 128).transpose(0, 2, 1))
        m["hbT"] = np.ascontiguousarray(inputs["hy_bias"][:nl].reshape(nl, 8, 128).transpose(0, 2, 1))
        m["hy_w1"] = np.ascontiguousarray(inputs["hy_ff_w1"][:nl])
        m["hy_w2"] = np.ascontiguousarray(inputs["hy_ff_w2"][:nl])
        m["hy_w3"] = np.ascontiguousarray(inputs["hy_ff_w3"][:nl])
        m["hy_b1"] = np.ascontiguousarray(inputs["hy_ff_b1"][:nl].reshape(nl, 64, 1))
        m["hy_b2"] = np.ascontiguousarray(inputs["hy_ff_b2"][:nl].reshape(nl, 64, 1))
        m["hy_fr"] = np.ascontiguousarray(inputs["hy_sin_freq"][:nl].reshape(nl, 64, 1))
        m.update(hyena_consts())
    if "moe_w1" in inputs:
        m["router_w"] = np.ascontiguousarray(inputs["router_w"][:nl])
        for k in ("moe_w1", "moe_w3", "moe_w2"):
            m[k] = np.ascontiguousarray(inputs[k][:nl])
        sel = np.zeros((16, 16, 128), np.float32)
        for e in range(16):
            sel[e, e, :] = 1.0
        m["selm"] = sel.reshape(16, 16 * 128)
        si = np.zeros((128, 5), np.float32)
        for c in range(4):
            si[:, c] = np.arange(128) + 128 * c
        si[:, 4] = np.arange(128)
        m["slotidx"] = si
        m["iota512"] = np.tile(np.arange(512, dtype=np.float32)[None, :], (128, 1))
        m["ustrict"] = (kk < qq).astype(np.float32)
    return m


def kernel(**inputs):
    inputs = {k: np.asarray(v) for k, v in inputs.items()}
    nb = inputs["x"].shape[0]
    nc = build(dict(nl=DEPTH, phases=("diff", "swa", "hy", "merge", "moe")))
    in_maps = [host_inputs(inputs, b, DEPTH) for b in range(nb)]
    res = run_bass_kernel_spmd(nc, in_maps, core_ids=list(range(nb)))
    out = np.stack([np.asarray(res.results[b]["outT"]).T for b in range(nb)], 0)
    return np.ascontiguousarray(out.astype(np.float32))
```

```python
import contextlib
import math
import numpy as np
import ml_dtypes
import concourse.bass as bass
import concourse.mybir as mybir
from concourse.bass_utils import run_bass_kernel_spmd

F32 = mybir.dt.float32
BF16 = mybir.dt.bfloat16
AF = mybir.ActivationFunctionType
ALU = mybir.AluOpType
AX = mybir.AxisListType

D = 1024
L = 4096
LC = 256
T = L + LC
DEPTH = 4
D_IN = 10752
NE = 16
CAP = 512
CAPC = 32
EPS = 1e-6
NF = 4224
NFC = 384
TCH = [(i * 512, 512) for i in range(8)] + [(L, LC)]


class Buf:
    __slots__ = ("name", "w", "r", "sem", "cnt")

    def __init__(self, name):
        self.name = name
        self.w = {}
        self.r = {}
        self.sem = None
        self.cnt = 0


class Sched:
    def __init__(self, nc, es):
        self.nc, self.es = nc, es
        self.E = {}
        for n, h in (("pe", nc.tensor), ("dve", nc.vector), ("act", nc.scalar),
                     ("pool", nc.gpsimd), ("sp", nc.sync)):
            self.E[n] = dict(h=h, sem=es.enter_context(nc.semaphore("s_" + n)), cnt=0, seen={})
        self.dma_bufs = []
        self.nbuf = 0
        self.persist = True
        self.pool = []

    def buf(self, name):
        self.nbuf += 1
        b = Buf("%s_%d" % (name, self.nbuf))
        b.w["_persist"] = self.persist
        return b

    @staticmethod
    def _add(evs, d):
        for k, sv in d.items():
            if k == "_persist":
                continue
            sem, v = sv
            if k not in evs or evs[k][1] < v:
                evs[k] = (sem, v)

    def _waits(self, e, evs):
        E = self.E[e]
        for name, (sem, val) in evs.items():
            if E["seen"].get(name, 0) < val:
                E["h"].wait_ge(sem, val)
                E["seen"][name] = val

    def op(self, e, fn, reads=(), writes=(), skip_self=False, drain_self=False):
        evs = {}
        for b in reads:
            self._add(evs, b.w)
        for b in writes:
            self._add(evs, b.w)
            self._add(evs, b.r)
        if skip_self:
            evs.pop("s_" + e, None)
        if drain_self and self.E[e]["cnt"]:
            evs["s_" + e] = (self.E[e]["sem"], self.E[e]["cnt"])
        self._waits(e, evs)
        E = self.E[e]
        ins = fn(E["h"])
        E["cnt"] += 1
        ins.then_inc(E["sem"], 1)
        key, ev = "s_" + e, (E["sem"], E["cnt"])
        for b in reads:
            b.r[key] = ev
        for b in writes:
            b.w[key] = ev

    def dma(self, q, out_ap, in_ap, dst, srcs=(), **kw):
        evs = {}
        self._add(evs, dst.w)
        self._add(evs, dst.r)
        for b in srcs:
            self._add(evs, b.w)
        self._waits(q, evs)
        if dst.sem is None:
            if self.pool and not dst.w["_persist"]:
                dst.name, dst.sem, dst.cnt = self.pool.pop()
            else:
                dst.sem = self.es.enter_context(self.nc.semaphore("d_" + dst.name))
            self.dma_bufs.append(dst)
        ins = self.E[q]["h"].dma_start(out=out_ap, in_=in_ap, **kw)
        dst.cnt += 16
        ins.then_inc(dst.sem, 16)
        key, ev = "d_" + dst.name, (dst.sem, dst.cnt)
        dst.w[key] = ev
        for b in srcs:
            b.r[key] = ev

    def barrier(self):
        evs = {}
        for n, E in self.E.items():
            if E["cnt"]:
                evs["s_" + n] = (E["sem"], E["cnt"])
        for b in self.dma_bufs:
            evs["d_" + b.name] = (b.sem, b.cnt)
        for n in self.E:
            self._waits(n, evs)
        keep = []
        for b in self.dma_bufs:
            if b.w["_persist"]:
                keep.append(b)
            else:
                self.pool.append((b.name, b.sem, b.cnt))
        self.dma_bufs = keep

    def barrier_known(self):
        pass


class Ctx:
    pass


def _mm_group(S, ps, out_ap, pairs, reads):
    def fn(h):
        ins = None
        n = len(pairs)
        for i, (a, b) in enumerate(pairs):
            ins = h.matmul(out_ap, a, b, start=(i == 0), stop=(i == n - 1))
        return ins
    S.op("pe", fn, reads=reads, writes=[ps])


def _mm1(S, ps, out_ap, a, b, reads, start, stop, drain=False):
    S.op("pe", lambda h: h.matmul(out_ap, a, b, start=start, stop=stop), reads=reads, writes=[ps], skip_self=True, drain_self=drain)


def build(cfg):
    nl = cfg["nl"]
    debug = cfg.get("debug", ())
    stop_after = cfg.get("stop_after", None)
    nc = bass.Bass("TRN2", target_bir_lowering=False)
    es = contextlib.ExitStack()
    K = Ctx()
    K.nc, K.es, K.cfg = nc, es, cfg
    S = Sched(nc, es)
    K.S = S

    def din(name, shape, dt=F32):
        return nc.dram_tensor(name, list(shape), dt, kind="ExternalInput").ap()

    def scratch(name, shape, dt):
        kind = "ExternalOutput" if name in debug else "Internal"
        return nc.dram_tensor(name, list(shape), dt, kind=kind).ap()

    I = {}
    I["xT"] = din("xT", [D, T])
    I["ccT"] = din("ccT", [128, 8, 2])
    I["w_mod"] = din("w_mod", [nl, D, 6 * D])
    I["b_modT"] = din("b_modT", [nl, 128, 48])
    I["g1T"] = din("g1T", [nl, 128, 8])
    I["g2T"] = din("g2T", [nl, 128, 8])
    I["w_in"] = din("w_in", [nl, D, D_IN])
    I["ropeC"] = din("ropeC", [128, L])
    I["ropeS"] = din("ropeS", [128, L])
    I["ident"] = din("ident", [128, 128])
    I["diff_lambda"] = din("diff_lambda", [nl, 256])
    I["sublnT"] = din("sublnT", [nl, 128, 1])
    I["swa_sink"] = din("swa_sink", [nl, 16])
    I["trilo"] = din("trilo", [128, 128])
    I["triup"] = din("triup", [128, 128])
    I["w_branch"] = din("w_branch", [nl, 3, D, D])
    I["w_out"] = din("w_out", [nl, D, D])
    I["fgT"] = din("fgT", [128, 8])
    if "moe" in cfg.get("phases", ()):
        I["router_w"] = din("router_w", [nl, D, NE])
        I["moe_w1"] = din("moe_w1", [nl, NE, D, D])
        I["moe_w3"] = din("moe_w3", [nl, NE, D, D])
        I["moe_w2"] = din("moe_w2", [nl, NE, D, D])
        I["selm"] = din("selm", [16, 16 * 128])
        I["slotidx"] = din("slotidx", [128, 5])
        I["iota512"] = din("iota512", [128, 512])
        I["ustrict"] = din("ustrict", [128, 128])
    if "hy" in cfg.get("phases", ()):
        I["cwT"] = din("cwT", [nl, 128, 24, 3])
        I["cbT"] = din("cbT", [nl, 128, 24])
        I["hbT"] = din("hbT", [nl, 128, 8])
        I["hy_w1"] = din("hy_w1", [nl, 33, 64])
        I["hy_w2"] = din("hy_w2", [nl, 64, 64])
        I["hy_w3"] = din("hy_w3", [nl, 64, 2048])
        I["hy_b1"] = din("hy_b1", [nl, 64, 1])
        I["hy_b2"] = din("hy_b2", [nl, 64, 1])
        I["hy_fr"] = din("hy_fr", [nl, 64, 1])
        I["embT"] = din("embT", [33, L])
        I["embTc"] = din("embTc", [33, LC])
        I["negt"] = din("negt", [128, 32])
        I["negtc"] = din("negtc", [128, 2])
        I["deltas"] = din("deltas", [1, D])
        I["dftC"] = din("dftC", [NF, NF], BF16)
        I["dftS"] = din("dftS", [NF, NF], BF16)
        I["dftCc"] = din("dftCc", [NFC, NFC], BF16)
        I["dftSc"] = din("dftSc", [NFC, NFC], BF16)
        I["wf"] = din("wf", [128, NF // 128])
        I["wfc"] = din("wfc", [128, NFC // 128])
    K.I = I
    K.outT = nc.dram_tensor("outT", [D, L], F32, kind="ExternalOutput").ap()
    K.outT_b = S.buf("outT")

    R = {}
    R["XT"] = (scratch("XT", [D, T], F32), S.buf("XT"))
    R["QdT"] = (scratch("QdT", [D, T], BF16), S.buf("QdT"))
    R["KdT"] = (scratch("KdT", [D, T], BF16), S.buf("KdT"))
    R["Vd"] = (scratch("Vd", [T, D], BF16), S.buf("Vd"))
    R["HyT"] = (scratch("HyT", [3 * D, T], BF16), S.buf("HyT"))
    R["QsT"] = (scratch("QsT", [D, T], BF16), S.buf("QsT"))
    R["KsT"] = (scratch("KsT", [256, T], BF16), S.buf("KsT"))
    R["Vs"] = (scratch("Vs", [T, 256], BF16), S.buf("Vs"))
    R["GT"] = (scratch("GT", [3 * D, T], BF16), S.buf("GT"))
    R["YdT"] = (scratch("YdT", [D, T], BF16), S.buf("YdT"))
    R["YhT"] = (scratch("YhT", [D, T], BF16), S.buf("YhT"))
    R["YsT"] = (scratch("YsT", [D, T], BF16), S.buf("YsT"))
    R["X0T"] = (scratch("X0T", [D, T], BF16), S.buf("X0T"))
    R["UT"] = (scratch("UT", [D, T], BF16), S.buf("UT"))
    R["KF"] = (scratch("KF", [2, NF, D], F32), S.buf("KF"))
    R["KFc"] = (scratch("KFc", [2, NFC, D], F32), S.buf("KFc"))
    R["YF"] = (scratch("YF", [2, NF, D], BF16), S.buf("YF"))
    R["YFc"] = (scratch("YFc", [2, NFC, D], BF16), S.buf("YFc"))
    R["YE"] = (scratch("YE", [NE, CAP + CAPC, D], BF16), S.buf("YE"))
    R["XT1"] = (scratch("XT1", [D, T], F32), S.buf("XT1"))
    R["PT"] = (scratch("PT", [2, 16, T], F32), S.buf("PT"))
    R["LG"] = (scratch("LG", [128, T // 128, NE], F32), S.buf("LG"))
    R["H2d"] = (scratch("H2d", [128, T // 128, D], BF16), S.buf("H2d"))
    K.R = R

    uid = [0]

    def sb(name, shape, dt, stack=es):
        uid[0] += 1
        t = stack.enter_context(nc.sbuf_tensor("sb%d_%s" % (uid[0], name), list(shape), dt))
        return t, S.buf(name)

    def pst(name, shape, dt, stack):
        uid[0] += 1
        t = stack.enter_context(nc.psum_tensor("ps%d_%s" % (uid[0], name), list(shape), dt))
        return t, S.buf(name)
    K.sb, K.pst = sb, pst

    ident_f, ident_f_b = sb("ident_f", [128, 128], F32)
    ident_b, ident_b_b = sb("ident_b", [128, 128], BF16)
    ones_f, ones_f_b = sb("ones_f", [128, 128], F32)
    ones_b, ones_b_b = sb("ones_b", [128, 128], BF16)
    S.dma("sp", ident_f[:], I["ident"][:, :], ident_f_b)
    S.dma("pool", ident_b[:], I["ident"][:, :], ident_b_b)
    S.op("dve", lambda h: h.memset(ones_f[:], 1.0), writes=[ones_f_b])
    S.op("dve", lambda h: h.memset(ones_b[:], 1.0), writes=[ones_b_b])
    K.ident_f, K.ident_b, K.ones_f, K.ones_b = (ident_f, ident_f_b), (ident_b, ident_b_b), (ones_f, ones_f_b), (ones_b, ones_b_b)

    sc, sc_b = sb("sc", [128, 8, 2], F32)
    S.dma("sp", sc[:], I["ccT"][:, :, :], sc_b)
    S.op("act", lambda h: h.activation(out=sc[:], in_=sc[:], func=AF.Silu), reads=[sc_b], writes=[sc_b])
    K.sc = (sc, sc_b)
    modT, modT_b = sb("modT", [128, 48, 2], F32)
    K.modT = (modT, modT_b)
    A1, A1_b = sb("A1", [128, 8, 2], F32)
    A2, A2_b = sb("A2", [128, 8, 2], F32)
    K.A1, K.A2 = (A1, A1_b), (A2, A2_b)

    with contextlib.ExitStack() as ph:
        xc, xc_b = sb("xcp", [128, 8, 512], F32, ph)
        for (t0, tn) in TCH:
            S.dma("sp", xc[:, :, :tn], I["xT"].rearrange("(k p) t -> p k t", p=128)[:, :, t0:t0 + tn], xc_b)
            S.dma("sp", R["XT"][0].rearrange("(k p) t -> p k t", p=128)[:, :, t0:t0 + tn], xc[:, :, :tn], R["XT"][1], [xc_b])
        S.barrier()

    S.persist = False
    phases = cfg.get("phases", ("diff", "swa", "merge"))
    if "swa" not in phases:
        with contextlib.ExitStack() as ph:
            zt, zt_b = sb("zt2", [128, T], BF16, ph)
            S.op("dve", lambda h: h.memset(zt[:], 0.0), writes=[zt_b])
            for k in range(8):
                S.dma("sp", R["YsT"][0][k * 128:(k + 1) * 128, :], zt[:], R["YsT"][1], [zt_b])
            S.barrier()
    if "hy" not in phases:
        with contextlib.ExitStack() as ph:
            zt, zt_b = sb("zt", [128, T], BF16, ph)
            S.op("dve", lambda h: h.memset(zt[:], 0.0), writes=[zt_b])
            for k in range(8):
                S.dma("sp", R["YhT"][0][k * 128:(k + 1) * 128, :], zt[:], R["YhT"][1], [zt_b])
            S.barrier()
    for l in range(nl):
        ctx_out = l < DEPTH - 1
        phase_mod(K, l)
        with contextlib.ExitStack() as ph:
            hT, hT_b = sb("hT", [128, 8, T], BF16, ph)
            phase_norm(K, ph, R["XT"], K.A1, 0, (hT, hT_b))
            if stop_after == "norm1":
                dbg = scratch("dbg_hT", [D, T], BF16)
                S.dma("sp", dbg.rearrange("(k p) t -> p k t", p=128), hT[:], S.buf("dbg"), [hT_b])
                S.barrier()
                break
            phase_inproj(K, ph, l, (hT, hT_b))
            S.barrier()
        if stop_after == "inproj":
            break
        if "diff" in phases:
            phase_diff(K, l, ctx_out)
        if stop_after == "diff":
            break
        if "swa" in phases:
            phase_swa(K, l, ctx_out)
        if "hy" in phases:
            phase_hyena(K, l, ctx_out)
        if stop_after == "hy":
            break
        if stop_after == "swa":
            break
        if "merge" in phases:
            phase_merge(K, l)
        if stop_after == "merge":
            break
        if "moe" in phases:
            phase_moe(K, l, ctx_out)
        if stop_after == "moe":
            break
    if stop_after is None:
        phase_final(K)

    S.barrier()
    es.close()
    return nc


def phase_mod(K, l):
    nc, S, I = K.nc, K.S, K.I
    modT, modT_b = K.modT
    sc, sc_b = K.sc
    with contextlib.ExitStack() as ph:
        wts = [K.sb("wmod%d" % i, [128, 8, 512], F32, ph) for i in range(2)]
        bm, bm_b = K.sb("bmodT", [128, 48], F32, ph)
        g1, g1_b = K.sb("g1T", [128, 8], F32, ph)
        g2, g2_b = K.sb("g2T", [128, 8], F32, ph)
        ps, ps_b = K.pst("ps_mod", [128, 512], F32, ph)
        S.dma("sp", bm[:], I["b_modT"][l], bm_b)
        S.dma("sp", g1[:], I["g1T"][l], g1_b)
        S.dma("sp", g2[:], I["g2T"][l], g2_b)
        for g in range(12):
            wt, wt_b = wts[g % 2]
            S.dma("sp", wt[:], I["w_mod"][l].rearrange("(k p) n -> p k n", p=128)[:, :, g * 512:(g + 1) * 512], wt_b)
            for m in range(4):
                mc = g * 4 + m
                _mm_group(S, ps_b, ps[:, 0:2],
                          [(wt[:, k, m * 128:(m + 1) * 128], sc[:, k, :]) for k in range(8)],
                          [wt_b, sc_b])
                S.op("dve", lambda h, mc=mc: h.tensor_scalar(out=modT[:, mc, :], in0=ps[:, 0:2], scalar1=bm[:, mc:mc + 1],
                                                            scalar2=None, op0=ALU.add),
                     reads=[ps_b, bm_b], writes=[modT_b])
        for (A, A_b), (g, g_b), j in ((K.A1, (g1, g1_b), 1), (K.A2, (g2, g2_b), 4)):
            for r in range(2):
                S.op("dve", lambda h, A=A, g=g, j=j, r=r: h.scalar_tensor_tensor(
                    out=A[:, :, r], in0=modT[:, j * 8:(j + 1) * 8, r], scalar=1.0, in1=g[:], op0=ALU.add, op1=ALU.mult),
                    reads=[modT_b, g_b], writes=[A_b])
        S.barrier()


def phase_norm(K, ph, X, Acoef, shift_j, out, out_f32_cb=None):
    nc, S = K.nc, K.S
    XT, XT_b = X
    A, A_b = Acoef
    modT, modT_b = K.modT
    hT, hT_b = out
    ones_f, ones_f_b = K.ones_f
    with contextlib.ExitStack() as st:
        xs = [K.sb("nx%d" % i, [128, 8, 512], F32, st) for i in range(2)]
        sq, sq_b = K.sb("nsq", [128, 8, 512], F32, st)
        rstd, rstd_b = K.sb("nrstd", [128, 512], F32, st)
        tmp, tmp_b = K.sb("ntmp", [128, 512], F32, st)
        ps, ps_b = K.pst("ps_norm", [128, 512], F32, st)
        for ci, (t0, tn) in enumerate(TCH):
            r = 0 if t0 < L else 1
            x, x_b = xs[ci % 2]
            S.dma("sp", x[:, :, :tn], XT.rearrange("(k p) t -> p k t", p=128)[:, :, t0:t0 + tn], x_b, [XT_b])
            S.op("act", lambda h: h.activation(out=sq[:, :, :tn], in_=x[:, :, :tn], func=AF.Square), reads=[x_b], writes=[sq_b])
            _mm_group(S, ps_b, ps[:, :tn], [(ones_f[:], sq[:, k, :tn]) for k in range(8)], [ones_f_b, sq_b])
            S.op("dve", lambda h: h.tensor_scalar(out=rstd[:, :tn], in0=ps[:, :tn], scalar1=1.0 / D, scalar2=EPS,
                                                  op0=ALU.mult, op1=ALU.add), reads=[ps_b], writes=[rstd_b])
            S.op("act", lambda h: h.activation(out=rstd[:, :tn], in_=rstd[:, :tn], func=AF.Sqrt), reads=[rstd_b], writes=[rstd_b])
            S.op("dve", lambda h: h.reciprocal(out=rstd[:, :tn], in_=rstd[:, :tn]), reads=[rstd_b], writes=[rstd_b])
            for k in range(8):
                S.op("dve", lambda h, k=k: h.tensor_tensor(out=tmp[:, :tn], in0=x[:, k, :tn], in1=rstd[:, :tn], op=ALU.mult),
                     reads=[x_b, rstd_b], writes=[tmp_b])
                S.op("act", lambda h, k=k: h.activation(out=hT[:, k, t0:t0 + tn], in_=tmp[:, :tn], func=AF.Identity,
                                                        scale=A[:, k, r:r + 1], bias=modT[:, shift_j * 8 + k, r:r + 1]),
                     reads=[tmp_b, A_b, modT_b], writes=[hT_b])
                if out_f32_cb is not None:
                    out_f32_cb(ci, k, t0, tn, r, tmp, tmp_b)
        S.barrier()


def _inproj_groups():
    g = []
    g += [("qk", "QdT", 0), ("qk", "QdT", 512), ("qk", "KdT", 0), ("qk", "KdT", 512)]
    g += [("v", "Vd", 0), ("v", "Vd", 512)]
    g += [("plain", "HyT", i * 512) for i in range(6)]
    g += [("qk", "QsT", 0), ("qk", "QsT", 512)]
    g += [("kv", None, 0)]
    g += [("gate", "GT", i * 512) for i in range(6)]
    return g


def phase_inproj(K, ph, l, hTb):
    nc, S, I, R = K.nc, K.S, K.I, K.R
    hT, hT_b = hTb
    with contextlib.ExitStack() as st:
        wts = [K.sb("wi%d" % i, [128, 8, 512], BF16, st) for i in range(2)]
        wsw, wsw_b = K.sb("wsw", [128, 8, 512], BF16, st)
        rC, rC_b = K.sb("ropeC", [128, L], F32, st)
        rS, rS_b = K.sb("ropeS", [128, L], F32, st)
        stg = [K.sb("stg%d" % i, [128, T], BF16, st) for i in range(2)]
        vst = [K.sb("vst%d" % i, [128, 512], BF16, st) for i in range(2)]
        t1, t1_b = K.sb("rt1", [128, 512], F32, st)
        t2, t2_b = K.sb("rt2", [128, 512], F32, st)
        psA = [K.pst("psA%d" % i, [128, 512], F32, st) for i in range(2)]
        psB = [K.pst("psB%d" % i, [128, 512], F32, st) for i in range(2)]
        S.dma("sp", rC[:], I["ropeC"][:, :], rC_b)
        S.dma("sp", rS[:], I["ropeS"][:, :], rS_b)
        win = I["w_in"][l].rearrange("(k p) n -> p k n", p=128)
        cnt = dict(s=0, p=0, v=0)

        def fm_chunk(wt, wt_b, m, kind, dst, row0):
            sg, sg_b = stg[cnt["s"] % 2]
            cnt["s"] += 1
            for (t0, tn) in TCH:
                pa, pa_b = psA[cnt["p"] % 2]
                pb, pb_b = psB[cnt["p"] % 2]
                cnt["p"] += 1
                _mm_group(S, pa_b, pa[:, :tn], [(wt[:, k, m * 128:(m + 1) * 128], hT[:, k, t0:t0 + tn]) for k in range(8)],
                          [wt_b, hT_b])
                if kind == "qk" and t0 < L:
                    _mm_group(S, pb_b, pb[:, :tn], [(wsw[:, k, m * 128:(m + 1) * 128], hT[:, k, t0:t0 + tn]) for k in range(8)],
                              [wsw_b, hT_b])
                    S.op("dve", lambda h: h.tensor_tensor(out=t1[:, :tn], in0=pa[:, :tn], in1=rC[:, t0:t0 + tn], op=ALU.mult),
                         reads=[pa_b, rC_b], writes=[t1_b])
                    S.op("dve", lambda h: h.tensor_tensor(out=t2[:, :tn], in0=pb[:, :tn], in1=rS[:, t0:t0 + tn], op=ALU.mult),
                         reads=[pb_b, rS_b], writes=[t2_b])
                    S.op("pool", lambda h: h.tensor_tensor(out=sg[:, t0:t0 + tn], in0=t1[:, :tn], in1=t2[:, :tn], op=ALU.add),
                         reads=[t1_b, t2_b], writes=[sg_b])
                elif kind == "gate":
                    S.op("act", lambda h: h.activation(out=sg[:, t0:t0 + tn], in_=pa[:, :tn], func=AF.Sigmoid),
                         reads=[pa_b], writes=[sg_b])
                else:
                    S.op("act", lambda h: h.activation(out=sg[:, t0:t0 + tn], in_=pa[:, :tn], func=AF.Copy),
                         reads=[pa_b], writes=[sg_b])
            S.dma("sp", R[dst][0][row0:row0 + 128, :], sg[:], R[dst][1], [sg_b])

        def tm_cols(wt, wt_b, c0, cn, dst, col0):
            for tt in range(T // 128):
                pa, pa_b = psA[cnt["p"] % 2]
                cnt["p"] += 1
                vs, vs_b = vst[cnt["v"] % 2]
                cnt["v"] += 1
                _mm_group(S, pa_b, pa[:, :cn], [(hT[:, k, tt * 128:(tt + 1) * 128], wt[:, k, c0:c0 + cn]) for k in range(8)],
                          [wt_b, hT_b])
                S.op("act", lambda h: h.activation(out=vs[:, :cn], in_=pa[:, :cn], func=AF.Copy), reads=[pa_b], writes=[vs_b])
                S.dma("sp", R[dst][0][tt * 128:(tt + 1) * 128, col0:col0 + cn], vs[:, :cn], R[dst][1], [vs_b])

        def make_swapped(wt, wt_b, ncols):
            src = wt[:, :, :ncols].rearrange("p k (q s f) -> p k q s f", s=2, f=16)
            dstv = wsw[:, :, :ncols].rearrange("p k (q s f) -> p k q s f", s=2, f=16)
            for k in range(8):
                S.op("pool", lambda h, k=k: h.tensor_copy(out=dstv[:, k, :, 0, :], in_=src[:, k, :, 1, :]), reads=[wt_b], writes=[wsw_b])
                S.op("pool", lambda h, k=k: h.tensor_copy(out=dstv[:, k, :, 1, :], in_=src[:, k, :, 0, :]), reads=[wt_b], writes=[wsw_b])

        for gi, (kind, dst, row0) in enumerate(_inproj_groups()):
            wt, wt_b = wts[gi % 2]
            S.dma("pool", wt[:], win[:, :, gi * 512:(gi + 1) * 512], wt_b)
            if kind == "qk":
                make_swapped(wt, wt_b, 512)
                for m in range(4):
                    fm_chunk(wt, wt_b, m, "qk", dst, row0 + m * 128)
            elif kind == "v":
                tm_cols(wt, wt_b, 0, 512, dst, row0)
            elif kind == "kv":
                make_swapped(wt, wt_b, 256)
                for m in range(2):
                    fm_chunk(wt, wt_b, m, "qk", "KsT", m * 128)
                tm_cols(wt, wt_b, 256, 256, "Vs", 0)
            else:
                for m in range(4):
                    fm_chunk(wt, wt_b, m, kind, dst, row0 + m * 128)
        S.barrier()


def _bcast_rows(ap, n):
    return bass.AP(ap.tensor, ap.offset, [[0, 128], [1, n]])


def phase_diff(K, l, ctx_out):
    nc, S, I, R = K.nc, K.S, K.I, K.R
    ones_f, ones_f_b = K.ones_f
    ones_b, ones_b_b = K.ones_b
    lambda_init = 0.8 - 0.6 * math.exp(-0.3 * l)
    with contextlib.ExitStack() as st:
        lp, lp_b = K.sb("lp", [128, 256], F32, st)
        lsc, lsc_b = K.sb("lsc", [128, 4], F32, st)
        gsc, gsc_b = K.sb("gsc", [128, 1], F32, st)
        S.dma("sp", lp[:], _bcast_rows(I["diff_lambda"][l], 256), lp_b)
        S.dma("sp", gsc[:], I["sublnT"][l], gsc_b)
        S.op("dve", lambda h: h.tensor_tensor(out=lp[:, 0:64], in0=lp[:, 0:64], in1=lp[:, 64:128], op=ALU.mult), reads=[lp_b], writes=[lp_b])
        S.op("dve", lambda h: h.tensor_tensor(out=lp[:, 128:192], in0=lp[:, 128:192], in1=lp[:, 192:256], op=ALU.mult), reads=[lp_b], writes=[lp_b])
        S.op("dve", lambda h: h.reduce_sum(out=lsc[:, 0:1], in_=lp[:, 0:64], axis=AX.X), reads=[lp_b], writes=[lsc_b])
        S.op("dve", lambda h: h.reduce_sum(out=lsc[:, 1:2], in_=lp[:, 128:192], axis=AX.X), reads=[lp_b], writes=[lsc_b])
        S.op("act", lambda h: h.activation(out=lsc[:, 0:2], in_=lsc[:, 0:2], func=AF.Exp), reads=[lsc_b], writes=[lsc_b])
        S.op("dve", lambda h: h.scalar_tensor_tensor(out=lsc[:, 2:3], in0=lsc[:, 1:2], scalar=-lambda_init, in1=lsc[:, 0:1],
                                                     op0=ALU.add, op1=ALU.subtract), reads=[lsc_b], writes=[lsc_b])
        S.op("dve", lambda h: h.tensor_scalar(out=gsc[:], in0=gsc[:], scalar1=1.0 - lambda_init, scalar2=None, op0=ALU.mult),
             reads=[gsc_b], writes=[gsc_b])

        QT = [K.sb("dQT%d" % i, [128, T], BF16, st) for i in range(2)]
        KT = [K.sb("dKT%d" % i, [128, T], BF16, st) for i in range(2)]
        VV = [K.sb("dV%d" % i, [128, T // 128, 128], BF16, st) for i in range(2)]
        pt = [K.sb("dP%d" % i, [128, 2, 512], BF16, st) for i in range(2)]
        psS = [K.pst("dpsS%d" % i, [128, 2, 512], F32, st) for i in range(2)]
        acc = [[K.sb("dacc%d%d" % (m, i), [128, 512], F32, st) for i in range(2)] for m in range(2)]
        psO = [K.pst("dpsO%d" % m, [128, 512], F32, st) for m in range(2)]
        psZ = [K.pst("dpsZ%d" % m, [128, 512], F32, st) for m in range(2)]
        rz = [K.sb("drz%d" % m, [128, 512], F32, st) for m in range(2)]
        oo = [K.sb("doo%d" % m, [128, 512], F32, st) for m in range(2)]
        of, of_b = K.sb("dof", [128, 512], F32, st)
        sqf, sqf_b = K.sb("dsq", [128, 512], F32, st)
        rs, rs_b = K.sb("drs", [128, 512], F32, st)
        ost = [K.sb("dost%d" % i, [128, 512], BF16, st) for i in range(2)]
        nch = 0
        for hd in range(8):
            q, q_b = QT[hd % 2]
            k, k_b = KT[hd % 2]
            v, v_b = VV[hd % 2]
            S.dma("sp", q[:], R["QdT"][0][hd * 128:(hd + 1) * 128, :], q_b, [R["QdT"][1]])
            S.dma("sp", k[:], R["KdT"][0][hd * 128:(hd + 1) * 128, :], k_b, [R["KdT"][1]])
            S.dma("sp", v[:], R["Vd"][0][:, hd * 128:(hd + 1) * 128].rearrange("(t p) e -> p t e", p=128), v_b, [R["Vd"][1]])
            for (q0, qn) in TCH:
                if q0 >= L and not ctx_out:
                    continue
                kts = list(range(T // 128)) if q0 < L else [32, 33]
                cpar = nch % 2

                def qk(i, kt, m, drain=False):
                    ps, ps_b = psS[i % 2]
                    _mm1(S, ps_b, ps[:, m, :qn], k[m * 64:(m + 1) * 64, kt * 128:(kt + 1) * 128], q[m * 64:(m + 1) * 64, q0:q0 + qn],
                         [k_b, q_b], True, True, drain=drain)

                def expo(i):
                    ps, ps_b = psS[i % 2]
                    p, p_b = pt[i % 2]
                    for m in range(2):
                        S.op("act", lambda h, m=m: h.activation(out=p[:, m, :qn], in_=ps[:, m, :qn], func=AF.Exp, scale=0.125), reads=[ps_b], writes=[p_b])
                qk(0, kts[0], 0, drain=True)
                qk(0, kts[0], 1, drain=True)
                expo(0)
                for i, kt in enumerate(kts):
                    first, last = i == 0, i == len(kts) - 1
                    p, p_b = pt[i % 2]
                    for m in range(2):
                        if not last:
                            qk(i + 1, kts[i + 1], m, drain=(i == 0 and m == 0))
                        _mm1(S, psO[m][1], psO[m][0][:, :qn], v[:, kt, :], p[:, m, :qn], [v_b, p_b], first, last)
                        a_, a_b = acc[m][cpar]
                        eng = "dve" if m == 0 else "pool"
                        if first:
                            S.op(eng, lambda h, a_=a_, m=m: h.tensor_copy(out=a_[:, :qn], in_=p[:, m, :qn]), reads=[p_b], writes=[a_b])
                        else:
                            S.op(eng, lambda h, a_=a_, m=m: h.tensor_tensor(out=a_[:, :qn], in0=a_[:, :qn], in1=p[:, m, :qn], op=ALU.add),
                                 reads=[p_b, a_b], writes=[a_b])
                    if not last:
                        expo(i + 1)
                for m in range(2):
                    a_, a_b = acc[m][cpar]
                    _mm_group(S, psZ[m][1], psZ[m][0][:, :qn], [(ones_f[:], a_[:, :qn])], [ones_f_b, a_b])
                for m in range(2):
                    S.op("dve", lambda h, m=m: h.reciprocal(out=rz[m][0][:, :qn], in_=psZ[m][0][:, :qn]), reads=[psZ[m][1]], writes=[rz[m][1]])
                    S.op("dve", lambda h, m=m: h.tensor_tensor(out=oo[m][0][:, :qn], in0=psO[m][0][:, :qn], in1=rz[m][0][:, :qn], op=ALU.mult),
                         reads=[psO[m][1], rz[m][1]], writes=[oo[m][1]])
                S.op("dve", lambda h: h.scalar_tensor_tensor(out=of[:, :qn], in0=oo[1][0][:, :qn], scalar=lsc[:, 2:3], in1=oo[0][0][:, :qn],
                                                              op0=ALU.mult, op1=ALU.add), reads=[oo[0][1], oo[1][1], lsc_b], writes=[of_b])
                S.op("pool", lambda h: h.tensor_tensor(out=sqf[:, :qn], in0=of[:, :qn], in1=of[:, :qn], op=ALU.mult), reads=[of_b], writes=[sqf_b])
                ps, ps_b = psZ[0]
                _mm_group(S, ps_b, ps[:, :qn], [(ones_f[:], sqf[:, :qn])], [ones_f_b, sqf_b])
                S.op("dve", lambda h: h.tensor_scalar(out=rs[:, :qn], in0=ps[:, :qn], scalar1=1.0 / 128, scalar2=EPS, op0=ALU.mult, op1=ALU.add),
                     reads=[ps_b], writes=[rs_b])
                S.op("act", lambda h: h.activation(out=rs[:, :qn], in_=rs[:, :qn], func=AF.Sqrt), reads=[rs_b], writes=[rs_b])
                S.op("dve", lambda h: h.reciprocal(out=rs[:, :qn], in_=rs[:, :qn]), reads=[rs_b], writes=[rs_b])
                S.op("dve", lambda h: h.tensor_tensor(out=of[:, :qn], in0=of[:, :qn], in1=rs[:, :qn], op=ALU.mult), reads=[of_b, rs_b], writes=[of_b])
                o, o_b = ost[nch % 2]
                nch += 1
                S.op("act", lambda h, o=o: h.activation(out=o[:, :qn], in_=of[:, :qn], func=AF.Copy, scale=gsc[:, 0:1]), reads=[of_b, gsc_b], writes=[o_b])
                S.dma("sp", R["YdT"][0][hd * 128:(hd + 1) * 128, q0:q0 + qn], o[:, :qn], R["YdT"][1], [o_b])
        S.barrier()


def phase_swa(K, l, ctx_out):
    nc, S, I, R = K.nc, K.S, K.I, K.R
    ones_b, ones_b_b = K.ones_b
    with contextlib.ExitStack() as st:
        es_, es_b = K.sb("esink", [128, 16], F32, st)
        S.dma("sp", es_[:], _bcast_rows(I["swa_sink"][l], 16), es_b)
        S.op("act", lambda h: h.activation(out=es_[:], in_=es_[:], func=AF.Exp), reads=[es_b], writes=[es_b])
        mlo, mlo_b = K.sb("mlo", [128, 128], BF16, st)
        mup, mup_b = K.sb("mup", [128, 128], BF16, st)
        S.dma("pool", mlo[:], I["trilo"][:, :], mlo_b)
        S.dma("pool", mup[:], I["triup"][:, :], mup_b)
        QC = [[K.sb("sQ%d%d" % (c, i), [128, T], BF16, st) for i in range(2)] for c in range(2)]
        KT = [K.sb("sKT%d" % i, [128, T], BF16, st) for i in range(2)]
        VV = [K.sb("sV%d" % i, [128, T // 128, 64], BF16, st) for i in range(2)]
        pt = [K.sb("sP%d" % i, [128, 512], BF16, st) for i in range(2)]
        psS = [K.pst("spsS%d" % i, [128, 512], F32, st) for i in range(2)]
        psO = [K.pst("spsO%d" % i, [128, 512], F32, st) for i in range(2)]
        psZ = [K.pst("spsZ%d" % i, [128, 512], F32, st) for i in range(2)]
        zz, zz_b = K.sb("szz", [64, 512], F32, st)
        ost = [K.sb("sost%d" % i, [64, 512], BF16, st) for i in range(2)]
        nblk = 0
        npt = 0
        for kh in range(4):
            k, k_b = KT[kh % 2]
            v, v_b = VV[kh % 2]
            for half in range(2):
                S.dma("sp", k[half * 64:(half + 1) * 64, :], R["KsT"][0][kh * 64:(kh + 1) * 64, :], k_b, [R["KsT"][1]])
            S.dma("sp", v[:], R["Vs"][0][:, kh * 64:(kh + 1) * 64].rearrange("(t p) e -> p t e", p=128), v_b, [R["Vs"][1]])
            qc = []
            for c in range(2):
                qq, qq_b = QC[c][kh % 2]
                S.dma("sp", qq[:], R["QsT"][0][kh * 256 + c * 128: kh * 256 + (c + 1) * 128, :], qq_b, [R["QsT"][1]])
                qc.append((qq, qq_b))
            nqb = T // 128 if ctx_out else L // 128
            dbgl = K.cfg.get("swa_dbg", 9)
            if dbgl < 9:
                nqb = 2 if kh == 0 else 0
            steps = []
            for qb in range(nqb):
                if qb < 32:
                    kts = [(kk, mk) for kk, mk in ((qb - 1, "lo"), (qb, None), (qb + 1, "up")) if 0 <= kk < 32] + [(32, None), (33, None)]
                else:
                    kts = [(32, None), (33, None)]
                for i, (kt, mk) in enumerate(kts):
                    steps.append((qb, kt, mk, i == 0, i == len(kts) - 1))

            def stage_a(si):
                qb, kt, mk, first, last = steps[si]
                ps, ps_b = psS[si % 2]
                p, p_b = pt[si % 2]
                for gi, g in enumerate((0, 2, 1, 3)):
                    r0 = (g % 2) * 64
                    qq, qq_b = qc[g // 2]
                    _mm1(S, ps_b, ps[:, g * 128:(g + 1) * 128], k[r0:r0 + 64, kt * 128:(kt + 1) * 128], qq[r0:r0 + 64, qb * 128:(qb + 1) * 128],
                         [k_b, qq_b], True, True, drain=(gi == 2 or (gi == 0 and si <= 1)))
                S.op("act", lambda h: h.activation(out=p[:], in_=ps[:], func=AF.Exp, scale=0.125), reads=[ps_b], writes=[p_b])
                if mk is not None:
                    mt, mt_b = (mlo, mlo_b) if mk == "lo" else (mup, mup_b)
                    for g in range(4):
                        S.op("pool", lambda h, g=g, mt=mt: h.tensor_tensor(out=p[:, g * 128:(g + 1) * 128], in0=p[:, g * 128:(g + 1) * 128], in1=mt[:], op=ALU.mult),
                             reads=[p_b, mt_b], writes=[p_b])

            if steps:
                stage_a(0)
            for si, (qb, kt, mk, first, last) in enumerate(steps):
                if si + 1 < len(steps):
                    stage_a(si + 1)
                po, po_b = psO[qb % 2]
                pz, pz_b = psZ[qb % 2]
                p, p_b = pt[si % 2]
                _mm1(S, po_b, po[0:64, :], v[:, kt, :], p[:], [v_b, p_b], first, last)
                _mm1(S, pz_b, pz[0:64, :], ones_b[:, 0:64], p[:], [ones_b_b, p_b], first, last)
                if not last:
                    continue
                for g in range(4):
                    S.op("dve", lambda h, g=g: h.tensor_scalar(out=zz[:, g * 128:(g + 1) * 128], in0=pz[0:64, g * 128:(g + 1) * 128],
                                                               scalar1=es_[0:64, kh * 4 + g:kh * 4 + g + 1], scalar2=None, op0=ALU.add),
                         reads=[pz_b, es_b], writes=[zz_b])
                S.op("dve", lambda h: h.reciprocal(out=zz[:], in_=zz[:]), reads=[zz_b], writes=[zz_b])
                o, o_b = ost[qb % 2]
                S.op("dve", lambda h, o=o: h.tensor_tensor(out=o[:], in0=po[0:64, :], in1=zz[:], op=ALU.mult),
                     reads=[po_b, zz_b], writes=[o_b])
                S.dma("sp", R["YsT"][0][kh * 256:(kh + 1) * 256, qb * 128:(qb + 1) * 128].rearrange("(g d) q -> d g q", g=4),
                      o[:].rearrange("p (g q) -> p g q", g=4), R["YsT"][1], [o_b])
        S.barrier()


def phase_merge(K, l):
    nc, S, I, R = K.nc, K.S, K.I, K.R
    modT, modT_b = K.modT
    with contextlib.ExitStack() as st:
        wb, wb_b = K.sb("wbr", [128, 24, D], BF16, st)
        wo, wo_b = K.sb("wout", [128, 8, D], BF16, st)
        for b in range(3):
            S.dma("pool", wb[:, b * 8:(b + 1) * 8, :], I["w_branch"][l, b].rearrange("(k p) n -> p k n", p=128), wb_b)
        S.dma("pool", wo[:], I["w_out"][l].rearrange("(k p) n -> p k n", p=128), wo_b)
        yb = [K.sb("my%d" % b, [128, 8, 512], BF16, st) for b in range(3)]
        gt, gt_b = K.sb("mgt", [128, 24, 512], BF16, st)
        mg, mg_b = K.sb("mmg", [128, 8, 512], F32, st)
        mgb, mgb_b = K.sb("mmgb", [128, 8, 512], BF16, st)
        tmp = [K.sb("mtmp%d" % i, [128, 512], F32, st) for i in range(2)]
        xx, xx_b = K.sb("mx", [128, 8, 512], F32, st)
        pss = [K.pst("mps%d" % i, [128, 512], F32, st) for i in range(4)]
        npz = 0
        ntmp = 0
        XTv = R["XT"][0].rearrange("(k p) t -> p k t", p=128)
        for (t0, tn) in TCH:
            r = 0 if t0 < L else 1
            for b, nm in enumerate(("YdT", "YhT", "YsT")):
                S.dma("sp", yb[b][0][:, :, :tn], R[nm][0].rearrange("(k p) t -> p k t", p=128)[:, :, t0:t0 + tn], yb[b][1], [R[nm][1]])
            S.dma("sp", gt[:, :, :tn], R["GT"][0].rearrange("(k p) t -> p k t", p=128)[:, :, t0:t0 + tn], gt_b, [R["GT"][1]])
            S.dma("sp", xx[:, :, :tn], XTv[:, :, t0:t0 + tn], xx_b, [R["XT"][1]])
            for m in range(8):
                for b in range(3):
                    ps, ps_b = pss[npz % 4]
                    npz += 1
                    _mm_group(S, ps_b, ps[:, :tn], [(wb[:, b * 8 + k, m * 128:(m + 1) * 128], yb[b][0][:, k, :tn]) for k in range(8)],
                              [wb_b, yb[b][1]])
                    if b == 0:
                        S.op("dve", lambda h, m=m, ps=ps: h.tensor_tensor(out=mg[:, m, :tn], in0=ps[:, :tn], in1=gt[:, m, :tn], op=ALU.mult),
                             reads=[ps_b, gt_b], writes=[mg_b])
                    else:
                        tp, tp_b = tmp[ntmp % 2]
                        ntmp += 1
                        S.op("dve", lambda h, m=m, b=b, ps=ps, tp=tp: h.tensor_tensor(out=tp[:, :tn], in0=ps[:, :tn], in1=gt[:, b * 8 + m, :tn], op=ALU.mult),
                             reads=[ps_b, gt_b], writes=[tp_b])
                        S.op("pool", lambda h, m=m, tp=tp: h.tensor_tensor(out=mg[:, m, :tn], in0=mg[:, m, :tn], in1=tp[:, :tn], op=ALU.add),
                             reads=[tp_b, mg_b], writes=[mg_b])
                S.op("act", lambda h, m=m: h.activation(out=mgb[:, m, :tn], in_=mg[:, m, :tn], func=AF.Copy), reads=[mg_b], writes=[mgb_b])
            for m in range(8):
                ps, ps_b = pss[npz % 4]
                npz += 1
                _mm_group(S, ps_b, ps[:, :tn], [(wo[:, k, m * 128:(m + 1) * 128], mgb[:, k, :tn]) for k in range(8)], [wo_b, mgb_b])
                S.op("dve", lambda h, m=m, ps=ps: h.scalar_tensor_tensor(out=xx[:, m, :tn], in0=ps[:, :tn], scalar=modT[:, 16 + m, r:r + 1],
                                                                         in1=xx[:, m, :tn], op0=ALU.mult, op1=ALU.add),
                     reads=[ps_b, modT_b, xx_b], writes=[xx_b])
            S.dma("sp", XTv[:, :, t0:t0 + tn], xx[:, :, :tn], R["XT"][1], [xx_b])
        S.barrier()


PI = math.pi


def _hy_filter(K, l, Lx, embT_ap, negt_ap, Cm, Sm, wf_ap, KFs):
    nc, S, I, R = K.nc, K.S, K.I, K.R
    ones_b, ones_b_b = K.ones_b
    nlt = Lx // 128
    nft = (Lx + 128) // 128
    nch = max(1, Lx // 512)
    cw = min(512, Lx)
    with contextlib.ExitStack() as st:
        w1, w1_b = K.sb("fw1", [33, 64], F32, st)
        w2, w2_b = K.sb("fw2", [64, 64], F32, st)
        w3, w3_b = K.sb("fw3", [64, 2048], F32, st)
        b1, b1_b = K.sb("fb1", [64, 1], F32, st)
        b2, b2_b = K.sb("fb2", [64, 1], F32, st)
        fr, fr_b = K.sb("ffr", [64, 1], F32, st)
        emb, emb_b = K.sb("femb", [33, Lx], F32, st)
        ngt, ngt_b = K.sb("fngt", [128, nlt], F32, st)
        dl, dl_b = K.sb("fdl", [128, D], F32, st)
        wf, wf_b = K.sb("fwf", [128, nft], F32, st)
        z1, z1_b = K.sb("fz1", [64, Lx], F32, st)
        z2, z2_b = K.sb("fz2", [64, Lx], F32, st)
        rn, rn_b = K.sb("frn", [128, D], F32, st)
        dec, dec_b = K.sb("fdec", [128, D], F32, st)
        hd = [K.sb("fhd%d" % i, [128, D], F32, st) for i in range(2)]
        ab, ab_b = K.sb("fab", [128, 512], BF16, st)
        gsel, gsel_b = K.sb("fgsel", [64, 512], F32, st)
        AT, AT_b = K.sb("fAT", [128, nlt, D], BF16, st)
        for t_, src in ((w1, I["hy_w1"][l]), (w2, I["hy_w2"][l]), (w3, I["hy_w3"][l]), (b1, I["hy_b1"][l]), (b2, I["hy_b2"][l]),
                        (fr, I["hy_fr"][l]), (emb, embT_ap), (ngt, negt_ap), (wf, wf_ap)):
            pass
        S.dma("sp", w1[:], I["hy_w1"][l], w1_b)
        S.dma("sp", w2[:], I["hy_w2"][l], w2_b)
        S.dma("sp", w3[:], I["hy_w3"][l], w3_b)
        S.dma("sp", b1[:], I["hy_b1"][l], b1_b)
        S.dma("sp", b2[:], I["hy_b2"][l], b2_b)
        S.dma("sp", fr[:], I["hy_fr"][l], fr_b)
        S.dma("sp", emb[:], embT_ap, emb_b)
        S.dma("sp", ngt[:], negt_ap, ngt_b)
        S.dma("sp", wf[:], wf_ap, wf_b)
        S.dma("sp", dl[:], _bcast_rows(I["deltas"][0], D), dl_b)
        psm = [K.pst("fps%d" % i, [128, 512], F32, st) for i in range(4)]
        psN = [K.pst("fpsN%d" % i, [128, 512], F32, st) for i in range(2)]
        for (wm, wm_b, bb, bb_b, src, src_b, dst, dst_b) in ((w1, w1_b, b1, b1_b, emb, emb_b, z1, z1_b), (w2, w2_b, b2, b2_b, z1, z1_b, z2, z2_b)):
            for ci in range(nch):
                ps, ps_b = psm[ci % 4]
                sl = slice(ci * cw, (ci + 1) * cw)
                _mm_group(S, ps_b, ps[0:64, :cw], [(wm[:], src[:, sl])], [wm_b, src_b])
                S.op("dve", lambda h, ps=ps, sl=sl, bb=bb, dst=dst: h.tensor_scalar(out=dst[:, sl], in0=ps[0:64, :cw], scalar1=bb[:, 0:1], scalar2=fr[:, 0:1],
                                                                                 op0=ALU.add, op1=ALU.mult), reads=[ps_b, bb_b, fr_b], writes=[dst_b])
                for _ in range(2):
                    for cop, sh in ((ALU.is_gt, -2.0 * PI), (ALU.is_lt, 2.0 * PI)):
                        thr = PI if cop == ALU.is_gt else -PI
                        S.op("dve", lambda h, sl=sl, dst=dst, cop=cop, thr=thr: h.tensor_scalar(out=gsel[:, :cw], in0=dst[:, sl], scalar1=thr, scalar2=None, op0=cop),
                             reads=[dst_b], writes=[gsel_b])
                        S.op("dve", lambda h, sl=sl, dst=dst, sh=sh: h.scalar_tensor_tensor(out=dst[:, sl], in0=gsel[:, :cw], scalar=sh, in1=dst[:, sl],
                                                                                          op0=ALU.mult, op1=ALU.add), reads=[gsel_b, dst_b], writes=[dst_b])
                S.op("act", lambda h, sl=sl, dst=dst: h.activation(out=dst[:, sl], in_=dst[:, sl], func=AF.Sin), reads=[dst_b], writes=[dst_b])

        def htile(lt, cb):
            S.op("act", lambda h: h.activation(out=dec[:], in_=dl[:], func=AF.Exp, scale=ngt[:, lt:lt + 1]), reads=[dl_b, ngt_b], writes=[dec_b])
            for j in range(4):
                ps, ps_b = psm[j]
                _mm_group(S, ps_b, ps[:], [(z2[:, lt * 128:(lt + 1) * 128], w3[:, j * 512:(j + 1) * 512])], [z2_b, w3_b])
            cb(lt)

        for lt in range(nlt):
            def cb1(lt):
                for j in range(4):
                    ps, ps_b = psm[j]
                    half = j % 2
                    tf, tf_b = hd[j // 2]
                    S.op("dve", lambda h, ps=ps, half=half, tf=tf: h.tensor_tensor(out=tf[:, half * 512:(half + 1) * 512], in0=ps[:],
                                                                                   in1=dec[:, half * 512:(half + 1) * 512], op=ALU.mult),
                         reads=[ps_b, dec_b], writes=[tf_b])
                    S.op("act", lambda h, half=half, tf=tf: h.activation(out=ab[:], in_=tf[:, half * 512:(half + 1) * 512], func=AF.Abs),
                         reads=[tf_b], writes=[ab_b])
                    first = (lt == 0 and j < 2)
                    last = (lt == nlt - 1 and j >= 2)
                    _mm1(S, psN[half][1], psN[half][0][:], ones_b[:], ab[:], [ones_b_b, ab_b], first, last)
            htile(lt, cb1)
        for half in range(2):
            S.op("dve", lambda h, half=half: h.tensor_scalar(out=rn[:, half * 512:(half + 1) * 512], in0=psN[half][0][:], scalar1=EPS, scalar2=None, op0=ALU.add),
                 reads=[psN[half][1]], writes=[rn_b])
        S.op("dve", lambda h: h.reciprocal(out=rn[:], in_=rn[:]), reads=[rn_b], writes=[rn_b])
        for plane, op2 in (("C", ALU.add), ("S", ALU.subtract)):
            for lt in range(nlt):
                def cb2(lt):
                    for j in range(4):
                        ps, ps_b = psm[j]
                        half, dr = j % 2, j // 2
                        S.op("dve", lambda h, ps=ps, half=half, dr=dr: h.tensor_tensor(out=hd[dr][0][:, half * 512:(half + 1) * 512], in0=ps[:],
                                                                                      in1=dec[:, half * 512:(half + 1) * 512], op=ALU.mult),
                             reads=[ps_b, dec_b], writes=[hd[dr][1]])
                    if lt == 0:
                        S.op("dve", lambda h: h.memset(hd[1][0][0:1, :], 0.0), writes=[hd[1][1]])
                    S.op("pool", lambda h: h.tensor_tensor(out=hd[0][0][:], in0=hd[0][0][:], in1=hd[1][0][:], op=op2), reads=[hd[0][1], hd[1][1]], writes=[hd[0][1]])
                    S.op("pool", lambda h: h.tensor_tensor(out=AT[:, lt, :], in0=hd[0][0][:], in1=rn[:], op=ALU.mult), reads=[hd[0][1], rn_b], writes=[AT_b])
                htile(lt, cb2)
            pi = 0 if plane == "C" else 1

            def evac(ft, pc, ps_, pi=pi):
                src = pc if pi == 0 else ps_
                for hh in range(2):
                    o, o_b = hd[hh]
                    S.op("dve", lambda h, hh=hh, o=o: h.tensor_scalar(out=o[:, 0:512], in0=src[hh][0][:], scalar1=wf[:, ft:ft + 1], scalar2=None, op0=ALU.mult),
                         reads=[src[hh][1], wf_b], writes=[o_b])
                    S.dma("sp", KFs[0][pi, ft * 128:(ft + 1) * 128, hh * 512:(hh + 1) * 512], o[:, 0:512], KFs[1], [o_b])
            _fwd_dft(K, st, Cm, Sm, nlt, nft, AT, AT_b, 0, (plane,), evac, psm)
        S.barrier()


def _fwd_dft(K, st, Cm, Sm, ntt, nft, rhs, rhs_b, tt0, planes, evac, pspool):
    S = K.S
    with contextlib.ExitStack() as s2:
        blk = {p: [K.sb("dblk%s%d" % (p, i), [128, ntt, 128], BF16, s2) for i in range(2)] for p in planes}
        for ft in range(nft):
            pss = {}
            for pi, p in enumerate(("C", "S")):
                if p not in planes:
                    pss[p] = None
                    continue
                M = Cm if p == "C" else Sm
                b, b_b = blk[p][ft % 2]
                S.dma("sp", b[:], M.rearrange("(t p) f -> p t f", p=128)[:, 0:ntt, ft * 128:(ft + 1) * 128], b_b)
                pss[p] = [pspool[pi * 2 + hh] for hh in range(2)]
                for hh in range(2):
                    ps, ps_b = pss[p][hh]
                    _mm_group(S, ps_b, ps[:], [(b[:, t, :], rhs[:, tt0 + t, hh * 512:(hh + 1) * 512]) for t in range(ntt)], [b_b, rhs_b])
            evac(ft, pss["C"], pss["S"])


def phase_hyena(K, l, ctx_out):
    nc, S, I, R = K.nc, K.S, K.I, K.R
    ident_b, ident_b_b = K.ident_b
    _hy_filter(K, l, L, I["embT"][:, :], I["negt"][:, :], I["dftC"], I["dftS"], I["wf"][:, :], R["KF"])
    if ctx_out:
        _hy_filter(K, l, LC, I["embTc"][:, :], I["negtc"][:, :], I["dftCc"], I["dftSc"], I["wfc"][:, :], R["KFc"])
    segs = [(0, L)] + ([(L, LC)] if ctx_out else [])
    TT = T if ctx_out else L
    with contextlib.ExitStack() as st:
        utok, utok_b = K.sb("hutok", [128, T // 128, D], BF16, st)
        with contextlib.ExitStack() as s2:
            cw, cw_b = K.sb("hcw", [128, 24, 3], F32, s2)
            cb, cb_b = K.sb("hcb", [128, 24], F32, s2)
            S.dma("sp", cw[:], I["cwT"][l], cw_b)
            S.dma("sp", cb[:], I["cbT"][l], cb_b)
            xin = [K.sb("hxin%d" % i, [128, T], BF16, s2) for i in range(3)]
            yc = [K.sb("hyc%d" % i, [128, T], F32, s2) for i in range(3)]
            x0b, x0b_b = K.sb("hx0b", [128, T], BF16, s2)
            ub, ub_b = K.sb("hub", [128, T], BF16, s2)
            ptr = [K.pst("hptr%d" % i, [128, 1024], BF16, s2) for i in range(2)]
            ntr = 0
            for c in range(8):
                for s_ in range(3):
                    j = s_ * 8 + c
                    xi, xi_b = xin[s_]
                    y, y_b = yc[s_]
                    S.dma("sp", xi[:, :TT], R["HyT"][0][j * 128:(j + 1) * 128, 0:TT], xi_b, [R["HyT"][1]])
                    for (a0, n) in segs:
                        S.op("dve", lambda h, j=j, xi=xi, y=y: h.tensor_scalar(out=y[:, a0:a0 + n], in0=xi[:, a0:a0 + n], scalar1=cw[:, j, 1:2], scalar2=cb[:, j:j + 1],
                                                                            op0=ALU.mult, op1=ALU.add), reads=[xi_b, cw_b, cb_b], writes=[y_b])
                        S.op("dve", lambda h, j=j, xi=xi, y=y: h.scalar_tensor_tensor(out=y[:, a0 + 1:a0 + n], in0=xi[:, a0:a0 + n - 1], scalar=cw[:, j, 0:1],
                                                                                   in1=y[:, a0 + 1:a0 + n], op0=ALU.mult, op1=ALU.add),
                             reads=[xi_b, cw_b, y_b], writes=[y_b])
                        S.op("dve", lambda h, j=j, xi=xi, y=y: h.scalar_tensor_tensor(out=y[:, a0:a0 + n - 1], in0=xi[:, a0 + 1:a0 + n], scalar=cw[:, j, 2:3],
                                                                                   in1=y[:, a0:a0 + n - 1], op0=ALU.mult, op1=ALU.add),
                             reads=[xi_b, cw_b, y_b], writes=[y_b])
                S.op("act", lambda h: h.activation(out=x0b[:, :TT], in_=yc[0][0][:, :TT], func=AF.Copy), reads=[yc[0][1]], writes=[x0b_b])
                S.op("pool", lambda h: h.tensor_tensor(out=ub[:, :TT], in0=yc[1][0][:, :TT], in1=yc[2][0][:, :TT], op=ALU.mult),
                     reads=[yc[1][1], yc[2][1]], writes=[ub_b])
                S.dma("sp", R["X0T"][0][c * 128:(c + 1) * 128, 0:TT], x0b[:, :TT], R["X0T"][1], [x0b_b])
                S.dma("sp", R["UT"][0][c * 128:(c + 1) * 128, 0:TT], ub[:, :TT], R["UT"][1], [ub_b])
                for t8 in range(0, TT // 128, 8):
                    n8 = min(8, TT // 128 - t8)
                    pt_, pt_b = ptr[ntr % 2]
                    ntr += 1

                    def trf(h, t8=t8, n8=n8, pt_=pt_):
                        ins = None
                        for q in range(n8):
                            ins = h.transpose(pt_[:, q * 128:(q + 1) * 128], ub[:, (t8 + q) * 128:(t8 + q + 1) * 128], ident_b[:])
                        return ins
                    S.op("pe", trf, reads=[ub_b, ident_b_b], writes=[pt_b])
                    S.op("act", lambda h, t8=t8, n8=n8, pt_=pt_, c=c: h.activation(out=utok[:, t8:t8 + n8, c * 128:(c + 1) * 128],
                                                                                   in_=pt_[:, :n8 * 128].rearrange("p (q f) -> p q f", f=128), func=AF.Copy),
                         reads=[pt_b], writes=[utok_b])
            S.barrier()
        for si, (a0, n) in enumerate(segs):
            Cm, Sm = (I["dftC"], I["dftS"]) if si == 0 else (I["dftCc"], I["dftSc"])
            KFs = R["KF"] if si == 0 else R["KFc"]
            YFs = R["YF"] if si == 0 else R["YFc"]
            ntt = n // 128
            nft = (n + 128) // 128
            with contextlib.ExitStack() as s2:
                kf = [K.sb("hkf%d" % i, [128, 2, D], F32, s2) for i in range(2)]
                tq = [K.sb("htq%d" % i, [128, 512], F32, s2) for i in range(4)]
                yo = [K.sb("hyo%d" % i, [128, 2, D], BF16, s2) for i in range(2)]
                pspool = [K.pst("hps%d" % i, [128, 512], F32, s2) for i in range(4)]

                def evac(ft, pc, ps_, KFs=KFs, YFs=YFs):
                    k_, k_b = kf[ft % 2]
                    o, o_b = yo[ft % 2]
                    S.dma("sp", k_[:], KFs[0][:, ft * 128:(ft + 1) * 128, :].rearrange("a p c -> p a c"), k_b, [KFs[1]])
                    for hh in range(2):
                        cs = slice(hh * 512, (hh + 1) * 512)
                        ur, ur_b = pc[hh]
                        ui, ui_b = ps_[hh]
                        S.op("dve", lambda h: h.tensor_tensor(out=tq[0][0][:], in0=ur[:], in1=k_[:, 0, cs], op=ALU.mult), reads=[ur_b, k_b], writes=[tq[0][1]])
                        S.op("dve", lambda h: h.tensor_tensor(out=tq[1][0][:], in0=ui[:], in1=k_[:, 1, cs], op=ALU.mult), reads=[ui_b, k_b], writes=[tq[1][1]])
                        S.op("pool", lambda h: h.tensor_tensor(out=o[:, 0, cs], in0=tq[0][0][:], in1=tq[1][0][:], op=ALU.subtract),
                             reads=[tq[0][1], tq[1][1]], writes=[o_b])
                        S.op("dve", lambda h: h.tensor_tensor(out=tq[2][0][:], in0=ur[:], in1=k_[:, 1, cs], op=ALU.mult), reads=[ur_b, k_b], writes=[tq[2][1]])
                        S.op("dve", lambda h: h.tensor_tensor(out=tq[3][0][:], in0=ui[:], in1=k_[:, 0, cs], op=ALU.mult), reads=[ui_b, k_b], writes=[tq[3][1]])
                        S.op("pool", lambda h: h.tensor_tensor(out=o[:, 1, cs], in0=tq[2][0][:], in1=tq[3][0][:], op=ALU.add),
                             reads=[tq[2][1], tq[3][1]], writes=[o_b])
                    S.dma("sp", YFs[0][:, ft * 128:(ft + 1) * 128, :].rearrange("a p c -> p a c"), o[:], YFs[1], [o_b])
                _fwd_dft(K, s2, Cm, Sm, ntt, nft, utok, utok_b, a0 // 128, ("C", "S"), evac, pspool)
                S.barrier()
    for si, (a0, n) in enumerate(segs):
        Cm, Sm = (I["dftC"], I["dftS"]) if si == 0 else (I["dftCc"], I["dftSc"])
        YFs = R["YF"] if si == 0 else R["YFc"]
        nft = (n + 128) // 128
        with contextlib.ExitStack() as s2:
            hb, hb_b = K.sb("ihb", [128, 8], F32, s2)
            S.dma("sp", hb[:], I["hbT"][l], hb_b)
            Yh = [K.sb("iY%d" % i, [128, nft, 512], BF16, s2) for i in range(2)]
            Cr = [K.sb("iC%d" % i, [128, nft, 256], BF16, s2) for i in range(2)]
            Sr = [K.sb("iS%d" % i, [128, nft, 256], BF16, s2) for i in range(2)]
            x0c = [K.sb("ix0%d" % i, [128, 4, 256], BF16, s2) for i in range(2)]
            uc = [K.sb("iu%d" % i, [128, 4, 256], BF16, s2) for i in range(2)]
            tmp, tmp_b = K.sb("itmp", [128, 256], F32, s2)
            og = [K.sb("iog%d" % i, [128, 4, 256], BF16, s2) for i in range(2)]
            psI = [K.pst("ips%d" % i, [128, 512], F32, s2) for i in range(2)]
            nk = 0
            npi = 0
            for half in range(2):
                for pl in range(2):
                    S.dma("sp", Yh[pl][0][:], YFs[0][pl, 0:nft * 128, half * 512:(half + 1) * 512].rearrange("(f p) c -> p f c", p=128), Yh[pl][1], [YFs[1]])
                for t0 in range(0, n, 256):
                    cr, cr_b = Cr[nk % 2]
                    sr, sr_b = Sr[nk % 2]
                    xx, xx_b = x0c[nk % 2]
                    uu, uu_b = uc[nk % 2]
                    oo, oo_b = og[nk % 2]
                    nk += 1
                    S.dma("sp", cr[:], Cm.rearrange("(f p) t -> p f t", p=128)[:, 0:nft, t0:t0 + 256], cr_b)
                    S.dma("sp", sr[:], Sm.rearrange("(f p) t -> p f t", p=128)[:, 0:nft, t0:t0 + 256], sr_b)
                    rows = slice(half * 512, (half + 1) * 512)
                    S.dma("sp", xx[:], R["X0T"][0][rows, a0 + t0:a0 + t0 + 256].rearrange("(c p) t -> p c t", p=128), xx_b, [R["X0T"][1]])
                    S.dma("sp", uu[:], R["UT"][0][rows, a0 + t0:a0 + t0 + 256].rearrange("(c p) t -> p c t", p=128), uu_b, [R["UT"][1]])
                    for ci in range(4):
                        ps, ps_b = psI[npi % 2]
                        npi += 1
                        pairs = [(Yh[0][0][:, f, ci * 128:(ci + 1) * 128], cr[:, f, :]) for f in range(nft)] + \
                                [(Yh[1][0][:, f, ci * 128:(ci + 1) * 128], sr[:, f, :]) for f in range(nft)]
                        _mm_group(S, ps_b, ps[:, 0:256], pairs, [Yh[0][1], Yh[1][1], cr_b, sr_b])
                        cidx = half * 4 + ci
                        S.op("dve", lambda h, ci=ci, cidx=cidx, ps=ps, uu=uu: h.scalar_tensor_tensor(out=tmp[:], in0=uu[:, ci, :], scalar=hb[:, cidx:cidx + 1],
                                                                                                    in1=ps[:, 0:256], op0=ALU.mult, op1=ALU.add),
                             reads=[uu_b, hb_b, ps_b], writes=[tmp_b])
                        S.op("pool", lambda h, ci=ci, oo=oo, xx=xx: h.tensor_tensor(out=oo[:, ci, :], in0=tmp[:], in1=xx[:, ci, :], op=ALU.mult),
                             reads=[tmp_b, xx_b], writes=[oo_b])
                    S.dma("sp", R["YhT"][0][rows, a0 + t0:a0 + t0 + 256].rearrange("(c p) t -> p c t", p=128), oo[:], R["YhT"][1], [oo_b])
            S.barrier()
    if not ctx_out:
        pass


def _bc_mid(ap2d, n):
    a = ap2d.ap
    return bass.AP(ap2d.tensor, ap2d.offset, [list(a[0]), [0, n], list(a[1])])


def phase_moe(K, l, ctx_out):
    nc, S, I, R = K.nc, K.S, K.I, K.R
    modT, modT_b = K.modT
    A2, A2_b = K.A2
    ones_f, ones_f_b = K.ones_f
    ones_b, ones_b_b = K.ones_b
    ident_f, ident_f_b = K.ident_f
    ident_b, ident_b_b = K.ident_b
    debug = K.cfg.get("debug", ())
    XTv = R["XT"][0].rearrange("(k p) t -> p k t", p=128)
    chunks = TCH if ctx_out else TCH[:8]
    ntt = 34 if ctx_out else 32
    groups = [(0, 32, CAP)] + ([(32, 34, CAPC)] if ctx_out else [])
    with contextlib.ExitStack() as st:
        lg, lg_b = K.sb("lg", [128, 34, NE], F32, st)
        aff, aff_b = K.sb("aff", [128, 34, NE], F32, st)
        psel, psel_b = K.sb("psel", [128, 34, NE], F32, st)
        wr, wr_b = K.sb("wr", [128, 8, NE], F32, st)
        S.dma("sp", wr[:], I["router_w"][l].rearrange("(k p) e -> p k e", p=128), wr_b)
        sH = contextlib.ExitStack()
        H2, H2_b = K.sb("H2", [128, 34, D], BF16, sH)
        if ctx_out is False:
            S.op("dve", lambda h: h.memset(lg[:, 32:34, :], 0.0), writes=[lg_b])
        with contextlib.ExitStack() as s2:
            xs = [K.sb("qx%d" % i, [128, 8, 512], F32, s2) for i in range(2)]
            sq, sq_b = K.sb("qsq", [128, 8, 512], F32, s2)
            rstd, rstd_b = K.sb("qrstd", [128, 512], F32, s2)
            tmp, tmp_b = K.sb("qtmp", [128, 512], F32, s2)
            h2f, h2f_b = K.sb("qh2f", [128, 8, 512], F32, s2)
            hTc, hTc_b = K.sb("qhTc", [128, 8, 512], BF16, s2)
            ps, ps_b = K.pst("qps", [128, 512], F32, s2)
            psl, psl_b = K.pst("qpsl", [128, 64], F32, s2)
            pstr = [K.pst("qpst%d" % i, [128, 1024], BF16, s2) for i in range(2)]
            ntr = 0
            for ci, (t0, tn) in enumerate(chunks):
                r = 0 if t0 < L else 1
                x, x_b = xs[ci % 2]
                S.dma("sp", x[:, :, :tn], XTv[:, :, t0:t0 + tn], x_b, [R["XT"][1]])
                if "XT1" in debug:
                    S.dma("sp", R["XT1"][0].rearrange("(k p) t -> p k t", p=128)[:, :, t0:t0 + tn], x[:, :, :tn], R["XT1"][1], [x_b])
                S.op("act", lambda h: h.activation(out=sq[:, :, :tn], in_=x[:, :, :tn], func=AF.Square), reads=[x_b], writes=[sq_b])
                _mm_group(S, ps_b, ps[:, :tn], [(ones_f[:], sq[:, k, :tn]) for k in range(8)], [ones_f_b, sq_b])
                S.op("dve", lambda h: h.tensor_scalar(out=rstd[:, :tn], in0=ps[:, :tn], scalar1=1.0 / D, scalar2=EPS,
                                                      op0=ALU.mult, op1=ALU.add), reads=[ps_b], writes=[rstd_b])
                S.op("act", lambda h: h.activation(out=rstd[:, :tn], in_=rstd[:, :tn], func=AF.Sqrt), reads=[rstd_b], writes=[rstd_b])
                S.op("dve", lambda h: h.reciprocal(out=rstd[:, :tn], in_=rstd[:, :tn]), reads=[rstd_b], writes=[rstd_b])
                for k in range(8):
                    S.op("dve", lambda h, k=k: h.tensor_tensor(out=tmp[:, :tn], in0=x[:, k, :tn], in1=rstd[:, :tn], op=ALU.mult),
                         reads=[x_b, rstd_b], writes=[tmp_b])
                    S.op("act", lambda h, k=k: h.activation(out=h2f[:, k, :tn], in_=tmp[:, :tn], func=AF.Identity,
                                                            scale=A2[:, k, r:r + 1], bias=modT[:, 24 + k, r:r + 1]),
                         reads=[tmp_b, A2_b, modT_b], writes=[h2f_b])
                    S.op("pool", lambda h, k=k: h.tensor_copy(out=hTc[:, k, :tn], in_=h2f[:, k, :tn]), reads=[h2f_b], writes=[hTc_b])
                nj = tn // 128
                for j in range(nj):
                    tt = t0 // 128 + j
                    _mm_group(S, psl_b, psl[:, j * 16:(j + 1) * 16],
                              [(h2f[:, k, j * 128:(j + 1) * 128], wr[:, k, :]) for k in range(8)], [h2f_b, wr_b])
                    pt_, pt_b = pstr[ntr % 2]
                    ntr += 1

                    def trf(h, j=j, pt_=pt_):
                        ins = None
                        for k in range(8):
                            ins = h.transpose(pt_[:, k * 128:(k + 1) * 128], hTc[:, k, j * 128:(j + 1) * 128], ident_b[:])
                        return ins
                    S.op("pe", trf, reads=[hTc_b, ident_b_b], writes=[pt_b])
                    S.op("act", lambda h, tt=tt, pt_=pt_: h.activation(out=H2[:, tt, :], in_=pt_[:], func=AF.Copy), reads=[pt_b], writes=[H2_b])
                S.op("dve", lambda h: h.tensor_copy(out=lg[:, t0 // 128:t0 // 128 + nj, :].rearrange("p t e -> p (t e)"),
                                                    in_=psl[:, :nj * 16]), reads=[psl_b], writes=[lg_b])
            S.barrier()
        if "LG" in debug:
            S.dma("sp", R["LG"][0], lg[:], R["LG"][1], [lg_b])
            S.dma("sp", R["H2d"][0], H2[:], R["H2d"][1], [H2_b])
        with contextlib.ExitStack() as s2:
            se, se_b = K.sb("rse", [128, 34], F32, s2)
            lo, lo_b = K.sb("rlo", [128, NE], F32, s2)
            mid, mid_b = K.sb("rmid", [128, NE], F32, s2)
            cmpt, cmp_b = K.sb("rcmp", [128, 32, NE], F32, s2)
            cntp, cntp_b = K.sb("rcntp", [128, NE], F32, s2)
            ge, ge_b = K.sb("rge", [128, NE], F32, s2)
            mk, mk_b = K.sb("rmk", [128, 34, NE], F32, s2)
            mkb, mkb_b = K.sb("rmkb", [128, 34, NE], BF16, s2)
            tot, tot_b = K.sb("rtot", [128, 34, NE], F32, s2)
            base, base_b = K.sb("rbase", [128, 34, NE], F32, s2)
            posT, posT_b = K.sb("posT", [16, T], F32, s2)
            affT, affT_b = K.sb("affT", [16, T], F32, s2)
            us, us_b = K.sb("rus", [128, 128], BF16, s2)
            S.dma("pool", us[:], I["ustrict"][:, :], us_b)
            psc, psc_b = K.pst("rpsc", [128, 512], F32, s2)
            psw, psw_b = K.pst("rpsw", [128, 512], F32, s2)
            pst_, pst_b = K.pst("rpst", [128, 512], F32, s2)
            S.op("act", lambda h: h.activation(out=aff[:].rearrange("p t e -> p (t e)"), in_=lg[:].rearrange("p t e -> p (t e)"), func=AF.Exp),
                 reads=[lg_b], writes=[aff_b])
            S.op("dve", lambda h: h.reduce_sum(out=se[:], in_=aff[:], axis=AX.X), reads=[aff_b], writes=[se_b])
            S.op("dve", lambda h: h.reciprocal(out=se[:], in_=se[:]), reads=[se_b], writes=[se_b])
            for tt in range(34):
                S.op("dve", lambda h, tt=tt: h.tensor_scalar(out=aff[:, tt, :], in0=aff[:, tt, :], scalar1=se[:, tt:tt + 1], scalar2=None, op0=ALU.mult),
                     reads=[aff_b, se_b], writes=[aff_b])
            S.op("dve", lambda h: h.memset(mk[:], 0.0), writes=[mk_b])
            S.op("dve", lambda h: h.memset(base[:], 0.0), writes=[base_b])
            for (ta, tb, cap) in groups:
                nt = tb - ta
                S.op("dve", lambda h: h.memset(lo[:], 0.0), writes=[lo_b])
                for it in range(30):
                    w = 0.5 ** (it + 1)
                    S.op("dve", lambda h: h.tensor_scalar(out=mid[:], in0=lo[:], scalar1=w, scalar2=None, op0=ALU.add), reads=[lo_b], writes=[mid_b])
                    S.op("dve", lambda h: h.tensor_tensor(out=cmpt[:, :nt, :], in0=aff[:, ta:tb, :], in1=_bc_mid(mid[:], nt), op=ALU.is_ge),
                         reads=[aff_b, mid_b], writes=[cmp_b])
                    S.op("dve", lambda h: h.reduce_sum(out=cntp[:], in_=cmpt[:, :nt, :].rearrange("p t e -> p e t"), axis=AX.X),
                         reads=[cmp_b], writes=[cntp_b])
                    _mm_group(S, psc_b, psc[:, 0:NE], [(ones_f[:], cntp[:])], [ones_f_b, cntp_b])
                    S.op("dve", lambda h: h.tensor_scalar(out=ge[:], in0=psc[:, 0:NE], scalar1=cap - 0.5, scalar2=None, op0=ALU.is_ge),
                         reads=[psc_b], writes=[ge_b])
                    S.op("dve", lambda h: h.scalar_tensor_tensor(out=lo[:], in0=ge[:], scalar=w, in1=lo[:], op0=ALU.mult, op1=ALU.add),
                         reads=[ge_b, lo_b], writes=[lo_b])
                S.op("dve", lambda h: h.tensor_tensor(out=mk[:, ta:tb, :], in0=aff[:, ta:tb, :], in1=_bc_mid(lo[:], nt), op=ALU.is_ge),
                     reads=[aff_b, lo_b], writes=[mk_b])
                S.op("dve", lambda h: h.tensor_copy(out=mkb[:, ta:tb, :], in_=mk[:, ta:tb, :]), reads=[mk_b], writes=[mkb_b])
                mflat = mkb[:, ta:tb, :].rearrange("p t e -> p (t e)")
                _mm_group(S, psw_b, psw[:, :nt * NE], [(us[:], mflat)], [us_b, mkb_b])
                _mm_group(S, pst_b, pst_[:, :nt * NE], [(ones_b[:], mflat)], [ones_b_b, mkb_b])
                S.op("dve", lambda h: h.tensor_copy(out=tot[:, ta:tb, :].rearrange("p t e -> p (t e)"), in_=pst_[:, :nt * NE]),
                     reads=[pst_b], writes=[tot_b])
                for t in range(ta + 1, tb):
                    S.op("dve", lambda h, t=t: h.tensor_tensor(out=base[:, t, :], in0=base[:, t - 1, :], in1=tot[:, t - 1, :], op=ALU.add),
                         reads=[base_b, tot_b], writes=[base_b])
                S.op("dve", lambda h: h.tensor_tensor(out=psel[:, ta:tb, :].rearrange("p t e -> p (t e)"), in0=psw[:, :nt * NE],
                                                      in1=base[:, ta:tb, :].rearrange("p t e -> p (t e)"), op=ALU.add),
                     reads=[psw_b, base_b], writes=[psel_b])
                S.op("dve", lambda h: h.scalar_tensor_tensor(out=psel[:, ta:tb, :], in0=psel[:, ta:tb, :], scalar=1.0, in1=mk[:, ta:tb, :],
                                                             op0=ALU.add, op1=ALU.mult), reads=[psel_b, mk_b], writes=[psel_b])
                S.op("dve", lambda h: h.tensor_scalar(out=psel[:, ta:tb, :], in0=psel[:, ta:tb, :], scalar1=-1.0, scalar2=None, op0=ALU.add),
                     reads=[psel_b], writes=[psel_b])
            S.op("dve", lambda h: h.tensor_tensor(out=aff[:], in0=aff[:], in1=mk[:], op=ALU.mult), reads=[aff_b, mk_b], writes=[aff_b])
            for src, src_b, dstT, dstT_b in ((psel, psel_b, posT, posT_b), (aff, aff_b, affT, affT_b)):
                for t4 in range(0, ntt, 4):
                    n4 = min(4, ntt - t4)

                    def trf(h, t4=t4, n4=n4, src=src):
                        ins = None
                        for j in range(n4):
                            ins = h.transpose(psc[0:16, j * 128:(j + 1) * 128], src[:, t4 + j, :], ident_f[:])
                        return ins
                    S.op("pe", trf, reads=[src_b, ident_f_b], writes=[psc_b])
                    S.op("act", lambda h, t4=t4, n4=n4, dstT=dstT: h.activation(out=dstT[:, t4 * 128:(t4 + n4) * 128], in_=psc[0:16, :n4 * 128], func=AF.Copy),
                         reads=[psc_b], writes=[dstT_b])
            S.dma("sp", R["PT"][0][0, :, 0:ntt * 128], posT[:, 0:ntt * 128], R["PT"][1], [posT_b])
            S.dma("sp", R["PT"][0][1, :, 0:ntt * 128], affT[:, 0:ntt * 128], R["PT"][1], [affT_b])
            S.barrier()
        NS = CAP + CAPC
        with contextlib.ExitStack() as s3:
            w1, w1_b = K.sb("ew1", [128, 8, D], BF16, s3)
            w3, w3_b = K.sb("ew3", [128, 8, D], BF16, s3)
            w2, w2_b = K.sb("ew2", [128, 8, D], BF16, s3)
            Se, Se_b = K.sb("eS", [128, 32, 512], BF16, s3)
            Sc, Sc_b = K.sb("eSc", [128, 2, CAPC], BF16, s3)
            io, io_b = K.sb("eio", [128, 512], F32, s3)
            S.dma("sp", io[:], I["iota512"][:, :], io_b)
            xg, xg_b = K.sb("exg", [128, 8, NS], BF16, s3)
            gT, gT_b = K.sb("egT", [128, 8, NS], BF16, s3)
            sil, sil_b = K.sb("esil", [128, NS], F32, s3)
            yst = [K.sb("eyst%d" % i, [128, 512], BF16, s3) for i in range(2)]
            psG = [K.pst("epsG%d" % i, [128, 512], F32, s3) for i in range(2)]
            psA, psA_b = K.pst("epsA", [128, 512], F32, s3)
            psB, psB_b = K.pst("epsB", [128, 512], F32, s3)
            psC, psC_b = K.pst("epsC", [128, 512], F32, s3)
            psY = [K.pst("epsY%d" % i, [128, 512], F32, s3) for i in range(2)]
            ng = 0
            ny = 0
            for e in range(NE):
                S.dma("pool", w1[:], I["moe_w1"][l, e].rearrange("(k p) n -> p k n", p=128), w1_b)
                S.dma("pool", w3[:], I["moe_w3"][l, e].rearrange("(k p) n -> p k n", p=128), w3_b)
                S.dma("pool", w2[:], I["moe_w2"][l, e].rearrange("(k p) n -> p k n", p=128), w2_b)
                for tt in range(32):
                    S.op("dve", lambda h, tt=tt: h.tensor_scalar(out=Se[:, tt, :], in0=io[:], scalar1=psel[:, tt, e:e + 1], scalar2=None, op0=ALU.is_equal),
                         reads=[io_b, psel_b], writes=[Se_b])
                if ctx_out:
                    for tt in range(32, 34):
                        S.op("dve", lambda h, tt=tt: h.tensor_scalar(out=Sc[:, tt - 32, :], in0=io[:, 0:CAPC], scalar1=psel[:, tt, e:e + 1], scalar2=None,
                                                                     op0=ALU.is_equal), reads=[io_b, psel_b], writes=[Sc_b])
                for dk in range(8):
                    pg, pg_b = psG[ng % 2]
                    ng += 1
                    _mm_group(S, pg_b, pg[:], [(H2[:, tt, dk * 128:(dk + 1) * 128], Se[:, tt, :]) for tt in range(32)], [H2_b, Se_b])
                    S.op("act", lambda h, dk=dk, pg=pg: h.activation(out=xg[:, dk, 0:CAP], in_=pg[:], func=AF.Copy), reads=[pg_b], writes=[xg_b])
                    if ctx_out:
                        _mm_group(S, psC_b, psC[:, 0:CAPC], [(H2[:, tt, dk * 128:(dk + 1) * 128], Sc[:, tt - 32, :]) for tt in (32, 33)], [H2_b, Sc_b])
                        S.op("act", lambda h, dk=dk: h.activation(out=xg[:, dk, CAP:NS], in_=psC[:, 0:CAPC], func=AF.Copy), reads=[psC_b], writes=[xg_b])
                for m in range(8):
                    _mm_group(S, psA_b, psA[:], [(w1[:, k, m * 128:(m + 1) * 128], xg[:, k, 0:CAP]) for k in range(8)], [w1_b, xg_b])
                    _mm_group(S, psB_b, psB[:], [(w3[:, k, m * 128:(m + 1) * 128], xg[:, k, 0:CAP]) for k in range(8)], [w3_b, xg_b])
                    S.op("act", lambda h: h.activation(out=sil[:, 0:CAP], in_=psA[:], func=AF.Silu), reads=[psA_b], writes=[sil_b])
                    S.op("dve", lambda h, m=m: h.tensor_tensor(out=gT[:, m, 0:CAP], in0=psB[:], in1=sil[:, 0:CAP], op=ALU.mult),
                         reads=[psB_b, sil_b], writes=[gT_b])
                    if ctx_out:
                        _mm_group(S, psC_b, psC[:, 0:CAPC], [(w1[:, k, m * 128:(m + 1) * 128], xg[:, k, CAP:NS]) for k in range(8)], [w1_b, xg_b])
                        S.op("act", lambda h: h.activation(out=sil[:, CAP:NS], in_=psC[:, 0:CAPC], func=AF.Silu), reads=[psC_b], writes=[sil_b])
                        _mm_group(S, psC_b, psC[:, 0:CAPC], [(w3[:, k, m * 128:(m + 1) * 128], xg[:, k, CAP:NS]) for k in range(8)], [w3_b, xg_b])
                        S.op("dve", lambda h, m=m: h.tensor_tensor(out=gT[:, m, CAP:NS], in0=psC[:, 0:CAPC], in1=sil[:, CAP:NS], op=ALU.mult),
                             reads=[psC_b, sil_b], writes=[gT_b])
                for c in range(5 if ctx_out else 4):
                    rows = 128 if c < 4 else CAPC
                    for dh in range(2):
                        py, py_b = psY[ny % 2]
                        ys, ys_b = yst[ny % 2]
                        ny += 1
                        _mm_group(S, py_b, py[0:rows, :], [(gT[:, f, c * 128:c * 128 + rows], w2[:, f, dh * 512:(dh + 1) * 512]) for f in range(8)],
                                  [gT_b, w2_b])
                        S.op("act", lambda h, py=py, ys=ys, rows=rows: h.activation(out=ys[0:rows, :], in_=py[0:rows, :], func=AF.Copy),
                             reads=[py_b], writes=[ys_b])
                        S.dma("sp", R["YE"][0][e, c * 128:c * 128 + rows, dh * 512:(dh + 1) * 512], ys[0:rows, :], R["YE"][1], [ys_b])
            S.barrier()
        sH.close()
        with contextlib.ExitStack() as s4:
            YEs, YEs_b = K.sb("cYE", [128, NE, 5, 512], BF16, s4)
            ST, ST_b = K.sb("cST", [128, NE, 4, 512], BF16, s4)
            abc = [K.sb("cabc%d" % i, [128, 512], F32, s4) for i in range(2)]
            xh, xh_b = K.sb("cxh", [128, 4, 512], F32, s4)
            selm, selm_b = K.sb("cselm", [16, NE, 128], F32, s4)
            sidx, sidx_b = K.sb("csidx", [128, 5], F32, s4)
            posT, posT_b = K.sb("cposT", [16, T], F32, s4)
            affT, affT_b = K.sb("caffT", [16, T], F32, s4)
            S.dma("sp", posT[:, 0:ntt * 128], R["PT"][0][0, :, 0:ntt * 128], posT_b, [R["PT"][1]])
            S.dma("sp", affT[:, 0:ntt * 128], R["PT"][0][1, :, 0:ntt * 128], affT_b, [R["PT"][1]])
            S.dma("sp", selm[:], I["selm"].rearrange("k (e m) -> k e m", e=NE), selm_b)
            S.dma("sp", sidx[:], I["slotidx"][:, :], sidx_b)
            psP = [K.pst("cpsP%d" % i, [128, 512], F32, s4) for i in range(2)]
            psQ = [K.pst("cpsQ%d" % i, [128, 512], F32, s4) for i in range(2)]
            psO = [K.pst("cpsO%d" % i, [128, 512], F32, s4) for i in range(2)]
            nb_ = 0
            no = 0
            for dh in range(2):
                for e in range(NE):
                    S.dma("sp", YEs[:, e, 0:4, :], R["YE"][0][e, 0:CAP, dh * 512:(dh + 1) * 512].rearrange("(c p) d -> p c d", p=128), YEs_b, [R["YE"][1]])
                    if ctx_out:
                        S.dma("sp", YEs[0:CAPC, e, 4, :], R["YE"][0][e, CAP:NS, dh * 512:(dh + 1) * 512], YEs_b, [R["YE"][1]])
                for (t0, tn) in chunks:
                    r = 0 if t0 < L else 1
                    lat = t0 < L
                    for e in range(NE):
                        pp, pp_b = psP[nb_ % 2]
                        pq, pq_b = psQ[nb_ % 2]
                        ab, ab_b = abc[nb_ % 2]
                        nb_ += 1
                        _mm_group(S, pp_b, pp[:, :tn], [(selm[:, e, :], posT[:, t0:t0 + tn])], [selm_b, posT_b])
                        _mm_group(S, pq_b, pq[:, :tn], [(selm[:, e, :], affT[:, t0:t0 + tn])], [selm_b, affT_b])
                        S.op("act", lambda h, ab=ab, pq=pq: h.activation(out=ab[:, :tn], in_=pq[:, :tn], func=AF.Copy), reads=[pq_b], writes=[ab_b])
                        if lat:
                            for c in range(4):
                                S.op("dve", lambda h, c=c, pp=pp, ab=ab: h.scalar_tensor_tensor(out=ST[:, e, c, :tn], in0=pp[:, :tn], scalar=sidx[:, c:c + 1],
                                                                                               in1=ab[:, :tn], op0=ALU.is_equal, op1=ALU.mult),
                                     reads=[pp_b, ab_b, sidx_b], writes=[ST_b])
                        else:
                            S.op("dve", lambda h, pp=pp, ab=ab: h.scalar_tensor_tensor(out=ST[0:CAPC, e, 0, :tn], in0=pp[0:CAPC, :tn], scalar=sidx[0:CAPC, 4:5],
                                                                                      in1=ab[0:CAPC, :tn], op0=ALU.is_equal, op1=ALU.mult),
                                 reads=[pp_b, ab_b, sidx_b], writes=[ST_b])
                    S.dma("sp", xh[:, :, :tn], XTv[:, dh * 4:(dh + 1) * 4, t0:t0 + tn], xh_b, [R["XT"][1]])
                    for m in range(4):
                        po, po_b = psO[no % 2]
                        no += 1
                        if lat:
                            pairs = [(YEs[:, e, c, m * 128:(m + 1) * 128], ST[:, e, c, :tn]) for e in range(NE) for c in range(4)]
                        else:
                            pairs = [(YEs[0:CAPC, e, 4, m * 128:(m + 1) * 128], ST[0:CAPC, e, 0, :tn]) for e in range(NE)]
                        _mm_group(S, po_b, po[:, :tn], pairs, [YEs_b, ST_b])
                        S.op("dve", lambda h, m=m, po=po: h.scalar_tensor_tensor(out=xh[:, m, :tn], in0=po[:, :tn], scalar=modT[:, 40 + dh * 4 + m, r:r + 1],
                                                                                 in1=xh[:, m, :tn], op0=ALU.mult, op1=ALU.add),
                             reads=[po_b, modT_b, xh_b], writes=[xh_b])
                    S.dma("sp", XTv[:, dh * 4:(dh + 1) * 4, t0:t0 + tn], xh[:, :, :tn], R["XT"][1], [xh_b])
            S.barrier()


def phase_final(K):
    nc, S, I, R = K.nc, K.S, K.I, K.R
    ones_f, ones_f_b = K.ones_f
    XTv = R["XT"][0].rearrange("(k p) t -> p k t", p=128)
    with contextlib.ExitStack() as st:
        fg, fg_b = K.sb("fg", [128, 8], F32, st)
        S.dma("sp", fg[:], I["fgT"][:, :], fg_b)
        xs = [K.sb("fx%d" % i, [128, 8, 512], F32, st) for i in range(2)]
        sq, sq_b = K.sb("fsq", [128, 8, 512], F32, st)
        rstd, rstd_b = K.sb("frstd", [128, 512], F32, st)
        ps, ps_b = K.pst("ps_fin", [128, 512], F32, st)
        for ci in range(8):
            t0 = ci * 512
            x, x_b = xs[ci % 2]
            S.dma("sp", x[:], XTv[:, :, t0:t0 + 512], x_b, [R["XT"][1]])
            S.op("act", lambda h: h.activation(out=sq[:], in_=x[:], func=AF.Square), reads=[x_b], writes=[sq_b])
            _mm_group(S, ps_b, ps[:], [(ones_f[:], sq[:, k, :]) for k in range(8)], [ones_f_b, sq_b])
            S.op("dve", lambda h: h.tensor_scalar(out=rstd[:], in0=ps[:], scalar1=1.0 / D, scalar2=EPS, op0=ALU.mult, op1=ALU.add),
                 reads=[ps_b], writes=[rstd_b])
            S.op("act", lambda h: h.activation(out=rstd[:], in_=rstd[:], func=AF.Sqrt), reads=[rstd_b], writes=[rstd_b])
            S.op("dve", lambda h: h.reciprocal(out=rstd[:], in_=rstd[:]), reads=[rstd_b], writes=[rstd_b])
            for k in range(8):
                S.op("dve", lambda h, k=k: h.scalar_tensor_tensor(out=sq[:, k, :], in0=x[:, k, :], scalar=fg[:, k:k + 1], in1=rstd[:],
                                                                  op0=ALU.mult, op1=ALU.mult), reads=[x_b, rstd_b, fg_b], writes=[sq_b])
            S.dma("sp", K.outT.rearrange("(k p) t -> p k t", p=128)[:, :, t0:t0 + 512], sq[:], K.outT_b, [sq_b])
        S.barrier()


def rope_tables():
    rows = L // 64
    row = np.repeat(np.arange(rows, dtype=np.float32), 64)
    col = np.tile(np.arange(64, dtype=np.float32), rows)
    inv = (10000.0 ** (-np.arange(16, dtype=np.float32) / 16)).astype(np.float32)
    C = np.zeros((64, L), np.float32)
    Sg = np.zeros((64, L), np.float32)
    for j in range(64):
        a, hf, f = j // 32, (j % 32) // 16, j % 16
        pos = row if a == 0 else col
        ang = (pos * inv[f]).astype(np.float32)
        C[j] = np.cos(ang)
        Sg[j] = np.sin(ang) * (-1.0 if hf == 0 else 1.0)
    return np.concatenate([C, C], 0), np.concatenate([Sg, Sg], 0)


_HC = {}


def hyena_consts():
    if _HC:
        return _HC
    f32 = np.float32

    def emb_of(Lx):
        t = np.linspace(0.0, 1.0, Lx, dtype=f32)[:, None]
        w = (2.0 * math.pi * np.arange(Lx, dtype=f32)[:, None] / Lx).astype(f32)
        f = np.linspace(1e-4, 15, 16, dtype=f32)[None, :]
        e = np.concatenate([t, np.cos(f * w), -np.sin(f * w)], axis=-1).astype(f32)
        return np.ascontiguousarray(e.T), t[:, 0]
    eT, t = emb_of(L)
    eTc, tc = emb_of(LC)
    _HC["embT"], _HC["embTc"] = eT, eTc
    _HC["negt"] = np.ascontiguousarray((-t).reshape(L // 128, 128).T)
    _HC["negtc"] = np.ascontiguousarray((-tc).reshape(LC // 128, 128).T)
    _HC["deltas"] = np.abs(np.linspace(math.log(1e-2) / 0.3, math.log(1e-2) / 1.5, D, dtype=f32)).reshape(1, D).astype(f32)

    def dft(Lx, npad):
        N = 2 * Lx
        a = np.arange(Lx + 1, dtype=np.int64)
        ph = (a[:, None] * a[None, :]) % N
        ang = ph.astype(np.float64) * (2.0 * math.pi / N)
        C = np.zeros((npad, npad), f32)
        S_ = np.zeros((npad, npad), f32)
        C[:Lx + 1, :Lx + 1] = np.cos(ang)
        S_[:Lx + 1, :Lx + 1] = np.sin(ang)
        wfv = np.zeros(npad, f32)
        wfv[:Lx + 1] = 2.0 / N
        wfv[0] = 1.0 / N
        wfv[Lx] = 1.0 / N
        return C.astype(ml_dtypes.bfloat16), S_.astype(ml_dtypes.bfloat16), np.ascontiguousarray(wfv.reshape(npad // 128, 128).T)
    _HC["dftC"], _HC["dftS"], _HC["wf"] = dft(L, NF)
    _HC["dftCc"], _HC["dftSc"], _HC["wfc"] = dft(LC, NFC)
    return _HC


def host_inputs(inputs, b, nl):
    x, c, ctx, c_ctx = inputs["x"], inputs["c"], inputs["ctx"], inputs["c_ctx"]
    m = {}
    m["xT"] = np.ascontiguousarray(np.concatenate([x[b], ctx[b]], 0).T)
    cc = np.stack([c[b], c_ctx], 0)
    m["ccT"] = np.ascontiguousarray(cc.reshape(2, 8, 128).transpose(2, 1, 0))
    m["w_mod"] = np.ascontiguousarray(inputs["w_mod"][:nl])
    m["b_modT"] = np.ascontiguousarray(inputs["b_mod"][:nl].reshape(nl, 48, 128).transpose(0, 2, 1))
    m["g1T"] = np.ascontiguousarray(inputs["norm1_g"][:nl].reshape(nl, 8, 128).transpose(0, 2, 1))
    m["g2T"] = np.ascontiguousarray(inputs["norm2_g"][:nl].reshape(nl, 8, 128).transpose(0, 2, 1))
    m["w_in"] = np.ascontiguousarray(inputs["w_in"][:nl])
    rc, rs = rope_tables()
    m["ropeC"], m["ropeS"] = rc, rs
    m["ident"] = np.eye(128, dtype=np.float32)
    m["diff_lambda"] = np.ascontiguousarray(inputs["diff_lambda"][:nl].reshape(nl, 256))
    m["sublnT"] = np.ascontiguousarray(inputs["diff_subln_g"][:nl].reshape(nl, 128, 1))
    m["swa_sink"] = np.ascontiguousarray(inputs["swa_sink"][:nl])
    kk, qq = np.meshgrid(np.arange(128), np.arange(128), indexing="ij")
    m["trilo"] = (kk >= qq).astype(np.float32)
    m["triup"] = (kk <= qq).astype(np.float32)
    m["w_branch"] = np.ascontiguousarray(inputs["w_branch"][:nl])
    m["w_out"] = np.ascontiguousarray(inputs["w_out"][:nl])
    m["fgT"] = np.ascontiguousarray(inputs["final_g"].reshape(8, 128).T)
    if "hy_ff_w1" in inputs:
        cwv = inputs["hy_conv_w"][:nl]
        m["cwT"] = np.ascontiguousarray(cwv.reshape(nl, 3, 24, 128).transpose(0, 3, 2, 1))
        m["cbT"] = np.ascontiguousarray(inputs["hy_conv_b"][:nl].reshape(nl, 24, 128).transpose(0, 2, 1))
        m["hbT"] = np.ascontiguousarray(inputs["hy_bias"][:nl].reshape(nl, 8, 128).transpose(0, 2, 1))
        m["hy_w1"] = np.ascontiguousarray(inputs["hy_ff_w1"][:nl])
        m["hy_w2"] = np.ascontiguousarray(inputs["hy_ff_w2"][:nl])
        m["hy_w3"] = np.ascontiguousarray(inputs["hy_ff_w3"][:nl])
        m["hy_b1"] = np.ascontiguousarray(inputs["hy_ff_b1"][:nl].reshape(nl, 64, 1))
        m["hy_b2"] = np.ascontiguousarray(inputs["hy_ff_b2"][:nl].reshape(nl, 64, 1))
        m["hy_fr"] = np.ascontiguousarray(inputs["hy_sin_freq"][:nl].reshape(nl, 64, 1))
        m.update(hyena_consts())
    if "moe_w1" in inputs:
        m["router_w"] = np.ascontiguousarray(inputs["router_w"][:nl])
        for k in ("moe_w1", "moe_w3", "moe_w2"):
            m[k] = np.ascontiguousarray(inputs[k][:nl])
        sel = np.zeros((16, 16, 128), np.float32)
        for e in range(16):
            sel[e, e, :] = 1.0
        m["selm"] = sel.reshape(16, 16 * 128)
        si = np.zeros((128, 5), np.float32)
        for c in range(4):
            si[:, c] = np.arange(128) + 128 * c
        si[:, 4] = np.arange(128)
        m["slotidx"] = si
        m["iota512"] = np.tile(np.arange(512, dtype=np.float32)[None, :], (128, 1))
        m["ustrict"] = (kk < qq).astype(np.float32)
    return m


def kernel(**inputs):
    inputs = {k: np.asarray(v) for k, v in inputs.items()}
    nb = inputs["x"].shape[0]
    nc = build(dict(nl=DEPTH, phases=("diff", "swa", "hy", "merge", "moe")))
    in_maps = [host_inputs(inputs, b, DEPTH) for b in range(nb)]
    res = run_bass_kernel_spmd(nc, in_maps, core_ids=list(range(nb)))
    out = np.stack([np.asarray(res.results[b]["outT"]).T for b in range(nb)], 0)
    return np.ascontiguousarray(out.astype(np.float32))
```

```python
import contextlib
import math
import numpy as np
import ml_dtypes
import concourse.bass as bass
import concourse.mybir as mybir
from concourse.bass_utils import run_bass_kernel_spmd

F32 = mybir.dt.float32
BF16 = mybir.dt.bfloat16
AF = mybir.ActivationFunctionType
ALU = mybir.AluOpType
AX = mybir.AxisListType

D = 1024
L = 4096
LC = 256
T = L + LC
DEPTH = 4
D_IN = 10752
NE = 16
CAP = 512
CAPC = 32
EPS = 1e-6
NF = 4224
NFC = 384
TCH = [(i * 512, 512) for i in range(8)] + [(L, LC)]


class Buf:
    __slots__ = ("name", "w", "r", "sem", "cnt")

    def __init__(self, name):
        self.name = name
        self.w = {}
        self.r = {}
        self.sem = None
        self.cnt = 0


class Sched:
    def __init__(self, nc, es):
        self.nc, self.es = nc, es
        self.E = {}
        for n, h in (("pe", nc.tensor), ("dve", nc.vector), ("act", nc.scalar),
                     ("pool", nc.gpsimd), ("sp", nc.sync)):
            self.E[n] = dict(h=h, sem=es.enter_context(nc.semaphore("s_" + n)), cnt=0, seen={})
        self.dma_bufs = []
        self.nbuf = 0
        self.persist = True
        self.pool = []

    def buf(self, name):
        self.nbuf += 1
        b = Buf("%s_%d" % (name, self.nbuf))
        b.w["_persist"] = self.persist
        return b

    @staticmethod
    def _add(evs, d):
        for k, sv in d.items():
            if k == "_persist":
                continue
            sem, v = sv
            if k not in evs or evs[k][1] < v:
                evs[k] = (sem, v)

    def _waits(self, e, evs):
        E = self.E[e]
        for name, (sem, val) in evs.items():
            if E["seen"].get(name, 0) < val:
                E["h"].wait_ge(sem, val)
                E["seen"][name] = val

    def op(self, e, fn, reads=(), writes=(), skip_self=False, drain_self=False):
        evs = {}
        for b in reads:
            self._add(evs, b.w)
        for b in writes:
            self._add(evs, b.w)
            self._add(evs, b.r)
        if skip_self:
            evs.pop("s_" + e, None)
        if drain_self and self.E[e]["cnt"]:
            evs["s_" + e] = (self.E[e]["sem"], self.E[e]["cnt"])
        self._waits(e, evs)
        E = self.E[e]
        ins = fn(E["h"])
        E["cnt"] += 1
        ins.then_inc(E["sem"], 1)
        key, ev = "s_" + e, (E["sem"], E["cnt"])
        for b in reads:
            b.r[key] = ev
        for b in writes:
            b.w[key] = ev

    def dma(self, q, out_ap, in_ap, dst, srcs=(), **kw):
        evs = {}
        self._add(evs, dst.w)
        self._add(evs, dst.r)
        for b in srcs:
            self._add(evs, b.w)
        self._waits(q, evs)
        if dst.sem is None:
            if self.pool and not dst.w["_persist"]:
                dst.name, dst.sem, dst.cnt = self.pool.pop()
            else:
                dst.sem = self.es.enter_context(self.nc.semaphore("d_" + dst.name))
            self.dma_bufs.append(dst)
        ins = self.E[q]["h"].dma_start(out=out_ap, in_=in_ap, **kw)
        dst.cnt += 16
        ins.then_inc(dst.sem, 16)
        key, ev = "d_" + dst.name, (dst.sem, dst.cnt)
        dst.w[key] = ev
        for b in srcs:
            b.r[key] = ev

    def barrier(self):
        evs = {}
        for n, E in self.E.items():
            if E["cnt"]:
                evs["s_" + n] = (E["sem"], E["cnt"])
        for b in self.dma_bufs:
            evs["d_" + b.name] = (b.sem, b.cnt)
        for n in self.E:
            self._waits(n, evs)
        keep = []
        for b in self.dma_bufs:
            if b.w["_persist"]:
                keep.append(b)
            else:
                self.pool.append((b.name, b.sem, b.cnt))
        self.dma_bufs = keep

    def barrier_known(self):
        pass


class Ctx:
    pass


def _mm_group(S, ps, out_ap, pairs, reads):
    def fn(h):
        ins = None
        n = len(pairs)
        for i, (a, b) in enumerate(pairs):
            ins = h.matmul(out_ap, a, b, start=(i == 0), stop=(i == n - 1))
        return ins
    S.op("pe", fn, reads=reads, writes=[ps])


def _mm1(S, ps, out_ap, a, b, reads, start, stop, drain=False):
    S.op("pe", lambda h: h.matmul(out_ap, a, b, start=start, stop=stop), reads=reads, writes=[ps], skip_self=True, drain_self=drain)


def build(cfg):
    nl = cfg["nl"]
    debug = cfg.get("debug", ())
    stop_after = cfg.get("stop_after", None)
    nc = bass.Bass("TRN2", target_bir_lowering=False)
    es = contextlib.ExitStack()
    K = Ctx()
    K.nc, K.es, K.cfg = nc, es, cfg
    S = Sched(nc, es)
    K.S = S

    def din(name, shape, dt=F32):
        return nc.dram_tensor(name, list(shape), dt, kind="ExternalInput").ap()

    def scratch(name, shape, dt):
        kind = "ExternalOutput" if name in debug else "Internal"
        return nc.dram_tensor(name, list(shape), dt, kind=kind).ap()

    I = {}
    I["xT"] = din("xT", [D, T])
    I["ccT"] = din("ccT", [128, 8, 2])
    I["w_mod"] = din("w_mod", [nl, D, 6 * D])
    I["b_modT"] = din("b_modT", [nl, 128, 48])
    I["g1T"] = din("g1T", [nl, 128, 8])
    I["g2T"] = din("g2T", [nl, 128, 8])
    I["w_in"] = din("w_in", [nl, D, D_IN])
    I["ropeC"] = din("ropeC", [128, L])
    I["ropeS"] = din("ropeS", [128, L])
    I["ident"] = din("ident", [128, 128])
    I["diff_lambda"] = din("diff_lambda", [nl, 256])
    I["sublnT"] = din("sublnT", [nl, 128, 1])
    I["swa_sink"] = din("swa_sink", [nl, 16])
    I["trilo"] = din("trilo", [128, 128])
    I["triup"] = din("triup", [128, 128])
    I["w_branch"] = din("w_branch", [nl, 3, D, D])
    I["w_out"] = din("w_out", [nl, D, D])
    I["fgT"] = din("fgT", [128, 8])
    if "moe" in cfg.get("phases", ()):
        I["router_w"] = din("router_w", [nl, D, NE])
        I["moe_w1"] = din("moe_w1", [nl, NE, D, D])
        I["moe_w3"] = din("moe_w3", [nl, NE, D, D])
        I["moe_w2"] = din("moe_w2", [nl, NE, D, D])
        I["selm"] = din("selm", [16, 16 * 128])
        I["slotidx"] = din("slotidx", [128, 5])
        I["iota512"] = din("iota512", [128, 512])
        I["ustrict"] = din("ustrict", [128, 128])
    if "hy" in cfg.get("phases", ()):
        I["cwT"] = din("cwT", [nl, 128, 24, 3])
        I["cbT"] = din("cbT", [nl, 128, 24])
        I["hbT"] = din("hbT", [nl, 128, 8])
        I["hy_w1"] = din("hy_w1", [nl, 33, 64])
        I["hy_w2"] = din("hy_w2", [nl, 64, 64])
        I["hy_w3"] = din("hy_w3", [nl, 64, 2048])
        I["hy_b1"] = din("hy_b1", [nl, 64, 1])
        I["hy_b2"] = din("hy_b2", [nl, 64, 1])
        I["hy_fr"] = din("hy_fr", [nl, 64, 1])
        I["embT"] = din("embT", [33, L])
        I["embTc"] = din("embTc", [33, LC])
        I["negt"] = din("negt", [128, 32])
        I["negtc"] = din("negtc", [128, 2])
        I["deltas"] = din("deltas", [1, D])
        I["dftC"] = din("dftC", [NF, NF], BF16)
        I["dftS"] = din("dftS", [NF, NF], BF16)
        I["dftCc"] = din("dftCc", [NFC, NFC], BF16)
        I["dftSc"] = din("dftSc", [NFC, NFC], BF16)
        I["wf"] = din("wf", [128, NF // 128])
        I["wfc"] = din("wfc", [128, NFC // 128])
    K.I = I
    K.outT = nc.dram_tensor("outT", [D, L], F32, kind="ExternalOutput").ap()
    K.outT_b = S.buf("outT")

    R = {}
    R["XT"] = (scratch("XT", [D, T], F32), S.buf("XT"))
    R["QdT"] = (scratch("QdT", [D, T], BF16), S.buf("QdT"))
    R["KdT"] = (scratch("KdT", [D, T], BF16), S.buf("KdT"))
    R["Vd"] = (scratch("Vd", [T, D], BF16), S.buf("Vd"))
    R["HyT"] = (scratch("HyT", [3 * D, T], BF16), S.buf("HyT"))
    R["QsT"] = (scratch("QsT", [D, T], BF16), S.buf("QsT"))
    R["KsT"] = (scratch("KsT", [256, T], BF16), S.buf("KsT"))
    R["Vs"] = (scratch("Vs", [T, 256], BF16), S.buf("Vs"))
    R["GT"] = (scratch("GT", [3 * D, T], BF16), S.buf("GT"))
    R["YdT"] = (scratch("YdT", [D, T], BF16), S.buf("YdT"))
    R["YhT"] = (scratch("YhT", [D, T], BF16), S.buf("YhT"))
    R["YsT"] = (scratch("YsT", [D, T], BF16), S.buf("YsT"))
    R["X0T"] = (scratch("X0T", [D, T], BF16), S.buf("X0T"))
    R["UT"] = (scratch("UT", [D, T], BF16), S.buf("UT"))
    R["KF"] = (scratch("KF", [2, NF, D], F32), S.buf("KF"))
    R["KFc"] = (scratch("KFc", [2, NFC, D], F32), S.buf("KFc"))
    R["YF"] = (scratch("YF", [2, NF, D], BF16), S.buf("YF"))
    R["YFc"] = (scratch("YFc", [2, NFC, D], BF16), S.buf("YFc"))
    R["YE"] = (scratch("YE", [NE, CAP + CAPC, D], BF16), S.buf("YE"))
    R["XT1"] = (scratch("XT1", [D, T], F32), S.buf("XT1"))
    R["PT"] = (scratch("PT", [2, 16, T], F32), S.buf("PT"))
    R["LG"] = (scratch("LG", [128, T // 128, NE], F32), S.buf("LG"))
    R["H2d"] = (scratch("H2d", [128, T // 128, D], BF16), S.buf("H2d"))
    K.R = R

    uid = [0]

    def sb(name, shape, dt, stack=es):
        uid[0] += 1
        t = stack.enter_context(nc.sbuf_tensor("sb%d_%s" % (uid[0], name), list(shape), dt))
        return t, S.buf(name)

    def pst(name, shape, dt, stack):
        uid[0] += 1
        t = stack.enter_context(nc.psum_tensor("ps%d_%s" % (uid[0], name), list(shape), dt))
        return t, S.buf(name)
    K.sb, K.pst = sb, pst

    ident_f, ident_f_b = sb("ident_f", [128, 128], F32)
    ident_b, ident_b_b = sb("ident_b", [128, 128], BF16)
    ones_f, ones_f_b = sb("ones_f", [128, 128], F32)
    ones_b, ones_b_b = sb("ones_b", [128, 128], BF16)
    S.dma("sp", ident_f[:], I["ident"][:, :], ident_f_b)
    S.dma("pool", ident_b[:], I["ident"][:, :], ident_b_b)
    S.op("dve", lambda h: h.memset(ones_f[:], 1.0), writes=[ones_f_b])
    S.op("dve", lambda h: h.memset(ones_b[:], 1.0), writes=[ones_b_b])
    K.ident_f, K.ident_b, K.ones_f, K.ones_b = (ident_f, ident_f_b), (ident_b, ident_b_b), (ones_f, ones_f_b), (ones_b, ones_b_b)

    sc, sc_b = sb("sc", [128, 8, 2], F32)
    S.dma("sp", sc[:], I["ccT"][:, :, :], sc_b)
    S.op("act", lambda h: h.activation(out=sc[:], in_=sc[:], func=AF.Silu), reads=[sc_b], writes=[sc_b])
    K.sc = (sc, sc_b)
    modT, modT_b = sb("modT", [128, 48, 2], F32)
    K.modT = (modT, modT_b)
    A1, A1_b = sb("A1", [128, 8, 2], F32)
    A2, A2_b = sb("A2", [128, 8, 2], F32)
    K.A1, K.A2 = (A1, A1_b), (A2, A2_b)

    with contextlib.ExitStack() as ph:
        xc, xc_b = sb("xcp", [128, 8, 512], F32, ph)
        for (t0, tn) in TCH:
            S.dma("sp", xc[:, :, :tn], I["xT"].rearrange("(k p) t -> p k t", p=128)[:, :, t0:t0 + tn], xc_b)
            S.dma("sp", R["XT"][0].rearrange("(k p) t -> p k t", p=128)[:, :, t0:t0 + tn], xc[:, :, :tn], R["XT"][1], [xc_b])
        S.barrier()

    S.persist = False
    phases = cfg.get("phases", ("diff", "swa", "merge"))
    if "swa" not in phases:
        with contextlib.ExitStack() as ph:
            zt, zt_b = sb("zt2", [128, T], BF16, ph)
            S.op("dve", lambda h: h.memset(zt[:], 0.0), writes=[zt_b])
            for k in range(8):
                S.dma("sp", R["YsT"][0][k * 128:(k + 1) * 128, :], zt[:], R["YsT"][1], [zt_b])
            S.barrier()
    if "hy" not in phases:
        with contextlib.ExitStack() as ph:
            zt, zt_b = sb("zt", [128, T], BF16, ph)
            S.op("dve", lambda h: h.memset(zt[:], 0.0), writes=[zt_b])
            for k in range(8):
                S.dma("sp", R["YhT"][0][k * 128:(k + 1) * 128, :], zt[:], R["YhT"][1], [zt_b])
            S.barrier()
    for l in range(nl):
        ctx_out = l < DEPTH - 1
        phase_mod(K, l)
        with contextlib.ExitStack() as ph:
            hT, hT_b = sb("hT", [128, 8, T], BF16, ph)
            phase_norm(K, ph, R["XT"], K.A1, 0, (hT, hT_b))
            if stop_after == "norm1":
                dbg = scratch("dbg_hT", [D, T], BF16)
                S.dma("sp", dbg.rearrange("(k p) t -> p k t", p=128), hT[:], S.buf("dbg"), [hT_b])
                S.barrier()
                break
            phase_inproj(K, ph, l, (hT, hT_b))
            S.barrier()
        if stop_after == "inproj":
            break
        if "diff" in phases:
            phase_diff(K, l, ctx_out)
        if stop_after == "diff":
            break
        if "swa" in phases:
            phase_swa(K, l, ctx_out)
        if "hy" in phases:
            phase_hyena(K, l, ctx_out)
        if stop_after == "hy":
            break
        if stop_after == "swa":
            break
        if "merge" in phases:
            phase_merge(K, l)
        if stop_after == "merge":
            break
        if "moe" in phases:
            phase_moe(K, l, ctx_out)
        if stop_after == "moe":
            break
    if stop_after is None:
        phase_final(K)

    S.barrier()
    es.close()
    return nc


def phase_mod(K, l):
    nc, S, I = K.nc, K.S, K.I
    modT, modT_b = K.modT
    sc, sc_b = K.sc
    with contextlib.ExitStack() as ph:
        wts = [K.sb("wmod%d" % i, [128, 8, 512], F32, ph) for i in range(2)]
        bm, bm_b = K.sb("bmodT", [128, 48], F32, ph)
        g1, g1_b = K.sb("g1T", [128, 8], F32, ph)
        g2, g2_b = K.sb("g2T", [128, 8], F32, ph)
        ps, ps_b = K.pst("ps_mod", [128, 512], F32, ph)
        S.dma("sp", bm[:], I["b_modT"][l], bm_b)
        S.dma("sp", g1[:], I["g1T"][l], g1_b)
        S.dma("sp", g2[:], I["g2T"][l], g2_b)
        for g in range(12):
            wt, wt_b = wts[g % 2]
            S.dma("sp", wt[:], I["w_mod"][l].rearrange("(k p) n -> p k n", p=128)[:, :, g * 512:(g + 1) * 512], wt_b)
            for m in range(4):
                mc = g * 4 + m
                _mm_group(S, ps_b, ps[:, 0:2],
                          [(wt[:, k, m * 128:(m + 1) * 128], sc[:, k, :]) for k in range(8)],
                          [wt_b, sc_b])
                S.op("dve", lambda h, mc=mc: h.tensor_scalar(out=modT[:, mc, :], in0=ps[:, 0:2], scalar1=bm[:, mc:mc + 1],
                                                            scalar2=None, op0=ALU.add),
                     reads=[ps_b, bm_b], writes=[modT_b])
        for (A, A_b), (g, g_b), j in ((K.A1, (g1, g1_b), 1), (K.A2, (g2, g2_b), 4)):
            for r in range(2):
                S.op("dve", lambda h, A=A, g=g, j=j, r=r: h.scalar_tensor_tensor(
                    out=A[:, :, r], in0=modT[:, j * 8:(j + 1) * 8, r], scalar=1.0, in1=g[:], op0=ALU.add, op1=ALU.mult),
                    reads=[modT_b, g_b], writes=[A_b])
        S.barrier()


def phase_norm(K, ph, X, Acoef, shift_j, out, out_f32_cb=None):
    nc, S = K.nc, K.S
    XT, XT_b = X
    A, A_b = Acoef
    modT, modT_b = K.modT
    hT, hT_b = out
    ones_f, ones_f_b = K.ones_f
    with contextlib.ExitStack() as st:
        xs = [K.sb("nx%d" % i, [128, 8, 512], F32, st) for i in range(2)]
        sq, sq_b = K.sb("nsq", [128, 8, 512], F32, st)
        rstd, rstd_b = K.sb("nrstd", [128, 512], F32, st)
        tmp, tmp_b = K.sb("ntmp", [128, 512], F32, st)
        ps, ps_b = K.pst("ps_norm", [128, 512], F32, st)
        for ci, (t0, tn) in enumerate(TCH):
            r = 0 if t0 < L else 1
            x, x_b = xs[ci % 2]
            S.dma("sp", x[:, :, :tn], XT.rearrange("(k p) t -> p k t", p=128)[:, :, t0:t0 + tn], x_b, [XT_b])
            S.op("act", lambda h: h.activation(out=sq[:, :, :tn], in_=x[:, :, :tn], func=AF.Square), reads=[x_b], writes=[sq_b])
            _mm_group(S, ps_b, ps[:, :tn], [(ones_f[:], sq[:, k, :tn]) for k in range(8)], [ones_f_b, sq_b])
            S.op("dve", lambda h: h.tensor_scalar(out=rstd[:, :tn], in0=ps[:, :tn], scalar1=1.0 / D, scalar2=EPS,
                                                  op0=ALU.mult, op1=ALU.add), reads=[ps_b], writes=[rstd_b])
            S.op("act", lambda h: h.activation(out=rstd[:, :tn], in_=rstd[:, :tn], func=AF.Sqrt), reads=[rstd_b], writes=[rstd_b])
            S.op("dve", lambda h: h.reciprocal(out=rstd[:, :tn], in_=rstd[:, :tn]), reads=[rstd_b], writes=[rstd_b])
            for k in range(8):
                S.op("dve", lambda h, k=k: h.tensor_tensor(out=tmp[:, :tn], in0=x[:, k, :tn], in1=rstd[:, :tn], op=ALU.mult),
                     reads=[x_b, rstd_b], writes=[tmp_b])
                S.op("act", lambda h, k=k: h.activation(out=hT[:, k, t0:t0 + tn], in_=tmp[:, :tn], func=AF.Identity,
                                                        scale=A[:, k, r:r + 1], bias=modT[:, shift_j * 8 + k, r:r + 1]),
                     reads=[tmp_b, A_b, modT_b], writes=[hT_b])
                if out_f32_cb is not None:
                    out_f32_cb(ci, k, t0, tn, r, tmp, tmp_b)
        S.barrier()


def _inproj_groups():
    g = []
    g += [("qk", "QdT", 0), ("qk", "QdT", 512), ("qk", "KdT", 0), ("qk", "KdT", 512)]
    g += [("v", "Vd", 0), ("v", "Vd", 512)]
    g += [("plain", "HyT", i * 512) for i in range(6)]
    g += [("qk", "QsT", 0), ("qk", "QsT", 512)]
    g += [("kv", None, 0)]
    g += [("gate", "GT", i * 512) for i in range(6)]
    return g


def phase_inproj(K, ph, l, hTb):
    nc, S, I, R = K.nc, K.S, K.I, K.R
    hT, hT_b = hTb
    with contextlib.ExitStack() as st:
        wts = [K.sb("wi%d" % i, [128, 8, 512], BF16, st) for i in range(2)]
        wsw, wsw_b = K.sb("wsw", [128, 8, 512], BF16, st)
        rC, rC_b = K.sb("ropeC", [128, L], F32, st)
        rS, rS_b = K.sb("ropeS", [128, L], F32, st)
        stg = [K.sb("stg%d" % i, [128, T], BF16, st) for i in range(2)]
        vst = [K.sb("vst%d" % i, [128, 512], BF16, st) for i in range(2)]
        t1, t1_b = K.sb("rt1", [128, 512], F32, st)
        t2, t2_b = K.sb("rt2", [128, 512], F32, st)
        psA = [K.pst("psA%d" % i, [128, 512], F32, st) for i in range(2)]
        psB = [K.pst("psB%d" % i, [128, 512], F32, st) for i in range(2)]
        S.dma("sp", rC[:], I["ropeC"][:, :], rC_b)
        S.dma("sp", rS[:], I["ropeS"][:, :], rS_b)
        win = I["w_in"][l].rearrange("(k p) n -> p k n", p=128)
        cnt = dict(s=0, p=0, v=0)

        def fm_chunk(wt, wt_b, m, kind, dst, row0):
            sg, sg_b = stg[cnt["s"] % 2]
            cnt["s"] += 1
            for (t0, tn) in TCH:
                pa, pa_b = psA[cnt["p"] % 2]
                pb, pb_b = psB[cnt["p"] % 2]
                cnt["p"] += 1
                _mm_group(S, pa_b, pa[:, :tn], [(wt[:, k, m * 128:(m + 1) * 128], hT[:, k, t0:t0 + tn]) for k in range(8)],
                          [wt_b, hT_b])
                if kind == "qk" and t0 < L:
                    _mm_group(S, pb_b, pb[:, :tn], [(wsw[:, k, m * 128:(m + 1) * 128], hT[:, k, t0:t0 + tn]) for k in range(8)],
                              [wsw_b, hT_b])
                    S.op("dve", lambda h: h.tensor_tensor(out=t1[:, :tn], in0=pa[:, :tn], in1=rC[:, t0:t0 + tn], op=ALU.mult),
                         reads=[pa_b, rC_b], writes=[t1_b])
                    S.op("dve", lambda h: h.tensor_tensor(out=t2[:, :tn], in0=pb[:, :tn], in1=rS[:, t0:t0 + tn], op=ALU.mult),
                         reads=[pb_b, rS_b], writes=[t2_b])
                    S.op("pool", lambda h: h.tensor_tensor(out=sg[:, t0:t0 + tn], in0=t1[:, :tn], in1=t2[:, :tn], op=ALU.add),
                         reads=[t1_b, t2_b], writes=[sg_b])
                elif kind == "gate":
                    S.op("act", lambda h: h.activation(out=sg[:, t0:t0 + tn], in_=pa[:, :tn], func=AF.Sigmoid),
                         reads=[pa_b], writes=[sg_b])
                else:
                    S.op("act", lambda h: h.activation(out=sg[:, t0:t0 + tn], in_=pa[:, :tn], func=AF.Copy),
                         reads=[pa_b], writes=[sg_b])
            S.dma("sp", R[dst][0][row0:row0 + 128, :], sg[:], R[dst][1], [sg_b])

        def tm_cols(wt, wt_b, c0, cn, dst, col0):
            for tt in range(T // 128):
                pa, pa_b = psA[cnt["p"] % 2]
                cnt["p"] += 1
                vs, vs_b = vst[cnt["v"] % 2]
                cnt["v"] += 1
                _mm_group(S, pa_b, pa[:, :cn], [(hT[:, k, tt * 128:(tt + 1) * 128], wt[:, k, c0:c0 + cn]) for k in range(8)],
                          [wt_b, hT_b])
                S.op("act", lambda h: h.activation(out=vs[:, :cn], in_=pa[:, :cn], func=AF.Copy), reads=[pa_b], writes=[vs_b])
                S.dma("sp", R[dst][0][tt * 128:(tt + 1) * 128, col0:col0 + cn], vs[:, :cn], R[dst][1], [vs_b])

        def make_swapped(wt, wt_b, ncols):
            src = wt[:, :, :ncols].rearrange("p k (q s f) -> p k q s f", s=2, f=16)
            dstv = wsw[:, :, :ncols].rearrange("p k (q s f) -> p k q s f", s=2, f=16)
            for k in range(8):
                S.op("pool", lambda h, k=k: h.tensor_copy(out=dstv[:, k, :, 0, :], in_=src[:, k, :, 1, :]), reads=[wt_b], writes=[wsw_b])
                S.op("pool", lambda h, k=k: h.tensor_copy(out=dstv[:, k, :, 1, :], in_=src[:, k, :, 0, :]), reads=[wt_b], writes=[wsw_b])

        for gi, (kind, dst, row0) in enumerate(_inproj_groups()):
            wt, wt_b = wts[gi % 2]
            S.dma("pool", wt[:], win[:, :, gi * 512:(gi + 1) * 512], wt_b)
            if kind == "qk":
                make_swapped(wt, wt_b, 512)
                for m in range(4):
                    fm_chunk(wt, wt_b, m, "qk", dst, row0 + m * 128)
            elif kind == "v":
                tm_cols(wt, wt_b, 0, 512, dst, row0)
            elif kind == "kv":
                make_swapped(wt, wt_b, 256)
                for m in range(2):
                    fm_chunk(wt, wt_b, m, "qk", "KsT", m * 128)
                tm_cols(wt, wt_b, 256, 256, "Vs", 0)
            else:
                for m in range(4):
                    fm_chunk(wt, wt_b, m, kind, dst, row0 + m * 128)
        S.barrier()


def _bcast_rows(ap, n):
    return bass.AP(ap.tensor, ap.offset, [[0, 128], [1, n]])


def phase_diff(K, l, ctx_out):
    nc, S, I, R = K.nc, K.S, K.I, K.R
    ones_f, ones_f_b = K.ones_f
    ones_b, ones_b_b = K.ones_b
    lambda_init = 0.8 - 0.6 * math.exp(-0.3 * l)
    with contextlib.ExitStack() as st:
        lp, lp_b = K.sb("lp", [128, 256], F32, st)
        lsc, lsc_b = K.sb("lsc", [128, 4], F32, st)
        gsc, gsc_b = K.sb("gsc", [128, 1], F32, st)
        S.dma("sp", lp[:], _bcast_rows(I["diff_lambda"][l], 256), lp_b)
        S.dma("sp", gsc[:], I["sublnT"][l], gsc_b)
        S.op("dve", lambda h: h.tensor_tensor(out=lp[:, 0:64], in0=lp[:, 0:64], in1=lp[:, 64:128], op=ALU.mult), reads=[lp_b], writes=[lp_b])
        S.op("dve", lambda h: h.tensor_tensor(out=lp[:, 128:192], in0=lp[:, 128:192], in1=lp[:, 192:256], op=ALU.mult), reads=[lp_b], writes=[lp_b])
        S.op("dve", lambda h: h.reduce_sum(out=lsc[:, 0:1], in_=lp[:, 0:64], axis=AX.X), reads=[lp_b], writes=[lsc_b])
        S.op("dve", lambda h: h.reduce_sum(out=lsc[:, 1:2], in_=lp[:, 128:192], axis=AX.X), reads=[lp_b], writes=[lsc_b])
        S.op("act", lambda h: h.activation(out=lsc[:, 0:2], in_=lsc[:, 0:2], func=AF.Exp), reads=[lsc_b], writes=[lsc_b])
        S.op("dve", lambda h: h.scalar_tensor_tensor(out=lsc[:, 2:3], in0=lsc[:, 1:2], scalar=-lambda_init, in1=lsc[:, 0:1],
                                                     op0=ALU.add, op1=ALU.subtract), reads=[lsc_b], writes=[lsc_b])
        S.op("dve", lambda h: h.tensor_scalar(out=gsc[:], in0=gsc[:], scalar1=1.0 - lambda_init, scalar2=None, op0=ALU.mult),
             reads=[gsc_b], writes=[gsc_b])

        QT = [K.sb("dQT%d" % i, [128, T], BF16, st) for i in range(2)]
        KT = [K.sb("dKT%d" % i, [128, T], BF16, st) for i in range(2)]
        VV = [K.sb("dV%d" % i, [128, T // 128, 128], BF16, st) for i in range(2)]
        pt = [K.sb("dP%d" % i, [128, 2, 512], BF16, st) for i in range(2)]
        psS = [K.pst("dpsS%d" % i, [128, 2, 512], F32, st) for i in range(2)]
        acc = [[K.sb("dacc%d%d" % (m, i), [128, 512], F32, st) for i in range(2)] for m in range(2)]
        acc_hi = [S.buf("dacchi%d" % i) for i in range(2)]
        pt_mb = [[S.buf("dPm%d%d" % (i, m)) for m in range(2)] for i in range(2)]
        ps_mb = [[S.buf("dSm%d%d" % (i, m)) for m in range(2)] for i in range(2)]
        psO = [K.pst("dpsO%d" % m, [128, 512], F32, st) for m in range(2)]
        psZ = [K.pst("dpsZ%d" % m, [128, 512], F32, st) for m in range(2)]
        rz = [K.sb("drz%d" % m, [128, 512], F32, st) for m in range(2)]
        oo = [K.sb("doo%d" % m, [128, 512], F32, st) for m in range(2)]
        of, of_b = K.sb("dof", [128, 512], F32, st)
        sqf, sqf_b = K.sb("dsq", [128, 512], F32, st)
        rs, rs_b = K.sb("drs", [128, 512], F32, st)
        ost = [K.sb("dost%d" % i, [128, 512], BF16, st) for i in range(2)]
        nch = 0
        for hd in range(8):
            q, q_b = QT[hd % 2]
            k, k_b = KT[hd % 2]
            v, v_b = VV[hd % 2]
            S.dma("sp", q[:], R["QdT"][0][hd * 128:(hd + 1) * 128, :], q_b, [R["QdT"][1]])
            S.dma("sp", k[:], R["KdT"][0][hd * 128:(hd + 1) * 128, :], k_b, [R["KdT"][1]])
            S.dma("sp", v[:], R["Vd"][0][:, hd * 128:(hd + 1) * 128].rearrange("(t p) e -> p t e", p=128), v_b, [R["Vd"][1]])
            for (q0, qn) in TCH:
                if q0 >= L and not ctx_out:
                    continue
                kts = list(range(T // 128)) if q0 < L else [32, 33]
                cpar = nch % 2

                def qk(i, kt, m, drain=False):
                    ps = psS[i % 2][0]
                    _mm1(S, ps_mb[i % 2][m], ps[:, m, :qn], k[m * 64:(m + 1) * 64, kt * 128:(kt + 1) * 128], q[m * 64:(m + 1) * 64, q0:q0 + qn],
                         [k_b, q_b], True, True, drain=drain)
                    p = pt[i % 2][0]
                    S.op("act", lambda h: h.activation(out=p[:, m, :qn], in_=ps[:, m, :qn], func=AF.Exp, scale=0.125),
                         reads=[ps_mb[i % 2][m]], writes=[pt_mb[i % 2][m]])
                qk(0, kts[0], 0, drain=True)
                qk(0, kts[0], 1, drain=True)
                hq = qn // 2
                for i, kt in enumerate(kts):
                    first, last = i == 0, i == len(kts) - 1
                    p = pt[i % 2][0]
                    for m in range(2):
                        p_b = pt_mb[i % 2][m]
                        if not last:
                            qk(i + 1, kts[i + 1], m, drain=(i == 0 and m == 0))
                        _mm1(S, psO[m][1], psO[m][0][:, :qn], v[:, kt, :], p[:, m, :qn], [v_b, p_b], first, last)
                        a_, a_b = acc[m][cpar]
                        if m == 0:
                            parts = [("dve", 0, qn, a_b)]
                        else:
                            parts = [("pool", 0, hq, a_b), ("dve", hq, qn, acc_hi[cpar])]
                        for eng, c0, c1, ab_ in parts:
                            if first:
                                S.op(eng, lambda h, a_=a_, m=m, c0=c0, c1=c1: h.tensor_copy(out=a_[:, c0:c1], in_=p[:, m, c0:c1]), reads=[p_b], writes=[ab_])
                            else:
                                S.op(eng, lambda h, a_=a_, m=m, c0=c0, c1=c1: h.tensor_tensor(out=a_[:, c0:c1], in0=a_[:, c0:c1], in1=p[:, m, c0:c1], op=ALU.add),
                                     reads=[p_b, ab_], writes=[ab_])
                for m in range(2):
                    a_, a_b = acc[m][cpar]
                    _mm_group(S, psZ[m][1], psZ[m][0][:, :qn], [(ones_f[:], a_[:, :qn])], [ones_f_b, a_b] + ([acc_hi[cpar]] if m == 1 else []))
                for m in range(2):
                    S.op("act", lambda h, m=m: h.activation(out=rz[m][0][:, :qn], in_=psZ[m][0][:, :qn], func=AF.Ln), reads=[psZ[m][1]], writes=[rz[m][1]])
                    S.op("act", lambda h, m=m: h.activation(out=rz[m][0][:, :qn], in_=rz[m][0][:, :qn], func=AF.Exp, scale=-1.0), reads=[rz[m][1]], writes=[rz[m][1]])
                    S.op("dve", lambda h, m=m: h.tensor_tensor(out=oo[m][0][:, :qn], in0=psO[m][0][:, :qn], in1=rz[m][0][:, :qn], op=ALU.mult),
                         reads=[psO[m][1], rz[m][1]], writes=[oo[m][1]])
                S.op("dve", lambda h: h.scalar_tensor_tensor(out=of[:, :qn], in0=oo[1][0][:, :qn], scalar=lsc[:, 2:3], in1=oo[0][0][:, :qn],
                                                              op0=ALU.mult, op1=ALU.add), reads=[oo[0][1], oo[1][1], lsc_b], writes=[of_b])
                S.op("pool", lambda h: h.tensor_tensor(out=sqf[:, :qn], in0=of[:, :qn], in1=of[:, :qn], op=ALU.mult), reads=[of_b], writes=[sqf_b])
                ps, ps_b = psZ[0]
                _mm_group(S, ps_b, ps[:, :qn], [(ones_f[:], sqf[:, :qn])], [ones_f_b, sqf_b])
                S.op("dve", lambda h: h.tensor_scalar(out=rs[:, :qn], in0=ps[:, :qn], scalar1=1.0 / 128, scalar2=EPS, op0=ALU.mult, op1=ALU.add),
                     reads=[ps_b], writes=[rs_b])
                S.op("act", lambda h: h.activation(out=rs[:, :qn], in_=rs[:, :qn], func=AF.Ln), reads=[rs_b], writes=[rs_b])
                S.op("act", lambda h: h.activation(out=rs[:, :qn], in_=rs[:, :qn], func=AF.Exp, scale=-0.5), reads=[rs_b], writes=[rs_b])
                S.op("dve", lambda h: h.tensor_tensor(out=of[:, :qn], in0=of[:, :qn], in1=rs[:, :qn], op=ALU.mult), reads=[of_b, rs_b], writes=[of_b])
                o, o_b = ost[nch % 2]
                nch += 1
                S.op("act", lambda h, o=o: h.activation(out=o[:, :qn], in_=of[:, :qn], func=AF.Copy, scale=gsc[:, 0:1]), reads=[of_b, gsc_b], writes=[o_b])
                S.dma("sp", R["YdT"][0][hd * 128:(hd + 1) * 128, q0:q0 + qn], o[:, :qn], R["YdT"][1], [o_b])
        S.barrier()


def phase_swa(K, l, ctx_out):
    nc, S, I, R = K.nc, K.S, K.I, K.R
    ones_b, ones_b_b = K.ones_b
    with contextlib.ExitStack() as st:
        es_, es_b = K.sb("esink", [128, 16], F32, st)
        S.dma("sp", es_[:], _bcast_rows(I["swa_sink"][l], 16), es_b)
        S.op("act", lambda h: h.activation(out=es_[:], in_=es_[:], func=AF.Exp), reads=[es_b], writes=[es_b])
        mlo, mlo_b = K.sb("mlo", [128, 128], BF16, st)
        mup, mup_b = K.sb("mup", [128, 128], BF16, st)
        S.dma("pool", mlo[:], I["trilo"][:, :], mlo_b)
        S.dma("pool", mup[:], I["triup"][:, :], mup_b)
        QC = [[K.sb("sQ%d%d" % (c, i), [128, T], BF16, st) for i in range(2)] for c in range(2)]
        KT = [K.sb("sKT%d" % i, [128, T], BF16, st) for i in range(2)]
        VV = [K.sb("sV%d" % i, [128, T // 128, 64], BF16, st) for i in range(2)]
        pt = [K.sb("sP%d" % i, [128, 512], BF16, st) for i in range(2)]
        psS = [K.pst("spsS%d" % i, [128, 512], F32, st) for i in range(2)]
        psO = [K.pst("spsO%d" % i, [128, 512], F32, st) for i in range(2)]
        psZ = [K.pst("spsZ%d" % i, [128, 512], F32, st) for i in range(2)]
        zz, zz_b = K.sb("szz", [64, 512], F32, st)
        ost = [K.sb("sost%d" % i, [64, 512], BF16, st) for i in range(2)]
        nblk = 0
        npt = 0
        for kh in range(4):
            k, k_b = KT[kh % 2]
            v, v_b = VV[kh % 2]
            for half in range(2):
                S.dma("sp", k[half * 64:(half + 1) * 64, :], R["KsT"][0][kh * 64:(kh + 1) * 64, :], k_b, [R["KsT"][1]])
            S.dma("sp", v[:], R["Vs"][0][:, kh * 64:(kh + 1) * 64].rearrange("(t p) e -> p t e", p=128), v_b, [R["Vs"][1]])
            qc = []
            for c in range(2):
                qq, qq_b = QC[c][kh % 2]
                S.dma("sp", qq[:], R["QsT"][0][kh * 256 + c * 128: kh * 256 + (c + 1) * 128, :], qq_b, [R["QsT"][1]])
                qc.append((qq, qq_b))
            nqb = T // 128 if ctx_out else L // 128
            dbgl = K.cfg.get("swa_dbg", 9)
            if dbgl < 9:
                nqb = 2 if kh == 0 else 0
            steps = []
            for qb in range(nqb):
                if qb < 32:
                    kts = [(kk, mk) for kk, mk in ((qb - 1, "lo"), (qb, None), (qb + 1, "up")) if 0 <= kk < 32] + [(32, None), (33, None)]
                else:
                    kts = [(32, None), (33, None)]
                for i, (kt, mk) in enumerate(kts):
                    steps.append((qb, kt, mk, i == 0, i == len(kts) - 1))

            def stage_a(si):
                qb, kt, mk, first, last = steps[si]
                ps, ps_b = psS[si % 2]
                p, p_b = pt[si % 2]
                for gi, g in enumerate((0, 2, 1, 3)):
                    r0 = (g % 2) * 64
                    qq, qq_b = qc[g // 2]
                    _mm1(S, ps_b, ps[:, g * 128:(g + 1) * 128], k[r0:r0 + 64, kt * 128:(kt + 1) * 128], qq[r0:r0 + 64, qb * 128:(qb + 1) * 128],
                         [k_b, qq_b], True, True, drain=(gi == 2 or (gi == 0 and si <= 1)))
                S.op("act", lambda h: h.activation(out=p[:], in_=ps[:], func=AF.Exp, scale=0.125), reads=[ps_b], writes=[p_b])
                if mk is not None:
                    mt, mt_b = (mlo, mlo_b) if mk == "lo" else (mup, mup_b)
                    for g in range(4):
                        S.op("pool", lambda h, g=g, mt=mt: h.tensor_tensor(out=p[:, g * 128:(g + 1) * 128], in0=p[:, g * 128:(g + 1) * 128], in1=mt[:], op=ALU.mult),
                             reads=[p_b, mt_b], writes=[p_b])

            if steps:
                stage_a(0)
            for si, (qb, kt, mk, first, last) in enumerate(steps):
                if si + 1 < len(steps):
                    stage_a(si + 1)
                po, po_b = psO[qb % 2]
                pz, pz_b = psZ[qb % 2]
                p, p_b = pt[si % 2]
                _mm1(S, po_b, po[0:64, :], v[:, kt, :], p[:], [v_b, p_b], first, last)
                _mm1(S, pz_b, pz[0:64, :], ones_b[:, 0:64], p[:], [ones_b_b, p_b], first, last)
                if not last:
                    continue
                for g in range(4):
                    S.op("dve", lambda h, g=g: h.tensor_scalar(out=zz[:, g * 128:(g + 1) * 128], in0=pz[0:64, g * 128:(g + 1) * 128],
                                                               scalar1=es_[0:64, kh * 4 + g:kh * 4 + g + 1], scalar2=None, op0=ALU.add),
                         reads=[pz_b, es_b], writes=[zz_b])
                S.op("dve", lambda h: h.reciprocal(out=zz[:], in_=zz[:]), reads=[zz_b], writes=[zz_b])
                o, o_b = ost[qb % 2]
                S.op("dve", lambda h, o=o: h.tensor_tensor(out=o[:], in0=po[0:64, :], in1=zz[:], op=ALU.mult),
                     reads=[po_b, zz_b], writes=[o_b])
                S.dma("sp", R["YsT"][0][kh * 256:(kh + 1) * 256, qb * 128:(qb + 1) * 128].rearrange("(g d) q -> d g q", g=4),
                      o[:].rearrange("p (g q) -> p g q", g=4), R["YsT"][1], [o_b])
        S.barrier()


def phase_merge(K, l):
    nc, S, I, R = K.nc, K.S, K.I, K.R
    modT, modT_b = K.modT
    with contextlib.ExitStack() as st:
        wb, wb_b = K.sb("wbr", [128, 24, D], BF16, st)
        wo, wo_b = K.sb("wout", [128, 8, D], BF16, st)
        for b in range(3):
            S.dma("pool", wb[:, b * 8:(b + 1) * 8, :], I["w_branch"][l, b].rearrange("(k p) n -> p k n", p=128), wb_b)
        S.dma("pool", wo[:], I["w_out"][l].rearrange("(k p) n -> p k n", p=128), wo_b)
        yb = [K.sb("my%d" % b, [128, 8, 512], BF16, st) for b in range(3)]
        gt, gt_b = K.sb("mgt", [128, 24, 512], BF16, st)
        mg, mg_b = K.sb("mmg", [128, 8, 512], F32, st)
        mgb, mgb_b = K.sb("mmgb", [128, 8, 512], BF16, st)
        tmp = [K.sb("mtmp%d" % i, [128, 512], F32, st) for i in range(2)]
        xx, xx_b = K.sb("mx", [128, 8, 512], F32, st)
        pss = [K.pst("mps%d" % i, [128, 512], F32, st) for i in range(4)]
        npz = 0
        ntmp = 0
        XTv = R["XT"][0].rearrange("(k p) t -> p k t", p=128)
        for (t0, tn) in TCH:
            r = 0 if t0 < L else 1
            for b, nm in enumerate(("YdT", "YhT", "YsT")):
                S.dma("sp", yb[b][0][:, :, :tn], R[nm][0].rearrange("(k p) t -> p k t", p=128)[:, :, t0:t0 + tn], yb[b][1], [R[nm][1]])
            S.dma("sp", gt[:, :, :tn], R["GT"][0].rearrange("(k p) t -> p k t", p=128)[:, :, t0:t0 + tn], gt_b, [R["GT"][1]])
            S.dma("sp", xx[:, :, :tn], XTv[:, :, t0:t0 + tn], xx_b, [R["XT"][1]])
            for m in range(8):
                for b in range(3):
                    ps, ps_b = pss[npz % 4]
                    npz += 1
                    _mm_group(S, ps_b, ps[:, :tn], [(wb[:, b * 8 + k, m * 128:(m + 1) * 128], yb[b][0][:, k, :tn]) for k in range(8)],
                              [wb_b, yb[b][1]])
                    if b == 0:
                        S.op("dve", lambda h, m=m, ps=ps: h.tensor_tensor(out=mg[:, m, :tn], in0=ps[:, :tn], in1=gt[:, m, :tn], op=ALU.mult),
                             reads=[ps_b, gt_b], writes=[mg_b])
                    else:
                        tp, tp_b = tmp[ntmp % 2]
                        ntmp += 1
                        S.op("dve", lambda h, m=m, b=b, ps=ps, tp=tp: h.tensor_tensor(out=tp[:, :tn], in0=ps[:, :tn], in1=gt[:, b * 8 + m, :tn], op=ALU.mult),
                             reads=[ps_b, gt_b], writes=[tp_b])
                        S.op("pool", lambda h, m=m, tp=tp: h.tensor_tensor(out=mg[:, m, :tn], in0=mg[:, m, :tn], in1=tp[:, :tn], op=ALU.add),
                             reads=[tp_b, mg_b], writes=[mg_b])
                S.op("act", lambda h, m=m: h.activation(out=mgb[:, m, :tn], in_=mg[:, m, :tn], func=AF.Copy), reads=[mg_b], writes=[mgb_b])
            for m in range(8):
                ps, ps_b = pss[npz % 4]
                npz += 1
                _mm_group(S, ps_b, ps[:, :tn], [(wo[:, k, m * 128:(m + 1) * 128], mgb[:, k, :tn]) for k in range(8)], [wo_b, mgb_b])
                S.op("dve", lambda h, m=m, ps=ps: h.scalar_tensor_tensor(out=xx[:, m, :tn], in0=ps[:, :tn], scalar=modT[:, 16 + m, r:r + 1],
                                                                         in1=xx[:, m, :tn], op0=ALU.mult, op1=ALU.add),
                     reads=[ps_b, modT_b, xx_b], writes=[xx_b])
            S.dma("sp", XTv[:, :, t0:t0 + tn], xx[:, :, :tn], R["XT"][1], [xx_b])
        S.barrier()


PI = math.pi


def _hy_filter(K, l, Lx, embT_ap, negt_ap, Cm, Sm, wf_ap, KFs):
    nc, S, I, R = K.nc, K.S, K.I, K.R
    ones_b, ones_b_b = K.ones_b
    nlt = Lx // 128
    nft = (Lx + 128) // 128
    nch = max(1, Lx // 512)
    cw = min(512, Lx)
    with contextlib.ExitStack() as st:
        w1, w1_b = K.sb("fw1", [33, 64], F32, st)
        w2, w2_b = K.sb("fw2", [64, 64], F32, st)
        w3, w3_b = K.sb("fw3", [64, 2048], F32, st)
        b1, b1_b = K.sb("fb1", [64, 1], F32, st)
        b2, b2_b = K.sb("fb2", [64, 1], F32, st)
        fr, fr_b = K.sb("ffr", [64, 1], F32, st)
        emb, emb_b = K.sb("femb", [33, Lx], F32, st)
        ngt, ngt_b = K.sb("fngt", [128, nlt], F32, st)
        dl, dl_b = K.sb("fdl", [128, D], F32, st)
        wf, wf_b = K.sb("fwf", [128, nft], F32, st)
        z1, z1_b = K.sb("fz1", [64, Lx], F32, st)
        z2, z2_b = K.sb("fz2", [64, Lx], F32, st)
        rn, rn_b = K.sb("frn", [128, D], F32, st)
        dec, dec_b = K.sb("fdec", [128, D], F32, st)
        hd = [K.sb("fhd%d" % i, [128, D], F32, st) for i in range(2)]
        ab, ab_b = K.sb("fab", [128, 512], BF16, st)
        gsel, gsel_b = K.sb("fgsel", [64, 512], F32, st)
        AT, AT_b = K.sb("fAT", [128, nlt, D], BF16, st)
        for t_, src in ((w1, I["hy_w1"][l]), (w2, I["hy_w2"][l]), (w3, I["hy_w3"][l]), (b1, I["hy_b1"][l]), (b2, I["hy_b2"][l]),
                        (fr, I["hy_fr"][l]), (emb, embT_ap), (ngt, negt_ap), (wf, wf_ap)):
            pass
        S.dma("sp", w1[:], I["hy_w1"][l], w1_b)
        S.dma("sp", w2[:], I["hy_w2"][l], w2_b)
        S.dma("sp", w3[:], I["hy_w3"][l], w3_b)
        S.dma("sp", b1[:], I["hy_b1"][l], b1_b)
        S.dma("sp", b2[:], I["hy_b2"][l], b2_b)
        S.dma("sp", fr[:], I["hy_fr"][l], fr_b)
        S.dma("sp", emb[:], embT_ap, emb_b)
        S.dma("sp", ngt[:], negt_ap, ngt_b)
        S.dma("sp", wf[:], wf_ap, wf_b)
        S.dma("sp", dl[:], _bcast_rows(I["deltas"][0], D), dl_b)
        psm = [K.pst("fps%d" % i, [128, 512], F32, st) for i in range(4)]
        psN = [K.pst("fpsN%d" % i, [128, 512], F32, st) for i in range(2)]
        for (wm, wm_b, bb, bb_b, src, src_b, dst, dst_b) in ((w1, w1_b, b1, b1_b, emb, emb_b, z1, z1_b), (w2, w2_b, b2, b2_b, z1, z1_b, z2, z2_b)):
            for ci in range(nch):
                ps, ps_b = psm[ci % 4]
                sl = slice(ci * cw, (ci + 1) * cw)
                _mm_group(S, ps_b, ps[0:64, :cw], [(wm[:], src[:, sl])], [wm_b, src_b])
                S.op("dve", lambda h, ps=ps, sl=sl, bb=bb, dst=dst: h.tensor_scalar(out=dst[:, sl], in0=ps[0:64, :cw], scalar1=bb[:, 0:1], scalar2=fr[:, 0:1],
                                                                                 op0=ALU.add, op1=ALU.mult), reads=[ps_b, bb_b, fr_b], writes=[dst_b])
                for _ in range(2):
                    for cop, sh in ((ALU.is_gt, -2.0 * PI), (ALU.is_lt, 2.0 * PI)):
                        thr = PI if cop == ALU.is_gt else -PI
                        S.op("dve", lambda h, sl=sl, dst=dst, cop=cop, thr=thr: h.tensor_scalar(out=gsel[:, :cw], in0=dst[:, sl], scalar1=thr, scalar2=None, op0=cop),
                             reads=[dst_b], writes=[gsel_b])
                        S.op("dve", lambda h, sl=sl, dst=dst, sh=sh: h.scalar_tensor_tensor(out=dst[:, sl], in0=gsel[:, :cw], scalar=sh, in1=dst[:, sl],
                                                                                          op0=ALU.mult, op1=ALU.add), reads=[gsel_b, dst_b], writes=[dst_b])
                S.op("act", lambda h, sl=sl, dst=dst: h.activation(out=dst[:, sl], in_=dst[:, sl], func=AF.Sin), reads=[dst_b], writes=[dst_b])

        def htile(lt, cb):
            S.op("act", lambda h: h.activation(out=dec[:], in_=dl[:], func=AF.Exp, scale=ngt[:, lt:lt + 1]), reads=[dl_b, ngt_b], writes=[dec_b])
            for j in range(4):
                ps, ps_b = psm[j]
                _mm_group(S, ps_b, ps[:], [(z2[:, lt * 128:(lt + 1) * 128], w3[:, j * 512:(j + 1) * 512])], [z2_b, w3_b])
            cb(lt)

        for lt in range(nlt):
            def cb1(lt):
                for j in range(4):
                    ps, ps_b = psm[j]
                    half = j % 2
                    tf, tf_b = hd[j // 2]
                    S.op("dve", lambda h, ps=ps, half=half, tf=tf: h.tensor_tensor(out=tf[:, half * 512:(half + 1) * 512], in0=ps[:],
                                                                                   in1=dec[:, half * 512:(half + 1) * 512], op=ALU.mult),
                         reads=[ps_b, dec_b], writes=[tf_b])
                    S.op("act", lambda h, half=half, tf=tf: h.activation(out=ab[:], in_=tf[:, half * 512:(half + 1) * 512], func=AF.Abs),
                         reads=[tf_b], writes=[ab_b])
                    first = (lt == 0 and j < 2)
                    last = (lt == nlt - 1 and j >= 2)
                    _mm1(S, psN[half][1], psN[half][0][:], ones_b[:], ab[:], [ones_b_b, ab_b], first, last)
            htile(lt, cb1)
        for half in range(2):
            S.op("dve", lambda h, half=half: h.tensor_scalar(out=rn[:, half * 512:(half + 1) * 512], in0=psN[half][0][:], scalar1=EPS, scalar2=None, op0=ALU.add),
                 reads=[psN[half][1]], writes=[rn_b])
        S.op("dve", lambda h: h.reciprocal(out=rn[:], in_=rn[:]), reads=[rn_b], writes=[rn_b])
        for plane, op2 in (("C", ALU.add), ("S", ALU.subtract)):
            for lt in range(nlt):
                def cb2(lt):
                    for j in range(4):
                        ps, ps_b = psm[j]
                        half, dr = j % 2, j // 2
                        S.op("dve", lambda h, ps=ps, half=half, dr=dr: h.tensor_tensor(out=hd[dr][0][:, half * 512:(half + 1) * 512], in0=ps[:],
                                                                                      in1=dec[:, half * 512:(half + 1) * 512], op=ALU.mult),
                             reads=[ps_b, dec_b], writes=[hd[dr][1]])
                    if lt == 0:
                        S.op("dve", lambda h: h.memset(hd[1][0][0:1, :], 0.0), writes=[hd[1][1]])
                    S.op("pool", lambda h: h.tensor_tensor(out=hd[0][0][:], in0=hd[0][0][:], in1=hd[1][0][:], op=op2), reads=[hd[0][1], hd[1][1]], writes=[hd[0][1]])
                    S.op("pool", lambda h: h.tensor_tensor(out=AT[:, lt, :], in0=hd[0][0][:], in1=rn[:], op=ALU.mult), reads=[hd[0][1], rn_b], writes=[AT_b])
                htile(lt, cb2)
            pi = 0 if plane == "C" else 1

            def evac(ft, pc, ps_, pi=pi):
                src = pc if pi == 0 else ps_
                for hh in range(2):
                    o, o_b = hd[hh]
                    S.op("dve", lambda h, hh=hh, o=o: h.tensor_scalar(out=o[:, 0:512], in0=src[hh][0][:], scalar1=wf[:, ft:ft + 1], scalar2=None, op0=ALU.mult),
                         reads=[src[hh][1], wf_b], writes=[o_b])
                    S.dma("sp", KFs[0][pi, ft * 128:(ft + 1) * 128, hh * 512:(hh + 1) * 512], o[:, 0:512], KFs[1], [o_b])
            _fwd_dft(K, st, Cm, Sm, nlt, nft, AT, AT_b, 0, (plane,), evac, psm)
        S.barrier()


def _fwd_dft(K, st, Cm, Sm, ntt, nft, rhs, rhs_b, tt0, planes, evac, pspool):
    S = K.S
    with contextlib.ExitStack() as s2:
        blk = {p: [K.sb("dblk%s%d" % (p, i), [128, ntt, 128], BF16, s2) for i in range(2)] for p in planes}
        for ft in range(nft):
            pss = {}
            for pi, p in enumerate(("C", "S")):
                if p not in planes:
                    pss[p] = None
                    continue
                M = Cm if p == "C" else Sm
                b, b_b = blk[p][ft % 2]
                S.dma("sp", b[:], M.rearrange("(t p) f -> p t f", p=128)[:, 0:ntt, ft * 128:(ft + 1) * 128], b_b)
                pss[p] = [pspool[pi * 2 + hh] for hh in range(2)]
                for hh in range(2):
                    ps, ps_b = pss[p][hh]
                    _mm_group(S, ps_b, ps[:], [(b[:, t, :], rhs[:, tt0 + t, hh * 512:(hh + 1) * 512]) for t in range(ntt)], [b_b, rhs_b])
            evac(ft, pss["C"], pss["S"])


def phase_hyena(K, l, ctx_out):
    nc, S, I, R = K.nc, K.S, K.I, K.R
    ident_b, ident_b_b = K.ident_b
    _hy_filter(K, l, L, I["embT"][:, :], I["negt"][:, :], I["dftC"], I["dftS"], I["wf"][:, :], R["KF"])
    if ctx_out:
        _hy_filter(K, l, LC, I["embTc"][:, :], I["negtc"][:, :], I["dftCc"], I["dftSc"], I["wfc"][:, :], R["KFc"])
    segs = [(0, L)] + ([(L, LC)] if ctx_out else [])
    TT = T if ctx_out else L
    with contextlib.ExitStack() as st:
        utok, utok_b = K.sb("hutok", [128, T // 128, D], BF16, st)
        with contextlib.ExitStack() as s2:
            cw, cw_b = K.sb("hcw", [128, 24, 3], F32, s2)
            cb, cb_b = K.sb("hcb", [128, 24], F32, s2)
            S.dma("sp", cw[:], I["cwT"][l], cw_b)
            S.dma("sp", cb[:], I["cbT"][l], cb_b)
            xin = [K.sb("hxin%d" % i, [128, T], BF16, s2) for i in range(3)]
            yc = [K.sb("hyc%d" % i, [128, T], F32, s2) for i in range(3)]
            x0b, x0b_b = K.sb("hx0b", [128, T], BF16, s2)
            ub, ub_b = K.sb("hub", [128, T], BF16, s2)
            ptr = [K.pst("hptr%d" % i, [128, 1024], BF16, s2) for i in range(2)]
            ntr = 0
            for c in range(8):
                for s_ in range(3):
                    j = s_ * 8 + c
                    xi, xi_b = xin[s_]
                    y, y_b = yc[s_]
                    S.dma("sp", xi[:, :TT], R["HyT"][0][j * 128:(j + 1) * 128, 0:TT], xi_b, [R["HyT"][1]])
                    for (a0, n) in segs:
                        S.op("dve", lambda h, j=j, xi=xi, y=y: h.tensor_scalar(out=y[:, a0:a0 + n], in0=xi[:, a0:a0 + n], scalar1=cw[:, j, 1:2], scalar2=cb[:, j:j + 1],
                                                                            op0=ALU.mult, op1=ALU.add), reads=[xi_b, cw_b, cb_b], writes=[y_b])
                        S.op("dve", lambda h, j=j, xi=xi, y=y: h.scalar_tensor_tensor(out=y[:, a0 + 1:a0 + n], in0=xi[:, a0:a0 + n - 1], scalar=cw[:, j, 0:1],
                                                                                   in1=y[:, a0 + 1:a0 + n], op0=ALU.mult, op1=ALU.add),
                             reads=[xi_b, cw_b, y_b], writes=[y_b])
                        S.op("dve", lambda h, j=j, xi=xi, y=y: h.scalar_tensor_tensor(out=y[:, a0:a0 + n - 1], in0=xi[:, a0 + 1:a0 + n], scalar=cw[:, j, 2:3],
                                                                                   in1=y[:, a0:a0 + n - 1], op0=ALU.mult, op1=ALU.add),
                             reads=[xi_b, cw_b, y_b], writes=[y_b])
                S.op("act", lambda h: h.activation(out=x0b[:, :TT], in_=yc[0][0][:, :TT], func=AF.Copy), reads=[yc[0][1]], writes=[x0b_b])
                S.op("pool", lambda h: h.tensor_tensor(out=ub[:, :TT], in0=yc[1][0][:, :TT], in1=yc[2][0][:, :TT], op=ALU.mult),
                     reads=[yc[1][1], yc[2][1]], writes=[ub_b])
                S.dma("sp", R["X0T"][0][c * 128:(c + 1) * 128, 0:TT], x0b[:, :TT], R["X0T"][1], [x0b_b])
                S.dma("sp", R["UT"][0][c * 128:(c + 1) * 128, 0:TT], ub[:, :TT], R["UT"][1], [ub_b])
                for t8 in range(0, TT // 128, 8):
                    n8 = min(8, TT // 128 - t8)
                    pt_, pt_b = ptr[ntr % 2]
                    ntr += 1

                    def trf(h, t8=t8, n8=n8, pt_=pt_):
                        ins = None
                        for q in range(n8):
                            ins = h.transpose(pt_[:, q * 128:(q + 1) * 128], ub[:, (t8 + q) * 128:(t8 + q + 1) * 128], ident_b[:])
                        return ins
                    S.op("pe", trf, reads=[ub_b, ident_b_b], writes=[pt_b])
                    S.op("act", lambda h, t8=t8, n8=n8, pt_=pt_, c=c: h.activation(out=utok[:, t8:t8 + n8, c * 128:(c + 1) * 128],
                                                                                   in_=pt_[:, :n8 * 128].rearrange("p (q f) -> p q f", f=128), func=AF.Copy),
                         reads=[pt_b], writes=[utok_b])
            S.barrier()
        for si, (a0, n) in enumerate(segs):
            Cm, Sm = (I["dftC"], I["dftS"]) if si == 0 else (I["dftCc"], I["dftSc"])
            KFs = R["KF"] if si == 0 else R["KFc"]
            YFs = R["YF"] if si == 0 else R["YFc"]
            ntt = n // 128
            nft = (n + 128) // 128
            with contextlib.ExitStack() as s2:
                kf = [K.sb("hkf%d" % i, [128, 2, D], F32, s2) for i in range(2)]
                tq = [K.sb("htq%d" % i, [128, 512], F32, s2) for i in range(4)]
                yo = [K.sb("hyo%d" % i, [128, 2, D], BF16, s2) for i in range(2)]
                pspool = [K.pst("hps%d" % i, [128, 512], F32, s2) for i in range(4)]

                def evac(ft, pc, ps_, KFs=KFs, YFs=YFs):
                    k_, k_b = kf[ft % 2]
                    o, o_b = yo[ft % 2]
                    S.dma("sp", k_[:], KFs[0][:, ft * 128:(ft + 1) * 128, :].rearrange("a p c -> p a c"), k_b, [KFs[1]])
                    for hh in range(2):
                        cs = slice(hh * 512, (hh + 1) * 512)
                        ur, ur_b = pc[hh]
                        ui, ui_b = ps_[hh]
                        S.op("dve", lambda h: h.tensor_tensor(out=tq[0][0][:], in0=ur[:], in1=k_[:, 0, cs], op=ALU.mult), reads=[ur_b, k_b], writes=[tq[0][1]])
                        S.op("dve", lambda h: h.tensor_tensor(out=tq[1][0][:], in0=ui[:], in1=k_[:, 1, cs], op=ALU.mult), reads=[ui_b, k_b], writes=[tq[1][1]])
                        S.op("pool", lambda h: h.tensor_tensor(out=o[:, 0, cs], in0=tq[0][0][:], in1=tq[1][0][:], op=ALU.subtract),
                             reads=[tq[0][1], tq[1][1]], writes=[o_b])
                        S.op("dve", lambda h: h.tensor_tensor(out=tq[2][0][:], in0=ur[:], in1=k_[:, 1, cs], op=ALU.mult), reads=[ur_b, k_b], writes=[tq[2][1]])
                        S.op("dve", lambda h: h.tensor_tensor(out=tq[3][0][:], in0=ui[:], in1=k_[:, 0, cs], op=ALU.mult), reads=[ui_b, k_b], writes=[tq[3][1]])
                        S.op("pool", lambda h: h.tensor_tensor(out=o[:, 1, cs], in0=tq[2][0][:], in1=tq[3][0][:], op=ALU.add),
                             reads=[tq[2][1], tq[3][1]], writes=[o_b])
                    S.dma("sp", YFs[0][:, ft * 128:(ft + 1) * 128, :].rearrange("a p c -> p a c"), o[:], YFs[1], [o_b])
                _fwd_dft(K, s2, Cm, Sm, ntt, nft, utok, utok_b, a0 // 128, ("C", "S"), evac, pspool)
                S.barrier()
    for si, (a0, n) in enumerate(segs):
        Cm, Sm = (I["dftC"], I["dftS"]) if si == 0 else (I["dftCc"], I["dftSc"])
        YFs = R["YF"] if si == 0 else R["YFc"]
        nft = (n + 128) // 128
        with contextlib.ExitStack() as s2:
            hb, hb_b = K.sb("ihb", [128, 8], F32, s2)
            S.dma("sp", hb[:], I["hbT"][l], hb_b)
            Yh = [K.sb("iY%d" % i, [128, nft, 512], BF16, s2) for i in range(2)]
            Cr = [K.sb("iC%d" % i, [128, nft, 256], BF16, s2) for i in range(2)]
            Sr = [K.sb("iS%d" % i, [128, nft, 256], BF16, s2) for i in range(2)]
            x0c = [K.sb("ix0%d" % i, [128, 4, 256], BF16, s2) for i in range(2)]
            uc = [K.sb("iu%d" % i, [128, 4, 256], BF16, s2) for i in range(2)]
            tmp, tmp_b = K.sb("itmp", [128, 256], F32, s2)
            og = [K.sb("iog%d" % i, [128, 4, 256], BF16, s2) for i in range(2)]
            psI = [K.pst("ips%d" % i, [128, 512], F32, s2) for i in range(2)]
            nk = 0
            npi = 0
            for half in range(2):
                for pl in range(2):
                    S.dma("sp", Yh[pl][0][:], YFs[0][pl, 0:nft * 128, half * 512:(half + 1) * 512].rearrange("(f p) c -> p f c", p=128), Yh[pl][1], [YFs[1]])
                for t0 in range(0, n, 256):
                    cr, cr_b = Cr[nk % 2]
                    sr, sr_b = Sr[nk % 2]
                    xx, xx_b = x0c[nk % 2]
                    uu, uu_b = uc[nk % 2]
                    oo, oo_b = og[nk % 2]
                    nk += 1
                    S.dma("sp", cr[:], Cm.rearrange("(f p) t -> p f t", p=128)[:, 0:nft, t0:t0 + 256], cr_b)
                    S.dma("sp", sr[:], Sm.rearrange("(f p) t -> p f t", p=128)[:, 0:nft, t0:t0 + 256], sr_b)
                    rows = slice(half * 512, (half + 1) * 512)
                    S.dma("sp", xx[:], R["X0T"][0][rows, a0 + t0:a0 + t0 + 256].rearrange("(c p) t -> p c t", p=128), xx_b, [R["X0T"][1]])
                    S.dma("sp", uu[:], R["UT"][0][rows, a0 + t0:a0 + t0 + 256].rearrange("(c p) t -> p c t", p=128), uu_b, [R["UT"][1]])
                    for ci in range(4):
                        ps, ps_b = psI[npi % 2]
                        npi += 1
                        pairs = [(Yh[0][0][:, f, ci * 128:(ci + 1) * 128], cr[:, f, :]) for f in range(nft)] + \
                                [(Yh[1][0][:, f, ci * 128:(ci + 1) * 128], sr[:, f, :]) for f in range(nft)]
                        _mm_group(S, ps_b, ps[:, 0:256], pairs, [Yh[0][1], Yh[1][1], cr_b, sr_b])
                        cidx = half * 4 + ci
                        S.op("dve", lambda h, ci=ci, cidx=cidx, ps=ps, uu=uu: h.scalar_tensor_tensor(out=tmp[:], in0=uu[:, ci, :], scalar=hb[:, cidx:cidx + 1],
                                                                                                    in1=ps[:, 0:256], op0=ALU.mult, op1=ALU.add),
                             reads=[uu_b, hb_b, ps_b], writes=[tmp_b])
                        S.op("pool", lambda h, ci=ci, oo=oo, xx=xx: h.tensor_tensor(out=oo[:, ci, :], in0=tmp[:], in1=xx[:, ci, :], op=ALU.mult),
                             reads=[tmp_b, xx_b], writes=[oo_b])
                    S.dma("sp", R["YhT"][0][rows, a0 + t0:a0 + t0 + 256].rearrange("(c p) t -> p c t", p=128), oo[:], R["YhT"][1], [oo_b])
            S.barrier()
    if not ctx_out:
        pass


def _bc_mid(ap2d, n):
    a = ap2d.ap
    return bass.AP(ap2d.tensor, ap2d.offset, [list(a[0]), [0, n], list(a[1])])


def phase_moe(K, l, ctx_out):
    nc, S, I, R = K.nc, K.S, K.I, K.R
    modT, modT_b = K.modT
    A2, A2_b = K.A2
    ones_f, ones_f_b = K.ones_f
    ones_b, ones_b_b = K.ones_b
    ident_f, ident_f_b = K.ident_f
    ident_b, ident_b_b = K.ident_b
    debug = K.cfg.get("debug", ())
    XTv = R["XT"][0].rearrange("(k p) t -> p k t", p=128)
    chunks = TCH if ctx_out else TCH[:8]
    ntt = 34 if ctx_out else 32
    groups = [(0, 32, CAP)] + ([(32, 34, CAPC)] if ctx_out else [])
    with contextlib.ExitStack() as st:
        lg, lg_b = K.sb("lg", [128, 34, NE], F32, st)
        aff, aff_b = K.sb("aff", [128, 34, NE], F32, st)
        psel, psel_b = K.sb("psel", [128, 34, NE], F32, st)
        wr, wr_b = K.sb("wr", [128, 8, NE], F32, st)
        S.dma("sp", wr[:], I["router_w"][l].rearrange("(k p) e -> p k e", p=128), wr_b)
        sH = contextlib.ExitStack()
        H2, H2_b = K.sb("H2", [128, 34, D], BF16, sH)
        if ctx_out is False:
            S.op("dve", lambda h: h.memset(lg[:, 32:34, :], 0.0), writes=[lg_b])
        with contextlib.ExitStack() as s2:
            xs = [K.sb("qx%d" % i, [128, 8, 512], F32, s2) for i in range(2)]
            sq, sq_b = K.sb("qsq", [128, 8, 512], F32, s2)
            rstd, rstd_b = K.sb("qrstd", [128, 512], F32, s2)
            tmp, tmp_b = K.sb("qtmp", [128, 512], F32, s2)
            h2f, h2f_b = K.sb("qh2f", [128, 8, 512], F32, s2)
            hTc, hTc_b = K.sb("qhTc", [128, 8, 512], BF16, s2)
            ps, ps_b = K.pst("qps", [128, 512], F32, s2)
            psl, psl_b = K.pst("qpsl", [128, 64], F32, s2)
            pstr = [K.pst("qpst%d" % i, [128, 1024], BF16, s2) for i in range(2)]
            ntr = 0
            for ci, (t0, tn) in enumerate(chunks):
                r = 0 if t0 < L else 1
                x, x_b = xs[ci % 2]
                S.dma("sp", x[:, :, :tn], XTv[:, :, t0:t0 + tn], x_b, [R["XT"][1]])
                if "XT1" in debug:
                    S.dma("sp", R["XT1"][0].rearrange("(k p) t -> p k t", p=128)[:, :, t0:t0 + tn], x[:, :, :tn], R["XT1"][1], [x_b])
                S.op("act", lambda h: h.activation(out=sq[:, :, :tn], in_=x[:, :, :tn], func=AF.Square), reads=[x_b], writes=[sq_b])
                _mm_group(S, ps_b, ps[:, :tn], [(ones_f[:], sq[:, k, :tn]) for k in range(8)], [ones_f_b, sq_b])
                S.op("dve", lambda h: h.tensor_scalar(out=rstd[:, :tn], in0=ps[:, :tn], scalar1=1.0 / D, scalar2=EPS,
                                                      op0=ALU.mult, op1=ALU.add), reads=[ps_b], writes=[rstd_b])
                S.op("act", lambda h: h.activation(out=rstd[:, :tn], in_=rstd[:, :tn], func=AF.Sqrt), reads=[rstd_b], writes=[rstd_b])
                S.op("dve", lambda h: h.reciprocal(out=rstd[:, :tn], in_=rstd[:, :tn]), reads=[rstd_b], writes=[rstd_b])
                for k in range(8):
                    S.op("dve", lambda h, k=k: h.tensor_tensor(out=tmp[:, :tn], in0=x[:, k, :tn], in1=rstd[:, :tn], op=ALU.mult),
                         reads=[x_b, rstd_b], writes=[tmp_b])
                    S.op("act", lambda h, k=k: h.activation(out=h2f[:, k, :tn], in_=tmp[:, :tn], func=AF.Identity,
                                                            scale=A2[:, k, r:r + 1], bias=modT[:, 24 + k, r:r + 1]),
                         reads=[tmp_b, A2_b, modT_b], writes=[h2f_b])
                    S.op("pool", lambda h, k=k: h.tensor_copy(out=hTc[:, k, :tn], in_=h2f[:, k, :tn]), reads=[h2f_b], writes=[hTc_b])
                nj = tn // 128
                for j in range(nj):
                    tt = t0 // 128 + j
                    _mm_group(S, psl_b, psl[:, j * 16:(j + 1) * 16],
                              [(h2f[:, k, j * 128:(j + 1) * 128], wr[:, k, :]) for k in range(8)], [h2f_b, wr_b])
                    pt_, pt_b = pstr[ntr % 2]
                    ntr += 1

                    def trf(h, j=j, pt_=pt_):
                        ins = None
                        for k in range(8):
                            ins = h.transpose(pt_[:, k * 128:(k + 1) * 128], hTc[:, k, j * 128:(j + 1) * 128], ident_b[:])
                        return ins
                    S.op("pe", trf, reads=[hTc_b, ident_b_b], writes=[pt_b])
                    S.op("act", lambda h, tt=tt, pt_=pt_: h.activation(out=H2[:, tt, :], in_=pt_[:], func=AF.Copy), reads=[pt_b], writes=[H2_b])
                S.op("dve", lambda h: h.tensor_copy(out=lg[:, t0 // 128:t0 // 128 + nj, :].rearrange("p t e -> p (t e)"),
                                                    in_=psl[:, :nj * 16]), reads=[psl_b], writes=[lg_b])
            S.barrier()
        if "LG" in debug:
            S.dma("sp", R["LG"][0], lg[:], R["LG"][1], [lg_b])
            S.dma("sp", R["H2d"][0], H2[:], R["H2d"][1], [H2_b])
        with contextlib.ExitStack() as s2:
            se, se_b = K.sb("rse", [128, 34], F32, s2)
            lo, lo_b = K.sb("rlo", [128, NE], F32, s2)
            mid, mid_b = K.sb("rmid", [128, NE], F32, s2)
            cmpt, cmp_b = K.sb("rcmp", [128, 32, NE], F32, s2)
            cntp, cntp_b = K.sb("rcntp", [128, NE], F32, s2)
            ge, ge_b = K.sb("rge", [128, NE], F32, s2)
            mk, mk_b = K.sb("rmk", [128, 34, NE], F32, s2)
            mkb, mkb_b = K.sb("rmkb", [128, 34, NE], BF16, s2)
            tot, tot_b = K.sb("rtot", [128, 34, NE], F32, s2)
            base, base_b = K.sb("rbase", [128, 34, NE], F32, s2)
            posT, posT_b = K.sb("posT", [16, T], F32, s2)
            affT, affT_b = K.sb("affT", [16, T], F32, s2)
            us, us_b = K.sb("rus", [128, 128], BF16, s2)
            S.dma("pool", us[:], I["ustrict"][:, :], us_b)
            psc, psc_b = K.pst("rpsc", [128, 512], F32, s2)
            psw, psw_b = K.pst("rpsw", [128, 512], F32, s2)
            pst_, pst_b = K.pst("rpst", [128, 512], F32, s2)
            S.op("act", lambda h: h.activation(out=aff[:].rearrange("p t e -> p (t e)"), in_=lg[:].rearrange("p t e -> p (t e)"), func=AF.Exp),
                 reads=[lg_b], writes=[aff_b])
            S.op("dve", lambda h: h.reduce_sum(out=se[:], in_=aff[:], axis=AX.X), reads=[aff_b], writes=[se_b])
            S.op("dve", lambda h: h.reciprocal(out=se[:], in_=se[:]), reads=[se_b], writes=[se_b])
            for tt in range(34):
                S.op("dve", lambda h, tt=tt: h.tensor_scalar(out=aff[:, tt, :], in0=aff[:, tt, :], scalar1=se[:, tt:tt + 1], scalar2=None, op0=ALU.mult),
                     reads=[aff_b, se_b], writes=[aff_b])
            S.op("dve", lambda h: h.memset(mk[:], 0.0), writes=[mk_b])
            S.op("dve", lambda h: h.memset(base[:], 0.0), writes=[base_b])
            for (ta, tb, cap) in groups:
                nt = tb - ta
                S.op("dve", lambda h: h.memset(lo[:], 0.0), writes=[lo_b])
                for it in range(30):
                    w = 0.5 ** (it + 1)
                    S.op("dve", lambda h: h.tensor_scalar(out=mid[:], in0=lo[:], scalar1=w, scalar2=None, op0=ALU.add), reads=[lo_b], writes=[mid_b])
                    S.op("dve", lambda h: h.tensor_tensor(out=cmpt[:, :nt, :], in0=aff[:, ta:tb, :], in1=_bc_mid(mid[:], nt), op=ALU.is_ge),
                         reads=[aff_b, mid_b], writes=[cmp_b])
                    S.op("dve", lambda h: h.reduce_sum(out=cntp[:], in_=cmpt[:, :nt, :].rearrange("p t e -> p e t"), axis=AX.X),
                         reads=[cmp_b], writes=[cntp_b])
                    _mm_group(S, psc_b, psc[:, 0:NE], [(ones_f[:], cntp[:])], [ones_f_b, cntp_b])
                    S.op("dve", lambda h: h.tensor_scalar(out=ge[:], in0=psc[:, 0:NE], scalar1=cap - 0.5, scalar2=None, op0=ALU.is_ge),
                         reads=[psc_b], writes=[ge_b])
                    S.op("dve", lambda h: h.scalar_tensor_tensor(out=lo[:], in0=ge[:], scalar=w, in1=lo[:], op0=ALU.mult, op1=ALU.add),
                         reads=[ge_b, lo_b], writes=[lo_b])
                S.op("dve", lambda h: h.tensor_tensor(out=mk[:, ta:tb, :], in0=aff[:, ta:tb, :], in1=_bc_mid(lo[:], nt), op=ALU.is_ge),
                     reads=[aff_b, lo_b], writes=[mk_b])
                S.op("dve", lambda h: h.tensor_copy(out=mkb[:, ta:tb, :], in_=mk[:, ta:tb, :]), reads=[mk_b], writes=[mkb_b])
                mflat = mkb[:, ta:tb, :].rearrange("p t e -> p (t e)")
                _mm_group(S, psw_b, psw[:, :nt * NE], [(us[:], mflat)], [us_b, mkb_b])
                _mm_group(S, pst_b, pst_[:, :nt * NE], [(ones_b[:], mflat)], [ones_b_b, mkb_b])
                S.op("dve", lambda h: h.tensor_copy(out=tot[:, ta:tb, :].rearrange("p t e -> p (t e)"), in_=pst_[:, :nt * NE]),
                     reads=[pst_b], writes=[tot_b])
                for t in range(ta + 1, tb):
                    S.op("dve", lambda h, t=t: h.tensor_tensor(out=base[:, t, :], in0=base[:, t - 1, :], in1=tot[:, t - 1, :], op=ALU.add),
                         reads=[base_b, tot_b], writes=[base_b])
                S.op("dve", lambda h: h.tensor_tensor(out=psel[:, ta:tb, :].rearrange("p t e -> p (t e)"), in0=psw[:, :nt * NE],
                                                      in1=base[:, ta:tb, :].rearrange("p t e -> p (t e)"), op=ALU.add),
                     reads=[psw_b, base_b], writes=[psel_b])
                S.op("dve", lambda h: h.scalar_tensor_tensor(out=psel[:, ta:tb, :], in0=psel[:, ta:tb, :], scalar=1.0, in1=mk[:, ta:tb, :],
                                                             op0=ALU.add, op1=ALU.mult), reads=[psel_b, mk_b], writes=[psel_b])
                S.op("dve", lambda h: h.tensor_scalar(out=psel[:, ta:tb, :], in0=psel[:, ta:tb, :], scalar1=-1.0, scalar2=None, op0=ALU.add),
                     reads=[psel_b], writes=[psel_b])
            S.op("dve", lambda h: h.tensor_tensor(out=aff[:], in0=aff[:], in1=mk[:], op=ALU.mult), reads=[aff_b, mk_b], writes=[aff_b])
            for src, src_b, dstT, dstT_b in ((psel, psel_b, posT, posT_b), (aff, aff_b, affT, affT_b)):
                for t4 in range(0, ntt, 4):
                    n4 = min(4, ntt - t4)

                    def trf(h, t4=t4, n4=n4, src=src):
                        ins = None
                        for j in range(n4):
                            ins = h.transpose(psc[0:16, j * 128:(j + 1) * 128], src[:, t4 + j, :], ident_f[:])
                        return ins
                    S.op("pe", trf, reads=[src_b, ident_f_b], writes=[psc_b])
                    S.op("act", lambda h, t4=t4, n4=n4, dstT=dstT: h.activation(out=dstT[:, t4 * 128:(t4 + n4) * 128], in_=psc[0:16, :n4 * 128], func=AF.Copy),
                         reads=[psc_b], writes=[dstT_b])
            S.dma("sp", R["PT"][0][0, :, 0:ntt * 128], posT[:, 0:ntt * 128], R["PT"][1], [posT_b])
            S.dma("sp", R["PT"][0][1, :, 0:ntt * 128], affT[:, 0:ntt * 128], R["PT"][1], [affT_b])
            S.barrier()
        NS = CAP + CAPC
        with contextlib.ExitStack() as s3:
            w1, w1_b = K.sb("ew1", [128, 8, D], BF16, s3)
            w3, w3_b = K.sb("ew3", [128, 8, D], BF16, s3)
            w2, w2_b = K.sb("ew2", [128, 8, D], BF16, s3)
            Se, Se_b = K.sb("eS", [128, 32, 512], BF16, s3)
            Sc, Sc_b = K.sb("eSc", [128, 2, CAPC], BF16, s3)
            io, io_b = K.sb("eio", [128, 512], F32, s3)
            S.dma("sp", io[:], I["iota512"][:, :], io_b)
            xg, xg_b = K.sb("exg", [128, 8, NS], BF16, s3)
            gT, gT_b = K.sb("egT", [128, 8, NS], BF16, s3)
            sil, sil_b = K.sb("esil", [128, NS], F32, s3)
            yst = [K.sb("eyst%d" % i, [128, 512], BF16, s3) for i in range(2)]
            psG = [K.pst("epsG%d" % i, [128, 512], F32, s3) for i in range(2)]
            psA, psA_b = K.pst("epsA", [128, 512], F32, s3)
            psB, psB_b = K.pst("epsB", [128, 512], F32, s3)
            psC, psC_b = K.pst("epsC", [128, 512], F32, s3)
            psY = [K.pst("epsY%d" % i, [128, 512], F32, s3) for i in range(2)]
            ng = 0
            ny = 0
            for e in range(NE):
                S.dma("pool", w1[:], I["moe_w1"][l, e].rearrange("(k p) n -> p k n", p=128), w1_b)
                S.dma("pool", w3[:], I["moe_w3"][l, e].rearrange("(k p) n -> p k n", p=128), w3_b)
                S.dma("pool", w2[:], I["moe_w2"][l, e].rearrange("(k p) n -> p k n", p=128), w2_b)
                for tt in range(32):
                    S.op("dve", lambda h, tt=tt: h.tensor_scalar(out=Se[:, tt, :], in0=io[:], scalar1=psel[:, tt, e:e + 1], scalar2=None, op0=ALU.is_equal),
                         reads=[io_b, psel_b], writes=[Se_b])
                if ctx_out:
                    for tt in range(32, 34):
                        S.op("dve", lambda h, tt=tt: h.tensor_scalar(out=Sc[:, tt - 32, :], in0=io[:, 0:CAPC], scalar1=psel[:, tt, e:e + 1], scalar2=None,
                                                                     op0=ALU.is_equal), reads=[io_b, psel_b], writes=[Sc_b])
                for dk in range(8):
                    pg, pg_b = psG[ng % 2]
                    ng += 1
                    _mm_group(S, pg_b, pg[:], [(H2[:, tt, dk * 128:(dk + 1) * 128], Se[:, tt, :]) for tt in range(32)], [H2_b, Se_b])
                    S.op("act", lambda h, dk=dk, pg=pg: h.activation(out=xg[:, dk, 0:CAP], in_=pg[:], func=AF.Copy), reads=[pg_b], writes=[xg_b])
                    if ctx_out:
                        _mm_group(S, psC_b, psC[:, 0:CAPC], [(H2[:, tt, dk * 128:(dk + 1) * 128], Sc[:, tt - 32, :]) for tt in (32, 33)], [H2_b, Sc_b])
                        S.op("act", lambda h, dk=dk: h.activation(out=xg[:, dk, CAP:NS], in_=psC[:, 0:CAPC], func=AF.Copy), reads=[psC_b], writes=[xg_b])
                for m in range(8):
                    _mm_group(S, psA_b, psA[:], [(w1[:, k, m * 128:(m + 1) * 128], xg[:, k, 0:CAP]) for k in range(8)], [w1_b, xg_b])
                    _mm_group(S, psB_b, psB[:], [(w3[:, k, m * 128:(m + 1) * 128], xg[:, k, 0:CAP]) for k in range(8)], [w3_b, xg_b])
                    S.op("act", lambda h: h.activation(out=sil[:, 0:CAP], in_=psA[:], func=AF.Silu), reads=[psA_b], writes=[sil_b])
                    S.op("dve", lambda h, m=m: h.tensor_tensor(out=gT[:, m, 0:CAP], in0=psB[:], in1=sil[:, 0:CAP], op=ALU.mult),
                         reads=[psB_b, sil_b], writes=[gT_b])
                    if ctx_out:
                        _mm_group(S, psC_b, psC[:, 0:CAPC], [(w1[:, k, m * 128:(m + 1) * 128], xg[:, k, CAP:NS]) for k in range(8)], [w1_b, xg_b])
                        S.op("act", lambda h: h.activation(out=sil[:, CAP:NS], in_=psC[:, 0:CAPC], func=AF.Silu), reads=[psC_b], writes=[sil_b])
                        _mm_group(S, psC_b, psC[:, 0:CAPC], [(w3[:, k, m * 128:(m + 1) * 128], xg[:, k, CAP:NS]) for k in range(8)], [w3_b, xg_b])
                        S.op("dve", lambda h, m=m: h.tensor_tensor(out=gT[:, m, CAP:NS], in0=psC[:, 0:CAPC], in1=sil[:, CAP:NS], op=ALU.mult),
                             reads=[psC_b, sil_b], writes=[gT_b])
                for c in range(5 if ctx_out else 4):
                    rows = 128 if c < 4 else CAPC
                    for dh in range(2):
                        py, py_b = psY[ny % 2]
                        ys, ys_b = yst[ny % 2]
                        ny += 1
                        _mm_group(S, py_b, py[0:rows, :], [(gT[:, f, c * 128:c * 128 + rows], w2[:, f, dh * 512:(dh + 1) * 512]) for f in range(8)],
                                  [gT_b, w2_b])
                        S.op("act", lambda h, py=py, ys=ys, rows=rows: h.activation(out=ys[0:rows, :], in_=py[0:rows, :], func=AF.Copy),
                             reads=[py_b], writes=[ys_b])
                        S.dma("sp", R["YE"][0][e, c * 128:c * 128 + rows, dh * 512:(dh + 1) * 512], ys[0:rows, :], R["YE"][1], [ys_b])
            S.barrier()
        sH.close()
        with contextlib.ExitStack() as s4:
            YEs, YEs_b = K.sb("cYE", [128, NE, 5, 512], BF16, s4)
            ST, ST_b = K.sb("cST", [128, NE, 4, 512], BF16, s4)
            abc = [K.sb("cabc%d" % i, [128, 512], F32, s4) for i in range(2)]
            xh, xh_b = K.sb("cxh", [128, 4, 512], F32, s4)
            selm, selm_b = K.sb("cselm", [16, NE, 128], F32, s4)
            sidx, sidx_b = K.sb("csidx", [128, 5], F32, s4)
            posT, posT_b = K.sb("cposT", [16, T], F32, s4)
            affT, affT_b = K.sb("caffT", [16, T], F32, s4)
            S.dma("sp", posT[:, 0:ntt * 128], R["PT"][0][0, :, 0:ntt * 128], posT_b, [R["PT"][1]])
            S.dma("sp", affT[:, 0:ntt * 128], R["PT"][0][1, :, 0:ntt * 128], affT_b, [R["PT"][1]])
            S.dma("sp", selm[:], I["selm"].rearrange("k (e m) -> k e m", e=NE), selm_b)
            S.dma("sp", sidx[:], I["slotidx"][:, :], sidx_b)
            psP = [K.pst("cpsP%d" % i, [128, 512], F32, s4) for i in range(2)]
            psQ = [K.pst("cpsQ%d" % i, [128, 512], F32, s4) for i in range(2)]
            psO = [K.pst("cpsO%d" % i, [128, 512], F32, s4) for i in range(2)]
            nb_ = 0
            no = 0
            for dh in range(2):
                for e in range(NE):
                    S.dma("sp", YEs[:, e, 0:4, :], R["YE"][0][e, 0:CAP, dh * 512:(dh + 1) * 512].rearrange("(c p) d -> p c d", p=128), YEs_b, [R["YE"][1]])
                    if ctx_out:
                        S.dma("sp", YEs[0:CAPC, e, 4, :], R["YE"][0][e, CAP:NS, dh * 512:(dh + 1) * 512], YEs_b, [R["YE"][1]])
                for (t0, tn) in chunks:
                    r = 0 if t0 < L else 1
                    lat = t0 < L
                    for e in range(NE):
                        pp, pp_b = psP[nb_ % 2]
                        pq, pq_b = psQ[nb_ % 2]
                        ab, ab_b = abc[nb_ % 2]
                        nb_ += 1
                        _mm_group(S, pp_b, pp[:, :tn], [(selm[:, e, :], posT[:, t0:t0 + tn])], [selm_b, posT_b])
                        _mm_group(S, pq_b, pq[:, :tn], [(selm[:, e, :], affT[:, t0:t0 + tn])], [selm_b, affT_b])
                        S.op("act", lambda h, ab=ab, pq=pq: h.activation(out=ab[:, :tn], in_=pq[:, :tn], func=AF.Copy), reads=[pq_b], writes=[ab_b])
                        if lat:
                            for c in range(4):
                                S.op("dve", lambda h, c=c, pp=pp, ab=ab: h.scalar_tensor_tensor(out=ST[:, e, c, :tn], in0=pp[:, :tn], scalar=sidx[:, c:c + 1],
                                                                                               in1=ab[:, :tn], op0=ALU.is_equal, op1=ALU.mult),
                                     reads=[pp_b, ab_b, sidx_b], writes=[ST_b])
                        else:
                            S.op("dve", lambda h, pp=pp, ab=ab: h.scalar_tensor_tensor(out=ST[0:CAPC, e, 0, :tn], in0=pp[0:CAPC, :tn], scalar=sidx[0:CAPC, 4:5],
                                                                                      in1=ab[0:CAPC, :tn], op0=ALU.is_equal, op1=ALU.mult),
                                 reads=[pp_b, ab_b, sidx_b], writes=[ST_b])
                    S.dma("sp", xh[:, :, :tn], XTv[:, dh * 4:(dh + 1) * 4, t0:t0 + tn], xh_b, [R["XT"][1]])
                    for m in range(4):
                        po, po_b = psO[no % 2]
                        no += 1
                        if lat:
                            pairs = [(YEs[:, e, c, m * 128:(m + 1) * 128], ST[:, e, c, :tn]) for e in range(NE) for c in range(4)]
                        else:
                            pairs = [(YEs[0:CAPC, e, 4, m * 128:(m + 1) * 128], ST[0:CAPC, e, 0, :tn]) for e in range(NE)]
                        _mm_group(S, po_b, po[:, :tn], pairs, [YEs_b, ST_b])
                        S.op("dve", lambda h, m=m, po=po: h.scalar_tensor_tensor(out=xh[:, m, :tn], in0=po[:, :tn], scalar=modT[:, 40 + dh * 4 + m, r:r + 1],
                                                                                 in1=xh[:, m, :tn], op0=ALU.mult, op1=ALU.add),
                             reads=[po_b, modT_b, xh_b], writes=[xh_b])
                    S.dma("sp", XTv[:, dh * 4:(dh + 1) * 4, t0:t0 + tn], xh[:, :, :tn], R["XT"][1], [xh_b])
            S.barrier()


def phase_final(K):
    nc, S, I, R = K.nc, K.S, K.I, K.R
    ones_f, ones_f_b = K.ones_f
    XTv = R["XT"][0].rearrange("(k p) t -> p k t", p=128)
    with contextlib.ExitStack() as st:
        fg, fg_b = K.sb("fg", [128, 8], F32, st)
        S.dma("sp", fg[:], I["fgT"][:, :], fg_b)
        xs = [K.sb("fx%d" % i, [128, 8, 512], F32, st) for i in range(2)]
        sq, sq_b = K.sb("fsq", [128, 8, 512], F32, st)
        rstd, rstd_b = K.sb("frstd", [128, 512], F32, st)
        ps, ps_b = K.pst("ps_fin", [128, 512], F32, st)
        for ci in range(8):
            t0 = ci * 512
            x, x_b = xs[ci % 2]
            S.dma("sp", x[:], XTv[:, :, t0:t0 + 512], x_b, [R["XT"][1]])
            S.op("act", lambda h: h.activation(out=sq[:], in_=x[:], func=AF.Square), reads=[x_b], writes=[sq_b])
            _mm_group(S, ps_b, ps[:], [(ones_f[:], sq[:, k, :]) for k in range(8)], [ones_f_b, sq_b])
            S.op("dve", lambda h: h.tensor_scalar(out=rstd[:], in0=ps[:], scalar1=1.0 / D, scalar2=EPS, op0=ALU.mult, op1=ALU.add),
                 reads=[ps_b], writes=[rstd_b])
            S.op("act", lambda h: h.activation(out=rstd[:], in_=rstd[:], func=AF.Sqrt), reads=[rstd_b], writes=[rstd_b])
            S.op("dve", lambda h: h.reciprocal(out=rstd[:], in_=rstd[:]), reads=[rstd_b], writes=[rstd_b])
            for k in range(8):
                S.op("dve", lambda h, k=k: h.scalar_tensor_tensor(out=sq[:, k, :], in0=x[:, k, :], scalar=fg[:, k:k + 1], in1=rstd[:],
                                                                  op0=ALU.mult, op1=ALU.mult), reads=[x_b, rstd_b, fg_b], writes=[sq_b])
            S.dma("sp", K.outT.rearrange("(k p) t -> p k t", p=128)[:, :, t0:t0 + 512], sq[:], K.outT_b, [sq_b])
        S.barrier()


def rope_tables():
    rows = L // 64
    row = np.repeat(np.arange(rows, dtype=np.float32), 64)
    col = np.tile(np.arange(64, dtype=np.float32), rows)
    inv = (10000.0 ** (-np.arange(16, dtype=np.float32) / 16)).astype(np.float32)
    C = np.zeros((64, L), np.float32)
    Sg = np.zeros((64, L), np.float32)
    for j in range(64):
        a, hf, f = j // 32, (j % 32) // 16, j % 16
        pos = row if a == 0 else col
        ang = (pos * inv[f]).astype(np.float32)
        C[j] = np.cos(ang)
        Sg[j] = np.sin(ang) * (-1.0 if hf == 0 else 1.0)
    return np.concatenate([C, C], 0), np.concatenate([Sg, Sg], 0)


_HC = {}


def hyena_consts():
    if _HC:
        return _HC
    f32 = np.float32

    def emb_of(Lx):
        t = np.linspace(0.0, 1.0, Lx, dtype=f32)[:, None]
        w = (2.0 * math.pi * np.arange(Lx, dtype=f32)[:, None] / Lx).astype(f32)
        f = np.linspace(1e-4, 15, 16, dtype=f32)[None, :]
        e = np.concatenate([t, np.cos(f * w), -np.sin(f * w)], axis=-1).astype(f32)
        return np.ascontiguousarray(e.T), t[:, 0]
    eT, t = emb_of(L)
    eTc, tc = emb_of(LC)
    _HC["embT"], _HC["embTc"] = eT, eTc
    _HC["negt"] = np.ascontiguousarray((-t).reshape(L // 128, 128).T)
    _HC["negtc"] = np.ascontiguousarray((-tc).reshape(LC // 128, 128).T)
    _HC["deltas"] = np.abs(np.linspace(math.log(1e-2) / 0.3, math.log(1e-2) / 1.5, D, dtype=f32)).reshape(1, D).astype(f32)

    def dft(Lx, npad):
        N = 2 * Lx
        a = np.arange(Lx + 1, dtype=np.int64)
        ph = (a[:, None] * a[None, :]) % N
        ang = ph.astype(np.float64) * (2.0 * math.pi / N)
        C = np.zeros((npad, npad), f32)
        S_ = np.zeros((npad, npad), f32)
        C[:Lx + 1, :Lx + 1] = np.cos(ang)
        S_[:Lx + 1, :Lx + 1] = np.sin(ang)
        wfv = np.zeros(npad, f32)
        wfv[:Lx + 1] = 2.0 / N
        wfv[0] = 1.0 / N
        wfv[Lx] = 1.0 / N
        return C.astype(ml_dtypes.bfloat16), S_.astype(ml_dtypes.bfloat16), np.ascontiguousarray(wfv.reshape(npad // 128, 128).T)
    _HC["dftC"], _HC["dftS"], _HC["wf"] = dft(L, NF)
    _HC["dftCc"], _HC["dftSc"], _HC["wfc"] = dft(LC, NFC)
    return _HC


def host_inputs(inputs, b, nl):
    x, c, ctx, c_ctx = inputs["x"], inputs["c"], inputs["ctx"], inputs["c_ctx"]
    m = {}
    m["xT"] = np.ascontiguousarray(np.concatenate([x[b], ctx[b]], 0).T)
    cc = np.stack([c[b], c_ctx], 0)
    m["ccT"] = np.ascontiguousarray(cc.reshape(2, 8, 128).transpose(2, 1, 0))
    m["w_mod"] = np.ascontiguousarray(inputs["w_mod"][:nl])
    m["b_modT"] = np.ascontiguousarray(inputs["b_mod"][:nl].reshape(nl, 48, 128).transpose(0, 2, 1))
    m["g1T"] = np.ascontiguousarray(inputs["norm1_g"][:nl].reshape(nl, 8, 128).transpose(0, 2, 1))
    m["g2T"] = np.ascontiguousarray(inputs["norm2_g"][:nl].reshape(nl, 8, 128).transpose(0, 2, 1))
    m["w_in"] = np.ascontiguousarray(inputs["w_in"][:nl])
    rc, rs = rope_tables()
    m["ropeC"], m["ropeS"] = rc, rs
    m["ident"] = np.eye(128, dtype=np.float32)
    m["diff_lambda"] = np.ascontiguousarray(inputs["diff_lambda"][:nl].reshape(nl, 256))
    m["sublnT"] = np.ascontiguousarray(inputs["diff_subln_g"][:nl].reshape(nl, 128, 1))
    m["swa_sink"] = np.ascontiguousarray(inputs["swa_sink"][:nl])
    kk, qq = np.meshgrid(np.arange(128), np.arange(128), indexing="ij")
    m["trilo"] = (kk >= qq).astype(np.float32)
    m["triup"] = (kk <= qq).astype(np.float32)
    m["w_branch"] = np.ascontiguousarray(inputs["w_branch"][:nl])
    m["w_out"] = np.ascontiguousarray(inputs["w_out"][:nl])
    m["fgT"] = np.ascontiguousarray(inputs["final_g"].reshape(8, 128).T)
    if "hy_ff_w1" in inputs:
        cwv = inputs["hy_conv_w"][:nl]
        m["cwT"] = np.ascontiguousarray(cwv.reshape(nl, 3, 24, 128).transpose(0, 3, 2, 1))
        m["cbT"] = np.ascontiguousarray(inputs["hy_conv_b"][:nl].reshape(nl, 24, 128).transpose(0, 2, 1))
        m["hbT"] = np.ascontiguousarray(inputs["hy_bias"][:nl].reshape(nl, 8, 128).transpose(0, 2, 1))
        m["hy_w1"] = np.ascontiguousarray(inputs["hy_ff_w1"][:nl])
        m["hy_w2"] = np.ascontiguousarray(inputs["hy_ff_w2"][:nl])
        m["hy_w3"] = np.ascontiguousarray(inputs["hy_ff_w3"][:nl])
        m["hy_b1"] = np.ascontiguousarray(inputs["hy_ff_b1"][:nl].reshape(nl, 64, 1))
        m["hy_b2"] = np.ascontiguousarray(inputs["hy_ff_b2"][:nl].reshape(nl, 64, 1))
        m["hy_fr"] = np.ascontiguousarray(inputs["hy_sin_freq"][:nl].reshape(nl, 64, 1))
        m.update(hyena_consts())
    if "moe_w1" in inputs:
        m["router_w"] = np.ascontiguousarray(inputs["router_w"][:nl])
        for k in ("moe_w1", "moe_w3", "moe_w2"):
            m[k] = np.ascontiguousarray(inputs[k][:nl])
        sel = np.zeros((16, 16, 128), np.float32)
        for e in range(16):
            sel[e, e, :] = 1.0
        m["selm"] = sel.reshape(16, 16 * 128)
        si = np.zeros((128, 5), np.float32)
        for c in range(4):
            si[:, c] = np.arange(128) + 128 * c
        si[:, 4] = np.arange(128)
        m["slotidx"] = si
        m["iota512"] = np.tile(np.arange(512, dtype=np.float32)[None, :], (128, 1))
        m["ustrict"] = (kk < qq).astype(np.float32)
    return m


def kernel(**inputs):
    inputs = {k: np.asarray(v) for k, v in inputs.items()}
    nb = inputs["x"].shape[0]
    nc = build(dict(nl=DEPTH, phases=("diff", "swa", "hy", "merge", "moe")))
    in_maps = [host_inputs(inputs, b, DEPTH) for b in range(nb)]
    res = run_bass_kernel_spmd(nc, in_maps, core_ids=list(range(nb)))
    out = np.stack([np.asarray(res.results[b]["outT"]).T for b in range(nb)], 0)
    return np.ascontiguousarray(out.astype(np.float32))
```

```python
import contextlib
import math
import numpy as np
import ml_dtypes
import concourse.bass as bass
import concourse.mybir as mybir
from concourse.bass_utils import run_bass_kernel_spmd

F32 = mybir.dt.float32
BF16 = mybir.dt.bfloat16
AF = mybir.ActivationFunctionType
ALU = mybir.AluOpType
AX = mybir.AxisListType

D = 1024
L = 4096
LC = 256
T = L + LC
DEPTH = 4
D_IN = 10752
NE = 16
CAP = 512
CAPC = 32
EPS = 1e-6
NF = 4224
NFC = 384
TCH = [(i * 512, 512) for i in range(8)] + [(L, LC)]


class Buf:
    __slots__ = ("name", "w", "r", "sem", "cnt")

    def __init__(self, name):
        self.name = name
        self.w = {}
        self.r = {}
        self.sem = None
        self.cnt = 0


class Sched:
    def __init__(self, nc, es):
        self.nc, self.es = nc, es
        self.E = {}
        for n, h in (("pe", nc.tensor), ("dve", nc.vector), ("act", nc.scalar),
                     ("pool", nc.gpsimd), ("sp", nc.sync)):
            self.E[n] = dict(h=h, sem=es.enter_context(nc.semaphore("s_" + n)), cnt=0, seen={})
        self.dma_bufs = []
        self.nbuf = 0
        self.persist = True
        self.pool = []

    def buf(self, name):
        self.nbuf += 1
        b = Buf("%s_%d" % (name, self.nbuf))
        b.w["_persist"] = self.persist
        return b

    @staticmethod
    def _add(evs, d):
        for k, sv in d.items():
            if k == "_persist":
                continue
            sem, v = sv
            if k not in evs or evs[k][1] < v:
                evs[k] = (sem, v)

    def _waits(self, e, evs):
        E = self.E[e]
        for name, (sem, val) in evs.items():
            if E["seen"].get(name, 0) < val:
                E["h"].wait_ge(sem, val)
                E["seen"][name] = val

    def op(self, e, fn, reads=(), writes=(), skip_self=False, drain_self=False):
        evs = {}
        for b in reads:
            self._add(evs, b.w)
        for b in writes:
            self._add(evs, b.w)
            self._add(evs, b.r)
        if skip_self:
            evs.pop("s_" + e, None)
        if drain_self and self.E[e]["cnt"]:
            evs["s_" + e] = (self.E[e]["sem"], self.E[e]["cnt"])
        self._waits(e, evs)
        E = self.E[e]
        ins = fn(E["h"])
        E["cnt"] += 1
        ins.then_inc(E["sem"], 1)
        key, ev = "s_" + e, (E["sem"], E["cnt"])
        for b in reads:
            b.r[key] = ev
        for b in writes:
            b.w[key] = ev

    def dma(self, q, out_ap, in_ap, dst, srcs=(), **kw):
        evs = {}
        self._add(evs, dst.w)
        self._add(evs, dst.r)
        for b in srcs:
            self._add(evs, b.w)
        self._waits(q, evs)
        if dst.sem is None:
            if self.pool and not dst.w["_persist"]:
                dst.name, dst.sem, dst.cnt = self.pool.pop()
            else:
                dst.sem = self.es.enter_context(self.nc.semaphore("d_" + dst.name))
            self.dma_bufs.append(dst)
        ins = self.E[q]["h"].dma_start(out=out_ap, in_=in_ap, **kw)
        dst.cnt += 16
        ins.then_inc(dst.sem, 16)
        key, ev = "d_" + dst.name, (dst.sem, dst.cnt)
        dst.w[key] = ev
        for b in srcs:
            b.r[key] = ev

    def barrier(self):
        evs = {}
        for n, E in self.E.items():
            if E["cnt"]:
                evs["s_" + n] = (E["sem"], E["cnt"])
        for b in self.dma_bufs:
            evs["d_" + b.name] = (b.sem, b.cnt)
        for n in self.E:
            self._waits(n, evs)
        keep = []
        for b in self.dma_bufs:
            if b.w["_persist"]:
                keep.append(b)
            else:
                self.pool.append((b.name, b.sem, b.cnt))
        self.dma_bufs = keep

    def barrier_known(self):
        pass


class Ctx:
    pass


def _mm_group(S, ps, out_ap, pairs, reads):
    def fn(h):
        ins = None
        n = len(pairs)
        for i, (a, b) in enumerate(pairs):
            ins = h.matmul(out_ap, a, b, start=(i == 0), stop=(i == n - 1))
        return ins
    S.op("pe", fn, reads=reads, writes=[ps])


def _mm1(S, ps, out_ap, a, b, reads, start, stop, drain=False):
    S.op("pe", lambda h: h.matmul(out_ap, a, b, start=start, stop=stop), reads=reads, writes=[ps], skip_self=True, drain_self=drain)


def build(cfg):
    nl = cfg["nl"]
    debug = cfg.get("debug", ())
    stop_after = cfg.get("stop_after", None)
    nc = bass.Bass("TRN2", target_bir_lowering=False)
    es = contextlib.ExitStack()
    K = Ctx()
    K.nc, K.es, K.cfg = nc, es, cfg
    S = Sched(nc, es)
    K.S = S

    def din(name, shape, dt=F32):
        return nc.dram_tensor(name, list(shape), dt, kind="ExternalInput").ap()

    def scratch(name, shape, dt):
        kind = "ExternalOutput" if name in debug else "Internal"
        return nc.dram_tensor(name, list(shape), dt, kind=kind).ap()

    I = {}
    I["xT"] = din("xT", [D, T])
    I["ccT"] = din("ccT", [128, 8, 2])
    I["w_mod"] = din("w_mod", [nl, D, 6 * D])
    I["b_modT"] = din("b_modT", [nl, 128, 48])
    I["g1T"] = din("g1T", [nl, 128, 8])
    I["g2T"] = din("g2T", [nl, 128, 8])
    I["w_in"] = din("w_in", [nl, D, D_IN])
    I["ropeC"] = din("ropeC", [128, L])
    I["ropeS"] = din("ropeS", [128, L])
    I["ident"] = din("ident", [128, 128])
    I["diff_lambda"] = din("diff_lambda", [nl, 256])
    I["sublnT"] = din("sublnT", [nl, 128, 1])
    I["swa_sink"] = din("swa_sink", [nl, 16])
    I["trilo"] = din("trilo", [128, 128])
    I["triup"] = din("triup", [128, 128])
    I["w_branch"] = din("w_branch", [nl, 3, D, D])
    I["w_out"] = din("w_out", [nl, D, D])
    I["fgT"] = din("fgT", [128, 8])
    if "moe" in cfg.get("phases", ()):
        I["router_w"] = din("router_w", [nl, D, NE])
        I["moe_w1"] = din("moe_w1", [nl, NE, D, D])
        I["moe_w3"] = din("moe_w3", [nl, NE, D, D])
        I["moe_w2"] = din("moe_w2", [nl, NE, D, D])
        I["selm"] = din("selm", [16, 16 * 128])
        I["slotidx"] = din("slotidx", [128, 5])
        I["iota512"] = din("iota512", [128, 512])
        I["ustrict"] = din("ustrict", [128, 128])
    if "hy" in cfg.get("phases", ()):
        I["cwT"] = din("cwT", [nl, 128, 24, 3])
        I["cbT"] = din("cbT", [nl, 128, 24])
        I["hbT"] = din("hbT", [nl, 128, 8])
        I["hy_w1"] = din("hy_w1", [nl, 33, 64])
        I["hy_w2"] = din("hy_w2", [nl, 64, 64])
        I["hy_w3"] = din("hy_w3", [nl, 64, 2048])
        I["hy_b1"] = din("hy_b1", [nl, 64, 1])
        I["hy_b2"] = din("hy_b2", [nl, 64, 1])
        I["hy_fr"] = din("hy_fr", [nl, 64, 1])
        I["embT"] = din("embT", [33, L])
        I["embTc"] = din("embTc", [33, LC])
        I["negt"] = din("negt", [128, 32])
        I["negtc"] = din("negtc", [128, 2])
        I["deltas"] = din("deltas", [1, D])
        I["dftC"] = din("dftC", [NF, NF], BF16)
        I["dftS"] = din("dftS", [NF, NF], BF16)
        I["dftCc"] = din("dftCc", [NFC, NFC], BF16)
        I["dftSc"] = din("dftSc", [NFC, NFC], BF16)
        I["wf"] = din("wf", [128, NF // 128])
        I["wfc"] = din("wfc", [128, NFC // 128])
    K.I = I
    K.outT = nc.dram_tensor("outT", [D, L], F32, kind="ExternalOutput").ap()
    K.outT_b = S.buf("outT")

    R = {}
    R["XT"] = (scratch("XT", [D, T], F32), S.buf("XT"))
    R["QdT"] = (scratch("QdT", [D, T], BF16), S.buf("QdT"))
    R["KdT"] = (scratch("KdT", [D, T], BF16), S.buf("KdT"))
    R["Vd"] = (scratch("Vd", [T, D], BF16), S.buf("Vd"))
    R["HyT"] = (scratch("HyT", [3 * D, T], BF16), S.buf("HyT"))
    R["QsT"] = (scratch("QsT", [D, T], BF16), S.buf("QsT"))
    R["KsT"] = (scratch("KsT", [256, T], BF16), S.buf("KsT"))
    R["Vs"] = (scratch("Vs", [T, 256], BF16), S.buf("Vs"))
    R["GT"] = (scratch("GT", [3 * D, T], BF16), S.buf("GT"))
    R["YdT"] = (scratch("YdT", [D, T], BF16), S.buf("YdT"))
    R["YhT"] = (scratch("YhT", [D, T], BF16), S.buf("YhT"))
    R["YsT"] = (scratch("YsT", [D, T], BF16), S.buf("YsT"))
    R["X0T"] = (scratch("X0T", [D, T], BF16), S.buf("X0T"))
    R["UT"] = (scratch("UT", [D, T], BF16), S.buf("UT"))
    R["KF"] = (scratch("KF", [2, NF, D], F32), S.buf("KF"))
    R["KFc"] = (scratch("KFc", [2, NFC, D], F32), S.buf("KFc"))
    R["YF"] = (scratch("YF", [2, NF, D], BF16), S.buf("YF"))
    R["YFc"] = (scratch("YFc", [2, NFC, D], BF16), S.buf("YFc"))
    R["YE"] = (scratch("YE", [NE, CAP + CAPC, D], BF16), S.buf("YE"))
    R["XT1"] = (scratch("XT1", [D, T], F32), S.buf("XT1"))
    R["PT"] = (scratch("PT", [2, 16, T], F32), S.buf("PT"))
    R["LG"] = (scratch("LG", [128, T // 128, NE], F32), S.buf("LG"))
    R["H2d"] = (scratch("H2d", [128, T // 128, D], BF16), S.buf("H2d"))
    K.R = R

    uid = [0]

    def sb(name, shape, dt, stack=es):
        uid[0] += 1
        t = stack.enter_context(nc.sbuf_tensor("sb%d_%s" % (uid[0], name), list(shape), dt))
        return t, S.buf(name)

    def pst(name, shape, dt, stack):
        uid[0] += 1
        t = stack.enter_context(nc.psum_tensor("ps%d_%s" % (uid[0], name), list(shape), dt))
        return t, S.buf(name)
    K.sb, K.pst = sb, pst

    ident_f, ident_f_b = sb("ident_f", [128, 128], F32)
    ident_b, ident_b_b = sb("ident_b", [128, 128], BF16)
    ones_f, ones_f_b = sb("ones_f", [128, 128], F32)
    ones_b, ones_b_b = sb("ones_b", [128, 128], BF16)
    S.dma("sp", ident_f[:], I["ident"][:, :], ident_f_b)
    S.dma("pool", ident_b[:], I["ident"][:, :], ident_b_b)
    S.op("dve", lambda h: h.memset(ones_f[:], 1.0), writes=[ones_f_b])
    S.op("dve", lambda h: h.memset(ones_b[:], 1.0), writes=[ones_b_b])
    K.ident_f, K.ident_b, K.ones_f, K.ones_b = (ident_f, ident_f_b), (ident_b, ident_b_b), (ones_f, ones_f_b), (ones_b, ones_b_b)

    sc, sc_b = sb("sc", [128, 8, 2], F32)
    S.dma("sp", sc[:], I["ccT"][:, :, :], sc_b)
    S.op("act", lambda h: h.activation(out=sc[:], in_=sc[:], func=AF.Silu), reads=[sc_b], writes=[sc_b])
    K.sc = (sc, sc_b)
    modT, modT_b = sb("modT", [128, 48, 2], F32)
    K.modT = (modT, modT_b)
    A1, A1_b = sb("A1", [128, 8, 2], F32)
    A2, A2_b = sb("A2", [128, 8, 2], F32)
    K.A1, K.A2 = (A1, A1_b), (A2, A2_b)

    with contextlib.ExitStack() as ph:
        xc, xc_b = sb("xcp", [128, 8, 512], F32, ph)
        for (t0, tn) in TCH:
            S.dma("sp", xc[:, :, :tn], I["xT"].rearrange("(k p) t -> p k t", p=128)[:, :, t0:t0 + tn], xc_b)
            S.dma("sp", R["XT"][0].rearrange("(k p) t -> p k t", p=128)[:, :, t0:t0 + tn], xc[:, :, :tn], R["XT"][1], [xc_b])
        S.barrier()

    S.persist = False
    phases = cfg.get("phases", ("diff", "swa", "merge"))
    if "swa" not in phases:
        with contextlib.ExitStack() as ph:
            zt, zt_b = sb("zt2", [128, T], BF16, ph)
            S.op("dve", lambda h: h.memset(zt[:], 0.0), writes=[zt_b])
            for k in range(8):
                S.dma("sp", R["YsT"][0][k * 128:(k + 1) * 128, :], zt[:], R["YsT"][1], [zt_b])
            S.barrier()
    if "hy" not in phases:
        with contextlib.ExitStack() as ph:
            zt, zt_b = sb("zt", [128, T], BF16, ph)
            S.op("dve", lambda h: h.memset(zt[:], 0.0), writes=[zt_b])
            for k in range(8):
                S.dma("sp", R["YhT"][0][k * 128:(k + 1) * 128, :], zt[:], R["YhT"][1], [zt_b])
            S.barrier()
    for l in range(nl):
        ctx_out = l < DEPTH - 1
        phase_mod(K, l)
        with contextlib.ExitStack() as ph:
            hT, hT_b = sb("hT", [128, 8, T], BF16, ph)
            phase_norm(K, ph, R["XT"], K.A1, 0, (hT, hT_b))
            if stop_after == "norm1":
                dbg = scratch("dbg_hT", [D, T], BF16)
                S.dma("sp", dbg.rearrange("(k p) t -> p k t", p=128), hT[:], S.buf("dbg"), [hT_b])
                S.barrier()
                break
            phase_inproj(K, ph, l, (hT, hT_b))
            S.barrier()
        if stop_after == "inproj":
            break
        if "diff" in phases:
            phase_diff(K, l, ctx_out)
        if stop_after == "diff":
            break
        if "swa" in phases:
            phase_swa(K, l, ctx_out)
        if "hy" in phases:
            phase_hyena(K, l, ctx_out)
        if stop_after == "hy":
            break
        if stop_after == "swa":
            break
        if "merge" in phases:
            phase_merge(K, l)
        if stop_after == "merge":
            break
        if "moe" in phases:
            phase_moe(K, l, ctx_out)
        if stop_after == "moe":
            break
    if stop_after is None:
        phase_final(K)

    S.barrier()
    es.close()
    return nc


def phase_mod(K, l):
    nc, S, I = K.nc, K.S, K.I
    modT, modT_b = K.modT
    sc, sc_b = K.sc
    with contextlib.ExitStack() as ph:
        wts = [K.sb("wmod%d" % i, [128, 8, 512], F32, ph) for i in range(2)]
        bm, bm_b = K.sb("bmodT", [128, 48], F32, ph)
        g1, g1_b = K.sb("g1T", [128, 8], F32, ph)
        g2, g2_b = K.sb("g2T", [128, 8], F32, ph)
        ps, ps_b = K.pst("ps_mod", [128, 512], F32, ph)
        S.dma("sp", bm[:], I["b_modT"][l], bm_b)
        S.dma("sp", g1[:], I["g1T"][l], g1_b)
        S.dma("sp", g2[:], I["g2T"][l], g2_b)
        for g in range(12):
            wt, wt_b = wts[g % 2]
            S.dma("sp", wt[:], I["w_mod"][l].rearrange("(k p) n -> p k n", p=128)[:, :, g * 512:(g + 1) * 512], wt_b)
            for m in range(4):
                mc = g * 4 + m
                _mm_group(S, ps_b, ps[:, 0:2],
                          [(wt[:, k, m * 128:(m + 1) * 128], sc[:, k, :]) for k in range(8)],
                          [wt_b, sc_b])
                S.op("dve", lambda h, mc=mc: h.tensor_scalar(out=modT[:, mc, :], in0=ps[:, 0:2], scalar1=bm[:, mc:mc + 1],
                                                            scalar2=None, op0=ALU.add),
                     reads=[ps_b, bm_b], writes=[modT_b])
        for (A, A_b), (g, g_b), j in ((K.A1, (g1, g1_b), 1), (K.A2, (g2, g2_b), 4)):
            for r in range(2):
                S.op("dve", lambda h, A=A, g=g, j=j, r=r: h.scalar_tensor_tensor(
                    out=A[:, :, r], in0=modT[:, j * 8:(j + 1) * 8, r], scalar=1.0, in1=g[:], op0=ALU.add, op1=ALU.mult),
                    reads=[modT_b, g_b], writes=[A_b])
        S.barrier()


def phase_norm(K, ph, X, Acoef, shift_j, out, out_f32_cb=None):
    nc, S = K.nc, K.S
    XT, XT_b = X
    A, A_b = Acoef
    modT, modT_b = K.modT
    hT, hT_b = out
    ones_f, ones_f_b = K.ones_f
    with contextlib.ExitStack() as st:
        xs = [K.sb("nx%d" % i, [128, 8, 512], F32, st) for i in range(2)]
        sq, sq_b = K.sb("nsq", [128, 8, 512], F32, st)
        rstd, rstd_b = K.sb("nrstd", [128, 512], F32, st)
        tmp, tmp_b = K.sb("ntmp", [128, 512], F32, st)
        ps, ps_b = K.pst("ps_norm", [128, 512], F32, st)
        for ci, (t0, tn) in enumerate(TCH):
            r = 0 if t0 < L else 1
            x, x_b = xs[ci % 2]
            S.dma("sp", x[:, :, :tn], XT.rearrange("(k p) t -> p k t", p=128)[:, :, t0:t0 + tn], x_b, [XT_b])
            S.op("act", lambda h: h.activation(out=sq[:, :, :tn], in_=x[:, :, :tn], func=AF.Square), reads=[x_b], writes=[sq_b])
            _mm_group(S, ps_b, ps[:, :tn], [(ones_f[:], sq[:, k, :tn]) for k in range(8)], [ones_f_b, sq_b])
            S.op("dve", lambda h: h.tensor_scalar(out=rstd[:, :tn], in0=ps[:, :tn], scalar1=1.0 / D, scalar2=EPS,
                                                  op0=ALU.mult, op1=ALU.add), reads=[ps_b], writes=[rstd_b])
            S.op("act", lambda h: h.activation(out=rstd[:, :tn], in_=rstd[:, :tn], func=AF.Sqrt), reads=[rstd_b], writes=[rstd_b])
            S.op("dve", lambda h: h.reciprocal(out=rstd[:, :tn], in_=rstd[:, :tn]), reads=[rstd_b], writes=[rstd_b])
            for k in range(8):
                S.op("dve", lambda h, k=k: h.tensor_tensor(out=tmp[:, :tn], in0=x[:, k, :tn], in1=rstd[:, :tn], op=ALU.mult),
                     reads=[x_b, rstd_b], writes=[tmp_b])
                S.op("act", lambda h, k=k: h.activation(out=hT[:, k, t0:t0 + tn], in_=tmp[:, :tn], func=AF.Identity,
                                                        scale=A[:, k, r:r + 1], bias=modT[:, shift_j * 8 + k, r:r + 1]),
                     reads=[tmp_b, A_b, modT_b], writes=[hT_b])
                if out_f32_cb is not None:
                    out_f32_cb(ci, k, t0, tn, r, tmp, tmp_b)
        S.barrier()


def _inproj_groups():
    g = []
    g += [("qk", "QdT", 0), ("qk", "QdT", 512), ("qk", "KdT", 0), ("qk", "KdT", 512)]
    g += [("v", "Vd", 0), ("v", "Vd", 512)]
    g += [("plain", "HyT", i * 512) for i in range(6)]
    g += [("qk", "QsT", 0), ("qk", "QsT", 512)]
    g += [("kv", None, 0)]
    g += [("gate", "GT", i * 512) for i in range(6)]
    return g


def phase_inproj(K, ph, l, hTb):
    nc, S, I, R = K.nc, K.S, K.I, K.R
    hT, hT_b = hTb
    with contextlib.ExitStack() as st:
        wts = [K.sb("wi%d" % i, [128, 8, 512], BF16, st) for i in range(2)]
        wsw, wsw_b = K.sb("wsw", [128, 8, 512], BF16, st)
        rC, rC_b = K.sb("ropeC", [128, L], F32, st)
        rS, rS_b = K.sb("ropeS", [128, L], F32, st)
        stg = [K.sb("stg%d" % i, [128, T], BF16, st) for i in range(2)]
        vst = [K.sb("vst%d" % i, [128, 512], BF16, st) for i in range(2)]
        t1, t1_b = K.sb("rt1", [128, 512], F32, st)
        t2, t2_b = K.sb("rt2", [128, 512], F32, st)
        psA = [K.pst("psA%d" % i, [128, 512], F32, st) for i in range(2)]
        psB = [K.pst("psB%d" % i, [128, 512], F32, st) for i in range(2)]
        S.dma("sp", rC[:], I["ropeC"][:, :], rC_b)
        S.dma("sp", rS[:], I["ropeS"][:, :], rS_b)
        win = I["w_in"][l].rearrange("(k p) n -> p k n", p=128)
        cnt = dict(s=0, p=0, v=0)

        def fm_chunk(wt, wt_b, m, kind, dst, row0):
            sg, sg_b = stg[cnt["s"] % 2]
            cnt["s"] += 1
            for (t0, tn) in TCH:
                pa, pa_b = psA[cnt["p"] % 2]
                pb, pb_b = psB[cnt["p"] % 2]
                cnt["p"] += 1
                _mm_group(S, pa_b, pa[:, :tn], [(wt[:, k, m * 128:(m + 1) * 128], hT[:, k, t0:t0 + tn]) for k in range(8)],
                          [wt_b, hT_b])
                if kind == "qk" and t0 < L:
                    _mm_group(S, pb_b, pb[:, :tn], [(wsw[:, k, m * 128:(m + 1) * 128], hT[:, k, t0:t0 + tn]) for k in range(8)],
                              [wsw_b, hT_b])
                    S.op("dve", lambda h: h.tensor_tensor(out=t1[:, :tn], in0=pa[:, :tn], in1=rC[:, t0:t0 + tn], op=ALU.mult),
                         reads=[pa_b, rC_b], writes=[t1_b])
                    S.op("dve", lambda h: h.tensor_tensor(out=t2[:, :tn], in0=pb[:, :tn], in1=rS[:, t0:t0 + tn], op=ALU.mult),
                         reads=[pb_b, rS_b], writes=[t2_b])
                    S.op("pool", lambda h: h.tensor_tensor(out=sg[:, t0:t0 + tn], in0=t1[:, :tn], in1=t2[:, :tn], op=ALU.add),
                         reads=[t1_b, t2_b], writes=[sg_b])
                elif kind == "gate":
                    S.op("act", lambda h: h.activation(out=sg[:, t0:t0 + tn], in_=pa[:, :tn], func=AF.Sigmoid),
                         reads=[pa_b], writes=[sg_b])
                else:
                    S.op("act", lambda h: h.activation(out=sg[:, t0:t0 + tn], in_=pa[:, :tn], func=AF.Copy),
                         reads=[pa_b], writes=[sg_b])
            S.dma("sp", R[dst][0][row0:row0 + 128, :], sg[:], R[dst][1], [sg_b])

        def tm_cols(wt, wt_b, c0, cn, dst, col0):
            for tt in range(T // 128):
                pa, pa_b = psA[cnt["p"] % 2]
                cnt["p"] += 1
                vs, vs_b = vst[cnt["v"] % 2]
                cnt["v"] += 1
                _mm_group(S, pa_b, pa[:, :cn], [(hT[:, k, tt * 128:(tt + 1) * 128], wt[:, k, c0:c0 + cn]) for k in range(8)],
                          [wt_b, hT_b])
                S.op("act", lambda h: h.activation(out=vs[:, :cn], in_=pa[:, :cn], func=AF.Copy), reads=[pa_b], writes=[vs_b])
                S.dma("sp", R[dst][0][tt * 128:(tt + 1) * 128, col0:col0 + cn], vs[:, :cn], R[dst][1], [vs_b])

        def make_swapped(wt, wt_b, ncols):
            src = wt[:, :, :ncols].rearrange("p k (q s f) -> p k q s f", s=2, f=16)
            dstv = wsw[:, :, :ncols].rearrange("p k (q s f) -> p k q s f", s=2, f=16)
            for k in range(8):
                S.op("pool", lambda h, k=k: h.tensor_copy(out=dstv[:, k, :, 0, :], in_=src[:, k, :, 1, :]), reads=[wt_b], writes=[wsw_b])
                S.op("pool", lambda h, k=k: h.tensor_copy(out=dstv[:, k, :, 1, :], in_=src[:, k, :, 0, :]), reads=[wt_b], writes=[wsw_b])

        for gi, (kind, dst, row0) in enumerate(_inproj_groups()):
            wt, wt_b = wts[gi % 2]
            S.dma("pool", wt[:], win[:, :, gi * 512:(gi + 1) * 512], wt_b)
            if kind == "qk":
                make_swapped(wt, wt_b, 512)
                for m in range(4):
                    fm_chunk(wt, wt_b, m, "qk", dst, row0 + m * 128)
            elif kind == "v":
                tm_cols(wt, wt_b, 0, 512, dst, row0)
            elif kind == "kv":
                make_swapped(wt, wt_b, 256)
                for m in range(2):
                    fm_chunk(wt, wt_b, m, "qk", "KsT", m * 128)
                tm_cols(wt, wt_b, 256, 256, "Vs", 0)
            else:
                for m in range(4):
                    fm_chunk(wt, wt_b, m, kind, dst, row0 + m * 128)
        S.barrier()


def _bcast_rows(ap, n):
    return bass.AP(ap.tensor, ap.offset, [[0, 128], [1, n]])


def phase_diff(K, l, ctx_out):
    nc, S, I, R = K.nc, K.S, K.I, K.R
    ones_f, ones_f_b = K.ones_f
    ones_b, ones_b_b = K.ones_b
    lambda_init = 0.8 - 0.6 * math.exp(-0.3 * l)
    with contextlib.ExitStack() as st:
        lp, lp_b = K.sb("lp", [128, 256], F32, st)
        lsc, lsc_b = K.sb("lsc", [128, 4], F32, st)
        gsc, gsc_b = K.sb("gsc", [128, 1], F32, st)
        S.dma("sp", lp[:], _bcast_rows(I["diff_lambda"][l], 256), lp_b)
        S.dma("sp", gsc[:], I["sublnT"][l], gsc_b)
        S.op("dve", lambda h: h.tensor_tensor(out=lp[:, 0:64], in0=lp[:, 0:64], in1=lp[:, 64:128], op=ALU.mult), reads=[lp_b], writes=[lp_b])
        S.op("dve", lambda h: h.tensor_tensor(out=lp[:, 128:192], in0=lp[:, 128:192], in1=lp[:, 192:256], op=ALU.mult), reads=[lp_b], writes=[lp_b])
        S.op("dve", lambda h: h.reduce_sum(out=lsc[:, 0:1], in_=lp[:, 0:64], axis=AX.X), reads=[lp_b], writes=[lsc_b])
        S.op("dve", lambda h: h.reduce_sum(out=lsc[:, 1:2], in_=lp[:, 128:192], axis=AX.X), reads=[lp_b], writes=[lsc_b])
        S.op("act", lambda h: h.activation(out=lsc[:, 0:2], in_=lsc[:, 0:2], func=AF.Exp), reads=[lsc_b], writes=[lsc_b])
        S.op("dve", lambda h: h.scalar_tensor_tensor(out=lsc[:, 2:3], in0=lsc[:, 1:2], scalar=-lambda_init, in1=lsc[:, 0:1],
                                                     op0=ALU.add, op1=ALU.subtract), reads=[lsc_b], writes=[lsc_b])
        S.op("dve", lambda h: h.tensor_scalar(out=gsc[:], in0=gsc[:], scalar1=1.0 - lambda_init, scalar2=None, op0=ALU.mult),
             reads=[gsc_b], writes=[gsc_b])

        QT = [K.sb("dQT%d" % i, [128, T], BF16, st) for i in range(2)]
        KT = [[K.sb("dKT%d%d" % (i, m), [128, T], BF16, st) for m in range(2)] for i in range(2)]
        for i in range(2):
            for m in range(2):
                S.op("dve", lambda h, i=i, m=m: h.memset(KT[i][m][0][(1 - m) * 64:(2 - m) * 64, :], 0.0), writes=[KT[i][m][1]])
        VV = [K.sb("dV%d" % i, [128, T // 128, 128], BF16, st) for i in range(2)]
        pt = [K.sb("dP%d" % i, [128, 2, 512], BF16, st) for i in range(2)]
        psS = [K.pst("dpsS%d" % i, [128, 2, 512], F32, st) for i in range(2)]
        acc = [[K.sb("dacc%d%d" % (m, i), [128, 512], F32, st) for i in range(2)] for m in range(2)]
        acc_hi = [S.buf("dacchi%d" % i) for i in range(2)]
        pt_mb = [[S.buf("dPm%d%d" % (i, m)) for m in range(2)] for i in range(2)]
        ps_mb = [[S.buf("dSm%d%d" % (i, m)) for m in range(2)] for i in range(2)]
        psO = [K.pst("dpsO%d" % m, [128, 512], F32, st) for m in range(2)]
        psZ = [K.pst("dpsZ%d" % m, [128, 512], F32, st) for m in range(2)]
        rz = [K.sb("drz%d" % m, [128, 512], F32, st) for m in range(2)]
        oo = [K.sb("doo%d" % m, [128, 512], F32, st) for m in range(2)]
        of, of_b = K.sb("dof", [128, 512], F32, st)
        sqf, sqf_b = K.sb("dsq", [128, 512], F32, st)
        rs, rs_b = K.sb("drs", [128, 512], F32, st)
        ost = [K.sb("dost%d" % i, [128, 512], BF16, st) for i in range(2)]
        nch = 0
        for hd in range(8):
            q, q_b = QT[hd % 2]
            kz = KT[hd % 2]
            v, v_b = VV[hd % 2]
            S.dma("sp", q[:], R["QdT"][0][hd * 128:(hd + 1) * 128, :], q_b, [R["QdT"][1]])
            for m in range(2):
                S.dma("sp", kz[m][0][m * 64:(m + 1) * 64, :], R["KdT"][0][hd * 128 + m * 64:hd * 128 + (m + 1) * 64, :], kz[m][1], [R["KdT"][1]])
            S.dma("sp", v[:], R["Vd"][0][:, hd * 128:(hd + 1) * 128].rearrange("(t p) e -> p t e", p=128), v_b, [R["Vd"][1]])
            for (q0, qn) in TCH:
                if q0 >= L and not ctx_out:
                    continue
                kts = list(range(T // 128)) if q0 < L else [32, 33]
                cpar = nch % 2

                def qk(i, kt, m, drain=False):
                    ps = psS[i % 2][0]
                    _mm1(S, ps_mb[i % 2][m], ps[:, m, :qn], kz[m][0][:, kt * 128:(kt + 1) * 128], q[:, q0:q0 + qn],
                         [kz[m][1], q_b], True, True)
                    p = pt[i % 2][0]
                    S.op("act", lambda h: h.activation(out=p[:, m, :qn], in_=ps[:, m, :qn], func=AF.Exp, scale=0.125),
                         reads=[ps_mb[i % 2][m]], writes=[pt_mb[i % 2][m]])
                qk(0, kts[0], 0, drain=True)
                qk(0, kts[0], 1, drain=True)
                hq = qn // 2
                for i, kt in enumerate(kts):
                    first, last = i == 0, i == len(kts) - 1
                    p = pt[i % 2][0]
                    for m in range(2):
                        p_b = pt_mb[i % 2][m]
                        if not last:
                            qk(i + 1, kts[i + 1], m, drain=(i == 0 and m == 0))
                        _mm1(S, psO[m][1], psO[m][0][:, :qn], v[:, kt, :], p[:, m, :qn], [v_b, p_b], first, last)
                        a_, a_b = acc[m][cpar]
                        if m == 0:
                            parts = [("dve", 0, qn, a_b)]
                        else:
                            parts = [("pool", 0, hq, a_b), ("dve", hq, qn, acc_hi[cpar])]
                        for eng, c0, c1, ab_ in parts:
                            if first:
                                S.op(eng, lambda h, a_=a_, m=m, c0=c0, c1=c1: h.tensor_copy(out=a_[:, c0:c1], in_=p[:, m, c0:c1]), reads=[p_b], writes=[ab_])
                            else:
                                S.op(eng, lambda h, a_=a_, m=m, c0=c0, c1=c1: h.tensor_tensor(out=a_[:, c0:c1], in0=a_[:, c0:c1], in1=p[:, m, c0:c1], op=ALU.add),
                                     reads=[p_b, ab_], writes=[ab_])
                for m in range(2):
                    a_, a_b = acc[m][cpar]
                    _mm_group(S, psZ[m][1], psZ[m][0][:, :qn], [(ones_f[:], a_[:, :qn])], [ones_f_b, a_b] + ([acc_hi[cpar]] if m == 1 else []))
                for m in range(2):
                    S.op("act", lambda h, m=m: h.activation(out=rz[m][0][:, :qn], in_=psZ[m][0][:, :qn], func=AF.Ln), reads=[psZ[m][1]], writes=[rz[m][1]])
                    S.op("act", lambda h, m=m: h.activation(out=rz[m][0][:, :qn], in_=rz[m][0][:, :qn], func=AF.Exp, scale=-1.0), reads=[rz[m][1]], writes=[rz[m][1]])
                    S.op("dve", lambda h, m=m: h.tensor_tensor(out=oo[m][0][:, :qn], in0=psO[m][0][:, :qn], in1=rz[m][0][:, :qn], op=ALU.mult),
                         reads=[psO[m][1], rz[m][1]], writes=[oo[m][1]])
                S.op("dve", lambda h: h.scalar_tensor_tensor(out=of[:, :qn], in0=oo[1][0][:, :qn], scalar=lsc[:, 2:3], in1=oo[0][0][:, :qn],
                                                              op0=ALU.mult, op1=ALU.add), reads=[oo[0][1], oo[1][1], lsc_b], writes=[of_b])
                S.op("pool", lambda h: h.tensor_tensor(out=sqf[:, :qn], in0=of[:, :qn], in1=of[:, :qn], op=ALU.mult), reads=[of_b], writes=[sqf_b])
                ps, ps_b = psZ[0]
                _mm_group(S, ps_b, ps[:, :qn], [(ones_f[:], sqf[:, :qn])], [ones_f_b, sqf_b])
                S.op("dve", lambda h: h.tensor_scalar(out=rs[:, :qn], in0=ps[:, :qn], scalar1=1.0 / 128, scalar2=EPS, op0=ALU.mult, op1=ALU.add),
                     reads=[ps_b], writes=[rs_b])
                S.op("act", lambda h: h.activation(out=rs[:, :qn], in_=rs[:, :qn], func=AF.Ln), reads=[rs_b], writes=[rs_b])
                S.op("act", lambda h: h.activation(out=rs[:, :qn], in_=rs[:, :qn], func=AF.Exp, scale=-0.5), reads=[rs_b], writes=[rs_b])
                S.op("dve", lambda h: h.tensor_tensor(out=of[:, :qn], in0=of[:, :qn], in1=rs[:, :qn], op=ALU.mult), reads=[of_b, rs_b], writes=[of_b])
                o, o_b = ost[nch % 2]
                nch += 1
                S.op("act", lambda h, o=o: h.activation(out=o[:, :qn], in_=of[:, :qn], func=AF.Copy, scale=gsc[:, 0:1]), reads=[of_b, gsc_b], writes=[o_b])
                S.dma("sp", R["YdT"][0][hd * 128:(hd + 1) * 128, q0:q0 + qn], o[:, :qn], R["YdT"][1], [o_b])
        S.barrier()


def phase_swa(K, l, ctx_out):
    nc, S, I, R = K.nc, K.S, K.I, K.R
    ones_b, ones_b_b = K.ones_b
    with contextlib.ExitStack() as st:
        es_, es_b = K.sb("esink", [128, 16], F32, st)
        S.dma("sp", es_[:], _bcast_rows(I["swa_sink"][l], 16), es_b)
        S.op("act", lambda h: h.activation(out=es_[:], in_=es_[:], func=AF.Exp), reads=[es_b], writes=[es_b])
        mlo, mlo_b = K.sb("mlo", [128, 128], BF16, st)
        mup, mup_b = K.sb("mup", [128, 128], BF16, st)
        S.dma("pool", mlo[:], I["trilo"][:, :], mlo_b)
        S.dma("pool", mup[:], I["triup"][:, :], mup_b)
        QC = [[K.sb("sQ%d%d" % (c, i), [128, T], BF16, st) for i in range(2)] for c in range(2)]
        KT = [[K.sb("sKT%d%d" % (i, hf), [128, T], BF16, st) for hf in range(2)] for i in range(2)]
        for i in range(2):
            for hf in range(2):
                S.op("dve", lambda h, i=i, hf=hf: h.memset(KT[i][hf][0][(1 - hf) * 64:(2 - hf) * 64, :], 0.0), writes=[KT[i][hf][1]])
        VV = [K.sb("sV%d" % i, [128, T // 128, 64], BF16, st) for i in range(2)]
        pt = [K.sb("sP%d" % i, [128, 512], BF16, st) for i in range(2)]
        psS = [K.pst("spsS%d" % i, [128, 512], F32, st) for i in range(2)]
        psO = [K.pst("spsO%d" % i, [128, 512], F32, st) for i in range(2)]
        psZ = [K.pst("spsZ%d" % i, [128, 512], F32, st) for i in range(2)]
        zz, zz_b = K.sb("szz", [64, 512], F32, st)
        ost = [K.sb("sost%d" % i, [64, 512], BF16, st) for i in range(2)]
        nblk = 0
        npt = 0
        for kh in range(4):
            kz = KT[kh % 2]
            v, v_b = VV[kh % 2]
            for half in range(2):
                S.dma("sp", kz[half][0][half * 64:(half + 1) * 64, :], R["KsT"][0][kh * 64:(kh + 1) * 64, :], kz[half][1], [R["KsT"][1]])
            S.dma("sp", v[:], R["Vs"][0][:, kh * 64:(kh + 1) * 64].rearrange("(t p) e -> p t e", p=128), v_b, [R["Vs"][1]])
            qc = []
            for c in range(2):
                qq, qq_b = QC[c][kh % 2]
                S.dma("sp", qq[:], R["QsT"][0][kh * 256 + c * 128: kh * 256 + (c + 1) * 128, :], qq_b, [R["QsT"][1]])
                qc.append((qq, qq_b))
            nqb = T // 128 if ctx_out else L // 128
            dbgl = K.cfg.get("swa_dbg", 9)
            if dbgl < 9:
                nqb = 2 if kh == 0 else 0
            steps = []
            for qb in range(nqb):
                if qb < 32:
                    kts = [(kk, mk) for kk, mk in ((qb - 1, "lo"), (qb, None), (qb + 1, "up")) if 0 <= kk < 32] + [(32, None), (33, None)]
                else:
                    kts = [(32, None), (33, None)]
                for i, (kt, mk) in enumerate(kts):
                    steps.append((qb, kt, mk, i == 0, i == len(kts) - 1))

            def stage_a(si):
                qb, kt, mk, first, last = steps[si]
                ps, ps_b = psS[si % 2]
                p, p_b = pt[si % 2]
                for g in range(4):
                    qq, qq_b = qc[g // 2]
                    kk, kk_b = kz[g % 2]
                    _mm1(S, ps_b, ps[:, g * 128:(g + 1) * 128], kk[:, kt * 128:(kt + 1) * 128], qq[:, qb * 128:(qb + 1) * 128],
                         [kk_b, qq_b], True, True)
                S.op("act", lambda h: h.activation(out=p[:], in_=ps[:], func=AF.Exp, scale=0.125), reads=[ps_b], writes=[p_b])
                if mk is not None:
                    mt, mt_b = (mlo, mlo_b) if mk == "lo" else (mup, mup_b)
                    for g in range(4):
                        S.op("pool", lambda h, g=g, mt=mt: h.tensor_tensor(out=p[:, g * 128:(g + 1) * 128], in0=p[:, g * 128:(g + 1) * 128], in1=mt[:], op=ALU.mult),
                             reads=[p_b, mt_b], writes=[p_b])

            if steps:
                stage_a(0)
            for si, (qb, kt, mk, first, last) in enumerate(steps):
                if si + 1 < len(steps):
                    stage_a(si + 1)
                po, po_b = psO[qb % 2]
                pz, pz_b = psZ[qb % 2]
                p, p_b = pt[si % 2]
                _mm1(S, po_b, po[0:64, :], v[:, kt, :], p[:], [v_b, p_b], first, last)
                _mm1(S, pz_b, pz[0:64, :], ones_b[:, 0:64], p[:], [ones_b_b, p_b], first, last)
                if not last:
                    continue
                for g in range(4):
                    S.op("dve", lambda h, g=g: h.tensor_scalar(out=zz[:, g * 128:(g + 1) * 128], in0=pz[0:64, g * 128:(g + 1) * 128],
                                                               scalar1=es_[0:64, kh * 4 + g:kh * 4 + g + 1], scalar2=None, op0=ALU.add),
                         reads=[pz_b, es_b], writes=[zz_b])
                S.op("dve", lambda h: h.reciprocal(out=zz[:], in_=zz[:]), reads=[zz_b], writes=[zz_b])
                o, o_b = ost[qb % 2]
                S.op("dve", lambda h, o=o: h.tensor_tensor(out=o[:], in0=po[0:64, :], in1=zz[:], op=ALU.mult),
                     reads=[po_b, zz_b], writes=[o_b])
                S.dma("sp", R["YsT"][0][kh * 256:(kh + 1) * 256, qb * 128:(qb + 1) * 128].rearrange("(g d) q -> d g q", g=4),
                      o[:].rearrange("p (g q) -> p g q", g=4), R["YsT"][1], [o_b])
        S.barrier()


def phase_merge(K, l):
    nc, S, I, R = K.nc, K.S, K.I, K.R
    modT, modT_b = K.modT
    with contextlib.ExitStack() as st:
        wb, wb_b = K.sb("wbr", [128, 24, D], BF16, st)
        wo, wo_b = K.sb("wout", [128, 8, D], BF16, st)
        for b in range(3):
            S.dma("pool", wb[:, b * 8:(b + 1) * 8, :], I["w_branch"][l, b].rearrange("(k p) n -> p k n", p=128), wb_b)
        S.dma("pool", wo[:], I["w_out"][l].rearrange("(k p) n -> p k n", p=128), wo_b)
        yb = [K.sb("my%d" % b, [128, 8, 512], BF16, st) for b in range(3)]
        gt, gt_b = K.sb("mgt", [128, 24, 512], BF16, st)
        mg, mg_b = K.sb("mmg", [128, 8, 512], F32, st)
        mgb, mgb_b = K.sb("mmgb", [128, 8, 512], BF16, st)
        tmp = [K.sb("mtmp%d" % i, [128, 512], F32, st) for i in range(2)]
        xx, xx_b = K.sb("mx", [128, 8, 512], F32, st)
        pss = [K.pst("mps%d" % i, [128, 512], F32, st) for i in range(4)]
        npz = 0
        ntmp = 0
        XTv = R["XT"][0].rearrange("(k p) t -> p k t", p=128)
        for (t0, tn) in TCH:
            r = 0 if t0 < L else 1
            for b, nm in enumerate(("YdT", "YhT", "YsT")):
                S.dma("sp", yb[b][0][:, :, :tn], R[nm][0].rearrange("(k p) t -> p k t", p=128)[:, :, t0:t0 + tn], yb[b][1], [R[nm][1]])
            S.dma("sp", gt[:, :, :tn], R["GT"][0].rearrange("(k p) t -> p k t", p=128)[:, :, t0:t0 + tn], gt_b, [R["GT"][1]])
            S.dma("sp", xx[:, :, :tn], XTv[:, :, t0:t0 + tn], xx_b, [R["XT"][1]])
            for m in range(8):
                for b in range(3):
                    ps, ps_b = pss[npz % 4]
                    npz += 1
                    _mm_group(S, ps_b, ps[:, :tn], [(wb[:, b * 8 + k, m * 128:(m + 1) * 128], yb[b][0][:, k, :tn]) for k in range(8)],
                              [wb_b, yb[b][1]])
                    if b == 0:
                        S.op("dve", lambda h, m=m, ps=ps: h.tensor_tensor(out=mg[:, m, :tn], in0=ps[:, :tn], in1=gt[:, m, :tn], op=ALU.mult),
                             reads=[ps_b, gt_b], writes=[mg_b])
                    else:
                        tp, tp_b = tmp[ntmp % 2]
                        ntmp += 1
                        S.op("dve", lambda h, m=m, b=b, ps=ps, tp=tp: h.tensor_tensor(out=tp[:, :tn], in0=ps[:, :tn], in1=gt[:, b * 8 + m, :tn], op=ALU.mult),
                             reads=[ps_b, gt_b], writes=[tp_b])
                        S.op("pool", lambda h, m=m, tp=tp: h.tensor_tensor(out=mg[:, m, :tn], in0=mg[:, m, :tn], in1=tp[:, :tn], op=ALU.add),
                             reads=[tp_b, mg_b], writes=[mg_b])
                S.op("act", lambda h, m=m: h.activation(out=mgb[:, m, :tn], in_=mg[:, m, :tn], func=AF.Copy), reads=[mg_b], writes=[mgb_b])
            for m in range(8):
                ps, ps_b = pss[npz % 4]
                npz += 1
                _mm_group(S, ps_b, ps[:, :tn], [(wo[:, k, m * 128:(m + 1) * 128], mgb[:, k, :tn]) for k in range(8)], [wo_b, mgb_b])
                S.op("dve", lambda h, m=m, ps=ps: h.scalar_tensor_tensor(out=xx[:, m, :tn], in0=ps[:, :tn], scalar=modT[:, 16 + m, r:r + 1],
                                                                         in1=xx[:, m, :tn], op0=ALU.mult, op1=ALU.add),
                     reads=[ps_b, modT_b, xx_b], writes=[xx_b])
            S.dma("sp", XTv[:, :, t0:t0 + tn], xx[:, :, :tn], R["XT"][1], [xx_b])
        S.barrier()


PI = math.pi


def _hy_filter(K, l, Lx, embT_ap, negt_ap, Cm, Sm, wf_ap, KFs):
    nc, S, I, R = K.nc, K.S, K.I, K.R
    ones_b, ones_b_b = K.ones_b
    nlt = Lx // 128
    nft = (Lx + 128) // 128
    nch = max(1, Lx // 512)
    cw = min(512, Lx)
    with contextlib.ExitStack() as st:
        w1, w1_b = K.sb("fw1", [33, 64], F32, st)
        w2, w2_b = K.sb("fw2", [64, 64], F32, st)
        w3, w3_b = K.sb("fw3", [64, 2048], F32, st)
        b1, b1_b = K.sb("fb1", [64, 1], F32, st)
        b2, b2_b = K.sb("fb2", [64, 1], F32, st)
        fr, fr_b = K.sb("ffr", [64, 1], F32, st)
        emb, emb_b = K.sb("femb", [33, Lx], F32, st)
        ngt, ngt_b = K.sb("fngt", [128, nlt], F32, st)
        dl, dl_b = K.sb("fdl", [128, D], F32, st)
        wf, wf_b = K.sb("fwf", [128, nft], F32, st)
        z1, z1_b = K.sb("fz1", [64, Lx], F32, st)
        z2, z2_b = K.sb("fz2", [64, Lx], F32, st)
        rn, rn_b = K.sb("frn", [128, D], F32, st)
        dec, dec_b = K.sb("fdec", [128, D], F32, st)
        hd = [K.sb("fhd%d" % i, [128, D], F32, st) for i in range(2)]
        ab, ab_b = K.sb("fab", [128, 512], BF16, st)
        gsel, gsel_b = K.sb("fgsel", [64, 512], F32, st)
        AT, AT_b = K.sb("fAT", [128, nlt, D], BF16, st)
        for t_, src in ((w1, I["hy_w1"][l]), (w2, I["hy_w2"][l]), (w3, I["hy_w3"][l]), (b1, I["hy_b1"][l]), (b2, I["hy_b2"][l]),
                        (fr, I["hy_fr"][l]), (emb, embT_ap), (ngt, negt_ap), (wf, wf_ap)):
            pass
        S.dma("sp", w1[:], I["hy_w1"][l], w1_b)
        S.dma("sp", w2[:], I["hy_w2"][l], w2_b)
        S.dma("sp", w3[:], I["hy_w3"][l], w3_b)
        S.dma("sp", b1[:], I["hy_b1"][l], b1_b)
        S.dma("sp", b2[:], I["hy_b2"][l], b2_b)
        S.dma("sp", fr[:], I["hy_fr"][l], fr_b)
        S.dma("sp", emb[:], embT_ap, emb_b)
        S.dma("sp", ngt[:], negt_ap, ngt_b)
        S.dma("sp", wf[:], wf_ap, wf_b)
        S.dma("sp", dl[:], _bcast_rows(I["deltas"][0], D), dl_b)
        psm = [K.pst("fps%d" % i, [128, 512], F32, st) for i in range(4)]
        psN = [K.pst("fpsN%d" % i, [128, 512], F32, st) for i in range(2)]
        for (wm, wm_b, bb, bb_b, src, src_b, dst, dst_b) in ((w1, w1_b, b1, b1_b, emb, emb_b, z1, z1_b), (w2, w2_b, b2, b2_b, z1, z1_b, z2, z2_b)):
            for ci in range(nch):
                ps, ps_b = psm[ci % 4]
                sl = slice(ci * cw, (ci + 1) * cw)
                _mm_group(S, ps_b, ps[0:64, :cw], [(wm[:], src[:, sl])], [wm_b, src_b])
                S.op("dve", lambda h, ps=ps, sl=sl, bb=bb, dst=dst: h.tensor_scalar(out=dst[:, sl], in0=ps[0:64, :cw], scalar1=bb[:, 0:1], scalar2=fr[:, 0:1],
                                                                                 op0=ALU.add, op1=ALU.mult), reads=[ps_b, bb_b, fr_b], writes=[dst_b])
                for _ in range(2):
                    for cop, sh in ((ALU.is_gt, -2.0 * PI), (ALU.is_lt, 2.0 * PI)):
                        thr = PI if cop == ALU.is_gt else -PI
                        S.op("dve", lambda h, sl=sl, dst=dst, cop=cop, thr=thr: h.tensor_scalar(out=gsel[:, :cw], in0=dst[:, sl], scalar1=thr, scalar2=None, op0=cop),
                             reads=[dst_b], writes=[gsel_b])
                        S.op("dve", lambda h, sl=sl, dst=dst, sh=sh: h.scalar_tensor_tensor(out=dst[:, sl], in0=gsel[:, :cw], scalar=sh, in1=dst[:, sl],
                                                                                          op0=ALU.mult, op1=ALU.add), reads=[gsel_b, dst_b], writes=[dst_b])
                S.op("act", lambda h, sl=sl, dst=dst: h.activation(out=dst[:, sl], in_=dst[:, sl], func=AF.Sin), reads=[dst_b], writes=[dst_b])

        def htile(lt, cb):
            S.op("act", lambda h: h.activation(out=dec[:], in_=dl[:], func=AF.Exp, scale=ngt[:, lt:lt + 1]), reads=[dl_b, ngt_b], writes=[dec_b])
            for j in range(4):
                ps, ps_b = psm[j]
                _mm_group(S, ps_b, ps[:], [(z2[:, lt * 128:(lt + 1) * 128], w3[:, j * 512:(j + 1) * 512])], [z2_b, w3_b])
            cb(lt)

        for lt in range(nlt):
            def cb1(lt):
                for j in range(4):
                    ps, ps_b = psm[j]
                    half = j % 2
                    tf, tf_b = hd[j // 2]
                    S.op("dve", lambda h, ps=ps, half=half, tf=tf: h.tensor_tensor(out=tf[:, half * 512:(half + 1) * 512], in0=ps[:],
                                                                                   in1=dec[:, half * 512:(half + 1) * 512], op=ALU.mult),
                         reads=[ps_b, dec_b], writes=[tf_b])
                    S.op("act", lambda h, half=half, tf=tf: h.activation(out=ab[:], in_=tf[:, half * 512:(half + 1) * 512], func=AF.Abs),
                         reads=[tf_b], writes=[ab_b])
                    first = (lt == 0 and j < 2)
                    last = (lt == nlt - 1 and j >= 2)
                    _mm1(S, psN[half][1], psN[half][0][:], ones_b[:], ab[:], [ones_b_b, ab_b], first, last)
            htile(lt, cb1)
        for half in range(2):
            S.op("dve", lambda h, half=half: h.tensor_scalar(out=rn[:, half * 512:(half + 1) * 512], in0=psN[half][0][:], scalar1=EPS, scalar2=None, op0=ALU.add),
                 reads=[psN[half][1]], writes=[rn_b])
        S.op("dve", lambda h: h.reciprocal(out=rn[:], in_=rn[:]), reads=[rn_b], writes=[rn_b])
        for plane, op2 in (("C", ALU.add), ("S", ALU.subtract)):
            for lt in range(nlt):
                def cb2(lt):
                    for j in range(4):
                        ps, ps_b = psm[j]
                        half, dr = j % 2, j // 2
                        S.op("dve", lambda h, ps=ps, half=half, dr=dr: h.tensor_tensor(out=hd[dr][0][:, half * 512:(half + 1) * 512], in0=ps[:],
                                                                                      in1=dec[:, half * 512:(half + 1) * 512], op=ALU.mult),
                             reads=[ps_b, dec_b], writes=[hd[dr][1]])
                    if lt == 0:
                        S.op("dve", lambda h: h.memset(hd[1][0][0:1, :], 0.0), writes=[hd[1][1]])
                    S.op("pool", lambda h: h.tensor_tensor(out=hd[0][0][:], in0=hd[0][0][:], in1=hd[1][0][:], op=op2), reads=[hd[0][1], hd[1][1]], writes=[hd[0][1]])
                    S.op("pool", lambda h: h.tensor_tensor(out=AT[:, lt, :], in0=hd[0][0][:], in1=rn[:], op=ALU.mult), reads=[hd[0][1], rn_b], writes=[AT_b])
                htile(lt, cb2)
            pi = 0 if plane == "C" else 1

            def evac(ft, pc, ps_, pi=pi):
                src = pc if pi == 0 else ps_
                for hh in range(2):
                    o, o_b = hd[hh]
                    S.op("dve", lambda h, hh=hh, o=o: h.tensor_scalar(out=o[:, 0:512], in0=src[hh][0][:], scalar1=wf[:, ft:ft + 1], scalar2=None, op0=ALU.mult),
                         reads=[src[hh][1], wf_b], writes=[o_b])
                    S.dma("sp", KFs[0][pi, ft * 128:(ft + 1) * 128, hh * 512:(hh + 1) * 512], o[:, 0:512], KFs[1], [o_b])
            _fwd_dft(K, st, Cm, Sm, nlt, nft, AT, AT_b, 0, (plane,), evac, psm)
        S.barrier()


def _fwd_dft(K, st, Cm, Sm, ntt, nft, rhs, rhs_b, tt0, planes, evac, pspool):
    S = K.S
    with contextlib.ExitStack() as s2:
        blk = {p: [K.sb("dblk%s%d" % (p, i), [128, ntt, 128], BF16, s2) for i in range(2)] for p in planes}
        for ft in range(nft):
            pss = {}
            for pi, p in enumerate(("C", "S")):
                if p not in planes:
                    pss[p] = None
                    continue
                M = Cm if p == "C" else Sm
                b, b_b = blk[p][ft % 2]
                S.dma("sp", b[:], M.rearrange("(t p) f -> p t f", p=128)[:, 0:ntt, ft * 128:(ft + 1) * 128], b_b)
                pss[p] = [pspool[pi * 2 + hh] for hh in range(2)]
                for hh in range(2):
                    ps, ps_b = pss[p][hh]
                    _mm_group(S, ps_b, ps[:], [(b[:, t, :], rhs[:, tt0 + t, hh * 512:(hh + 1) * 512]) for t in range(ntt)], [b_b, rhs_b])
            evac(ft, pss["C"], pss["S"])


def phase_hyena(K, l, ctx_out):
    nc, S, I, R = K.nc, K.S, K.I, K.R
    ident_b, ident_b_b = K.ident_b
    _hy_filter(K, l, L, I["embT"][:, :], I["negt"][:, :], I["dftC"], I["dftS"], I["wf"][:, :], R["KF"])
    if ctx_out:
        _hy_filter(K, l, LC, I["embTc"][:, :], I["negtc"][:, :], I["dftCc"], I["dftSc"], I["wfc"][:, :], R["KFc"])
    segs = [(0, L)] + ([(L, LC)] if ctx_out else [])
    TT = T if ctx_out else L
    with contextlib.ExitStack() as st:
        utok, utok_b = K.sb("hutok", [128, T // 128, D], BF16, st)
        with contextlib.ExitStack() as s2:
            cw, cw_b = K.sb("hcw", [128, 24, 3], F32, s2)
            cb, cb_b = K.sb("hcb", [128, 24], F32, s2)
            S.dma("sp", cw[:], I["cwT"][l], cw_b)
            S.dma("sp", cb[:], I["cbT"][l], cb_b)
            xin = [K.sb("hxin%d" % i, [128, T], BF16, s2) for i in range(3)]
            yc = [K.sb("hyc%d" % i, [128, T], F32, s2) for i in range(3)]
            x0b, x0b_b = K.sb("hx0b", [128, T], BF16, s2)
            ub, ub_b = K.sb("hub", [128, T], BF16, s2)
            ptr = [K.pst("hptr%d" % i, [128, 1024], BF16, s2) for i in range(2)]
            ntr = 0
            for c in range(8):
                for s_ in range(3):
                    j = s_ * 8 + c
                    xi, xi_b = xin[s_]
                    y, y_b = yc[s_]
                    S.dma("sp", xi[:, :TT], R["HyT"][0][j * 128:(j + 1) * 128, 0:TT], xi_b, [R["HyT"][1]])
                    for (a0, n) in segs:
                        S.op("dve", lambda h, j=j, xi=xi, y=y: h.tensor_scalar(out=y[:, a0:a0 + n], in0=xi[:, a0:a0 + n], scalar1=cw[:, j, 1:2], scalar2=cb[:, j:j + 1],
                                                                            op0=ALU.mult, op1=ALU.add), reads=[xi_b, cw_b, cb_b], writes=[y_b])
                        S.op("dve", lambda h, j=j, xi=xi, y=y: h.scalar_tensor_tensor(out=y[:, a0 + 1:a0 + n], in0=xi[:, a0:a0 + n - 1], scalar=cw[:, j, 0:1],
                                                                                   in1=y[:, a0 + 1:a0 + n], op0=ALU.mult, op1=ALU.add),
                             reads=[xi_b, cw_b, y_b], writes=[y_b])
                        S.op("dve", lambda h, j=j, xi=xi, y=y: h.scalar_tensor_tensor(out=y[:, a0:a0 + n - 1], in0=xi[:, a0 + 1:a0 + n], scalar=cw[:, j, 2:3],
                                                                                   in1=y[:, a0:a0 + n - 1], op0=ALU.mult, op1=ALU.add),
                             reads=[xi_b, cw_b, y_b], writes=[y_b])
                S.op("act", lambda h: h.activation(out=x0b[:, :TT], in_=yc[0][0][:, :TT], func=AF.Copy), reads=[yc[0][1]], writes=[x0b_b])
                S.op("pool", lambda h: h.tensor_tensor(out=ub[:, :TT], in0=yc[1][0][:, :TT], in1=yc[2][0][:, :TT], op=ALU.mult),
                     reads=[yc[1][1], yc[2][1]], writes=[ub_b])
                S.dma("sp", R["X0T"][0][c * 128:(c + 1) * 128, 0:TT], x0b[:, :TT], R["X0T"][1], [x0b_b])
                S.dma("sp", R["UT"][0][c * 128:(c + 1) * 128, 0:TT], ub[:, :TT], R["UT"][1], [ub_b])
                for t8 in range(0, TT // 128, 8):
                    n8 = min(8, TT // 128 - t8)
                    pt_, pt_b = ptr[ntr % 2]
                    ntr += 1

                    def trf(h, t8=t8, n8=n8, pt_=pt_):
                        ins = None
                        for q in range(n8):
                            ins = h.transpose(pt_[:, q * 128:(q + 1) * 128], ub[:, (t8 + q) * 128:(t8 + q + 1) * 128], ident_b[:])
                        return ins
                    S.op("pe", trf, reads=[ub_b, ident_b_b], writes=[pt_b])
                    S.op("act", lambda h, t8=t8, n8=n8, pt_=pt_, c=c: h.activation(out=utok[:, t8:t8 + n8, c * 128:(c + 1) * 128],
                                                                                   in_=pt_[:, :n8 * 128].rearrange("p (q f) -> p q f", f=128), func=AF.Copy),
                         reads=[pt_b], writes=[utok_b])
            S.barrier()
        for si, (a0, n) in enumerate(segs):
            Cm, Sm = (I["dftC"], I["dftS"]) if si == 0 else (I["dftCc"], I["dftSc"])
            KFs = R["KF"] if si == 0 else R["KFc"]
            YFs = R["YF"] if si == 0 else R["YFc"]
            ntt = n // 128
            nft = (n + 128) // 128
            with contextlib.ExitStack() as s2:
                kf = [K.sb("hkf%d" % i, [128, 2, D], F32, s2) for i in range(2)]
                tq = [K.sb("htq%d" % i, [128, 512], F32, s2) for i in range(4)]
                yo = [K.sb("hyo%d" % i, [128, 2, D], BF16, s2) for i in range(2)]
                pspool = [K.pst("hps%d" % i, [128, 512], F32, s2) for i in range(4)]

                def evac(ft, pc, ps_, KFs=KFs, YFs=YFs):
                    k_, k_b = kf[ft % 2]
                    o, o_b = yo[ft % 2]
                    S.dma("sp", k_[:], KFs[0][:, ft * 128:(ft + 1) * 128, :].rearrange("a p c -> p a c"), k_b, [KFs[1]])
                    for hh in range(2):
                        cs = slice(hh * 512, (hh + 1) * 512)
                        ur, ur_b = pc[hh]
                        ui, ui_b = ps_[hh]
                        S.op("dve", lambda h: h.tensor_tensor(out=tq[0][0][:], in0=ur[:], in1=k_[:, 0, cs], op=ALU.mult), reads=[ur_b, k_b], writes=[tq[0][1]])
                        S.op("dve", lambda h: h.tensor_tensor(out=tq[1][0][:], in0=ui[:], in1=k_[:, 1, cs], op=ALU.mult), reads=[ui_b, k_b], writes=[tq[1][1]])
                        S.op("pool", lambda h: h.tensor_tensor(out=o[:, 0, cs], in0=tq[0][0][:], in1=tq[1][0][:], op=ALU.subtract),
                             reads=[tq[0][1], tq[1][1]], writes=[o_b])
                        S.op("dve", lambda h: h.tensor_tensor(out=tq[2][0][:], in0=ur[:], in1=k_[:, 1, cs], op=ALU.mult), reads=[ur_b, k_b], writes=[tq[2][1]])
                        S.op("dve", lambda h: h.tensor_tensor(out=tq[3][0][:], in0=ui[:], in1=k_[:, 0, cs], op=ALU.mult), reads=[ui_b, k_b], writes=[tq[3][1]])
                        S.op("pool", lambda h: h.tensor_tensor(out=o[:, 1, cs], in0=tq[2][0][:], in1=tq[3][0][:], op=ALU.add),
                             reads=[tq[2][1], tq[3][1]], writes=[o_b])
                    S.dma("sp", YFs[0][:, ft * 128:(ft + 1) * 128, :].rearrange("a p c -> p a c"), o[:], YFs[1], [o_b])
                _fwd_dft(K, s2, Cm, Sm, ntt, nft, utok, utok_b, a0 // 128, ("C", "S"), evac, pspool)
                S.barrier()
    for si, (a0, n) in enumerate(segs):
        Cm, Sm = (I["dftC"], I["dftS"]) if si == 0 else (I["dftCc"], I["dftSc"])
        YFs = R["YF"] if si == 0 else R["YFc"]
        nft = (n + 128) // 128
        with contextlib.ExitStack() as s2:
            hb, hb_b = K.sb("ihb", [128, 8], F32, s2)
            S.dma("sp", hb[:], I["hbT"][l], hb_b)
            Yh = [K.sb("iY%d" % i, [128, nft, 512], BF16, s2) for i in range(2)]
            Cr = [K.sb("iC%d" % i, [128, nft, 256], BF16, s2) for i in range(2)]
            Sr = [K.sb("iS%d" % i, [128, nft, 256], BF16, s2) for i in range(2)]
            x0c = [K.sb("ix0%d" % i, [128, 4, 256], BF16, s2) for i in range(2)]
            uc = [K.sb("iu%d" % i, [128, 4, 256], BF16, s2) for i in range(2)]
            tmp, tmp_b = K.sb("itmp", [128, 256], F32, s2)
            og = [K.sb("iog%d" % i, [128, 4, 256], BF16, s2) for i in range(2)]
            psI = [K.pst("ips%d" % i, [128, 512], F32, s2) for i in range(2)]
            nk = 0
            npi = 0
            for half in range(2):
                for pl in range(2):
                    S.dma("sp", Yh[pl][0][:], YFs[0][pl, 0:nft * 128, half * 512:(half + 1) * 512].rearrange("(f p) c -> p f c", p=128), Yh[pl][1], [YFs[1]])
                for t0 in range(0, n, 256):
                    cr, cr_b = Cr[nk % 2]
                    sr, sr_b = Sr[nk % 2]
                    xx, xx_b = x0c[nk % 2]
                    uu, uu_b = uc[nk % 2]
                    oo, oo_b = og[nk % 2]
                    nk += 1
                    S.dma("sp", cr[:], Cm.rearrange("(f p) t -> p f t", p=128)[:, 0:nft, t0:t0 + 256], cr_b)
                    S.dma("sp", sr[:], Sm.rearrange("(f p) t -> p f t", p=128)[:, 0:nft, t0:t0 + 256], sr_b)
                    rows = slice(half * 512, (half + 1) * 512)
                    S.dma("sp", xx[:], R["X0T"][0][rows, a0 + t0:a0 + t0 + 256].rearrange("(c p) t -> p c t", p=128), xx_b, [R["X0T"][1]])
                    S.dma("sp", uu[:], R["UT"][0][rows, a0 + t0:a0 + t0 + 256].rearrange("(c p) t -> p c t", p=128), uu_b, [R["UT"][1]])
                    for ci in range(4):
                        ps, ps_b = psI[npi % 2]
                        npi += 1
                        pairs = [(Yh[0][0][:, f, ci * 128:(ci + 1) * 128], cr[:, f, :]) for f in range(nft)] + \
                                [(Yh[1][0][:, f, ci * 128:(ci + 1) * 128], sr[:, f, :]) for f in range(nft)]
                        _mm_group(S, ps_b, ps[:, 0:256], pairs, [Yh[0][1], Yh[1][1], cr_b, sr_b])
                        cidx = half * 4 + ci
                        S.op("dve", lambda h, ci=ci, cidx=cidx, ps=ps, uu=uu: h.scalar_tensor_tensor(out=tmp[:], in0=uu[:, ci, :], scalar=hb[:, cidx:cidx + 1],
                                                                                                    in1=ps[:, 0:256], op0=ALU.mult, op1=ALU.add),
                             reads=[uu_b, hb_b, ps_b], writes=[tmp_b])
                        S.op("pool", lambda h, ci=ci, oo=oo, xx=xx: h.tensor_tensor(out=oo[:, ci, :], in0=tmp[:], in1=xx[:, ci, :], op=ALU.mult),
                             reads=[tmp_b, xx_b], writes=[oo_b])
                    S.dma("sp", R["YhT"][0][rows, a0 + t0:a0 + t0 + 256].rearrange("(c p) t -> p c t", p=128), oo[:], R["YhT"][1], [oo_b])
            S.barrier()
    if not ctx_out:
        pass


def _bc_mid(ap2d, n):
    a = ap2d.ap
    return bass.AP(ap2d.tensor, ap2d.offset, [list(a[0]), [0, n], list(a[1])])


def phase_moe(K, l, ctx_out):
    nc, S, I, R = K.nc, K.S, K.I, K.R
    modT, modT_b = K.modT
    A2, A2_b = K.A2
    ones_f, ones_f_b = K.ones_f
    ones_b, ones_b_b = K.ones_b
    ident_f, ident_f_b = K.ident_f
    ident_b, ident_b_b = K.ident_b
    debug = K.cfg.get("debug", ())
    XTv = R["XT"][0].rearrange("(k p) t -> p k t", p=128)
    chunks = TCH if ctx_out else TCH[:8]
    ntt = 34 if ctx_out else 32
    groups = [(0, 32, CAP)] + ([(32, 34, CAPC)] if ctx_out else [])
    with contextlib.ExitStack() as st:
        lg, lg_b = K.sb("lg", [128, 34, NE], F32, st)
        aff, aff_b = K.sb("aff", [128, 34, NE], F32, st)
        psel, psel_b = K.sb("psel", [128, 34, NE], F32, st)
        wr, wr_b = K.sb("wr", [128, 8, NE], F32, st)
        S.dma("sp", wr[:], I["router_w"][l].rearrange("(k p) e -> p k e", p=128), wr_b)
        sH = contextlib.ExitStack()
        H2, H2_b = K.sb("H2", [128, 34, D], BF16, sH)
        if ctx_out is False:
            S.op("dve", lambda h: h.memset(lg[:, 32:34, :], 0.0), writes=[lg_b])
        with contextlib.ExitStack() as s2:
            xs = [K.sb("qx%d" % i, [128, 8, 512], F32, s2) for i in range(2)]
            sq, sq_b = K.sb("qsq", [128, 8, 512], F32, s2)
            rstd, rstd_b = K.sb("qrstd", [128, 512], F32, s2)
            tmp, tmp_b = K.sb("qtmp", [128, 512], F32, s2)
            h2f, h2f_b = K.sb("qh2f", [128, 8, 512], F32, s2)
            hTc, hTc_b = K.sb("qhTc", [128, 8, 512], BF16, s2)
            ps, ps_b = K.pst("qps", [128, 512], F32, s2)
            psl, psl_b = K.pst("qpsl", [128, 64], F32, s2)
            pstr = [K.pst("qpst%d" % i, [128, 1024], BF16, s2) for i in range(2)]
            ntr = 0
            for ci, (t0, tn) in enumerate(chunks):
                r = 0 if t0 < L else 1
                x, x_b = xs[ci % 2]
                S.dma("sp", x[:, :, :tn], XTv[:, :, t0:t0 + tn], x_b, [R["XT"][1]])
                if "XT1" in debug:
                    S.dma("sp", R["XT1"][0].rearrange("(k p) t -> p k t", p=128)[:, :, t0:t0 + tn], x[:, :, :tn], R["XT1"][1], [x_b])
                S.op("act", lambda h: h.activation(out=sq[:, :, :tn], in_=x[:, :, :tn], func=AF.Square), reads=[x_b], writes=[sq_b])
                _mm_group(S, ps_b, ps[:, :tn], [(ones_f[:], sq[:, k, :tn]) for k in range(8)], [ones_f_b, sq_b])
                S.op("dve", lambda h: h.tensor_scalar(out=rstd[:, :tn], in0=ps[:, :tn], scalar1=1.0 / D, scalar2=EPS,
                                                      op0=ALU.mult, op1=ALU.add), reads=[ps_b], writes=[rstd_b])
                S.op("act", lambda h: h.activation(out=rstd[:, :tn], in_=rstd[:, :tn], func=AF.Sqrt), reads=[rstd_b], writes=[rstd_b])
                S.op("dve", lambda h: h.reciprocal(out=rstd[:, :tn], in_=rstd[:, :tn]), reads=[rstd_b], writes=[rstd_b])
                for k in range(8):
                    S.op("dve", lambda h, k=k: h.tensor_tensor(out=tmp[:, :tn], in0=x[:, k, :tn], in1=rstd[:, :tn], op=ALU.mult),
                         reads=[x_b, rstd_b], writes=[tmp_b])
                    S.op("act", lambda h, k=k: h.activation(out=h2f[:, k, :tn], in_=tmp[:, :tn], func=AF.Identity,
                                                            scale=A2[:, k, r:r + 1], bias=modT[:, 24 + k, r:r + 1]),
                         reads=[tmp_b, A2_b, modT_b], writes=[h2f_b])
                    S.op("pool", lambda h, k=k: h.tensor_copy(out=hTc[:, k, :tn], in_=h2f[:, k, :tn]), reads=[h2f_b], writes=[hTc_b])
                nj = tn // 128
                for j in range(nj):
                    tt = t0 // 128 + j
                    _mm_group(S, psl_b, psl[:, j * 16:(j + 1) * 16],
                              [(h2f[:, k, j * 128:(j + 1) * 128], wr[:, k, :]) for k in range(8)], [h2f_b, wr_b])
                    pt_, pt_b = pstr[ntr % 2]
                    ntr += 1

                    def trf(h, j=j, pt_=pt_):
                        ins = None
                        for k in range(8):
                            ins = h.transpose(pt_[:, k * 128:(k + 1) * 128], hTc[:, k, j * 128:(j + 1) * 128], ident_b[:])
                        return ins
                    S.op("pe", trf, reads=[hTc_b, ident_b_b], writes=[pt_b])
                    S.op("act", lambda h, tt=tt, pt_=pt_: h.activation(out=H2[:, tt, :], in_=pt_[:], func=AF.Copy), reads=[pt_b], writes=[H2_b])
                S.op("dve", lambda h: h.tensor_copy(out=lg[:, t0 // 128:t0 // 128 + nj, :].rearrange("p t e -> p (t e)"),
                                                    in_=psl[:, :nj * 16]), reads=[psl_b], writes=[lg_b])
            S.barrier()
        if "LG" in debug:
            S.dma("sp", R["LG"][0], lg[:], R["LG"][1], [lg_b])
            S.dma("sp", R["H2d"][0], H2[:], R["H2d"][1], [H2_b])
        with contextlib.ExitStack() as s2:
            se, se_b = K.sb("rse", [128, 34], F32, s2)
            lo, lo_b = K.sb("rlo", [128, NE], F32, s2)
            mid, mid_b = K.sb("rmid", [128, NE], F32, s2)
            cmpt, cmp_b = K.sb("rcmp", [128, 32, NE], F32, s2)
            cntp, cntp_b = K.sb("rcntp", [128, NE], F32, s2)
            ge, ge_b = K.sb("rge", [128, NE], F32, s2)
            mk, mk_b = K.sb("rmk", [128, 34, NE], F32, s2)
            mkb, mkb_b = K.sb("rmkb", [128, 34, NE], BF16, s2)
            tot, tot_b = K.sb("rtot", [128, 34, NE], F32, s2)
            base, base_b = K.sb("rbase", [128, 34, NE], F32, s2)
            posT, posT_b = K.sb("posT", [16, T], F32, s2)
            affT, affT_b = K.sb("affT", [16, T], F32, s2)
            us, us_b = K.sb("rus", [128, 128], BF16, s2)
            S.dma("pool", us[:], I["ustrict"][:, :], us_b)
            psc, psc_b = K.pst("rpsc", [128, 512], F32, s2)
            psw, psw_b = K.pst("rpsw", [128, 512], F32, s2)
            pst_, pst_b = K.pst("rpst", [128, 512], F32, s2)
            S.op("act", lambda h: h.activation(out=aff[:].rearrange("p t e -> p (t e)"), in_=lg[:].rearrange("p t e -> p (t e)"), func=AF.Exp),
                 reads=[lg_b], writes=[aff_b])
            S.op("dve", lambda h: h.reduce_sum(out=se[:], in_=aff[:], axis=AX.X), reads=[aff_b], writes=[se_b])
            S.op("dve", lambda h: h.reciprocal(out=se[:], in_=se[:]), reads=[se_b], writes=[se_b])
            for tt in range(34):
                S.op("dve", lambda h, tt=tt: h.tensor_scalar(out=aff[:, tt, :], in0=aff[:, tt, :], scalar1=se[:, tt:tt + 1], scalar2=None, op0=ALU.mult),
                     reads=[aff_b, se_b], writes=[aff_b])
            S.op("dve", lambda h: h.memset(mk[:], 0.0), writes=[mk_b])
            S.op("dve", lambda h: h.memset(base[:], 0.0), writes=[base_b])
            for (ta, tb, cap) in groups:
                nt = tb - ta
                S.op("dve", lambda h: h.memset(lo[:], 0.0), writes=[lo_b])
                for it in range(30):
                    w = 0.5 ** (it + 1)
                    S.op("dve", lambda h: h.tensor_scalar(out=mid[:], in0=lo[:], scalar1=w, scalar2=None, op0=ALU.add), reads=[lo_b], writes=[mid_b])
                    S.op("dve", lambda h: h.tensor_tensor(out=cmpt[:, :nt, :], in0=aff[:, ta:tb, :], in1=_bc_mid(mid[:], nt), op=ALU.is_ge),
                         reads=[aff_b, mid_b], writes=[cmp_b])
                    S.op("dve", lambda h: h.reduce_sum(out=cntp[:], in_=cmpt[:, :nt, :].rearrange("p t e -> p e t"), axis=AX.X),
                         reads=[cmp_b], writes=[cntp_b])
                    _mm_group(S, psc_b, psc[:, 0:NE], [(ones_f[:], cntp[:])], [ones_f_b, cntp_b])
                    S.op("dve", lambda h: h.tensor_scalar(out=ge[:], in0=psc[:, 0:NE], scalar1=cap - 0.5, scalar2=None, op0=ALU.is_ge),
                         reads=[psc_b], writes=[ge_b])
                    S.op("dve", lambda h: h.scalar_tensor_tensor(out=lo[:], in0=ge[:], scalar=w, in1=lo[:], op0=ALU.mult, op1=ALU.add),
                         reads=[ge_b, lo_b], writes=[lo_b])
                S.op("dve", lambda h: h.tensor_tensor(out=mk[:, ta:tb, :], in0=aff[:, ta:tb, :], in1=_bc_mid(lo[:], nt), op=ALU.is_ge),
                     reads=[aff_b, lo_b], writes=[mk_b])
                S.op("dve", lambda h: h.tensor_copy(out=mkb[:, ta:tb, :], in_=mk[:, ta:tb, :]), reads=[mk_b], writes=[mkb_b])
                mflat = mkb[:, ta:tb, :].rearrange("p t e -> p (t e)")
                _mm_group(S, psw_b, psw[:, :nt * NE], [(us[:], mflat)], [us_b, mkb_b])
                _mm_group(S, pst_b, pst_[:, :nt * NE], [(ones_b[:], mflat)], [ones_b_b, mkb_b])
                S.op("dve", lambda h: h.tensor_copy(out=tot[:, ta:tb, :].rearrange("p t e -> p (t e)"), in_=pst_[:, :nt * NE]),
                     reads=[pst_b], writes=[tot_b])
                for t in range(ta + 1, tb):
                    S.op("dve", lambda h, t=t: h.tensor_tensor(out=base[:, t, :], in0=base[:, t - 1, :], in1=tot[:, t - 1, :], op=ALU.add),
                         reads=[base_b, tot_b], writes=[base_b])
                S.op("dve", lambda h: h.tensor_tensor(out=psel[:, ta:tb, :].rearrange("p t e -> p (t e)"), in0=psw[:, :nt * NE],
                                                      in1=base[:, ta:tb, :].rearrange("p t e -> p (t e)"), op=ALU.add),
                     reads=[psw_b, base_b], writes=[psel_b])
                S.op("dve", lambda h: h.scalar_tensor_tensor(out=psel[:, ta:tb, :], in0=psel[:, ta:tb, :], scalar=1.0, in1=mk[:, ta:tb, :],
                                                             op0=ALU.add, op1=ALU.mult), reads=[psel_b, mk_b], writes=[psel_b])
                S.op("dve", lambda h: h.tensor_scalar(out=psel[:, ta:tb, :], in0=psel[:, ta:tb, :], scalar1=-1.0, scalar2=None, op0=ALU.add),
                     reads=[psel_b], writes=[psel_b])
            S.op("dve", lambda h: h.tensor_tensor(out=aff[:], in0=aff[:], in1=mk[:], op=ALU.mult), reads=[aff_b, mk_b], writes=[aff_b])
            for src, src_b, dstT, dstT_b in ((psel, psel_b, posT, posT_b), (aff, aff_b, affT, affT_b)):
                for t4 in range(0, ntt, 4):
                    n4 = min(4, ntt - t4)

                    def trf(h, t4=t4, n4=n4, src=src):
                        ins = None
                        for j in range(n4):
                            ins = h.transpose(psc[0:16, j * 128:(j + 1) * 128], src[:, t4 + j, :], ident_f[:])
                        return ins
                    S.op("pe", trf, reads=[src_b, ident_f_b], writes=[psc_b])
                    S.op("act", lambda h, t4=t4, n4=n4, dstT=dstT: h.activation(out=dstT[:, t4 * 128:(t4 + n4) * 128], in_=psc[0:16, :n4 * 128], func=AF.Copy),
                         reads=[psc_b], writes=[dstT_b])
            S.dma("sp", R["PT"][0][0, :, 0:ntt * 128], posT[:, 0:ntt * 128], R["PT"][1], [posT_b])
            S.dma("sp", R["PT"][0][1, :, 0:ntt * 128], affT[:, 0:ntt * 128], R["PT"][1], [affT_b])
            S.barrier()
        NS = CAP + CAPC
        with contextlib.ExitStack() as s3:
            w1, w1_b = K.sb("ew1", [128, 8, D], BF16, s3)
            w3, w3_b = K.sb("ew3", [128, 8, D], BF16, s3)
            w2, w2_b = K.sb("ew2", [128, 8, D], BF16, s3)
            Se, Se_b = K.sb("eS", [128, 32, 512], BF16, s3)
            Sc, Sc_b = K.sb("eSc", [128, 2, CAPC], BF16, s3)
            io, io_b = K.sb("eio", [128, 512], F32, s3)
            S.dma("sp", io[:], I["iota512"][:, :], io_b)
            xg, xg_b = K.sb("exg", [128, 8, NS], BF16, s3)
            gT, gT_b = K.sb("egT", [128, 8, NS], BF16, s3)
            sil, sil_b = K.sb("esil", [128, NS], F32, s3)
            yst = [K.sb("eyst%d" % i, [128, 512], BF16, s3) for i in range(2)]
            psG = [K.pst("epsG%d" % i, [128, 512], F32, s3) for i in range(2)]
            psA, psA_b = K.pst("epsA", [128, 512], F32, s3)
            psB, psB_b = K.pst("epsB", [128, 512], F32, s3)
            psC, psC_b = K.pst("epsC", [128, 512], F32, s3)
            psY = [K.pst("epsY%d" % i, [128, 512], F32, s3) for i in range(2)]
            ng = 0
            ny = 0
            for e in range(NE):
                S.dma("pool", w1[:], I["moe_w1"][l, e].rearrange("(k p) n -> p k n", p=128), w1_b)
                S.dma("pool", w3[:], I["moe_w3"][l, e].rearrange("(k p) n -> p k n", p=128), w3_b)
                S.dma("pool", w2[:], I["moe_w2"][l, e].rearrange("(k p) n -> p k n", p=128), w2_b)
                for tt in range(32):
                    S.op("dve", lambda h, tt=tt: h.tensor_scalar(out=Se[:, tt, :], in0=io[:], scalar1=psel[:, tt, e:e + 1], scalar2=None, op0=ALU.is_equal),
                         reads=[io_b, psel_b], writes=[Se_b])
                if ctx_out:
                    for tt in range(32, 34):
                        S.op("dve", lambda h, tt=tt: h.tensor_scalar(out=Sc[:, tt - 32, :], in0=io[:, 0:CAPC], scalar1=psel[:, tt, e:e + 1], scalar2=None,
                                                                     op0=ALU.is_equal), reads=[io_b, psel_b], writes=[Sc_b])
                for dk in range(8):
                    pg, pg_b = psG[ng % 2]
                    ng += 1
                    _mm_group(S, pg_b, pg[:], [(H2[:, tt, dk * 128:(dk + 1) * 128], Se[:, tt, :]) for tt in range(32)], [H2_b, Se_b])
                    S.op("act", lambda h, dk=dk, pg=pg: h.activation(out=xg[:, dk, 0:CAP], in_=pg[:], func=AF.Copy), reads=[pg_b], writes=[xg_b])
                    if ctx_out:
                        _mm_group(S, psC_b, psC[:, 0:CAPC], [(H2[:, tt, dk * 128:(dk + 1) * 128], Sc[:, tt - 32, :]) for tt in (32, 33)], [H2_b, Sc_b])
                        S.op("act", lambda h, dk=dk: h.activation(out=xg[:, dk, CAP:NS], in_=psC[:, 0:CAPC], func=AF.Copy), reads=[psC_b], writes=[xg_b])
                for m in range(8):
                    _mm_group(S, psA_b, psA[:], [(w1[:, k, m * 128:(m + 1) * 128], xg[:, k, 0:CAP]) for k in range(8)], [w1_b, xg_b])
                    _mm_group(S, psB_b, psB[:], [(w3[:, k, m * 128:(m + 1) * 128], xg[:, k, 0:CAP]) for k in range(8)], [w3_b, xg_b])
                    S.op("act", lambda h: h.activation(out=sil[:, 0:CAP], in_=psA[:], func=AF.Silu), reads=[psA_b], writes=[sil_b])
                    S.op("dve", lambda h, m=m: h.tensor_tensor(out=gT[:, m, 0:CAP], in0=psB[:], in1=sil[:, 0:CAP], op=ALU.mult),
                         reads=[psB_b, sil_b], writes=[gT_b])
                    if ctx_out:
                        _mm_group(S, psC_b, psC[:, 0:CAPC], [(w1[:, k, m * 128:(m + 1) * 128], xg[:, k, CAP:NS]) for k in range(8)], [w1_b, xg_b])
                        S.op("act", lambda h: h.activation(out=sil[:, CAP:NS], in_=psC[:, 0:CAPC], func=AF.Silu), reads=[psC_b], writes=[sil_b])
                        _mm_group(S, psC_b, psC[:, 0:CAPC], [(w3[:, k, m * 128:(m + 1) * 128], xg[:, k, CAP:NS]) for k in range(8)], [w3_b, xg_b])
                        S.op("dve", lambda h, m=m: h.tensor_tensor(out=gT[:, m, CAP:NS], in0=psC[:, 0:CAPC], in1=sil[:, CAP:NS], op=ALU.mult),
                             reads=[psC_b, sil_b], writes=[gT_b])
                for c in range(5 if ctx_out else 4):
                    rows = 128 if c < 4 else CAPC
                    for dh in range(2):
                        py, py_b = psY[ny % 2]
                        ys, ys_b = yst[ny % 2]
                        ny += 1
                        _mm_group(S, py_b, py[0:rows, :], [(gT[:, f, c * 128:c * 128 + rows], w2[:, f, dh * 512:(dh + 1) * 512]) for f in range(8)],
                                  [gT_b, w2_b])
                        S.op("act", lambda h, py=py, ys=ys, rows=rows: h.activation(out=ys[0:rows, :], in_=py[0:rows, :], func=AF.Copy),
                             reads=[py_b], writes=[ys_b])
                        S.dma("sp", R["YE"][0][e, c * 128:c * 128 + rows, dh * 512:(dh + 1) * 512], ys[0:rows, :], R["YE"][1], [ys_b])
            S.barrier()
        sH.close()
        with contextlib.ExitStack() as s4:
            YEs, YEs_b = K.sb("cYE", [128, NE, 5, 512], BF16, s4)
            ST, ST_b = K.sb("cST", [128, NE, 4, 512], BF16, s4)
            abc = [K.sb("cabc%d" % i, [128, 512], F32, s4) for i in range(2)]
            xh, xh_b = K.sb("cxh", [128, 4, 512], F32, s4)
            selm, selm_b = K.sb("cselm", [16, NE, 128], F32, s4)
            sidx, sidx_b = K.sb("csidx", [128, 5], F32, s4)
            posT, posT_b = K.sb("cposT", [16, T], F32, s4)
            affT, affT_b = K.sb("caffT", [16, T], F32, s4)
            S.dma("sp", posT[:, 0:ntt * 128], R["PT"][0][0, :, 0:ntt * 128], posT_b, [R["PT"][1]])
            S.dma("sp", affT[:, 0:ntt * 128], R["PT"][0][1, :, 0:ntt * 128], affT_b, [R["PT"][1]])
            S.dma("sp", selm[:], I["selm"].rearrange("k (e m) -> k e m", e=NE), selm_b)
            S.dma("sp", sidx[:], I["slotidx"][:, :], sidx_b)
            psP = [K.pst("cpsP%d" % i, [128, 512], F32, s4) for i in range(2)]
            psQ = [K.pst("cpsQ%d" % i, [128, 512], F32, s4) for i in range(2)]
            psO = [K.pst("cpsO%d" % i, [128, 512], F32, s4) for i in range(2)]
            nb_ = 0
            no = 0
            for dh in range(2):
                for e in range(NE):
                    S.dma("sp", YEs[:, e, 0:4, :], R["YE"][0][e, 0:CAP, dh * 512:(dh + 1) * 512].rearrange("(c p) d -> p c d", p=128), YEs_b, [R["YE"][1]])
                    if ctx_out:
                        S.dma("sp", YEs[0:CAPC, e, 4, :], R["YE"][0][e, CAP:NS, dh * 512:(dh + 1) * 512], YEs_b, [R["YE"][1]])
                for (t0, tn) in chunks:
                    r = 0 if t0 < L else 1
                    lat = t0 < L
                    for e in range(NE):
                        pp, pp_b = psP[nb_ % 2]
                        pq, pq_b = psQ[nb_ % 2]
                        ab, ab_b = abc[nb_ % 2]
                        nb_ += 1
                        _mm_group(S, pp_b, pp[:, :tn], [(selm[:, e, :], posT[:, t0:t0 + tn])], [selm_b, posT_b])
                        _mm_group(S, pq_b, pq[:, :tn], [(selm[:, e, :], affT[:, t0:t0 + tn])], [selm_b, affT_b])
                        S.op("act", lambda h, ab=ab, pq=pq: h.activation(out=ab[:, :tn], in_=pq[:, :tn], func=AF.Copy), reads=[pq_b], writes=[ab_b])
                        if lat:
                            for c in range(4):
                                S.op("dve", lambda h, c=c, pp=pp, ab=ab: h.scalar_tensor_tensor(out=ST[:, e, c, :tn], in0=pp[:, :tn], scalar=sidx[:, c:c + 1],
                                                                                               in1=ab[:, :tn], op0=ALU.is_equal, op1=ALU.mult),
                                     reads=[pp_b, ab_b, sidx_b], writes=[ST_b])
                        else:
                            S.op("dve", lambda h, pp=pp, ab=ab: h.scalar_tensor_tensor(out=ST[0:CAPC, e, 0, :tn], in0=pp[0:CAPC, :tn], scalar=sidx[0:CAPC, 4:5],
                                                                                      in1=ab[0:CAPC, :tn], op0=ALU.is_equal, op1=ALU.mult),
                                 reads=[pp_b, ab_b, sidx_b], writes=[ST_b])
                    S.dma("sp", xh[:, :, :tn], XTv[:, dh * 4:(dh + 1) * 4, t0:t0 + tn], xh_b, [R["XT"][1]])
                    for m in range(4):
                        po, po_b = psO[no % 2]
                        no += 1
                        if lat:
                            pairs = [(YEs[:, e, c, m * 128:(m + 1) * 128], ST[:, e, c, :tn]) for e in range(NE) for c in range(4)]
                        else:
                            pairs = [(YEs[0:CAPC, e, 4, m * 128:(m + 1) * 128], ST[0:CAPC, e, 0, :tn]) for e in range(NE)]
                        _mm_group(S, po_b, po[:, :tn], pairs, [YEs_b, ST_b])
                        S.op("dve", lambda h, m=m, po=po: h.scalar_tensor_tensor(out=xh[:, m, :tn], in0=po[:, :tn], scalar=modT[:, 40 + dh * 4 + m, r:r + 1],
                                                                                 in1=xh[:, m, :tn], op0=ALU.mult, op1=ALU.add),
                             reads=[po_b, modT_b, xh_b], writes=[xh_b])
                    S.dma("sp", XTv[:, dh * 4:(dh + 1) * 4, t0:t0 + tn], xh[:, :, :tn], R["XT"][1], [xh_b])
            S.barrier()


def phase_final(K):
    nc, S, I, R = K.nc, K.S, K.I, K.R
    ones_f, ones_f_b = K.ones_f
    XTv = R["XT"][0].rearrange("(k p) t -> p k t", p=128)
    with contextlib.ExitStack() as st:
        fg, fg_b = K.sb("fg", [128, 8], F32, st)
        S.dma("sp", fg[:], I["fgT"][:, :], fg_b)
        xs = [K.sb("fx%d" % i, [128, 8, 512], F32, st) for i in range(2)]
        sq, sq_b = K.sb("fsq", [128, 8, 512], F32, st)
        rstd, rstd_b = K.sb("frstd", [128, 512], F32, st)
        ps, ps_b = K.pst("ps_fin", [128, 512], F32, st)
        for ci in range(8):
            t0 = ci * 512
            x, x_b = xs[ci % 2]
            S.dma("sp", x[:], XTv[:, :, t0:t0 + 512], x_b, [R["XT"][1]])
            S.op("act", lambda h: h.activation(out=sq[:], in_=x[:], func=AF.Square), reads=[x_b], writes=[sq_b])
            _mm_group(S, ps_b, ps[:], [(ones_f[:], sq[:, k, :]) for k in range(8)], [ones_f_b, sq_b])
            S.op("dve", lambda h: h.tensor_scalar(out=rstd[:], in0=ps[:], scalar1=1.0 / D, scalar2=EPS, op0=ALU.mult, op1=ALU.add),
                 reads=[ps_b], writes=[rstd_b])
            S.op("act", lambda h: h.activation(out=rstd[:], in_=rstd[:], func=AF.Sqrt), reads=[rstd_b], writes=[rstd_b])
            S.op("dve", lambda h: h.reciprocal(out=rstd[:], in_=rstd[:]), reads=[rstd_b], writes=[rstd_b])
            for k in range(8):
                S.op("dve", lambda h, k=k: h.scalar_tensor_tensor(out=sq[:, k, :], in0=x[:, k, :], scalar=fg[:, k:k + 1], in1=rstd[:],
                                                                  op0=ALU.mult, op1=ALU.mult), reads=[x_b, rstd_b, fg_b], writes=[sq_b])
            S.dma("sp", K.outT.rearrange("(k p) t -> p k t", p=128)[:, :, t0:t0 + 512], sq[:], K.outT_b, [sq_b])
        S.barrier()


def rope_tables():
    rows = L // 64
    row = np.repeat(np.arange(rows, dtype=np.float32), 64)
    col = np.tile(np.arange(64, dtype=np.float32), rows)
    inv = (10000.0 ** (-np.arange(16, dtype=np.float32) / 16)).astype(np.float32)
    C = np.zeros((64, L), np.float32)
    Sg = np.zeros((64, L), np.float32)
    for j in range(64):
        a, hf, f = j // 32, (j % 32) // 16, j % 16
        pos = row if a == 0 else col
        ang = (pos * inv[f]).astype(np.float32)
        C[j] = np.cos(ang)
        Sg[j] = np.sin(ang) * (-1.0 if hf == 0 else 1.0)
    return np.concatenate([C, C], 0), np.concatenate([Sg, Sg], 0)


_HC = {}


def hyena_consts():
    if _HC:
        return _HC
    f32 = np.float32

    def emb_of(Lx):
        t = np.linspace(0.0, 1.0, Lx, dtype=f32)[:, None]
        w = (2.0 * math.pi * np.arange(Lx, dtype=f32)[:, None] / Lx).astype(f32)
        f = np.linspace(1e-4, 15, 16, dtype=f32)[None, :]
        e = np.concatenate([t, np.cos(f * w), -np.sin(f * w)], axis=-1).astype(f32)
        return np.ascontiguousarray(e.T), t[:, 0]
    eT, t = emb_of(L)
    eTc, tc = emb_of(LC)
    _HC["embT"], _HC["embTc"] = eT, eTc
    _HC["negt"] = np.ascontiguousarray((-t).reshape(L // 128, 128).T)
    _HC["negtc"] = np.ascontiguousarray((-tc).reshape(LC // 128, 128).T)
    _HC["deltas"] = np.abs(np.linspace(math.log(1e-2) / 0.3, math.log(1e-2) / 1.5, D, dtype=f32)).reshape(1, D).astype(f32)

    def dft(Lx, npad):
        N = 2 * Lx
        a = np.arange(Lx + 1, dtype=np.int64)
        ph = (a[:, None] * a[None, :]) % N
        ang = ph.astype(np.float64) * (2.0 * math.pi / N)
        C = np.zeros((npad, npad), f32)
        S_ = np.zeros((npad, npad), f32)
        C[:Lx + 1, :Lx + 1] = np.cos(ang)
        S_[:Lx + 1, :Lx + 1] = np.sin(ang)
        wfv = np.zeros(npad, f32)
        wfv[:Lx + 1] = 2.0 / N
        wfv[0] = 1.0 / N
        wfv[Lx] = 1.0 / N
        return C.astype(ml_dtypes.bfloat16), S_.astype(ml_dtypes.bfloat16), np.ascontiguousarray(wfv.reshape(npad // 128, 128).T)
    _HC["dftC"], _HC["dftS"], _HC["wf"] = dft(L, NF)
    _HC["dftCc"], _HC["dftSc"], _HC["wfc"] = dft(LC, NFC)
    return _HC


def host_inputs(inputs, b, nl):
    x, c, ctx, c_ctx = inputs["x"], inputs["c"], inputs["ctx"], inputs["c_ctx"]
    m = {}
    m["xT"] = np.ascontiguousarray(np.concatenate([x[b], ctx[b]], 0).T)
    cc = np.stack([c[b], c_ctx], 0)
    m["ccT"] = np.ascontiguousarray(cc.reshape(2, 8, 128).transpose(2, 1, 0))
    m["w_mod"] = np.ascontiguousarray(inputs["w_mod"][:nl])
    m["b_modT"] = np.ascontiguousarray(inputs["b_mod"][:nl].reshape(nl, 48, 128).transpose(0, 2, 1))
    m["g1T"] = np.ascontiguousarray(inputs["norm1_g"][:nl].reshape(nl, 8, 128).transpose(0, 2, 1))
    m["g2T"] = np.ascontiguousarray(inputs["norm2_g"][:nl].reshape(nl, 8, 128).transpose(0, 2, 1))
    m["w_in"] = np.ascontiguousarray(inputs["w_in"][:nl])
    rc, rs = rope_tables()
    m["ropeC"], m["ropeS"] = rc, rs
    m["ident"] = np.eye(128, dtype=np.float32)
    m["diff_lambda"] = np.ascontiguousarray(inputs["diff_lambda"][:nl].reshape(nl, 256))
    m["sublnT"] = np.ascontiguousarray(inputs["diff_subln_g"][:nl].reshape(nl, 128, 1))
    m["swa_sink"] = np.ascontiguousarray(inputs["swa_sink"][:nl])
    kk, qq = np.meshgrid(np.arange(128), np.arange(128), indexing="ij")
    m["trilo"] = (kk >= qq).astype(np.float32)
    m["triup"] = (kk <= qq).astype(np.float32)
    m["w_branch"] = np.ascontiguousarray(inputs["w_branch"][:nl])
    m["w_out"] = np.ascontiguousarray(inputs["w_out"][:nl])
    m["fgT"] = np.ascontiguousarray(inputs["final_g"].reshape(8, 128).T)
    if "hy_ff_w1" in inputs:
        cwv = inputs["hy_conv_w"][:nl]
        m["cwT"] = np.ascontiguousarray(cwv.reshape(nl, 3, 24, 128).transpose(0, 3, 2, 1))
        m["cbT"] = np.ascontiguousarray(inputs["hy_conv_b"][:nl].reshape(nl, 24, 128).transpose(0, 2, 1))
        m["hbT"] = np.ascontiguousarray(inputs["hy_bias"][:nl].reshape(nl, 8, 128).transpose(0, 2, 1))
        m["hy_w1"] = np.ascontiguousarray(inputs["hy_ff_w1"][:nl])
        m["hy_w2"] = np.ascontiguousarray(inputs["hy_ff_w2"][:nl])
        m["hy_w3"] = np.ascontiguousarray(inputs["hy_ff_w3"][:nl])
        m["hy_b1"] = np.ascontiguousarray(inputs["hy_ff_b1"][:nl].reshape(nl, 64, 1))
        m["hy_b2"] = np.ascontiguousarray(inputs["hy_ff_b2"][:nl].reshape(nl, 64, 1))
        m["hy_fr"] = np.ascontiguousarray(inputs["hy_sin_freq"][:nl].reshape(nl, 64, 1))
        m.update(hyena_consts())
    if "moe_w1" in inputs:
        m["router_w"] = np.ascontiguousarray(inputs["router_w"][:nl])
        for k in ("moe_w1", "moe_w3", "moe_w2"):
            m[k] = np.ascontiguousarray(inputs[k][:nl])
        sel = np.zeros((16, 16, 128), np.float32)
        for e in range(16):
            sel[e, e, :] = 1.0
        m["selm"] = sel.reshape(16, 16 * 128)
        si = np.zeros((128, 5), np.float32)
        for c in range(4):
            si[:, c] = np.arange(128) + 128 * c
        si[:, 4] = np.arange(128)
        m["slotidx"] = si
        m["iota512"] = np.tile(np.arange(512, dtype=np.float32)[None, :], (128, 1))
        m["ustrict"] = (kk < qq).astype(np.float32)
    return m


def kernel(**inputs):
    inputs = {k: np.asarray(v) for k, v in inputs.items()}
    nb = inputs["x"].shape[0]
    nc = build(dict(nl=DEPTH, phases=("diff", "swa", "hy", "merge", "moe")))
    in_maps = [host_inputs(inputs, b, DEPTH) for b in range(nb)]
    res = run_bass_kernel_spmd(nc, in_maps, core_ids=list(range(nb)))
    out = np.stack([np.asarray(res.results[b]["outT"]).T for b in range(nb)], 0)
    return np.ascontiguousarray(out.astype(np.float32))
```

```python
import contextlib
import math
import numpy as np
import ml_dtypes
import concourse.bass as bass
import concourse.mybir as mybir
from concourse.bass_utils import run_bass_kernel_spmd

F32 = mybir.dt.float32
BF16 = mybir.dt.bfloat16
AF = mybir.ActivationFunctionType
ALU = mybir.AluOpType
AX = mybir.AxisListType

D = 1024
L = 4096
LC = 256
T = L + LC
DEPTH = 4
D_IN = 10752
NE = 16
CAP = 512
CAPC = 32
EPS = 1e-6
NF = 4224
NFC = 384
TCH = [(i * 512, 512) for i in range(8)] + [(L, LC)]


class Buf:
    __slots__ = ("name", "w", "r", "sem", "cnt")

    def __init__(self, name):
        self.name = name
        self.w = {}
        self.r = {}
        self.sem = None
        self.cnt = 0


class Sched:
    def __init__(self, nc, es):
        self.nc, self.es = nc, es
        self.E = {}
        for n, h in (("pe", nc.tensor), ("dve", nc.vector), ("act", nc.scalar),
                     ("pool", nc.gpsimd), ("sp", nc.sync)):
            self.E[n] = dict(h=h, sem=es.enter_context(nc.semaphore("s_" + n)), cnt=0, seen={})
        self.dma_bufs = []
        self.nbuf = 0
        self.persist = True
        self.pool = []

    def buf(self, name):
        self.nbuf += 1
        b = Buf("%s_%d" % (name, self.nbuf))
        b.w["_persist"] = self.persist
        return b

    @staticmethod
    def _add(evs, d):
        for k, sv in d.items():
            if k == "_persist":
                continue
            sem, v = sv
            if k not in evs or evs[k][1] < v:
                evs[k] = (sem, v)

    def _waits(self, e, evs):
        E = self.E[e]
        for name, (sem, val) in evs.items():
            if E["seen"].get(name, 0) < val:
                E["h"].wait_ge(sem, val)
                E["seen"][name] = val

    def op(self, e, fn, reads=(), writes=(), skip_self=False, drain_self=False):
        evs = {}
        for b in reads:
            self._add(evs, b.w)
        for b in writes:
            self._add(evs, b.w)
            self._add(evs, b.r)
        if skip_self:
            evs.pop("s_" + e, None)
        if drain_self and self.E[e]["cnt"]:
            evs["s_" + e] = (self.E[e]["sem"], self.E[e]["cnt"])
        self._waits(e, evs)
        E = self.E[e]
        ins = fn(E["h"])
        E["cnt"] += 1
        ins.then_inc(E["sem"], 1)
        key, ev = "s_" + e, (E["sem"], E["cnt"])
        for b in reads:
            b.r[key] = ev
        for b in writes:
            b.w[key] = ev

    def dma(self, q, out_ap, in_ap, dst, srcs=(), **kw):
        evs = {}
        self._add(evs, dst.w)
        self._add(evs, dst.r)
        for b in srcs:
            self._add(evs, b.w)
        self._waits(q, evs)
        if dst.sem is None:
            if self.pool and not dst.w["_persist"]:
                dst.name, dst.sem, dst.cnt = self.pool.pop()
            else:
                dst.sem = self.es.enter_context(self.nc.semaphore("d_" + dst.name))
            self.dma_bufs.append(dst)
        ins = self.E[q]["h"].dma_start(out=out_ap, in_=in_ap, **kw)
        dst.cnt += 16
        ins.then_inc(dst.sem, 16)
        key, ev = "d_" + dst.name, (dst.sem, dst.cnt)
        dst.w[key] = ev
        for b in srcs:
            b.r[key] = ev

    def barrier(self):
        evs = {}
        for n, E in self.E.items():
            if E["cnt"]:
                evs["s_" + n] = (E["sem"], E["cnt"])
        for b in self.dma_bufs:
            evs["d_" + b.name] = (b.sem, b.cnt)
        for n in self.E:
            self._waits(n, evs)
        keep = []
        for b in self.dma_bufs:
            if b.w["_persist"]:
                keep.append(b)
            else:
                self.pool.append((b.name, b.sem, b.cnt))
        self.dma_bufs = keep

    def barrier_known(self):
        pass


class Ctx:
    pass


def _mm_group(S, ps, out_ap, pairs, reads):
    def fn(h):
        ins = None
        n = len(pairs)
        for i, (a, b) in enumerate(pairs):
            ins = h.matmul(out_ap, a, b, start=(i == 0), stop=(i == n - 1))
        return ins
    S.op("pe", fn, reads=reads, writes=[ps])


def _mm1(S, ps, out_ap, a, b, reads, start, stop, drain=False):
    S.op("pe", lambda h: h.matmul(out_ap, a, b, start=start, stop=stop), reads=reads, writes=[ps], skip_self=True, drain_self=drain)


def build(cfg):
    nl = cfg["nl"]
    debug = cfg.get("debug", ())
    stop_after = cfg.get("stop_after", None)
    nc = bass.Bass("TRN2", target_bir_lowering=False)
    es = contextlib.ExitStack()
    K = Ctx()
    K.nc, K.es, K.cfg = nc, es, cfg
    S = Sched(nc, es)
    K.S = S

    def din(name, shape, dt=F32):
        return nc.dram_tensor(name, list(shape), dt, kind="ExternalInput").ap()

    def scratch(name, shape, dt):
        kind = "ExternalOutput" if name in debug else "Internal"
        return nc.dram_tensor(name, list(shape), dt, kind=kind).ap()

    I = {}
    I["xT"] = din("xT", [D, T])
    I["ccT"] = din("ccT", [128, 8, 2])
    I["w_mod"] = din("w_mod", [nl, D, 6 * D])
    I["b_modT"] = din("b_modT", [nl, 128, 48])
    I["g1T"] = din("g1T", [nl, 128, 8])
    I["g2T"] = din("g2T", [nl, 128, 8])
    I["w_in"] = din("w_in", [nl, D, D_IN])
    I["ropeC"] = din("ropeC", [128, L])
    I["ropeS"] = din("ropeS", [128, L])
    I["ident"] = din("ident", [128, 128])
    I["diff_lambda"] = din("diff_lambda", [nl, 256])
    I["sublnT"] = din("sublnT", [nl, 128, 1])
    I["swa_sink"] = din("swa_sink", [nl, 16])
    I["trilo"] = din("trilo", [128, 128])
    I["triup"] = din("triup", [128, 128])
    I["w_branch"] = din("w_branch", [nl, 3, D, D])
    I["w_out"] = din("w_out", [nl, D, D])
    I["fgT"] = din("fgT", [128, 8])
    if "moe" in cfg.get("phases", ()):
        I["router_w"] = din("router_w", [nl, D, NE])
        I["moe_w1"] = din("moe_w1", [nl, NE, D, D])
        I["moe_w3"] = din("moe_w3", [nl, NE, D, D])
        I["moe_w2"] = din("moe_w2", [nl, NE, D, D])
        I["selm"] = din("selm", [16, 16 * 128])
        I["slotidx"] = din("slotidx", [128, 5])
        I["iota512"] = din("iota512", [128, 512])
        I["ustrict"] = din("ustrict", [128, 128])
    if "hy" in cfg.get("phases", ()):
        I["cwT"] = din("cwT", [nl, 128, 24, 3])
        I["cbT"] = din("cbT", [nl, 128, 24])
        I["hbT"] = din("hbT", [nl, 128, 8])
        I["hy_w1"] = din("hy_w1", [nl, 33, 64])
        I["hy_w2"] = din("hy_w2", [nl, 64, 64])
        I["hy_w3"] = din("hy_w3", [nl, 64, 2048])
        I["hy_b1"] = din("hy_b1", [nl, 64, 1])
        I["hy_b2"] = din("hy_b2", [nl, 64, 1])
        I["hy_fr"] = din("hy_fr", [nl, 64, 1])
        I["embT"] = din("embT", [33, L])
        I["embTc"] = din("embTc", [33, LC])
        I["negt"] = din("negt", [128, 32])
        I["negtc"] = din("negtc", [128, 2])
        I["deltas"] = din("deltas", [1, D])
        I["dftC"] = din("dftC", [NF, NF], BF16)
        I["dftS"] = din("dftS", [NF, NF], BF16)
        I["dftCc"] = din("dftCc", [NFC, NFC], BF16)
        I["dftSc"] = din("dftSc", [NFC, NFC], BF16)
        I["dftCt"] = din("dftCt", [NF // 128, 128, NF // 128, 128], BF16)
        I["dftSt"] = din("dftSt", [NF // 128, 128, NF // 128, 128], BF16)
        I["dftCct"] = din("dftCct", [NFC // 128, 128, NFC // 128, 128], BF16)
        I["dftSct"] = din("dftSct", [NFC // 128, 128, NFC // 128, 128], BF16)
        I["wf"] = din("wf", [128, NF // 128])
        I["wfc"] = din("wfc", [128, NFC // 128])
    K.I = I
    K.outT = nc.dram_tensor("outT", [D, L], F32, kind="ExternalOutput").ap()
    K.outT_b = S.buf("outT")

    R = {}
    R["XT"] = (scratch("XT", [D, T], F32), S.buf("XT"))
    R["QdT"] = (scratch("QdT", [D, T], BF16), S.buf("QdT"))
    R["KdT"] = (scratch("KdT", [D, T], BF16), S.buf("KdT"))
    R["Vd"] = (scratch("Vd", [T, D], BF16), S.buf("Vd"))
    R["HyT"] = (scratch("HyT", [3 * D, T], BF16), S.buf("HyT"))
    R["QsT"] = (scratch("QsT", [D, T], BF16), S.buf("QsT"))
    R["KsT"] = (scratch("KsT", [256, T], BF16), S.buf("KsT"))
    R["Vs"] = (scratch("Vs", [T, 256], BF16), S.buf("Vs"))
    R["GT"] = (scratch("GT", [3 * D, T], BF16), S.buf("GT"))
    R["YdT"] = (scratch("YdT", [D, T], BF16), S.buf("YdT"))
    R["YhT"] = (scratch("YhT", [D, T], BF16), S.buf("YhT"))
    R["YsT"] = (scratch("YsT", [D, T], BF16), S.buf("YsT"))
    R["X0T"] = (scratch("X0T", [D, T], BF16), S.buf("X0T"))
    R["UT"] = (scratch("UT", [D, T], BF16), S.buf("UT"))
    R["KF"] = (scratch("KF", [2, NF, D], F32), S.buf("KF"))
    R["KFc"] = (scratch("KFc", [2, NFC, D], F32), S.buf("KFc"))
    R["YF"] = (scratch("YF", [2, NF, D], BF16), S.buf("YF"))
    R["YFc"] = (scratch("YFc", [2, NFC, D], BF16), S.buf("YFc"))
    R["YE"] = (scratch("YE", [NE, CAP + CAPC, D], BF16), S.buf("YE"))
    R["XT1"] = (scratch("XT1", [D, T], F32), S.buf("XT1"))
    R["PT"] = (scratch("PT", [2, 16, T], F32), S.buf("PT"))
    R["LG"] = (scratch("LG", [128, T // 128, NE], F32), S.buf("LG"))
    R["H2d"] = (scratch("H2d", [128, T // 128, D], BF16), S.buf("H2d"))
    K.R = R

    uid = [0]

    def sb(name, shape, dt, stack=es):
        uid[0] += 1
        t = stack.enter_context(nc.sbuf_tensor("sb%d_%s" % (uid[0], name), list(shape), dt))
        return t, S.buf(name)

    def pst(name, shape, dt, stack):
        uid[0] += 1
        t = stack.enter_context(nc.psum_tensor("ps%d_%s" % (uid[0], name), list(shape), dt))
        return t, S.buf(name)
    K.sb, K.pst = sb, pst

    ident_f, ident_f_b = sb("ident_f", [128, 128], F32)
    ident_b, ident_b_b = sb("ident_b", [128, 128], BF16)
    ones_f, ones_f_b = sb("ones_f", [128, 128], F32)
    ones_b, ones_b_b = sb("ones_b", [128, 128], BF16)
    S.dma("sp", ident_f[:], I["ident"][:, :], ident_f_b)
    S.dma("pool", ident_b[:], I["ident"][:, :], ident_b_b)
    S.op("dve", lambda h: h.memset(ones_f[:], 1.0), writes=[ones_f_b])
    S.op("dve", lambda h: h.memset(ones_b[:], 1.0), writes=[ones_b_b])
    K.ident_f, K.ident_b, K.ones_f, K.ones_b = (ident_f, ident_f_b), (ident_b, ident_b_b), (ones_f, ones_f_b), (ones_b, ones_b_b)

    sc, sc_b = sb("sc", [128, 8, 2], F32)
    S.dma("sp", sc[:], I["ccT"][:, :, :], sc_b)
    S.op("act", lambda h: h.activation(out=sc[:], in_=sc[:], func=AF.Silu), reads=[sc_b], writes=[sc_b])
    K.sc = (sc, sc_b)
    modT, modT_b = sb("modT", [128, 48, 2], F32)
    K.modT = (modT, modT_b)
    A1, A1_b = sb("A1", [128, 8, 2], F32)
    A2, A2_b = sb("A2", [128, 8, 2], F32)
    K.A1, K.A2 = (A1, A1_b), (A2, A2_b)

    with contextlib.ExitStack() as ph:
        xc, xc_b = sb("xcp", [128, 8, 512], F32, ph)
        for (t0, tn) in TCH:
            S.dma("sp", xc[:, :, :tn], I["xT"].rearrange("(k p) t -> p k t", p=128)[:, :, t0:t0 + tn], xc_b)
            S.dma("sp", R["XT"][0].rearrange("(k p) t -> p k t", p=128)[:, :, t0:t0 + tn], xc[:, :, :tn], R["XT"][1], [xc_b])
        S.barrier()

    S.persist = False
    phases = cfg.get("phases", ("diff", "swa", "merge"))
    if "swa" not in phases:
        with contextlib.ExitStack() as ph:
            zt, zt_b = sb("zt2", [128, T], BF16, ph)
            S.op("dve", lambda h: h.memset(zt[:], 0.0), writes=[zt_b])
            for k in range(8):
                S.dma("sp", R["YsT"][0][k * 128:(k + 1) * 128, :], zt[:], R["YsT"][1], [zt_b])
            S.barrier()
    if "hy" not in phases:
        with contextlib.ExitStack() as ph:
            zt, zt_b = sb("zt", [128, T], BF16, ph)
            S.op("dve", lambda h: h.memset(zt[:], 0.0), writes=[zt_b])
            for k in range(8):
                S.dma("sp", R["YhT"][0][k * 128:(k + 1) * 128, :], zt[:], R["YhT"][1], [zt_b])
            S.barrier()
    for l in range(nl):
        ctx_out = l < DEPTH - 1
        phase_mod(K, l)
        with contextlib.ExitStack() as ph:
            hT, hT_b = sb("hT", [128, 8, T], BF16, ph)
            phase_norm(K, ph, R["XT"], K.A1, 0, (hT, hT_b))
            if stop_after == "norm1":
                dbg = scratch("dbg_hT", [D, T], BF16)
                S.dma("sp", dbg.rearrange("(k p) t -> p k t", p=128), hT[:], S.buf("dbg"), [hT_b])
                S.barrier()
                break
            phase_inproj(K, ph, l, (hT, hT_b))
            S.barrier()
        if stop_after == "inproj":
            break
        if "diff" in phases:
            phase_diff(K, l, ctx_out)
        if stop_after == "diff":
            break
        if "swa" in phases:
            phase_swa(K, l, ctx_out)
        if "hy" in phases:
            phase_hyena(K, l, ctx_out)
        if stop_after == "hy":
            break
        if stop_after == "swa":
            break
        if "merge" in phases:
            phase_merge(K, l)
        if stop_after == "merge":
            break
        if "moe" in phases:
            phase_moe(K, l, ctx_out)
        if stop_after == "moe":
            break
    if stop_after is None:
        phase_final(K)

    S.barrier()
    es.close()
    return nc


def phase_mod(K, l):
    nc, S, I = K.nc, K.S, K.I
    modT, modT_b = K.modT
    sc, sc_b = K.sc
    with contextlib.ExitStack() as ph:
        wts = [K.sb("wmod%d" % i, [128, 8, 512], F32, ph) for i in range(2)]
        bm, bm_b = K.sb("bmodT", [128, 48], F32, ph)
        g1, g1_b = K.sb("g1T", [128, 8], F32, ph)
        g2, g2_b = K.sb("g2T", [128, 8], F32, ph)
        ps, ps_b = K.pst("ps_mod", [128, 512], F32, ph)
        S.dma("sp", bm[:], I["b_modT"][l], bm_b)
        S.dma("sp", g1[:], I["g1T"][l], g1_b)
        S.dma("sp", g2[:], I["g2T"][l], g2_b)
        for g in range(12):
            wt, wt_b = wts[g % 2]
            S.dma("sp", wt[:], I["w_mod"][l].rearrange("(k p) n -> p k n", p=128)[:, :, g * 512:(g + 1) * 512], wt_b)
            for m in range(4):
                mc = g * 4 + m
                _mm_group(S, ps_b, ps[:, 0:2],
                          [(wt[:, k, m * 128:(m + 1) * 128], sc[:, k, :]) for k in range(8)],
                          [wt_b, sc_b])
                S.op("dve", lambda h, mc=mc: h.tensor_scalar(out=modT[:, mc, :], in0=ps[:, 0:2], scalar1=bm[:, mc:mc + 1],
                                                            scalar2=None, op0=ALU.add),
                     reads=[ps_b, bm_b], writes=[modT_b])
        for (A, A_b), (g, g_b), j in ((K.A1, (g1, g1_b), 1), (K.A2, (g2, g2_b), 4)):
            for r in range(2):
                S.op("dve", lambda h, A=A, g=g, j=j, r=r: h.scalar_tensor_tensor(
                    out=A[:, :, r], in0=modT[:, j * 8:(j + 1) * 8, r], scalar=1.0, in1=g[:], op0=ALU.add, op1=ALU.mult),
                    reads=[modT_b, g_b], writes=[A_b])
        S.barrier()


def phase_norm(K, ph, X, Acoef, shift_j, out, out_f32_cb=None):
    nc, S = K.nc, K.S
    XT, XT_b = X
    A, A_b = Acoef
    modT, modT_b = K.modT
    hT, hT_b = out
    ones_f, ones_f_b = K.ones_f
    with contextlib.ExitStack() as st:
        xs = [K.sb("nx%d" % i, [128, 8, 512], F32, st) for i in range(2)]
        sq, sq_b = K.sb("nsq", [128, 8, 512], F32, st)
        rstd, rstd_b = K.sb("nrstd", [128, 512], F32, st)
        tmp, tmp_b = K.sb("ntmp", [128, 512], F32, st)
        ps, ps_b = K.pst("ps_norm", [128, 512], F32, st)
        for ci, (t0, tn) in enumerate(TCH):
            r = 0 if t0 < L else 1
            x, x_b = xs[ci % 2]
            S.dma("sp", x[:, :, :tn], XT.rearrange("(k p) t -> p k t", p=128)[:, :, t0:t0 + tn], x_b, [XT_b])
            S.op("act", lambda h: h.activation(out=sq[:, :, :tn], in_=x[:, :, :tn], func=AF.Square), reads=[x_b], writes=[sq_b])
            _mm_group(S, ps_b, ps[:, :tn], [(ones_f[:], sq[:, k, :tn]) for k in range(8)], [ones_f_b, sq_b])
            S.op("dve", lambda h: h.tensor_scalar(out=rstd[:, :tn], in0=ps[:, :tn], scalar1=1.0 / D, scalar2=EPS,
                                                  op0=ALU.mult, op1=ALU.add), reads=[ps_b], writes=[rstd_b])
            S.op("act", lambda h: h.activation(out=rstd[:, :tn], in_=rstd[:, :tn], func=AF.Sqrt), reads=[rstd_b], writes=[rstd_b])
            S.op("dve", lambda h: h.reciprocal(out=rstd[:, :tn], in_=rstd[:, :tn]), reads=[rstd_b], writes=[rstd_b])
            for k in range(8):
                S.op("dve", lambda h, k=k: h.tensor_tensor(out=tmp[:, :tn], in0=x[:, k, :tn], in1=rstd[:, :tn], op=ALU.mult),
                     reads=[x_b, rstd_b], writes=[tmp_b])
                S.op("act", lambda h, k=k: h.activation(out=hT[:, k, t0:t0 + tn], in_=tmp[:, :tn], func=AF.Identity,
                                                        scale=A[:, k, r:r + 1], bias=modT[:, shift_j * 8 + k, r:r + 1]),
                     reads=[tmp_b, A_b, modT_b], writes=[hT_b])
                if out_f32_cb is not None:
                    out_f32_cb(ci, k, t0, tn, r, tmp, tmp_b)
        S.barrier()


def _inproj_groups():
    g = []
    g += [("qk", "QdT", 0), ("qk", "QdT", 512), ("qk", "KdT", 0), ("qk", "KdT", 512)]
    g += [("v", "Vd", 0), ("v", "Vd", 512)]
    g += [("plain", "HyT", i * 512) for i in range(6)]
    g += [("qk", "QsT", 0), ("qk", "QsT", 512)]
    g += [("kv", None, 0)]
    g += [("gate", "GT", i * 512) for i in range(6)]
    return g


def phase_inproj(K, ph, l, hTb):
    nc, S, I, R = K.nc, K.S, K.I, K.R
    hT, hT_b = hTb
    with contextlib.ExitStack() as st:
        wts = [K.sb("wi%d" % i, [128, 8, 512], BF16, st) for i in range(2)]
        wsw, wsw_b = K.sb("wsw", [128, 8, 512], BF16, st)
        rC, rC_b = K.sb("ropeC", [128, L], F32, st)
        rS, rS_b = K.sb("ropeS", [128, L], F32, st)
        stg = [K.sb("stg%d" % i, [128, T], BF16, st) for i in range(2)]
        vst = [K.sb("vst%d" % i, [128, 512], BF16, st) for i in range(2)]
        t1, t1_b = K.sb("rt1", [128, 512], F32, st)
        t2, t2_b = K.sb("rt2", [128, 512], F32, st)
        psA = [K.pst("psA%d" % i, [128, 512], F32, st) for i in range(2)]
        psB = [K.pst("psB%d" % i, [128, 512], F32, st) for i in range(2)]
        S.dma("sp", rC[:], I["ropeC"][:, :], rC_b)
        S.dma("sp", rS[:], I["ropeS"][:, :], rS_b)
        win = I["w_in"][l].rearrange("(k p) n -> p k n", p=128)
        cnt = dict(s=0, p=0, v=0)

        def fm_chunk(wt, wt_b, m, kind, dst, row0):
            sg, sg_b = stg[cnt["s"] % 2]
            cnt["s"] += 1
            for (t0, tn) in TCH:
                pa, pa_b = psA[cnt["p"] % 2]
                pb, pb_b = psB[cnt["p"] % 2]
                cnt["p"] += 1
                _mm_group(S, pa_b, pa[:, :tn], [(wt[:, k, m * 128:(m + 1) * 128], hT[:, k, t0:t0 + tn]) for k in range(8)],
                          [wt_b, hT_b])
                if kind == "qk" and t0 < L:
                    _mm_group(S, pb_b, pb[:, :tn], [(wsw[:, k, m * 128:(m + 1) * 128], hT[:, k, t0:t0 + tn]) for k in range(8)],
                              [wsw_b, hT_b])
                    S.op("dve", lambda h: h.tensor_tensor(out=t1[:, :tn], in0=pa[:, :tn], in1=rC[:, t0:t0 + tn], op=ALU.mult),
                         reads=[pa_b, rC_b], writes=[t1_b])
                    S.op("dve", lambda h: h.tensor_tensor(out=t2[:, :tn], in0=pb[:, :tn], in1=rS[:, t0:t0 + tn], op=ALU.mult),
                         reads=[pb_b, rS_b], writes=[t2_b])
                    S.op("pool", lambda h: h.tensor_tensor(out=sg[:, t0:t0 + tn], in0=t1[:, :tn], in1=t2[:, :tn], op=ALU.add),
                         reads=[t1_b, t2_b], writes=[sg_b])
                elif kind == "gate":
                    S.op("act", lambda h: h.activation(out=sg[:, t0:t0 + tn], in_=pa[:, :tn], func=AF.Sigmoid),
                         reads=[pa_b], writes=[sg_b])
                else:
                    S.op("act", lambda h: h.activation(out=sg[:, t0:t0 + tn], in_=pa[:, :tn], func=AF.Copy),
                         reads=[pa_b], writes=[sg_b])
            S.dma("sp", R[dst][0][row0:row0 + 128, :], sg[:], R[dst][1], [sg_b])

        def tm_cols(wt, wt_b, c0, cn, dst, col0):
            for tt in range(T // 128):
                pa, pa_b = psA[cnt["p"] % 2]
                cnt["p"] += 1
                vs, vs_b = vst[cnt["v"] % 2]
                cnt["v"] += 1
                _mm_group(S, pa_b, pa[:, :cn], [(hT[:, k, tt * 128:(tt + 1) * 128], wt[:, k, c0:c0 + cn]) for k in range(8)],
                          [wt_b, hT_b])
                S.op("act", lambda h: h.activation(out=vs[:, :cn], in_=pa[:, :cn], func=AF.Copy), reads=[pa_b], writes=[vs_b])
                S.dma("sp", R[dst][0][tt * 128:(tt + 1) * 128, col0:col0 + cn], vs[:, :cn], R[dst][1], [vs_b])

        def make_swapped(wt, wt_b, ncols):
            src = wt[:, :, :ncols].rearrange("p k (q s f) -> p k q s f", s=2, f=16)
            dstv = wsw[:, :, :ncols].rearrange("p k (q s f) -> p k q s f", s=2, f=16)
            for k in range(8):
                S.op("pool", lambda h, k=k: h.tensor_copy(out=dstv[:, k, :, 0, :], in_=src[:, k, :, 1, :]), reads=[wt_b], writes=[wsw_b])
                S.op("pool", lambda h, k=k: h.tensor_copy(out=dstv[:, k, :, 1, :], in_=src[:, k, :, 0, :]), reads=[wt_b], writes=[wsw_b])

        for gi, (kind, dst, row0) in enumerate(_inproj_groups()):
            wt, wt_b = wts[gi % 2]
            S.dma("pool", wt[:], win[:, :, gi * 512:(gi + 1) * 512], wt_b)
            if kind == "qk":
                make_swapped(wt, wt_b, 512)
                for m in range(4):
                    fm_chunk(wt, wt_b, m, "qk", dst, row0 + m * 128)
            elif kind == "v":
                tm_cols(wt, wt_b, 0, 512, dst, row0)
            elif kind == "kv":
                make_swapped(wt, wt_b, 256)
                for m in range(2):
                    fm_chunk(wt, wt_b, m, "qk", "KsT", m * 128)
                tm_cols(wt, wt_b, 256, 256, "Vs", 0)
            else:
                for m in range(4):
                    fm_chunk(wt, wt_b, m, kind, dst, row0 + m * 128)
        S.barrier()


def _bcast_rows(ap, n):
    return bass.AP(ap.tensor, ap.offset, [[0, 128], [1, n]])


def phase_diff(K, l, ctx_out):
    nc, S, I, R = K.nc, K.S, K.I, K.R
    ones_f, ones_f_b = K.ones_f
    ones_b, ones_b_b = K.ones_b
    lambda_init = 0.8 - 0.6 * math.exp(-0.3 * l)
    with contextlib.ExitStack() as st:
        lp, lp_b = K.sb("lp", [128, 256], F32, st)
        lsc, lsc_b = K.sb("lsc", [128, 4], F32, st)
        gsc, gsc_b = K.sb("gsc", [128, 1], F32, st)
        S.dma("sp", lp[:], _bcast_rows(I["diff_lambda"][l], 256), lp_b)
        S.dma("sp", gsc[:], I["sublnT"][l], gsc_b)
        S.op("dve", lambda h: h.tensor_tensor(out=lp[:, 0:64], in0=lp[:, 0:64], in1=lp[:, 64:128], op=ALU.mult), reads=[lp_b], writes=[lp_b])
        S.op("dve", lambda h: h.tensor_tensor(out=lp[:, 128:192], in0=lp[:, 128:192], in1=lp[:, 192:256], op=ALU.mult), reads=[lp_b], writes=[lp_b])
        S.op("dve", lambda h: h.reduce_sum(out=lsc[:, 0:1], in_=lp[:, 0:64], axis=AX.X), reads=[lp_b], writes=[lsc_b])
        S.op("dve", lambda h: h.reduce_sum(out=lsc[:, 1:2], in_=lp[:, 128:192], axis=AX.X), reads=[lp_b], writes=[lsc_b])
        S.op("act", lambda h: h.activation(out=lsc[:, 0:2], in_=lsc[:, 0:2], func=AF.Exp), reads=[lsc_b], writes=[lsc_b])
        S.op("dve", lambda h: h.scalar_tensor_tensor(out=lsc[:, 2:3], in0=lsc[:, 1:2], scalar=-lambda_init, in1=lsc[:, 0:1],
                                                     op0=ALU.add, op1=ALU.subtract), reads=[lsc_b], writes=[lsc_b])
        S.op("dve", lambda h: h.tensor_scalar(out=gsc[:], in0=gsc[:], scalar1=1.0 - lambda_init, scalar2=None, op0=ALU.mult),
             reads=[gsc_b], writes=[gsc_b])

        QT = [K.sb("dQT%d" % i, [128, T], BF16, st) for i in range(2)]
        KT = [[K.sb("dKT%d%d" % (i, m), [128, T], BF16, st) for m in range(2)] for i in range(2)]
        for i in range(2):
            for m in range(2):
                S.op("dve", lambda h, i=i, m=m: h.memset(KT[i][m][0][(1 - m) * 64:(2 - m) * 64, :], 0.0), writes=[KT[i][m][1]])
        VV = [K.sb("dV%d" % i, [128, T // 128, 128], BF16, st) for i in range(2)]
        pt = [K.sb("dP%d" % i, [128, 2, 512], BF16, st) for i in range(2)]
        psS = [K.pst("dpsS%d" % i, [128, 2, 512], F32, st) for i in range(2)]
        acc = [[K.sb("dacc%d%d" % (m, i), [128, 512], F32, st) for i in range(2)] for m in range(2)]
        acc_hi = [S.buf("dacchi%d" % i) for i in range(2)]
        pt_mb = [[S.buf("dPm%d%d" % (i, m)) for m in range(2)] for i in range(2)]
        ps_mb = [[S.buf("dSm%d%d" % (i, m)) for m in range(2)] for i in range(2)]
        psO = [K.pst("dpsO%d" % m, [128, 512], F32, st) for m in range(2)]
        psZ = [K.pst("dpsZ%d" % m, [128, 512], F32, st) for m in range(2)]
        rz = [K.sb("drz%d" % m, [128, 512], F32, st) for m in range(2)]
        oo = [K.sb("doo%d" % m, [128, 512], F32, st) for m in range(2)]
        of, of_b = K.sb("dof", [128, 512], F32, st)
        sqf, sqf_b = K.sb("dsq", [128, 512], F32, st)
        rs, rs_b = K.sb("drs", [128, 512], F32, st)
        ost = [K.sb("dost%d" % i, [128, 512], BF16, st) for i in range(2)]
        nch = 0
        for hd in range(8):
            q, q_b = QT[hd % 2]
            kz = KT[hd % 2]
            v, v_b = VV[hd % 2]
            S.dma("sp", q[:], R["QdT"][0][hd * 128:(hd + 1) * 128, :], q_b, [R["QdT"][1]])
            for m in range(2):
                S.dma("sp", kz[m][0][m * 64:(m + 1) * 64, :], R["KdT"][0][hd * 128 + m * 64:hd * 128 + (m + 1) * 64, :], kz[m][1], [R["KdT"][1]])
            S.dma("sp", v[:], R["Vd"][0][:, hd * 128:(hd + 1) * 128].rearrange("(t p) e -> p t e", p=128), v_b, [R["Vd"][1]])
            for (q0, qn) in TCH:
                if q0 >= L and not ctx_out:
                    continue
                kts = list(range(T // 128)) if q0 < L else [32, 33]
                cpar = nch % 2

                def qk(i, kt, m, drain=False):
                    ps = psS[i % 2][0]
                    _mm1(S, ps_mb[i % 2][m], ps[:, m, :qn], kz[m][0][:, kt * 128:(kt + 1) * 128], q[:, q0:q0 + qn],
                         [kz[m][1], q_b], True, True)
                    p = pt[i % 2][0]
                    S.op("act", lambda h: h.activation(out=p[:, m, :qn], in_=ps[:, m, :qn], func=AF.Exp, scale=0.125),
                         reads=[ps_mb[i % 2][m]], writes=[pt_mb[i % 2][m]])
                qk(0, kts[0], 0, drain=True)
                qk(0, kts[0], 1, drain=True)
                hq = qn // 2
                for i, kt in enumerate(kts):
                    first, last = i == 0, i == len(kts) - 1
                    p = pt[i % 2][0]
                    for m in range(2):
                        p_b = pt_mb[i % 2][m]
                        if not last:
                            qk(i + 1, kts[i + 1], m, drain=(i == 0 and m == 0))
                        _mm1(S, psO[m][1], psO[m][0][:, :qn], v[:, kt, :], p[:, m, :qn], [v_b, p_b], first, last)
                        a_, a_b = acc[m][cpar]
                        if m == 0:
                            parts = [("dve", 0, qn, a_b)]
                        else:
                            parts = [("pool", 0, hq, a_b), ("dve", hq, qn, acc_hi[cpar])]
                        for eng, c0, c1, ab_ in parts:
                            if first:
                                S.op(eng, lambda h, a_=a_, m=m, c0=c0, c1=c1: h.tensor_copy(out=a_[:, c0:c1], in_=p[:, m, c0:c1]), reads=[p_b], writes=[ab_])
                            else:
                                S.op(eng, lambda h, a_=a_, m=m, c0=c0, c1=c1: h.tensor_tensor(out=a_[:, c0:c1], in0=a_[:, c0:c1], in1=p[:, m, c0:c1], op=ALU.add),
                                     reads=[p_b, ab_], writes=[ab_])
                for m in range(2):
                    a_, a_b = acc[m][cpar]
                    _mm_group(S, psZ[m][1], psZ[m][0][:, :qn], [(ones_f[:], a_[:, :qn])], [ones_f_b, a_b] + ([acc_hi[cpar]] if m == 1 else []))
                for m in range(2):
                    S.op("act", lambda h, m=m: h.activation(out=rz[m][0][:, :qn], in_=psZ[m][0][:, :qn], func=AF.Ln), reads=[psZ[m][1]], writes=[rz[m][1]])
                    S.op("act", lambda h, m=m: h.activation(out=rz[m][0][:, :qn], in_=rz[m][0][:, :qn], func=AF.Exp, scale=-1.0), reads=[rz[m][1]], writes=[rz[m][1]])
                    S.op("dve", lambda h, m=m: h.tensor_tensor(out=oo[m][0][:, :qn], in0=psO[m][0][:, :qn], in1=rz[m][0][:, :qn], op=ALU.mult),
                         reads=[psO[m][1], rz[m][1]], writes=[oo[m][1]])
                S.op("dve", lambda h: h.scalar_tensor_tensor(out=of[:, :qn], in0=oo[1][0][:, :qn], scalar=lsc[:, 2:3], in1=oo[0][0][:, :qn],
                                                              op0=ALU.mult, op1=ALU.add), reads=[oo[0][1], oo[1][1], lsc_b], writes=[of_b])
                S.op("pool", lambda h: h.tensor_tensor(out=sqf[:, :qn], in0=of[:, :qn], in1=of[:, :qn], op=ALU.mult), reads=[of_b], writes=[sqf_b])
                ps, ps_b = psZ[0]
                _mm_group(S, ps_b, ps[:, :qn], [(ones_f[:], sqf[:, :qn])], [ones_f_b, sqf_b])
                S.op("dve", lambda h: h.tensor_scalar(out=rs[:, :qn], in0=ps[:, :qn], scalar1=1.0 / 128, scalar2=EPS, op0=ALU.mult, op1=ALU.add),
                     reads=[ps_b], writes=[rs_b])
                S.op("act", lambda h: h.activation(out=rs[:, :qn], in_=rs[:, :qn], func=AF.Ln), reads=[rs_b], writes=[rs_b])
                S.op("act", lambda h: h.activation(out=rs[:, :qn], in_=rs[:, :qn], func=AF.Exp, scale=-0.5), reads=[rs_b], writes=[rs_b])
                S.op("dve", lambda h: h.tensor_tensor(out=of[:, :qn], in0=of[:, :qn], in1=rs[:, :qn], op=ALU.mult), reads=[of_b, rs_b], writes=[of_b])
                o, o_b = ost[nch % 2]
                nch += 1
                S.op("act", lambda h, o=o: h.activation(out=o[:, :qn], in_=of[:, :qn], func=AF.Copy, scale=gsc[:, 0:1]), reads=[of_b, gsc_b], writes=[o_b])
                S.dma("sp", R["YdT"][0][hd * 128:(hd + 1) * 128, q0:q0 + qn], o[:, :qn], R["YdT"][1], [o_b])
        S.barrier()


def phase_swa(K, l, ctx_out):
    nc, S, I, R = K.nc, K.S, K.I, K.R
    ones_b, ones_b_b = K.ones_b
    with contextlib.ExitStack() as st:
        es_, es_b = K.sb("esink", [128, 16], F32, st)
        S.dma("sp", es_[:], _bcast_rows(I["swa_sink"][l], 16), es_b)
        S.op("act", lambda h: h.activation(out=es_[:], in_=es_[:], func=AF.Exp), reads=[es_b], writes=[es_b])
        mlo, mlo_b = K.sb("mlo", [128, 128], BF16, st)
        mup, mup_b = K.sb("mup", [128, 128], BF16, st)
        S.dma("pool", mlo[:], I["trilo"][:, :], mlo_b)
        S.dma("pool", mup[:], I["triup"][:, :], mup_b)
        QC = [[K.sb("sQ%d%d" % (c, i), [128, T], BF16, st) for i in range(2)] for c in range(2)]
        KT = [[K.sb("sKT%d%d" % (i, hf), [128, T], BF16, st) for hf in range(2)] for i in range(2)]
        for i in range(2):
            for hf in range(2):
                S.op("dve", lambda h, i=i, hf=hf: h.memset(KT[i][hf][0][(1 - hf) * 64:(2 - hf) * 64, :], 0.0), writes=[KT[i][hf][1]])
        VV = [K.sb("sV%d" % i, [128, T // 128, 64], BF16, st) for i in range(2)]
        pt = [K.sb("sP%d" % i, [128, 512], BF16, st) for i in range(2)]
        psS = [K.pst("spsS%d" % i, [128, 512], F32, st) for i in range(2)]
        psO = [K.pst("spsO%d" % i, [128, 512], F32, st) for i in range(2)]
        psZ = [K.pst("spsZ%d" % i, [128, 512], F32, st) for i in range(2)]
        zz, zz_b = K.sb("szz", [64, 512], F32, st)
        ost = [K.sb("sost%d" % i, [64, 512], BF16, st) for i in range(2)]
        nblk = 0
        npt = 0
        for kh in range(4):
            kz = KT[kh % 2]
            v, v_b = VV[kh % 2]
            for half in range(2):
                S.dma("sp", kz[half][0][half * 64:(half + 1) * 64, :], R["KsT"][0][kh * 64:(kh + 1) * 64, :], kz[half][1], [R["KsT"][1]])
            S.dma("sp", v[:], R["Vs"][0][:, kh * 64:(kh + 1) * 64].rearrange("(t p) e -> p t e", p=128), v_b, [R["Vs"][1]])
            qc = []
            for c in range(2):
                qq, qq_b = QC[c][kh % 2]
                S.dma("sp", qq[:], R["QsT"][0][kh * 256 + c * 128: kh * 256 + (c + 1) * 128, :], qq_b, [R["QsT"][1]])
                qc.append((qq, qq_b))
            nqb = T // 128 if ctx_out else L // 128
            dbgl = K.cfg.get("swa_dbg", 9)
            if dbgl < 9:
                nqb = 2 if kh == 0 else 0
            steps = []
            for qb in range(nqb):
                if qb < 32:
                    kts = [(kk, mk) for kk, mk in ((qb - 1, "lo"), (qb, None), (qb + 1, "up")) if 0 <= kk < 32] + [(32, None), (33, None)]
                else:
                    kts = [(32, None), (33, None)]
                for i, (kt, mk) in enumerate(kts):
                    steps.append((qb, kt, mk, i == 0, i == len(kts) - 1))

            def stage_a(si):
                qb, kt, mk, first, last = steps[si]
                ps, ps_b = psS[si % 2]
                p, p_b = pt[si % 2]
                for g in range(4):
                    qq, qq_b = qc[g // 2]
                    kk, kk_b = kz[g % 2]
                    _mm1(S, ps_b, ps[:, g * 128:(g + 1) * 128], kk[:, kt * 128:(kt + 1) * 128], qq[:, qb * 128:(qb + 1) * 128],
                         [kk_b, qq_b], True, True)
                S.op("act", lambda h: h.activation(out=p[:], in_=ps[:], func=AF.Exp, scale=0.125), reads=[ps_b], writes=[p_b])
                if mk is not None:
                    mt, mt_b = (mlo, mlo_b) if mk == "lo" else (mup, mup_b)
                    for g in range(4):
                        S.op("pool", lambda h, g=g, mt=mt: h.tensor_tensor(out=p[:, g * 128:(g + 1) * 128], in0=p[:, g * 128:(g + 1) * 128], in1=mt[:], op=ALU.mult),
                             reads=[p_b, mt_b], writes=[p_b])

            if steps:
                stage_a(0)
            for si, (qb, kt, mk, first, last) in enumerate(steps):
                if si + 1 < len(steps):
                    stage_a(si + 1)
                po, po_b = psO[qb % 2]
                pz, pz_b = psZ[qb % 2]
                p, p_b = pt[si % 2]
                _mm1(S, po_b, po[0:64, :], v[:, kt, :], p[:], [v_b, p_b], first, last)
                _mm1(S, pz_b, pz[0:64, :], ones_b[:, 0:64], p[:], [ones_b_b, p_b], first, last)
                if not last:
                    continue
                for g in range(4):
                    S.op("dve", lambda h, g=g: h.tensor_scalar(out=zz[:, g * 128:(g + 1) * 128], in0=pz[0:64, g * 128:(g + 1) * 128],
                                                               scalar1=es_[0:64, kh * 4 + g:kh * 4 + g + 1], scalar2=None, op0=ALU.add),
                         reads=[pz_b, es_b], writes=[zz_b])
                S.op("dve", lambda h: h.reciprocal(out=zz[:], in_=zz[:]), reads=[zz_b], writes=[zz_b])
                o, o_b = ost[qb % 2]
                S.op("dve", lambda h, o=o: h.tensor_tensor(out=o[:], in0=po[0:64, :], in1=zz[:], op=ALU.mult),
                     reads=[po_b, zz_b], writes=[o_b])
                S.dma("sp", R["YsT"][0][kh * 256:(kh + 1) * 256, qb * 128:(qb + 1) * 128].rearrange("(g d) q -> d g q", g=4),
                      o[:].rearrange("p (g q) -> p g q", g=4), R["YsT"][1], [o_b])
        S.barrier()


def phase_merge(K, l):
    nc, S, I, R = K.nc, K.S, K.I, K.R
    modT, modT_b = K.modT
    with contextlib.ExitStack() as st:
        wb, wb_b = K.sb("wbr", [128, 24, D], BF16, st)
        wo, wo_b = K.sb("wout", [128, 8, D], BF16, st)
        for b in range(3):
            S.dma("pool", wb[:, b * 8:(b + 1) * 8, :], I["w_branch"][l, b].rearrange("(k p) n -> p k n", p=128), wb_b)
        S.dma("pool", wo[:], I["w_out"][l].rearrange("(k p) n -> p k n", p=128), wo_b)
        yb = [K.sb("my%d" % b, [128, 8, 512], BF16, st) for b in range(3)]
        gt, gt_b = K.sb("mgt", [128, 24, 512], BF16, st)
        mg, mg_b = K.sb("mmg", [128, 8, 512], F32, st)
        mgb, mgb_b = K.sb("mmgb", [128, 8, 512], BF16, st)
        tmp = [K.sb("mtmp%d" % i, [128, 512], F32, st) for i in range(2)]
        xx, xx_b = K.sb("mx", [128, 8, 512], F32, st)
        pss = [K.pst("mps%d" % i, [128, 512], F32, st) for i in range(4)]
        npz = 0
        ntmp = 0
        XTv = R["XT"][0].rearrange("(k p) t -> p k t", p=128)
        for (t0, tn) in TCH:
            r = 0 if t0 < L else 1
            for b, nm in enumerate(("YdT", "YhT", "YsT")):
                S.dma("sp", yb[b][0][:, :, :tn], R[nm][0].rearrange("(k p) t -> p k t", p=128)[:, :, t0:t0 + tn], yb[b][1], [R[nm][1]])
            S.dma("sp", gt[:, :, :tn], R["GT"][0].rearrange("(k p) t -> p k t", p=128)[:, :, t0:t0 + tn], gt_b, [R["GT"][1]])
            S.dma("sp", xx[:, :, :tn], XTv[:, :, t0:t0 + tn], xx_b, [R["XT"][1]])
            for m in range(8):
                for b in range(3):
                    ps, ps_b = pss[npz % 4]
                    npz += 1
                    _mm_group(S, ps_b, ps[:, :tn], [(wb[:, b * 8 + k, m * 128:(m + 1) * 128], yb[b][0][:, k, :tn]) for k in range(8)],
                              [wb_b, yb[b][1]])
                    if b == 0:
                        S.op("dve", lambda h, m=m, ps=ps: h.tensor_tensor(out=mg[:, m, :tn], in0=ps[:, :tn], in1=gt[:, m, :tn], op=ALU.mult),
                             reads=[ps_b, gt_b], writes=[mg_b])
                    else:
                        tp, tp_b = tmp[ntmp % 2]
                        ntmp += 1
                        S.op("dve", lambda h, m=m, b=b, ps=ps, tp=tp: h.tensor_tensor(out=tp[:, :tn], in0=ps[:, :tn], in1=gt[:, b * 8 + m, :tn], op=ALU.mult),
                             reads=[ps_b, gt_b], writes=[tp_b])
                        S.op("pool", lambda h, m=m, tp=tp: h.tensor_tensor(out=mg[:, m, :tn], in0=mg[:, m, :tn], in1=tp[:, :tn], op=ALU.add),
                             reads=[tp_b, mg_b], writes=[mg_b])
                S.op("act", lambda h, m=m: h.activation(out=mgb[:, m, :tn], in_=mg[:, m, :tn], func=AF.Copy), reads=[mg_b], writes=[mgb_b])
            for m in range(8):
                ps, ps_b = pss[npz % 4]
                npz += 1
                _mm_group(S, ps_b, ps[:, :tn], [(wo[:, k, m * 128:(m + 1) * 128], mgb[:, k, :tn]) for k in range(8)], [wo_b, mgb_b])
                S.op("dve", lambda h, m=m, ps=ps: h.scalar_tensor_tensor(out=xx[:, m, :tn], in0=ps[:, :tn], scalar=modT[:, 16 + m, r:r + 1],
                                                                         in1=xx[:, m, :tn], op0=ALU.mult, op1=ALU.add),
                     reads=[ps_b, modT_b, xx_b], writes=[xx_b])
            S.dma("sp", XTv[:, :, t0:t0 + tn], xx[:, :, :tn], R["XT"][1], [xx_b])
        S.barrier()


PI = math.pi


def _hy_filter(K, l, Lx, embT_ap, negt_ap, Cm, Sm, wf_ap, KFs):
    nc, S, I, R = K.nc, K.S, K.I, K.R
    ones_b, ones_b_b = K.ones_b
    nlt = Lx // 128
    nft = (Lx + 128) // 128
    nch = max(1, Lx // 512)
    cw = min(512, Lx)
    with contextlib.ExitStack() as st:
        w1, w1_b = K.sb("fw1", [33, 64], F32, st)
        w2, w2_b = K.sb("fw2", [64, 64], F32, st)
        w3, w3_b = K.sb("fw3", [64, 2048], F32, st)
        b1, b1_b = K.sb("fb1", [64, 1], F32, st)
        b2, b2_b = K.sb("fb2", [64, 1], F32, st)
        fr, fr_b = K.sb("ffr", [64, 1], F32, st)
        emb, emb_b = K.sb("femb", [33, Lx], F32, st)
        ngt, ngt_b = K.sb("fngt", [128, nlt], F32, st)
        dl, dl_b = K.sb("fdl", [128, D], F32, st)
        wf, wf_b = K.sb("fwf", [128, nft], F32, st)
        z1, z1_b = K.sb("fz1", [64, Lx], F32, st)
        z2, z2_b = K.sb("fz2", [64, Lx], F32, st)
        rn, rn_b = K.sb("frn", [128, D], F32, st)
        dec, dec_b = K.sb("fdec", [128, D], F32, st)
        hd = [K.sb("fhd%d" % i, [128, D], F32, st) for i in range(2)]
        ab, ab_b = K.sb("fab", [128, 512], BF16, st)
        gsel, gsel_b = K.sb("fgsel", [64, 512], F32, st)
        AT, AT_b = K.sb("fAT", [128, nlt, D], BF16, st)
        for t_, src in ((w1, I["hy_w1"][l]), (w2, I["hy_w2"][l]), (w3, I["hy_w3"][l]), (b1, I["hy_b1"][l]), (b2, I["hy_b2"][l]),
                        (fr, I["hy_fr"][l]), (emb, embT_ap), (ngt, negt_ap), (wf, wf_ap)):
            pass
        S.dma("sp", w1[:], I["hy_w1"][l], w1_b)
        S.dma("sp", w2[:], I["hy_w2"][l], w2_b)
        S.dma("sp", w3[:], I["hy_w3"][l], w3_b)
        S.dma("sp", b1[:], I["hy_b1"][l], b1_b)
        S.dma("sp", b2[:], I["hy_b2"][l], b2_b)
        S.dma("sp", fr[:], I["hy_fr"][l], fr_b)
        S.dma("sp", emb[:], embT_ap, emb_b)
        S.dma("sp", ngt[:], negt_ap, ngt_b)
        S.dma("sp", wf[:], wf_ap, wf_b)
        S.dma("sp", dl[:], _bcast_rows(I["deltas"][0], D), dl_b)
        psm = [K.pst("fps%d" % i, [128, 512], F32, st) for i in range(4)]
        psN = [K.pst("fpsN%d" % i, [128, 512], F32, st) for i in range(2)]
        for (wm, wm_b, bb, bb_b, src, src_b, dst, dst_b) in ((w1, w1_b, b1, b1_b, emb, emb_b, z1, z1_b), (w2, w2_b, b2, b2_b, z1, z1_b, z2, z2_b)):
            for ci in range(nch):
                ps, ps_b = psm[ci % 4]
                sl = slice(ci * cw, (ci + 1) * cw)
                _mm_group(S, ps_b, ps[0:64, :cw], [(wm[:], src[:, sl])], [wm_b, src_b])
                S.op("dve", lambda h, ps=ps, sl=sl, bb=bb, dst=dst: h.tensor_scalar(out=dst[:, sl], in0=ps[0:64, :cw], scalar1=bb[:, 0:1], scalar2=fr[:, 0:1],
                                                                                 op0=ALU.add, op1=ALU.mult), reads=[ps_b, bb_b, fr_b], writes=[dst_b])
                for _ in range(2):
                    for cop, sh in ((ALU.is_gt, -2.0 * PI), (ALU.is_lt, 2.0 * PI)):
                        thr = PI if cop == ALU.is_gt else -PI
                        S.op("dve", lambda h, sl=sl, dst=dst, cop=cop, thr=thr: h.tensor_scalar(out=gsel[:, :cw], in0=dst[:, sl], scalar1=thr, scalar2=None, op0=cop),
                             reads=[dst_b], writes=[gsel_b])
                        S.op("dve", lambda h, sl=sl, dst=dst, sh=sh: h.scalar_tensor_tensor(out=dst[:, sl], in0=gsel[:, :cw], scalar=sh, in1=dst[:, sl],
                                                                                          op0=ALU.mult, op1=ALU.add), reads=[gsel_b, dst_b], writes=[dst_b])
                S.op("act", lambda h, sl=sl, dst=dst: h.activation(out=dst[:, sl], in_=dst[:, sl], func=AF.Sin), reads=[dst_b], writes=[dst_b])

        def htile(lt, cb):
            S.op("act", lambda h: h.activation(out=dec[:], in_=dl[:], func=AF.Exp, scale=ngt[:, lt:lt + 1]), reads=[dl_b, ngt_b], writes=[dec_b])
            for j in range(4):
                ps, ps_b = psm[j]
                _mm_group(S, ps_b, ps[:], [(z2[:, lt * 128:(lt + 1) * 128], w3[:, j * 512:(j + 1) * 512])], [z2_b, w3_b])
            cb(lt)

        for lt in range(nlt):
            def cb1(lt):
                for j in range(4):
                    ps, ps_b = psm[j]
                    half = j % 2
                    tf, tf_b = hd[j // 2]
                    S.op("dve", lambda h, ps=ps, half=half, tf=tf: h.tensor_tensor(out=tf[:, half * 512:(half + 1) * 512], in0=ps[:],
                                                                                   in1=dec[:, half * 512:(half + 1) * 512], op=ALU.mult),
                         reads=[ps_b, dec_b], writes=[tf_b])
                    S.op("act", lambda h, half=half, tf=tf: h.activation(out=ab[:], in_=tf[:, half * 512:(half + 1) * 512], func=AF.Abs),
                         reads=[tf_b], writes=[ab_b])
                    first = (lt == 0 and j < 2)
                    last = (lt == nlt - 1 and j >= 2)
                    _mm1(S, psN[half][1], psN[half][0][:], ones_b[:], ab[:], [ones_b_b, ab_b], first, last)
            htile(lt, cb1)
        for half in range(2):
            S.op("dve", lambda h, half=half: h.tensor_scalar(out=rn[:, half * 512:(half + 1) * 512], in0=psN[half][0][:], scalar1=EPS, scalar2=None, op0=ALU.add),
                 reads=[psN[half][1]], writes=[rn_b])
        S.op("dve", lambda h: h.reciprocal(out=rn[:], in_=rn[:]), reads=[rn_b], writes=[rn_b])
        for plane, op2 in (("C", ALU.add), ("S", ALU.subtract)):
            for lt in range(nlt):
                def cb2(lt):
                    for j in range(4):
                        ps, ps_b = psm[j]
                        half, dr = j % 2, j // 2
                        S.op("dve", lambda h, ps=ps, half=half, dr=dr: h.tensor_tensor(out=hd[dr][0][:, half * 512:(half + 1) * 512], in0=ps[:],
                                                                                      in1=dec[:, half * 512:(half + 1) * 512], op=ALU.mult),
                             reads=[ps_b, dec_b], writes=[hd[dr][1]])
                    if lt == 0:
                        S.op("dve", lambda h: h.memset(hd[1][0][0:1, :], 0.0), writes=[hd[1][1]])
                    S.op("pool", lambda h: h.tensor_tensor(out=hd[0][0][:], in0=hd[0][0][:], in1=hd[1][0][:], op=op2), reads=[hd[0][1], hd[1][1]], writes=[hd[0][1]])
                    S.op("pool", lambda h: h.tensor_tensor(out=AT[:, lt, :], in0=hd[0][0][:], in1=rn[:], op=ALU.mult), reads=[hd[0][1], rn_b], writes=[AT_b])
                htile(lt, cb2)
            pi = 0 if plane == "C" else 1

            def evac(ft, pc, ps_, pi=pi):
                src = pc if pi == 0 else ps_
                for hh in range(2):
                    o, o_b = hd[hh]
                    S.op("dve", lambda h, hh=hh, o=o: h.tensor_scalar(out=o[:, 0:512], in0=src[hh][0][:], scalar1=wf[:, ft:ft + 1], scalar2=None, op0=ALU.mult),
                         reads=[src[hh][1], wf_b], writes=[o_b])
                    S.dma("sp", KFs[0][pi, ft * 128:(ft + 1) * 128, hh * 512:(hh + 1) * 512], o[:, 0:512], KFs[1], [o_b])
            _fwd_dft(K, st, Cm, Sm, nlt, nft, AT, AT_b, 0, (plane,), evac, psm)
        S.barrier()


def _fwd_dft(K, st, Cm, Sm, ntt, nft, rhs, rhs_b, tt0, planes, evac, pspool):
    S = K.S
    with contextlib.ExitStack() as s2:
        blk = {p: [K.sb("dblk%s%d" % (p, i), [128, ntt, 128], BF16, s2) for i in range(2)] for p in planes}
        for ft in range(nft):
            pss = {}
            for pi, p in enumerate(("C", "S")):
                if p not in planes:
                    pss[p] = None
                    continue
                M = Cm if p == "C" else Sm
                b, b_b = blk[p][ft % 2]
                S.dma("sp", b[:], M[ft, :, 0:ntt, :], b_b)
                pss[p] = [pspool[pi * 2 + hh] for hh in range(2)]
                for hh in range(2):
                    ps, ps_b = pss[p][hh]
                    _mm_group(S, ps_b, ps[:], [(b[:, t, :], rhs[:, tt0 + t, hh * 512:(hh + 1) * 512]) for t in range(ntt)], [b_b, rhs_b])
            evac(ft, pss["C"], pss["S"])


def phase_hyena(K, l, ctx_out):
    nc, S, I, R = K.nc, K.S, K.I, K.R
    ident_b, ident_b_b = K.ident_b
    _hy_filter(K, l, L, I["embT"][:, :], I["negt"][:, :], I["dftCt"], I["dftSt"], I["wf"][:, :], R["KF"])
    if ctx_out:
        _hy_filter(K, l, LC, I["embTc"][:, :], I["negtc"][:, :], I["dftCct"], I["dftSct"], I["wfc"][:, :], R["KFc"])
    segs = [(0, L)] + ([(L, LC)] if ctx_out else [])
    TT = T if ctx_out else L
    with contextlib.ExitStack() as st:
        utok, utok_b = K.sb("hutok", [128, T // 128, D], BF16, st)
        with contextlib.ExitStack() as s2:
            cw, cw_b = K.sb("hcw", [128, 24, 3], F32, s2)
            cb, cb_b = K.sb("hcb", [128, 24], F32, s2)
            S.dma("sp", cw[:], I["cwT"][l], cw_b)
            S.dma("sp", cb[:], I["cbT"][l], cb_b)
            xin = [K.sb("hxin%d" % i, [128, T], BF16, s2) for i in range(3)]
            yc = [K.sb("hyc%d" % i, [128, T], F32, s2) for i in range(3)]
            x0b, x0b_b = K.sb("hx0b", [128, T], BF16, s2)
            ub, ub_b = K.sb("hub", [128, T], BF16, s2)
            ptr = [K.pst("hptr%d" % i, [128, 1024], BF16, s2) for i in range(2)]
            ntr = 0
            for c in range(8):
                for s_ in range(3):
                    j = s_ * 8 + c
                    xi, xi_b = xin[s_]
                    y, y_b = yc[s_]
                    S.dma("sp", xi[:, :TT], R["HyT"][0][j * 128:(j + 1) * 128, 0:TT], xi_b, [R["HyT"][1]])
                    for (a0, n) in segs:
                        S.op("dve", lambda h, j=j, xi=xi, y=y: h.tensor_scalar(out=y[:, a0:a0 + n], in0=xi[:, a0:a0 + n], scalar1=cw[:, j, 1:2], scalar2=cb[:, j:j + 1],
                                                                            op0=ALU.mult, op1=ALU.add), reads=[xi_b, cw_b, cb_b], writes=[y_b])
                        S.op("dve", lambda h, j=j, xi=xi, y=y: h.scalar_tensor_tensor(out=y[:, a0 + 1:a0 + n], in0=xi[:, a0:a0 + n - 1], scalar=cw[:, j, 0:1],
                                                                                   in1=y[:, a0 + 1:a0 + n], op0=ALU.mult, op1=ALU.add),
                             reads=[xi_b, cw_b, y_b], writes=[y_b])
                        S.op("dve", lambda h, j=j, xi=xi, y=y: h.scalar_tensor_tensor(out=y[:, a0:a0 + n - 1], in0=xi[:, a0 + 1:a0 + n], scalar=cw[:, j, 2:3],
                                                                                   in1=y[:, a0:a0 + n - 1], op0=ALU.mult, op1=ALU.add),
                             reads=[xi_b, cw_b, y_b], writes=[y_b])
                S.op("act", lambda h: h.activation(out=x0b[:, :TT], in_=yc[0][0][:, :TT], func=AF.Copy), reads=[yc[0][1]], writes=[x0b_b])
                S.op("pool", lambda h: h.tensor_tensor(out=ub[:, :TT], in0=yc[1][0][:, :TT], in1=yc[2][0][:, :TT], op=ALU.mult),
                     reads=[yc[1][1], yc[2][1]], writes=[ub_b])
                S.dma("sp", R["X0T"][0][c * 128:(c + 1) * 128, 0:TT], x0b[:, :TT], R["X0T"][1], [x0b_b])
                S.dma("sp", R["UT"][0][c * 128:(c + 1) * 128, 0:TT], ub[:, :TT], R["UT"][1], [ub_b])
                for t8 in range(0, TT // 128, 8):
                    n8 = min(8, TT // 128 - t8)
                    pt_, pt_b = ptr[ntr % 2]
                    ntr += 1

                    def trf(h, t8=t8, n8=n8, pt_=pt_):
                        ins = None
                        for q in range(n8):
                            ins = h.transpose(pt_[:, q * 128:(q + 1) * 128], ub[:, (t8 + q) * 128:(t8 + q + 1) * 128], ident_b[:])
                        return ins
                    S.op("pe", trf, reads=[ub_b, ident_b_b], writes=[pt_b])
                    S.op("act", lambda h, t8=t8, n8=n8, pt_=pt_, c=c: h.activation(out=utok[:, t8:t8 + n8, c * 128:(c + 1) * 128],
                                                                                   in_=pt_[:, :n8 * 128].rearrange("p (q f) -> p q f", f=128), func=AF.Copy),
                         reads=[pt_b], writes=[utok_b])
            S.barrier()
        for si, (a0, n) in enumerate(segs):
            Cm, Sm = (I["dftC"], I["dftS"]) if si == 0 else (I["dftCc"], I["dftSc"])
            KFs = R["KF"] if si == 0 else R["KFc"]
            YFs = R["YF"] if si == 0 else R["YFc"]
            ntt = n // 128
            nft = (n + 128) // 128
            with contextlib.ExitStack() as s2:
                kf = [K.sb("hkf%d" % i, [128, 2, D], F32, s2) for i in range(2)]
                tq = [K.sb("htq%d" % i, [128, 512], F32, s2) for i in range(4)]
                yo = [K.sb("hyo%d" % i, [128, 2, D], BF16, s2) for i in range(2)]
                pspool = [K.pst("hps%d" % i, [128, 512], F32, s2) for i in range(4)]

                def evac(ft, pc, ps_, KFs=KFs, YFs=YFs):
                    k_, k_b = kf[ft % 2]
                    o, o_b = yo[ft % 2]
                    S.dma("sp", k_[:], KFs[0][:, ft * 128:(ft + 1) * 128, :].rearrange("a p c -> p a c"), k_b, [KFs[1]])
                    for hh in range(2):
                        cs = slice(hh * 512, (hh + 1) * 512)
                        ur, ur_b = pc[hh]
                        ui, ui_b = ps_[hh]
                        S.op("dve", lambda h: h.tensor_tensor(out=tq[0][0][:], in0=ur[:], in1=k_[:, 0, cs], op=ALU.mult), reads=[ur_b, k_b], writes=[tq[0][1]])
                        S.op("dve", lambda h: h.tensor_tensor(out=tq[1][0][:], in0=ui[:], in1=k_[:, 1, cs], op=ALU.mult), reads=[ui_b, k_b], writes=[tq[1][1]])
                        S.op("pool", lambda h: h.tensor_tensor(out=o[:, 0, cs], in0=tq[0][0][:], in1=tq[1][0][:], op=ALU.subtract),
                             reads=[tq[0][1], tq[1][1]], writes=[o_b])
                        S.op("dve", lambda h: h.tensor_tensor(out=tq[2][0][:], in0=ur[:], in1=k_[:, 1, cs], op=ALU.mult), reads=[ur_b, k_b], writes=[tq[2][1]])
                        S.op("dve", lambda h: h.tensor_tensor(out=tq[3][0][:], in0=ui[:], in1=k_[:, 0, cs], op=ALU.mult), reads=[ui_b, k_b], writes=[tq[3][1]])
                        S.op("pool", lambda h: h.tensor_tensor(out=o[:, 1, cs], in0=tq[2][0][:], in1=tq[3][0][:], op=ALU.add),
                             reads=[tq[2][1], tq[3][1]], writes=[o_b])
                    S.dma("sp", YFs[0][:, ft * 128:(ft + 1) * 128, :].rearrange("a p c -> p a c"), o[:], YFs[1], [o_b])
                Cmt, Smt = (I["dftCt"], I["dftSt"]) if si == 0 else (I["dftCct"], I["dftSct"])
                _fwd_dft(K, s2, Cmt, Smt, ntt, nft, utok, utok_b, a0 // 128, ("C", "S"), evac, pspool)
                S.barrier()
    for si, (a0, n) in enumerate(segs):
        Cm, Sm = (I["dftC"], I["dftS"]) if si == 0 else (I["dftCc"], I["dftSc"])
        YFs = R["YF"] if si == 0 else R["YFc"]
        nft = (n + 128) // 128
        with contextlib.ExitStack() as s2:
            hb, hb_b = K.sb("ihb", [128, 8], F32, s2)
            S.dma("sp", hb[:], I["hbT"][l], hb_b)
            Yh = [K.sb("iY%d" % i, [128, nft, 512], BF16, s2) for i in range(2)]
            Cr = [K.sb("iC%d" % i, [128, nft, 256], BF16, s2) for i in range(2)]
            Sr = [K.sb("iS%d" % i, [128, nft, 256], BF16, s2) for i in range(2)]
            x0c = [K.sb("ix0%d" % i, [128, 4, 256], BF16, s2) for i in range(2)]
            uc = [K.sb("iu%d" % i, [128, 4, 256], BF16, s2) for i in range(2)]
            tmp, tmp_b = K.sb("itmp", [128, 256], F32, s2)
            og = [K.sb("iog%d" % i, [128, 4, 256], BF16, s2) for i in range(2)]
            psI = [K.pst("ips%d" % i, [128, 512], F32, s2) for i in range(2)]
            nk = 0
            npi = 0
            for half in range(2):
                for pl in range(2):
                    S.dma("sp", Yh[pl][0][:], YFs[0][pl, 0:nft * 128, half * 512:(half + 1) * 512].rearrange("(f p) c -> p f c", p=128), Yh[pl][1], [YFs[1]])
                for t0 in range(0, n, 256):
                    cr, cr_b = Cr[nk % 2]
                    sr, sr_b = Sr[nk % 2]
                    xx, xx_b = x0c[nk % 2]
                    uu, uu_b = uc[nk % 2]
                    oo, oo_b = og[nk % 2]
                    nk += 1
                    S.dma("sp", cr[:], Cm.rearrange("(f p) t -> p f t", p=128)[:, 0:nft, t0:t0 + 256], cr_b)
                    S.dma("sp", sr[:], Sm.rearrange("(f p) t -> p f t", p=128)[:, 0:nft, t0:t0 + 256], sr_b)
                    rows = slice(half * 512, (half + 1) * 512)
                    S.dma("sp", xx[:], R["X0T"][0][rows, a0 + t0:a0 + t0 + 256].rearrange("(c p) t -> p c t", p=128), xx_b, [R["X0T"][1]])
                    S.dma("sp", uu[:], R["UT"][0][rows, a0 + t0:a0 + t0 + 256].rearrange("(c p) t -> p c t", p=128), uu_b, [R["UT"][1]])
                    for ci in range(4):
                        ps, ps_b = psI[npi % 2]
                        npi += 1
                        pairs = [(Yh[0][0][:, f, ci * 128:(ci + 1) * 128], cr[:, f, :]) for f in range(nft)] + \
                                [(Yh[1][0][:, f, ci * 128:(ci + 1) * 128], sr[:, f, :]) for f in range(nft)]
                        _mm_group(S, ps_b, ps[:, 0:256], pairs, [Yh[0][1], Yh[1][1], cr_b, sr_b])
                        cidx = half * 4 + ci
                        S.op("dve", lambda h, ci=ci, cidx=cidx, ps=ps, uu=uu: h.scalar_tensor_tensor(out=tmp[:], in0=uu[:, ci, :], scalar=hb[:, cidx:cidx + 1],
                                                                                                    in1=ps[:, 0:256], op0=ALU.mult, op1=ALU.add),
                             reads=[uu_b, hb_b, ps_b], writes=[tmp_b])
                        S.op("pool", lambda h, ci=ci, oo=oo, xx=xx: h.tensor_tensor(out=oo[:, ci, :], in0=tmp[:], in1=xx[:, ci, :], op=ALU.mult),
                             reads=[tmp_b, xx_b], writes=[oo_b])
                    S.dma("sp", R["YhT"][0][rows, a0 + t0:a0 + t0 + 256].rearrange("(c p) t -> p c t", p=128), oo[:], R["YhT"][1], [oo_b])
            S.barrier()
    if not ctx_out:
        pass


def _bc_mid(ap2d, n):
    a = ap2d.ap
    return bass.AP(ap2d.tensor, ap2d.offset, [list(a[0]), [0, n], list(a[1])])


def phase_moe(K, l, ctx_out):
    nc, S, I, R = K.nc, K.S, K.I, K.R
    modT, modT_b = K.modT
    A2, A2_b = K.A2
    ones_f, ones_f_b = K.ones_f
    ones_b, ones_b_b = K.ones_b
    ident_f, ident_f_b = K.ident_f
    ident_b, ident_b_b = K.ident_b
    debug = K.cfg.get("debug", ())
    XTv = R["XT"][0].rearrange("(k p) t -> p k t", p=128)
    chunks = TCH if ctx_out else TCH[:8]
    ntt = 34 if ctx_out else 32
    groups = [(0, 32, CAP)] + ([(32, 34, CAPC)] if ctx_out else [])
    with contextlib.ExitStack() as st:
        lg, lg_b = K.sb("lg", [128, 34, NE], F32, st)
        aff, aff_b = K.sb("aff", [128, 34, NE], F32, st)
        psel, psel_b = K.sb("psel", [128, 34, NE], F32, st)
        wr, wr_b = K.sb("wr", [128, 8, NE], F32, st)
        S.dma("sp", wr[:], I["router_w"][l].rearrange("(k p) e -> p k e", p=128), wr_b)
        sH = contextlib.ExitStack()
        H2, H2_b = K.sb("H2", [128, 34, D], BF16, sH)
        if ctx_out is False:
            S.op("dve", lambda h: h.memset(lg[:, 32:34, :], 0.0), writes=[lg_b])
        with contextlib.ExitStack() as s2:
            xs = [K.sb("qx%d" % i, [128, 8, 512], F32, s2) for i in range(2)]
            sq, sq_b = K.sb("qsq", [128, 8, 512], F32, s2)
            rstd, rstd_b = K.sb("qrstd", [128, 512], F32, s2)
            tmp, tmp_b = K.sb("qtmp", [128, 512], F32, s2)
            h2f, h2f_b = K.sb("qh2f", [128, 8, 512], F32, s2)
            hTc, hTc_b = K.sb("qhTc", [128, 8, 512], BF16, s2)
            ps, ps_b = K.pst("qps", [128, 512], F32, s2)
            psl, psl_b = K.pst("qpsl", [128, 64], F32, s2)
            pstr = [K.pst("qpst%d" % i, [128, 1024], BF16, s2) for i in range(2)]
            ntr = 0
            for ci, (t0, tn) in enumerate(chunks):
                r = 0 if t0 < L else 1
                x, x_b = xs[ci % 2]
                S.dma("sp", x[:, :, :tn], XTv[:, :, t0:t0 + tn], x_b, [R["XT"][1]])
                if "XT1" in debug:
                    S.dma("sp", R["XT1"][0].rearrange("(k p) t -> p k t", p=128)[:, :, t0:t0 + tn], x[:, :, :tn], R["XT1"][1], [x_b])
                S.op("act", lambda h: h.activation(out=sq[:, :, :tn], in_=x[:, :, :tn], func=AF.Square), reads=[x_b], writes=[sq_b])
                _mm_group(S, ps_b, ps[:, :tn], [(ones_f[:], sq[:, k, :tn]) for k in range(8)], [ones_f_b, sq_b])
                S.op("dve", lambda h: h.tensor_scalar(out=rstd[:, :tn], in0=ps[:, :tn], scalar1=1.0 / D, scalar2=EPS,
                                                      op0=ALU.mult, op1=ALU.add), reads=[ps_b], writes=[rstd_b])
                S.op("act", lambda h: h.activation(out=rstd[:, :tn], in_=rstd[:, :tn], func=AF.Sqrt), reads=[rstd_b], writes=[rstd_b])
                S.op("dve", lambda h: h.reciprocal(out=rstd[:, :tn], in_=rstd[:, :tn]), reads=[rstd_b], writes=[rstd_b])
                for k in range(8):
                    S.op("dve", lambda h, k=k: h.tensor_tensor(out=tmp[:, :tn], in0=x[:, k, :tn], in1=rstd[:, :tn], op=ALU.mult),
                         reads=[x_b, rstd_b], writes=[tmp_b])
                    S.op("act", lambda h, k=k: h.activation(out=h2f[:, k, :tn], in_=tmp[:, :tn], func=AF.Identity,
                                                            scale=A2[:, k, r:r + 1], bias=modT[:, 24 + k, r:r + 1]),
                         reads=[tmp_b, A2_b, modT_b], writes=[h2f_b])
                    S.op("pool", lambda h, k=k: h.tensor_copy(out=hTc[:, k, :tn], in_=h2f[:, k, :tn]), reads=[h2f_b], writes=[hTc_b])
                nj = tn // 128
                for j in range(nj):
                    tt = t0 // 128 + j
                    _mm_group(S, psl_b, psl[:, j * 16:(j + 1) * 16],
                              [(h2f[:, k, j * 128:(j + 1) * 128], wr[:, k, :]) for k in range(8)], [h2f_b, wr_b])
                    pt_, pt_b = pstr[ntr % 2]
                    ntr += 1

                    def trf(h, j=j, pt_=pt_):
                        ins = None
                        for k in range(8):
                            ins = h.transpose(pt_[:, k * 128:(k + 1) * 128], hTc[:, k, j * 128:(j + 1) * 128], ident_b[:])
                        return ins
                    S.op("pe", trf, reads=[hTc_b, ident_b_b], writes=[pt_b])
                    S.op("act", lambda h, tt=tt, pt_=pt_: h.activation(out=H2[:, tt, :], in_=pt_[:], func=AF.Copy), reads=[pt_b], writes=[H2_b])
                S.op("dve", lambda h: h.tensor_copy(out=lg[:, t0 // 128:t0 // 128 + nj, :].rearrange("p t e -> p (t e)"),
                                                    in_=psl[:, :nj * 16]), reads=[psl_b], writes=[lg_b])
            S.barrier()
        if "LG" in debug:
            S.dma("sp", R["LG"][0], lg[:], R["LG"][1], [lg_b])
            S.dma("sp", R["H2d"][0], H2[:], R["H2d"][1], [H2_b])
        with contextlib.ExitStack() as s2:
            se, se_b = K.sb("rse", [128, 34], F32, s2)
            lo, lo_b = K.sb("rlo", [128, NE], F32, s2)
            mid, mid_b = K.sb("rmid", [128, NE], F32, s2)
            cmpt, cmp_b = K.sb("rcmp", [128, 32, NE], F32, s2)
            cntp, cntp_b = K.sb("rcntp", [128, NE], F32, s2)
            ge, ge_b = K.sb("rge", [128, NE], F32, s2)
            mk, mk_b = K.sb("rmk", [128, 34, NE], F32, s2)
            mkb, mkb_b = K.sb("rmkb", [128, 34, NE], BF16, s2)
            tot, tot_b = K.sb("rtot", [128, 34, NE], F32, s2)
            base, base_b = K.sb("rbase", [128, 34, NE], F32, s2)
            posT, posT_b = K.sb("posT", [16, T], F32, s2)
            affT, affT_b = K.sb("affT", [16, T], F32, s2)
            us, us_b = K.sb("rus", [128, 128], BF16, s2)
            S.dma("pool", us[:], I["ustrict"][:, :], us_b)
            psc, psc_b = K.pst("rpsc", [128, 512], F32, s2)
            psw, psw_b = K.pst("rpsw", [128, 512], F32, s2)
            pst_, pst_b = K.pst("rpst", [128, 512], F32, s2)
            S.op("act", lambda h: h.activation(out=aff[:].rearrange("p t e -> p (t e)"), in_=lg[:].rearrange("p t e -> p (t e)"), func=AF.Exp),
                 reads=[lg_b], writes=[aff_b])
            S.op("dve", lambda h: h.reduce_sum(out=se[:], in_=aff[:], axis=AX.X), reads=[aff_b], writes=[se_b])
            S.op("dve", lambda h: h.reciprocal(out=se[:], in_=se[:]), reads=[se_b], writes=[se_b])
            for tt in range(34):
                S.op("dve", lambda h, tt=tt: h.tensor_scalar(out=aff[:, tt, :], in0=aff[:, tt, :], scalar1=se[:, tt:tt + 1], scalar2=None, op0=ALU.mult),
                     reads=[aff_b, se_b], writes=[aff_b])
            S.op("dve", lambda h: h.memset(mk[:], 0.0), writes=[mk_b])
            S.op("dve", lambda h: h.memset(base[:], 0.0), writes=[base_b])
            for (ta, tb, cap) in groups:
                nt = tb - ta
                S.op("dve", lambda h: h.memset(lo[:], 0.0), writes=[lo_b])
                for it in range(30):
                    w = 0.5 ** (it + 1)
                    S.op("dve", lambda h: h.tensor_scalar(out=mid[:], in0=lo[:], scalar1=w, scalar2=None, op0=ALU.add), reads=[lo_b], writes=[mid_b])
                    S.op("dve", lambda h: h.tensor_tensor(out=cmpt[:, :nt, :], in0=aff[:, ta:tb, :], in1=_bc_mid(mid[:], nt), op=ALU.is_ge),
                         reads=[aff_b, mid_b], writes=[cmp_b])
                    S.op("dve", lambda h: h.reduce_sum(out=cntp[:], in_=cmpt[:, :nt, :].rearrange("p t e -> p e t"), axis=AX.X),
                         reads=[cmp_b], writes=[cntp_b])
                    _mm_group(S, psc_b, psc[:, 0:NE], [(ones_f[:], cntp[:])], [ones_f_b, cntp_b])
                    S.op("dve", lambda h: h.tensor_scalar(out=ge[:], in0=psc[:, 0:NE], scalar1=cap - 0.5, scalar2=None, op0=ALU.is_ge),
                         reads=[psc_b], writes=[ge_b])
                    S.op("dve", lambda h: h.scalar_tensor_tensor(out=lo[:], in0=ge[:], scalar=w, in1=lo[:], op0=ALU.mult, op1=ALU.add),
                         reads=[ge_b, lo_b], writes=[lo_b])
                S.op("dve", lambda h: h.tensor_tensor(out=mk[:, ta:tb, :], in0=aff[:, ta:tb, :], in1=_bc_mid(lo[:], nt), op=ALU.is_ge),
                     reads=[aff_b, lo_b], writes=[mk_b])
                S.op("dve", lambda h: h.tensor_copy(out=mkb[:, ta:tb, :], in_=mk[:, ta:tb, :]), reads=[mk_b], writes=[mkb_b])
                mflat = mkb[:, ta:tb, :].rearrange("p t e -> p (t e)")
                _mm_group(S, psw_b, psw[:, :nt * NE], [(us[:], mflat)], [us_b, mkb_b])
                _mm_group(S, pst_b, pst_[:, :nt * NE], [(ones_b[:], mflat)], [ones_b_b, mkb_b])
                S.op("dve", lambda h: h.tensor_copy(out=tot[:, ta:tb, :].rearrange("p t e -> p (t e)"), in_=pst_[:, :nt * NE]),
                     reads=[pst_b], writes=[tot_b])
                for t in range(ta + 1, tb):
                    S.op("dve", lambda h, t=t: h.tensor_tensor(out=base[:, t, :], in0=base[:, t - 1, :], in1=tot[:, t - 1, :], op=ALU.add),
                         reads=[base_b, tot_b], writes=[base_b])
                S.op("dve", lambda h: h.tensor_tensor(out=psel[:, ta:tb, :].rearrange("p t e -> p (t e)"), in0=psw[:, :nt * NE],
                                                      in1=base[:, ta:tb, :].rearrange("p t e -> p (t e)"), op=ALU.add),
                     reads=[psw_b, base_b], writes=[psel_b])
                S.op("dve", lambda h: h.scalar_tensor_tensor(out=psel[:, ta:tb, :], in0=psel[:, ta:tb, :], scalar=1.0, in1=mk[:, ta:tb, :],
                                                             op0=ALU.add, op1=ALU.mult), reads=[psel_b, mk_b], writes=[psel_b])
                S.op("dve", lambda h: h.tensor_scalar(out=psel[:, ta:tb, :], in0=psel[:, ta:tb, :], scalar1=-1.0, scalar2=None, op0=ALU.add),
                     reads=[psel_b], writes=[psel_b])
            S.op("dve", lambda h: h.tensor_tensor(out=aff[:], in0=aff[:], in1=mk[:], op=ALU.mult), reads=[aff_b, mk_b], writes=[aff_b])
            for src, src_b, dstT, dstT_b in ((psel, psel_b, posT, posT_b), (aff, aff_b, affT, affT_b)):
                for t4 in range(0, ntt, 4):
                    n4 = min(4, ntt - t4)

                    def trf(h, t4=t4, n4=n4, src=src):
                        ins = None
                        for j in range(n4):
                            ins = h.transpose(psc[0:16, j * 128:(j + 1) * 128], src[:, t4 + j, :], ident_f[:])
                        return ins
                    S.op("pe", trf, reads=[src_b, ident_f_b], writes=[psc_b])
                    S.op("act", lambda h, t4=t4, n4=n4, dstT=dstT: h.activation(out=dstT[:, t4 * 128:(t4 + n4) * 128], in_=psc[0:16, :n4 * 128], func=AF.Copy),
                         reads=[psc_b], writes=[dstT_b])
            S.dma("sp", R["PT"][0][0, :, 0:ntt * 128], posT[:, 0:ntt * 128], R["PT"][1], [posT_b])
            S.dma("sp", R["PT"][0][1, :, 0:ntt * 128], affT[:, 0:ntt * 128], R["PT"][1], [affT_b])
            S.barrier()
        NS = CAP + CAPC
        with contextlib.ExitStack() as s3:
            w1, w1_b = K.sb("ew1", [128, 8, D], BF16, s3)
            w3, w3_b = K.sb("ew3", [128, 8, D], BF16, s3)
            w2, w2_b = K.sb("ew2", [128, 8, D], BF16, s3)
            Se, Se_b = K.sb("eS", [128, 32, 512], BF16, s3)
            Sc, Sc_b = K.sb("eSc", [128, 2, CAPC], BF16, s3)
            io, io_b = K.sb("eio", [128, 512], F32, s3)
            S.dma("sp", io[:], I["iota512"][:, :], io_b)
            xg, xg_b = K.sb("exg", [128, 8, NS], BF16, s3)
            gT, gT_b = K.sb("egT", [128, 8, NS], BF16, s3)
            sil, sil_b = K.sb("esil", [128, NS], F32, s3)
            yst = [K.sb("eyst%d" % i, [128, 512], BF16, s3) for i in range(2)]
            psG = [K.pst("epsG%d" % i, [128, 512], F32, s3) for i in range(2)]
            psA, psA_b = K.pst("epsA", [128, 512], F32, s3)
            psB, psB_b = K.pst("epsB", [128, 512], F32, s3)
            psC, psC_b = K.pst("epsC", [128, 512], F32, s3)
            psY = [K.pst("epsY%d" % i, [128, 512], F32, s3) for i in range(2)]
            ng = 0
            ny = 0
            for e in range(NE):
                S.dma("pool", w1[:], I["moe_w1"][l, e].rearrange("(k p) n -> p k n", p=128), w1_b)
                S.dma("pool", w3[:], I["moe_w3"][l, e].rearrange("(k p) n -> p k n", p=128), w3_b)
                S.dma("pool", w2[:], I["moe_w2"][l, e].rearrange("(k p) n -> p k n", p=128), w2_b)
                for tt in range(32):
                    S.op("dve", lambda h, tt=tt: h.tensor_scalar(out=Se[:, tt, :], in0=io[:], scalar1=psel[:, tt, e:e + 1], scalar2=None, op0=ALU.is_equal),
                         reads=[io_b, psel_b], writes=[Se_b])
                if ctx_out:
                    for tt in range(32, 34):
                        S.op("dve", lambda h, tt=tt: h.tensor_scalar(out=Sc[:, tt - 32, :], in0=io[:, 0:CAPC], scalar1=psel[:, tt, e:e + 1], scalar2=None,
                                                                     op0=ALU.is_equal), reads=[io_b, psel_b], writes=[Sc_b])
                for dk in range(8):
                    pg, pg_b = psG[ng % 2]
                    ng += 1
                    _mm_group(S, pg_b, pg[:], [(H2[:, tt, dk * 128:(dk + 1) * 128], Se[:, tt, :]) for tt in range(32)], [H2_b, Se_b])
                    S.op("act", lambda h, dk=dk, pg=pg: h.activation(out=xg[:, dk, 0:CAP], in_=pg[:], func=AF.Copy), reads=[pg_b], writes=[xg_b])
                    if ctx_out:
                        _mm_group(S, psC_b, psC[:, 0:CAPC], [(H2[:, tt, dk * 128:(dk + 1) * 128], Sc[:, tt - 32, :]) for tt in (32, 33)], [H2_b, Sc_b])
                        S.op("act", lambda h, dk=dk: h.activation(out=xg[:, dk, CAP:NS], in_=psC[:, 0:CAPC], func=AF.Copy), reads=[psC_b], writes=[xg_b])
                for m in range(8):
                    _mm_group(S, psA_b, psA[:], [(w1[:, k, m * 128:(m + 1) * 128], xg[:, k, 0:CAP]) for k in range(8)], [w1_b, xg_b])
                    _mm_group(S, psB_b, psB[:], [(w3[:, k, m * 128:(m + 1) * 128], xg[:, k, 0:CAP]) for k in range(8)], [w3_b, xg_b])
                    S.op("act", lambda h: h.activation(out=sil[:, 0:CAP], in_=psA[:], func=AF.Silu), reads=[psA_b], writes=[sil_b])
                    S.op("dve", lambda h, m=m: h.tensor_tensor(out=gT[:, m, 0:CAP], in0=psB[:], in1=sil[:, 0:CAP], op=ALU.mult),
                         reads=[psB_b, sil_b], writes=[gT_b])
                    if ctx_out:
                        _mm_group(S, psC_b, psC[:, 0:CAPC], [(w1[:, k, m * 128:(m + 1) * 128], xg[:, k, CAP:NS]) for k in range(8)], [w1_b, xg_b])
                        S.op("act", lambda h: h.activation(out=sil[:, CAP:NS], in_=psC[:, 0:CAPC], func=AF.Silu), reads=[psC_b], writes=[sil_b])
                        _mm_group(S, psC_b, psC[:, 0:CAPC], [(w3[:, k, m * 128:(m + 1) * 128], xg[:, k, CAP:NS]) for k in range(8)], [w3_b, xg_b])
                        S.op("dve", lambda h, m=m: h.tensor_tensor(out=gT[:, m, CAP:NS], in0=psC[:, 0:CAPC], in1=sil[:, CAP:NS], op=ALU.mult),
                             reads=[psC_b, sil_b], writes=[gT_b])
                for c in range(5 if ctx_out else 4):
                    rows = 128 if c < 4 else CAPC
                    for dh in range(2):
                        py, py_b = psY[ny % 2]
                        ys, ys_b = yst[ny % 2]
                        ny += 1
                        _mm_group(S, py_b, py[0:rows, :], [(gT[:, f, c * 128:c * 128 + rows], w2[:, f, dh * 512:(dh + 1) * 512]) for f in range(8)],
                                  [gT_b, w2_b])
                        S.op("act", lambda h, py=py, ys=ys, rows=rows: h.activation(out=ys[0:rows, :], in_=py[0:rows, :], func=AF.Copy),
                             reads=[py_b], writes=[ys_b])
                        S.dma("sp", R["YE"][0][e, c * 128:c * 128 + rows, dh * 512:(dh + 1) * 512], ys[0:rows, :], R["YE"][1], [ys_b])
            S.barrier()
        sH.close()
        with contextlib.ExitStack() as s4:
            YEs, YEs_b = K.sb("cYE", [128, NE, 5, 512], BF16, s4)
            ST, ST_b = K.sb("cST", [128, NE, 4, 512], BF16, s4)
            abc = [K.sb("cabc%d" % i, [128, 512], F32, s4) for i in range(2)]
            xh, xh_b = K.sb("cxh", [128, 4, 512], F32, s4)
            selm, selm_b = K.sb("cselm", [16, NE, 128], F32, s4)
            sidx, sidx_b = K.sb("csidx", [128, 5], F32, s4)
            posT, posT_b = K.sb("cposT", [16, T], F32, s4)
            affT, affT_b = K.sb("caffT", [16, T], F32, s4)
            S.dma("sp", posT[:, 0:ntt * 128], R["PT"][0][0, :, 0:ntt * 128], posT_b, [R["PT"][1]])
            S.dma("sp", affT[:, 0:ntt * 128], R["PT"][0][1, :, 0:ntt * 128], affT_b, [R["PT"][1]])
            S.dma("sp", selm[:], I["selm"].rearrange("k (e m) -> k e m", e=NE), selm_b)
            S.dma("sp", sidx[:], I["slotidx"][:, :], sidx_b)
            psP = [K.pst("cpsP%d" % i, [128, 512], F32, s4) for i in range(2)]
            psQ = [K.pst("cpsQ%d" % i, [128, 512], F32, s4) for i in range(2)]
            psO = [K.pst("cpsO%d" % i, [128, 512], F32, s4) for i in range(2)]
            nb_ = 0
            no = 0
            for dh in range(2):
                for e in range(NE):
                    S.dma("sp", YEs[:, e, 0:4, :], R["YE"][0][e, 0:CAP, dh * 512:(dh + 1) * 512].rearrange("(c p) d -> p c d", p=128), YEs_b, [R["YE"][1]])
                    if ctx_out:
                        S.dma("sp", YEs[0:CAPC, e, 4, :], R["YE"][0][e, CAP:NS, dh * 512:(dh + 1) * 512], YEs_b, [R["YE"][1]])
                for (t0, tn) in chunks:
                    r = 0 if t0 < L else 1
                    lat = t0 < L
                    for e in range(NE):
                        pp, pp_b = psP[nb_ % 2]
                        pq, pq_b = psQ[nb_ % 2]
                        ab, ab_b = abc[nb_ % 2]
                        nb_ += 1
                        _mm_group(S, pp_b, pp[:, :tn], [(selm[:, e, :], posT[:, t0:t0 + tn])], [selm_b, posT_b])
                        _mm_group(S, pq_b, pq[:, :tn], [(selm[:, e, :], affT[:, t0:t0 + tn])], [selm_b, affT_b])
                        S.op("act", lambda h, ab=ab, pq=pq: h.activation(out=ab[:, :tn], in_=pq[:, :tn], func=AF.Copy), reads=[pq_b], writes=[ab_b])
                        if lat:
                            for c in range(4):
                                S.op("dve", lambda h, c=c, pp=pp, ab=ab: h.scalar_tensor_tensor(out=ST[:, e, c, :tn], in0=pp[:, :tn], scalar=sidx[:, c:c + 1],
                                                                                               in1=ab[:, :tn], op0=ALU.is_equal, op1=ALU.mult),
                                     reads=[pp_b, ab_b, sidx_b], writes=[ST_b])
                        else:
                            S.op("dve", lambda h, pp=pp, ab=ab: h.scalar_tensor_tensor(out=ST[0:CAPC, e, 0, :tn], in0=pp[0:CAPC, :tn], scalar=sidx[0:CAPC, 4:5],
                                                                                      in1=ab[0:CAPC, :tn], op0=ALU.is_equal, op1=ALU.mult),
                                 reads=[pp_b, ab_b, sidx_b], writes=[ST_b])
                    S.dma("sp", xh[:, :, :tn], XTv[:, dh * 4:(dh + 1) * 4, t0:t0 + tn], xh_b, [R["XT"][1]])
                    for m in range(4):
                        po, po_b = psO[no % 2]
                        no += 1
                        if lat:
                            pairs = [(YEs[:, e, c, m * 128:(m + 1) * 128], ST[:, e, c, :tn]) for e in range(NE) for c in range(4)]
                        else:
                            pairs = [(YEs[0:CAPC, e, 4, m * 128:(m + 1) * 128], ST[0:CAPC, e, 0, :tn]) for e in range(NE)]
                        _mm_group(S, po_b, po[:, :tn], pairs, [YEs_b, ST_b])
                        S.op("dve", lambda h, m=m, po=po: h.scalar_tensor_tensor(out=xh[:, m, :tn], in0=po[:, :tn], scalar=modT[:, 40 + dh * 4 + m, r:r + 1],
                                                                                 in1=xh[:, m, :tn], op0=ALU.mult, op1=ALU.add),
                             reads=[po_b, modT_b, xh_b], writes=[xh_b])
                    S.dma("sp", XTv[:, dh * 4:(dh + 1) * 4, t0:t0 + tn], xh[:, :, :tn], R["XT"][1], [xh_b])
            S.barrier()


def phase_final(K):
    nc, S, I, R = K.nc, K.S, K.I, K.R
    ones_f, ones_f_b = K.ones_f
    XTv = R["XT"][0].rearrange("(k p) t -> p k t", p=128)
    with contextlib.ExitStack() as st:
        fg, fg_b = K.sb("fg", [128, 8], F32, st)
        S.dma("sp", fg[:], I["fgT"][:, :], fg_b)
        xs = [K.sb("fx%d" % i, [128, 8, 512], F32, st) for i in range(2)]
        sq, sq_b = K.sb("fsq", [128, 8, 512], F32, st)
        rstd, rstd_b = K.sb("frstd", [128, 512], F32, st)
        ps, ps_b = K.pst("ps_fin", [128, 512], F32, st)
        for ci in range(8):
            t0 = ci * 512
            x, x_b = xs[ci % 2]
            S.dma("sp", x[:], XTv[:, :, t0:t0 + 512], x_b, [R["XT"][1]])
            S.op("act", lambda h: h.activation(out=sq[:], in_=x[:], func=AF.Square), reads=[x_b], writes=[sq_b])
            _mm_group(S, ps_b, ps[:], [(ones_f[:], sq[:, k, :]) for k in range(8)], [ones_f_b, sq_b])
            S.op("dve", lambda h: h.tensor_scalar(out=rstd[:], in0=ps[:], scalar1=1.0 / D, scalar2=EPS, op0=ALU.mult, op1=ALU.add),
                 reads=[ps_b], writes=[rstd_b])
            S.op("act", lambda h: h.activation(out=rstd[:], in_=rstd[:], func=AF.Sqrt), reads=[rstd_b], writes=[rstd_b])
            S.op("dve", lambda h: h.reciprocal(out=rstd[:], in_=rstd[:]), reads=[rstd_b], writes=[rstd_b])
            for k in range(8):
                S.op("dve", lambda h, k=k: h.scalar_tensor_tensor(out=sq[:, k, :], in0=x[:, k, :], scalar=fg[:, k:k + 1], in1=rstd[:],
                                                                  op0=ALU.mult, op1=ALU.mult), reads=[x_b, rstd_b, fg_b], writes=[sq_b])
            S.dma("sp", K.outT.rearrange("(k p) t -> p k t", p=128)[:, :, t0:t0 + 512], sq[:], K.outT_b, [sq_b])
        S.barrier()


def rope_tables():
    rows = L // 64
    row = np.repeat(np.arange(rows, dtype=np.float32), 64)
    col = np.tile(np.arange(64, dtype=np.float32), rows)
    inv = (10000.0 ** (-np.arange(16, dtype=np.float32) / 16)).astype(np.float32)
    C = np.zeros((64, L), np.float32)
    Sg = np.zeros((64, L), np.float32)
    for j in range(64):
        a, hf, f = j // 32, (j % 32) // 16, j % 16
        pos = row if a == 0 else col
        ang = (pos * inv[f]).astype(np.float32)
        C[j] = np.cos(ang)
        Sg[j] = np.sin(ang) * (-1.0 if hf == 0 else 1.0)
    return np.concatenate([C, C], 0), np.concatenate([Sg, Sg], 0)


_HC = {}


def hyena_consts():
    if _HC:
        return _HC
    f32 = np.float32

    def emb_of(Lx):
        t = np.linspace(0.0, 1.0, Lx, dtype=f32)[:, None]
        w = (2.0 * math.pi * np.arange(Lx, dtype=f32)[:, None] / Lx).astype(f32)
        f = np.linspace(1e-4, 15, 16, dtype=f32)[None, :]
        e = np.concatenate([t, np.cos(f * w), -np.sin(f * w)], axis=-1).astype(f32)
        return np.ascontiguousarray(e.T), t[:, 0]
    eT, t = emb_of(L)
    eTc, tc = emb_of(LC)
    _HC["embT"], _HC["embTc"] = eT, eTc
    _HC["negt"] = np.ascontiguousarray((-t).reshape(L // 128, 128).T)
    _HC["negtc"] = np.ascontiguousarray((-tc).reshape(LC // 128, 128).T)
    _HC["deltas"] = np.abs(np.linspace(math.log(1e-2) / 0.3, math.log(1e-2) / 1.5, D, dtype=f32)).reshape(1, D).astype(f32)

    def dft(Lx, npad):
        N = 2 * Lx
        a = np.arange(Lx + 1, dtype=np.int64)
        ph = (a[:, None] * a[None, :]) % N
        ang = ph.astype(np.float64) * (2.0 * math.pi / N)
        C = np.zeros((npad, npad), f32)
        S_ = np.zeros((npad, npad), f32)
        C[:Lx + 1, :Lx + 1] = np.cos(ang)
        S_[:Lx + 1, :Lx + 1] = np.sin(ang)
        wfv = np.zeros(npad, f32)
        wfv[:Lx + 1] = 2.0 / N
        wfv[0] = 1.0 / N
        wfv[Lx] = 1.0 / N
        return C.astype(ml_dtypes.bfloat16), S_.astype(ml_dtypes.bfloat16), np.ascontiguousarray(wfv.reshape(npad // 128, 128).T)
    _HC["dftC"], _HC["dftS"], _HC["wf"] = dft(L, NF)
    _HC["dftCc"], _HC["dftSc"], _HC["wfc"] = dft(LC, NFC)

    def tiled(Mx):
        n = Mx.shape[0] // 128
        return np.ascontiguousarray(Mx.reshape(n, 128, n, 128).transpose(2, 1, 0, 3))
    _HC["dftCt"], _HC["dftSt"] = tiled(_HC["dftC"]), tiled(_HC["dftS"])
    _HC["dftCct"], _HC["dftSct"] = tiled(_HC["dftCc"]), tiled(_HC["dftScc"] if "dftScc" in _HC else _HC["dftSc"])
    return _HC


def host_inputs(inputs, b, nl):
    x, c, ctx, c_ctx = inputs["x"], inputs["c"], inputs["ctx"], inputs["c_ctx"]
    m = {}
    m["xT"] = np.ascontiguousarray(np.concatenate([x[b], ctx[b]], 0).T)
    cc = np.stack([c[b], c_ctx], 0)
    m["ccT"] = np.ascontiguousarray(cc.reshape(2, 8, 128).transpose(2, 1, 0))
    m["w_mod"] = np.ascontiguousarray(inputs["w_mod"][:nl])
    m["b_modT"] = np.ascontiguousarray(inputs["b_mod"][:nl].reshape(nl, 48, 128).transpose(0, 2, 1))
    m["g1T"] = np.ascontiguousarray(inputs["norm1_g"][:nl].reshape(nl, 8, 128).transpose(0, 2, 1))
    m["g2T"] = np.ascontiguousarray(inputs["norm2_g"][:nl].reshape(nl, 8, 128).transpose(0, 2, 1))
    m["w_in"] = np.ascontiguousarray(inputs["w_in"][:nl])
    rc, rs = rope_tables()
    m["ropeC"], m["ropeS"] = rc, rs
    m["ident"] = np.eye(128, dtype=np.float32)
    m["diff_lambda"] = np.ascontiguousarray(inputs["diff_lambda"][:nl].reshape(nl, 256))
    m["sublnT"] = np.ascontiguousarray(inputs["diff_subln_g"][:nl].reshape(nl, 128, 1))
    m["swa_sink"] = np.ascontiguousarray(inputs["swa_sink"][:nl])
    kk, qq = np.meshgrid(np.arange(128), np.arange(128), indexing="ij")
    m["trilo"] = (kk >= qq).astype(np.float32)
    m["triup"] = (kk <= qq).astype(np.float32)
    m["w_branch"] = np.ascontiguousarray(inputs["w_branch"][:nl])
    m["w_out"] = np.ascontiguousarray(inputs["w_out"][:nl])
    m["fgT"] = np.ascontiguousarray(inputs["final_g"].reshape(8, 128).T)
    if "hy_ff_w1" in inputs:
        cwv = inputs["hy_conv_w"][:nl]
        m["cwT"] = np.ascontiguousarray(cwv.reshape(nl, 3, 24, 128).transpose(0, 3, 2, 1))
        m["cbT"] = np.ascontiguousarray(inputs["hy_conv_b"][:nl].reshape(nl, 24, 128).transpose(0, 2, 1))
        m["hbT"] = np.ascontiguousarray(inputs["hy_bias"][:nl].reshape(nl, 8, 128).transpose(0, 2, 1))
        m["hy_w1"] = np.ascontiguousarray(inputs["hy_ff_w1"][:nl])
        m["hy_w2"] = np.ascontiguousarray(inputs["hy_ff_w2"][:nl])
        m["hy_w3"] = np.ascontiguousarray(inputs["hy_ff_w3"][:nl])
        m["hy_b1"] = np.ascontiguousarray(inputs["hy_ff_b1"][:nl].reshape(nl, 64, 1))
        m["hy_b2"] = np.ascontiguousarray(inputs["hy_ff_b2"][:nl].reshape(nl, 64, 1))
        m["hy_fr"] = np.ascontiguousarray(inputs["hy_sin_freq"][:nl].reshape(nl, 64, 1))
        m.update(hyena_consts())
    if "moe_w1" in inputs:
        m["router_w"] = np.ascontiguousarray(inputs["router_w"][:nl])
        for k in ("moe_w1", "moe_w3", "moe_w2"):
            m[k] = np.ascontiguousarray(inputs[k][:nl])
        sel = np.zeros((16, 16, 128), np.float32)
        for e in range(16):
            sel[e, e, :] = 1.0
        m["selm"] = sel.reshape(16, 16 * 128)
        si = np.zeros((128, 5), np.float32)
        for c in range(4):
            si[:, c] = np.arange(128) + 128 * c
        si[:, 4] = np.arange(128)
        m["slotidx"] = si
        m["iota512"] = np.tile(np.arange(512, dtype=np.float32)[None, :], (128, 1))
        m["ustrict"] = (kk < qq).astype(np.float32)
    return m


def kernel(**inputs):
    inputs = {k: np.asarray(v) for k, v in inputs.items()}
    nb = inputs["x"].shape[0]
    nc = build(dict(nl=DEPTH, phases=("diff", "swa", "hy", "merge", "moe")))
    in_maps = [host_inputs(inputs, b, DEPTH) for b in range(nb)]
    res = run_bass_kernel_spmd(nc, in_maps, core_ids=list(range(nb)))
    out = np.stack([np.asarray(res.results[b]["outT"]).T for b in range(nb)], 0)
    return np.ascontiguousarray(out.astype(np.float32))
```

```python
import contextlib
import math
import numpy as np
import ml_dtypes
import concourse.bass as bass
import concourse.mybir as mybir
from concourse.bass_utils import run_bass_kernel_spmd

F32 = mybir.dt.float32
BF16 = mybir.dt.bfloat16
AF = mybir.ActivationFunctionType
ALU = mybir.AluOpType
AX = mybir.AxisListType

D = 1024
L = 4096
LC = 256
T = L + LC
DEPTH = 4
D_IN = 10752
NE = 16
CAP = 512
CAPC = 32
EPS = 1e-6
NF = 4224
NFC = 384
TCH = [(i * 512, 512) for i in range(8)] + [(L, LC)]


class Buf:
    __slots__ = ("name", "w", "r", "sem", "cnt")

    def __init__(self, name):
        self.name = name
        self.w = {}
        self.r = {}
        self.sem = None
        self.cnt = 0


class Sched:
    def __init__(self, nc, es):
        self.nc, self.es = nc, es
        self.E = {}
        for n, h in (("pe", nc.tensor), ("dve", nc.vector), ("act", nc.scalar),
                     ("pool", nc.gpsimd), ("sp", nc.sync)):
            self.E[n] = dict(h=h, sem=es.enter_context(nc.semaphore("s_" + n)), cnt=0, seen={})
        self.dma_bufs = []
        self.nbuf = 0
        self.persist = True
        self.pool = []

    def buf(self, name):
        self.nbuf += 1
        b = Buf("%s_%d" % (name, self.nbuf))
        b.w["_persist"] = self.persist
        return b

    @staticmethod
    def _add(evs, d):
        for k, sv in d.items():
            if k == "_persist":
                continue
            sem, v = sv
            if k not in evs or evs[k][1] < v:
                evs[k] = (sem, v)

    def _waits(self, e, evs):
        E = self.E[e]
        for name, (sem, val) in evs.items():
            if E["seen"].get(name, 0) < val:
                E["h"].wait_ge(sem, val)
                E["seen"][name] = val

    def op(self, e, fn, reads=(), writes=(), skip_self=False, drain_self=False):
        evs = {}
        for b in reads:
            self._add(evs, b.w)
        for b in writes:
            self._add(evs, b.w)
            self._add(evs, b.r)
        if skip_self:
            evs.pop("s_" + e, None)
        if drain_self and self.E[e]["cnt"]:
            evs["s_" + e] = (self.E[e]["sem"], self.E[e]["cnt"])
        self._waits(e, evs)
        E = self.E[e]
        ins = fn(E["h"])
        E["cnt"] += 1
        ins.then_inc(E["sem"], 1)
        key, ev = "s_" + e, (E["sem"], E["cnt"])
        for b in reads:
            b.r[key] = ev
        for b in writes:
            b.w[key] = ev

    def dma(self, q, out_ap, in_ap, dst, srcs=(), **kw):
        evs = {}
        self._add(evs, dst.w)
        self._add(evs, dst.r)
        for b in srcs:
            self._add(evs, b.w)
        self._waits(q, evs)
        if dst.sem is None:
            if self.pool and not dst.w["_persist"]:
                dst.name, dst.sem, dst.cnt = self.pool.pop()
            else:
                dst.sem = self.es.enter_context(self.nc.semaphore("d_" + dst.name))
            self.dma_bufs.append(dst)
        ins = self.E[q]["h"].dma_start(out=out_ap, in_=in_ap, **kw)
        dst.cnt += 16
        ins.then_inc(dst.sem, 16)
        key, ev = "d_" + dst.name, (dst.sem, dst.cnt)
        dst.w[key] = ev
        for b in srcs:
            b.r[key] = ev

    def barrier(self):
        evs = {}
        for n, E in self.E.items():
            if E["cnt"]:
                evs["s_" + n] = (E["sem"], E["cnt"])
        for b in self.dma_bufs:
            evs["d_" + b.name] = (b.sem, b.cnt)
        for n in self.E:
            self._waits(n, evs)
        keep = []
        for b in self.dma_bufs:
            if b.w["_persist"]:
                keep.append(b)
            else:
                self.pool.append((b.name, b.sem, b.cnt))
        self.dma_bufs = keep

    def barrier_known(self):
        pass


class Ctx:
    pass


def _mm_group(S, ps, out_ap, pairs, reads):
    def fn(h):
        ins = None
        n = len(pairs)
        for i, (a, b) in enumerate(pairs):
            ins = h.matmul(out_ap, a, b, start=(i == 0), stop=(i == n - 1))
        return ins
    S.op("pe", fn, reads=reads, writes=[ps])


def _mm1(S, ps, out_ap, a, b, reads, start, stop, drain=False):
    S.op("pe", lambda h: h.matmul(out_ap, a, b, start=start, stop=stop), reads=reads, writes=[ps], skip_self=True, drain_self=drain)


def build(cfg):
    nl = cfg["nl"]
    debug = cfg.get("debug", ())
    stop_after = cfg.get("stop_after", None)
    nc = bass.Bass("TRN2", target_bir_lowering=False)
    es = contextlib.ExitStack()
    K = Ctx()
    K.nc, K.es, K.cfg = nc, es, cfg
    S = Sched(nc, es)
    K.S = S

    def din(name, shape, dt=F32):
        return nc.dram_tensor(name, list(shape), dt, kind="ExternalInput").ap()

    def scratch(name, shape, dt):
        kind = "ExternalOutput" if name in debug else "Internal"
        return nc.dram_tensor(name, list(shape), dt, kind=kind).ap()

    I = {}
    I["xT"] = din("xT", [D, T])
    I["ccT"] = din("ccT", [128, 8, 2])
    I["w_mod"] = din("w_mod", [nl, D, 6 * D])
    I["b_modT"] = din("b_modT", [nl, 128, 48])
    I["g1T"] = din("g1T", [nl, 128, 8])
    I["g2T"] = din("g2T", [nl, 128, 8])
    I["w_in"] = din("w_in", [nl, D, D_IN])
    I["ropeC"] = din("ropeC", [128, L])
    I["ropeS"] = din("ropeS", [128, L])
    I["ident"] = din("ident", [128, 128])
    I["diff_lambda"] = din("diff_lambda", [nl, 256])
    I["sublnT"] = din("sublnT", [nl, 128, 1])
    I["swa_sink"] = din("swa_sink", [nl, 16])
    I["trilo"] = din("trilo", [128, 128])
    I["triup"] = din("triup", [128, 128])
    I["w_branch"] = din("w_branch", [nl, 3, D, D])
    I["w_out"] = din("w_out", [nl, D, D])
    I["fgT"] = din("fgT", [128, 8])
    if "moe" in cfg.get("phases", ()):
        I["router_w"] = din("router_w", [nl, D, NE])
        I["moe_w1"] = din("moe_w1", [nl, NE, D, D])
        I["moe_w3"] = din("moe_w3", [nl, NE, D, D])
        I["moe_w2"] = din("moe_w2", [nl, NE, D, D])
        I["selm"] = din("selm", [16, 16 * 128])
        I["slotidx"] = din("slotidx", [128, 5])
        I["iota512"] = din("iota512", [128, 512])
        I["ustrict"] = din("ustrict", [128, 128])
    if "hy" in cfg.get("phases", ()):
        I["cwT"] = din("cwT", [nl, 128, 24, 3])
        I["cbT"] = din("cbT", [nl, 128, 24])
        I["hbT"] = din("hbT", [nl, 128, 8])
        I["hy_w1"] = din("hy_w1", [nl, 33, 64])
        I["hy_w2"] = din("hy_w2", [nl, 64, 64])
        I["hy_w3"] = din("hy_w3", [nl, 64, 2048])
        I["hy_b1"] = din("hy_b1", [nl, 64, 1])
        I["hy_b2"] = din("hy_b2", [nl, 64, 1])
        I["hy_fr"] = din("hy_fr", [nl, 64, 1])
        I["embT"] = din("embT", [33, L])
        I["embTc"] = din("embTc", [33, LC])
        I["negt"] = din("negt", [128, 32])
        I["negtc"] = din("negtc", [128, 2])
        I["deltas"] = din("deltas", [1, D])
        I["dftC"] = din("dftC", [NF, NF], BF16)
        I["dftS"] = din("dftS", [NF, NF], BF16)
        I["dftCc"] = din("dftCc", [NFC, NFC], BF16)
        I["dftSc"] = din("dftSc", [NFC, NFC], BF16)
        I["wf"] = din("wf", [128, NF // 128])
        I["wfc"] = din("wfc", [128, NFC // 128])
    K.I = I
    K.outT = nc.dram_tensor("outT", [D, L], F32, kind="ExternalOutput").ap()
    K.outT_b = S.buf("outT")

    R = {}
    R["XT"] = (scratch("XT", [D, T], F32), S.buf("XT"))
    R["QdT"] = (scratch("QdT", [D, T], BF16), S.buf("QdT"))
    R["KdT"] = (scratch("KdT", [D, T], BF16), S.buf("KdT"))
    R["Vd"] = (scratch("Vd", [T, D], BF16), S.buf("Vd"))
    R["HyT"] = (scratch("HyT", [3 * D, T], BF16), S.buf("HyT"))
    R["QsT"] = (scratch("QsT", [D, T], BF16), S.buf("QsT"))
    R["KsT"] = (scratch("KsT", [256, T], BF16), S.buf("KsT"))
    R["Vs"] = (scratch("Vs", [T, 256], BF16), S.buf("Vs"))
    R["GT"] = (scratch("GT", [3 * D, T], BF16), S.buf("GT"))
    R["YdT"] = (scratch("YdT", [D, T], BF16), S.buf("YdT"))
    R["YhT"] = (scratch("YhT", [D, T], BF16), S.buf("YhT"))
    R["YsT"] = (scratch("YsT", [D, T], BF16), S.buf("YsT"))
    R["X0T"] = (scratch("X0T", [D, T], BF16), S.buf("X0T"))
    R["UT"] = (scratch("UT", [D, T], BF16), S.buf("UT"))
    R["KF"] = (scratch("KF", [2, NF, D], F32), S.buf("KF"))
    R["KFc"] = (scratch("KFc", [2, NFC, D], F32), S.buf("KFc"))
    R["YF"] = (scratch("YF", [2, NF, D], BF16), S.buf("YF"))
    R["YFc"] = (scratch("YFc", [2, NFC, D], BF16), S.buf("YFc"))
    R["YE"] = (scratch("YE", [NE, CAP + CAPC, D], BF16), S.buf("YE"))
    R["XT1"] = (scratch("XT1", [D, T], F32), S.buf("XT1"))
    R["PT"] = (scratch("PT", [2, 16, T], F32), S.buf("PT"))
    R["LG"] = (scratch("LG", [128, T // 128, NE], F32), S.buf("LG"))
    R["H2d"] = (scratch("H2d", [128, T // 128, D], BF16), S.buf("H2d"))
    K.R = R

    uid = [0]

    def sb(name, shape, dt, stack=es):
        uid[0] += 1
        t = stack.enter_context(nc.sbuf_tensor("sb%d_%s" % (uid[0], name), list(shape), dt))
        return t, S.buf(name)

    def pst(name, shape, dt, stack):
        uid[0] += 1
        t = stack.enter_context(nc.psum_tensor("ps%d_%s" % (uid[0], name), list(shape), dt))
        return t, S.buf(name)
    K.sb, K.pst = sb, pst

    ident_f, ident_f_b = sb("ident_f", [128, 128], F32)
    ident_b, ident_b_b = sb("ident_b", [128, 128], BF16)
    ones_f, ones_f_b = sb("ones_f", [128, 128], F32)
    ones_b, ones_b_b = sb("ones_b", [128, 128], BF16)
    S.dma("sp", ident_f[:], I["ident"][:, :], ident_f_b)
    S.dma("pool", ident_b[:], I["ident"][:, :], ident_b_b)
    S.op("dve", lambda h: h.memset(ones_f[:], 1.0), writes=[ones_f_b])
    S.op("dve", lambda h: h.memset(ones_b[:], 1.0), writes=[ones_b_b])
    K.ident_f, K.ident_b, K.ones_f, K.ones_b = (ident_f, ident_f_b), (ident_b, ident_b_b), (ones_f, ones_f_b), (ones_b, ones_b_b)

    sc, sc_b = sb("sc", [128, 8, 2], F32)
    S.dma("sp", sc[:], I["ccT"][:, :, :], sc_b)
    S.op("act", lambda h: h.activation(out=sc[:], in_=sc[:], func=AF.Silu), reads=[sc_b], writes=[sc_b])
    K.sc = (sc, sc_b)
    modT, modT_b = sb("modT", [128, 48, 2], F32)
    K.modT = (modT, modT_b)
    A1, A1_b = sb("A1", [128, 8, 2], F32)
    A2, A2_b = sb("A2", [128, 8, 2], F32)
    K.A1, K.A2 = (A1, A1_b), (A2, A2_b)

    with contextlib.ExitStack() as ph:
        xc, xc_b = sb("xcp", [128, 8, 512], F32, ph)
        for (t0, tn) in TCH:
            S.dma("sp", xc[:, :, :tn], I["xT"].rearrange("(k p) t -> p k t", p=128)[:, :, t0:t0 + tn], xc_b)
            S.dma("sp", R["XT"][0].rearrange("(k p) t -> p k t", p=128)[:, :, t0:t0 + tn], xc[:, :, :tn], R["XT"][1], [xc_b])
        S.barrier()

    S.persist = False
    phases = cfg.get("phases", ("diff", "swa", "merge"))
    if "swa" not in phases:
        with contextlib.ExitStack() as ph:
            zt, zt_b = sb("zt2", [128, T], BF16, ph)
            S.op("dve", lambda h: h.memset(zt[:], 0.0), writes=[zt_b])
            for k in range(8):
                S.dma("sp", R["YsT"][0][k * 128:(k + 1) * 128, :], zt[:], R["YsT"][1], [zt_b])
            S.barrier()
    if "hy" not in phases:
        with contextlib.ExitStack() as ph:
            zt, zt_b = sb("zt", [128, T], BF16, ph)
            S.op("dve", lambda h: h.memset(zt[:], 0.0), writes=[zt_b])
            for k in range(8):
                S.dma("sp", R["YhT"][0][k * 128:(k + 1) * 128, :], zt[:], R["YhT"][1], [zt_b])
            S.barrier()
    for l in range(nl):
        ctx_out = l < DEPTH - 1
        phase_mod(K, l)
        with contextlib.ExitStack() as ph:
            hT, hT_b = sb("hT", [128, 8, T], BF16, ph)
            phase_norm(K, ph, R["XT"], K.A1, 0, (hT, hT_b))
            if stop_after == "norm1":
                dbg = scratch("dbg_hT", [D, T], BF16)
                S.dma("sp", dbg.rearrange("(k p) t -> p k t", p=128), hT[:], S.buf("dbg"), [hT_b])
                S.barrier()
                break
            phase_inproj(K, ph, l, (hT, hT_b))
            S.barrier()
        if stop_after == "inproj":
            break
        if "diff" in phases:
            phase_diff(K, l, ctx_out)
        if stop_after == "diff":
            break
        if "swa" in phases:
            phase_swa(K, l, ctx_out)
        if "hy" in phases:
            phase_hyena(K, l, ctx_out)
        if stop_after == "hy":
            break
        if stop_after == "swa":
            break
        if "merge" in phases:
            phase_merge(K, l)
        if stop_after == "merge":
            break
        if "moe" in phases:
            phase_moe(K, l, ctx_out)
        if stop_after == "moe":
            break
    if stop_after is None:
        phase_final(K)

    S.barrier()
    es.close()
    return nc


def phase_mod(K, l):
    nc, S, I = K.nc, K.S, K.I
    modT, modT_b = K.modT
    sc, sc_b = K.sc
    with contextlib.ExitStack() as ph:
        wts = [K.sb("wmod%d" % i, [128, 8, 512], F32, ph) for i in range(2)]
        bm, bm_b = K.sb("bmodT", [128, 48], F32, ph)
        g1, g1_b = K.sb("g1T", [128, 8], F32, ph)
        g2, g2_b = K.sb("g2T", [128, 8], F32, ph)
        ps, ps_b = K.pst("ps_mod", [128, 512], F32, ph)
        S.dma("sp", bm[:], I["b_modT"][l], bm_b)
        S.dma("sp", g1[:], I["g1T"][l], g1_b)
        S.dma("sp", g2[:], I["g2T"][l], g2_b)
        for g in range(12):
            wt, wt_b = wts[g % 2]
            S.dma("sp", wt[:], I["w_mod"][l].rearrange("(k p) n -> p k n", p=128)[:, :, g * 512:(g + 1) * 512], wt_b)
            for m in range(4):
                mc = g * 4 + m
                _mm_group(S, ps_b, ps[:, 0:2],
                          [(wt[:, k, m * 128:(m + 1) * 128], sc[:, k, :]) for k in range(8)],
                          [wt_b, sc_b])
                S.op("dve", lambda h, mc=mc: h.tensor_scalar(out=modT[:, mc, :], in0=ps[:, 0:2], scalar1=bm[:, mc:mc + 1],
                                                            scalar2=None, op0=ALU.add),
                     reads=[ps_b, bm_b], writes=[modT_b])
        for (A, A_b), (g, g_b), j in ((K.A1, (g1, g1_b), 1), (K.A2, (g2, g2_b), 4)):
            for r in range(2):
                S.op("dve", lambda h, A=A, g=g, j=j, r=r: h.scalar_tensor_tensor(
                    out=A[:, :, r], in0=modT[:, j * 8:(j + 1) * 8, r], scalar=1.0, in1=g[:], op0=ALU.add, op1=ALU.mult),
                    reads=[modT_b, g_b], writes=[A_b])
        S.barrier()


def phase_norm(K, ph, X, Acoef, shift_j, out, out_f32_cb=None):
    nc, S = K.nc, K.S
    XT, XT_b = X
    A, A_b = Acoef
    modT, modT_b = K.modT
    hT, hT_b = out
    ones_f, ones_f_b = K.ones_f
    with contextlib.ExitStack() as st:
        xs = [K.sb("nx%d" % i, [128, 8, 512], F32, st) for i in range(2)]
        sq, sq_b = K.sb("nsq", [128, 8, 512], F32, st)
        rstd, rstd_b = K.sb("nrstd", [128, 512], F32, st)
        tmp, tmp_b = K.sb("ntmp", [128, 512], F32, st)
        ps, ps_b = K.pst("ps_norm", [128, 512], F32, st)
        for ci, (t0, tn) in enumerate(TCH):
            r = 0 if t0 < L else 1
            x, x_b = xs[ci % 2]
            S.dma("sp", x[:, :, :tn], XT.rearrange("(k p) t -> p k t", p=128)[:, :, t0:t0 + tn], x_b, [XT_b])
            S.op("act", lambda h: h.activation(out=sq[:, :, :tn], in_=x[:, :, :tn], func=AF.Square), reads=[x_b], writes=[sq_b])
            _mm_group(S, ps_b, ps[:, :tn], [(ones_f[:], sq[:, k, :tn]) for k in range(8)], [ones_f_b, sq_b])
            S.op("dve", lambda h: h.tensor_scalar(out=rstd[:, :tn], in0=ps[:, :tn], scalar1=1.0 / D, scalar2=EPS,
                                                  op0=ALU.mult, op1=ALU.add), reads=[ps_b], writes=[rstd_b])
            S.op("act", lambda h: h.activation(out=rstd[:, :tn], in_=rstd[:, :tn], func=AF.Sqrt), reads=[rstd_b], writes=[rstd_b])
            S.op("dve", lambda h: h.reciprocal(out=rstd[:, :tn], in_=rstd[:, :tn]), reads=[rstd_b], writes=[rstd_b])
            for k in range(8):
                S.op("dve", lambda h, k=k: h.tensor_tensor(out=tmp[:, :tn], in0=x[:, k, :tn], in1=rstd[:, :tn], op=ALU.mult),
                     reads=[x_b, rstd_b], writes=[tmp_b])
                S.op("act", lambda h, k=k: h.activation(out=hT[:, k, t0:t0 + tn], in_=tmp[:, :tn], func=AF.Identity,
                                                        scale=A[:, k, r:r + 1], bias=modT[:, shift_j * 8 + k, r:r + 1]),
                     reads=[tmp_b, A_b, modT_b], writes=[hT_b])
                if out_f32_cb is not None:
                    out_f32_cb(ci, k, t0, tn, r, tmp, tmp_b)
        S.barrier()


def _inproj_groups():
    g = []
    g += [("qk", "QdT", 0), ("qk", "QdT", 512), ("qk", "KdT", 0), ("qk", "KdT", 512)]
    g += [("v", "Vd", 0), ("v", "Vd", 512)]
    g += [("plain", "HyT", i * 512) for i in range(6)]
    g += [("qk", "QsT", 0), ("qk", "QsT", 512)]
    g += [("kv", None, 0)]
    g += [("gate", "GT", i * 512) for i in range(6)]
    return g


def phase_inproj(K, ph, l, hTb):
    nc, S, I, R = K.nc, K.S, K.I, K.R
    hT, hT_b = hTb
    with contextlib.ExitStack() as st:
        wts = [K.sb("wi%d" % i, [128, 8, 512], BF16, st) for i in range(2)]
        wsw, wsw_b = K.sb("wsw", [128, 8, 512], BF16, st)
        rC, rC_b = K.sb("ropeC", [128, L], F32, st)
        rS, rS_b = K.sb("ropeS", [128, L], F32, st)
        stg = [K.sb("stg%d" % i, [128, T], BF16, st) for i in range(2)]
        vst = [K.sb("vst%d" % i, [128, 512], BF16, st) for i in range(2)]
        t1, t1_b = K.sb("rt1", [128, 512], F32, st)
        t2, t2_b = K.sb("rt2", [128, 512], F32, st)
        psA = [K.pst("psA%d" % i, [128, 512], F32, st) for i in range(2)]
        psB = [K.pst("psB%d" % i, [128, 512], F32, st) for i in range(2)]
        S.dma("sp", rC[:], I["ropeC"][:, :], rC_b)
        S.dma("sp", rS[:], I["ropeS"][:, :], rS_b)
        win = I["w_in"][l].rearrange("(k p) n -> p k n", p=128)
        cnt = dict(s=0, p=0, v=0)

        def fm_chunk(wt, wt_b, m, kind, dst, row0):
            sg, sg_b = stg[cnt["s"] % 2]
            cnt["s"] += 1
            for (t0, tn) in TCH:
                pa, pa_b = psA[cnt["p"] % 2]
                pb, pb_b = psB[cnt["p"] % 2]
                cnt["p"] += 1
                _mm_group(S, pa_b, pa[:, :tn], [(wt[:, k, m * 128:(m + 1) * 128], hT[:, k, t0:t0 + tn]) for k in range(8)],
                          [wt_b, hT_b])
                if kind == "qk" and t0 < L:
                    _mm_group(S, pb_b, pb[:, :tn], [(wsw[:, k, m * 128:(m + 1) * 128], hT[:, k, t0:t0 + tn]) for k in range(8)],
                              [wsw_b, hT_b])
                    S.op("dve", lambda h: h.tensor_tensor(out=t1[:, :tn], in0=pa[:, :tn], in1=rC[:, t0:t0 + tn], op=ALU.mult),
                         reads=[pa_b, rC_b], writes=[t1_b])
                    S.op("dve", lambda h: h.tensor_tensor(out=t2[:, :tn], in0=pb[:, :tn], in1=rS[:, t0:t0 + tn], op=ALU.mult),
                         reads=[pb_b, rS_b], writes=[t2_b])
                    S.op("pool", lambda h: h.tensor_tensor(out=sg[:, t0:t0 + tn], in0=t1[:, :tn], in1=t2[:, :tn], op=ALU.add),
                         reads=[t1_b, t2_b], writes=[sg_b])
                elif kind == "gate":
                    S.op("act", lambda h: h.activation(out=sg[:, t0:t0 + tn], in_=pa[:, :tn], func=AF.Sigmoid),
                         reads=[pa_b], writes=[sg_b])
                else:
                    S.op("act", lambda h: h.activation(out=sg[:, t0:t0 + tn], in_=pa[:, :tn], func=AF.Copy),
                         reads=[pa_b], writes=[sg_b])
            S.dma("sp", R[dst][0][row0:row0 + 128, :], sg[:], R[dst][1], [sg_b])

        def tm_cols(wt, wt_b, c0, cn, dst, col0):
            for tt in range(T // 128):
                pa, pa_b = psA[cnt["p"] % 2]
                cnt["p"] += 1
                vs, vs_b = vst[cnt["v"] % 2]
                cnt["v"] += 1
                _mm_group(S, pa_b, pa[:, :cn], [(hT[:, k, tt * 128:(tt + 1) * 128], wt[:, k, c0:c0 + cn]) for k in range(8)],
                          [wt_b, hT_b])
                S.op("act", lambda h: h.activation(out=vs[:, :cn], in_=pa[:, :cn], func=AF.Copy), reads=[pa_b], writes=[vs_b])
                S.dma("sp", R[dst][0][tt * 128:(tt + 1) * 128, col0:col0 + cn], vs[:, :cn], R[dst][1], [vs_b])

        def make_swapped(wt, wt_b, ncols):
            src = wt[:, :, :ncols].rearrange("p k (q s f) -> p k q s f", s=2, f=16)
            dstv = wsw[:, :, :ncols].rearrange("p k (q s f) -> p k q s f", s=2, f=16)
            for k in range(8):
                S.op("pool", lambda h, k=k: h.tensor_copy(out=dstv[:, k, :, 0, :], in_=src[:, k, :, 1, :]), reads=[wt_b], writes=[wsw_b])
                S.op("pool", lambda h, k=k: h.tensor_copy(out=dstv[:, k, :, 1, :], in_=src[:, k, :, 0, :]), reads=[wt_b], writes=[wsw_b])

        for gi, (kind, dst, row0) in enumerate(_inproj_groups()):
            wt, wt_b = wts[gi % 2]
            S.dma("pool", wt[:], win[:, :, gi * 512:(gi + 1) * 512], wt_b)
            if kind == "qk":
                make_swapped(wt, wt_b, 512)
                for m in range(4):
                    fm_chunk(wt, wt_b, m, "qk", dst, row0 + m * 128)
            elif kind == "v":
                tm_cols(wt, wt_b, 0, 512, dst, row0)
            elif kind == "kv":
                make_swapped(wt, wt_b, 256)
                for m in range(2):
                    fm_chunk(wt, wt_b, m, "qk", "KsT", m * 128)
                tm_cols(wt, wt_b, 256, 256, "Vs", 0)
            else:
                for m in range(4):
                    fm_chunk(wt, wt_b, m, kind, dst, row0 + m * 128)
        S.barrier()


def _bcast_rows(ap, n):
    return bass.AP(ap.tensor, ap.offset, [[0, 128], [1, n]])


def phase_diff(K, l, ctx_out):
    nc, S, I, R = K.nc, K.S, K.I, K.R
    ones_f, ones_f_b = K.ones_f
    ones_b, ones_b_b = K.ones_b
    lambda_init = 0.8 - 0.6 * math.exp(-0.3 * l)
    with contextlib.ExitStack() as st:
        lp, lp_b = K.sb("lp", [128, 256], F32, st)
        lsc, lsc_b = K.sb("lsc", [128, 4], F32, st)
        gsc, gsc_b = K.sb("gsc", [128, 1], F32, st)
        S.dma("sp", lp[:], _bcast_rows(I["diff_lambda"][l], 256), lp_b)
        S.dma("sp", gsc[:], I["sublnT"][l], gsc_b)
        S.op("dve", lambda h: h.tensor_tensor(out=lp[:, 0:64], in0=lp[:, 0:64], in1=lp[:, 64:128], op=ALU.mult), reads=[lp_b], writes=[lp_b])
        S.op("dve", lambda h: h.tensor_tensor(out=lp[:, 128:192], in0=lp[:, 128:192], in1=lp[:, 192:256], op=ALU.mult), reads=[lp_b], writes=[lp_b])
        S.op("dve", lambda h: h.reduce_sum(out=lsc[:, 0:1], in_=lp[:, 0:64], axis=AX.X), reads=[lp_b], writes=[lsc_b])
        S.op("dve", lambda h: h.reduce_sum(out=lsc[:, 1:2], in_=lp[:, 128:192], axis=AX.X), reads=[lp_b], writes=[lsc_b])
        S.op("act", lambda h: h.activation(out=lsc[:, 0:2], in_=lsc[:, 0:2], func=AF.Exp), reads=[lsc_b], writes=[lsc_b])
        S.op("dve", lambda h: h.scalar_tensor_tensor(out=lsc[:, 2:3], in0=lsc[:, 1:2], scalar=-lambda_init, in1=lsc[:, 0:1],
                                                     op0=ALU.add, op1=ALU.subtract), reads=[lsc_b], writes=[lsc_b])
        S.op("dve", lambda h: h.tensor_scalar(out=gsc[:], in0=gsc[:], scalar1=1.0 - lambda_init, scalar2=None, op0=ALU.mult),
             reads=[gsc_b], writes=[gsc_b])

        QT = [K.sb("dQT%d" % i, [128, T], BF16, st) for i in range(2)]
        KT = [[K.sb("dKT%d%d" % (i, m), [128, T], BF16, st) for m in range(2)] for i in range(2)]
        for i in range(2):
            for m in range(2):
                S.op("dve", lambda h, i=i, m=m: h.memset(KT[i][m][0][(1 - m) * 64:(2 - m) * 64, :], 0.0), writes=[KT[i][m][1]])
        VV = [K.sb("dV%d" % i, [128, T // 128, 128], BF16, st) for i in range(2)]
        pt = [K.sb("dP%d" % i, [128, 2, 512], BF16, st) for i in range(2)]
        psS = [K.pst("dpsS%d" % i, [128, 2, 512], F32, st) for i in range(2)]
        acc = [[K.sb("dacc%d%d" % (m, i), [128, 512], F32, st) for i in range(2)] for m in range(2)]
        acc_hi = [S.buf("dacchi%d" % i) for i in range(2)]
        pt_mb = [[S.buf("dPm%d%d" % (i, m)) for m in range(2)] for i in range(2)]
        ps_mb = [[S.buf("dSm%d%d" % (i, m)) for m in range(2)] for i in range(2)]
        psO = [K.pst("dpsO%d" % m, [128, 512], F32, st) for m in range(2)]
        psZ = [K.pst("dpsZ%d" % m, [128, 512], F32, st) for m in range(2)]
        rz = [K.sb("drz%d" % m, [128, 512], F32, st) for m in range(2)]
        oo = [K.sb("doo%d" % m, [128, 512], F32, st) for m in range(2)]
        of, of_b = K.sb("dof", [128, 512], F32, st)
        sqf, sqf_b = K.sb("dsq", [128, 512], F32, st)
        rs, rs_b = K.sb("drs", [128, 512], F32, st)
        ost = [K.sb("dost%d" % i, [128, 512], BF16, st) for i in range(2)]
        nch = 0
        for hd in range(8):
            q, q_b = QT[hd % 2]
            kz = KT[hd % 2]
            v, v_b = VV[hd % 2]
            S.dma("sp", q[:], R["QdT"][0][hd * 128:(hd + 1) * 128, :], q_b, [R["QdT"][1]])
            for m in range(2):
                S.dma("sp", kz[m][0][m * 64:(m + 1) * 64, :], R["KdT"][0][hd * 128 + m * 64:hd * 128 + (m + 1) * 64, :], kz[m][1], [R["KdT"][1]])
            S.dma("sp", v[:], R["Vd"][0][:, hd * 128:(hd + 1) * 128].rearrange("(t p) e -> p t e", p=128), v_b, [R["Vd"][1]])
            for (q0, qn) in TCH:
                if q0 >= L and not ctx_out:
                    continue
                kts = list(range(T // 128)) if q0 < L else [32, 33]
                cpar = nch % 2

                def qk(i, kt, m, drain=False):
                    ps = psS[i % 2][0]
                    _mm1(S, ps_mb[i % 2][m], ps[:, m, :qn], kz[m][0][:, kt * 128:(kt + 1) * 128], q[:, q0:q0 + qn],
                         [kz[m][1], q_b], True, True)
                    p = pt[i % 2][0]
                    S.op("act", lambda h: h.activation(out=p[:, m, :qn], in_=ps[:, m, :qn], func=AF.Exp, scale=0.125),
                         reads=[ps_mb[i % 2][m]], writes=[pt_mb[i % 2][m]])
                qk(0, kts[0], 0, drain=True)
                qk(0, kts[0], 1, drain=True)
                hq = qn // 2
                for i, kt in enumerate(kts):
                    first, last = i == 0, i == len(kts) - 1
                    p = pt[i % 2][0]
                    for m in range(2):
                        p_b = pt_mb[i % 2][m]
                        if not last:
                            qk(i + 1, kts[i + 1], m, drain=(i == 0 and m == 0))
                        _mm1(S, psO[m][1], psO[m][0][:, :qn], v[:, kt, :], p[:, m, :qn], [v_b, p_b], first, last)
                        a_, a_b = acc[m][cpar]
                        if m == 0:
                            parts = [("dve", 0, qn, a_b)]
                        else:
                            parts = [("pool", 0, hq, a_b), ("dve", hq, qn, acc_hi[cpar])]
                        for eng, c0, c1, ab_ in parts:
                            if first:
                                S.op(eng, lambda h, a_=a_, m=m, c0=c0, c1=c1: h.tensor_copy(out=a_[:, c0:c1], in_=p[:, m, c0:c1]), reads=[p_b], writes=[ab_])
                            else:
                                S.op(eng, lambda h, a_=a_, m=m, c0=c0, c1=c1: h.tensor_tensor(out=a_[:, c0:c1], in0=a_[:, c0:c1], in1=p[:, m, c0:c1], op=ALU.add),
                                     reads=[p_b, ab_], writes=[ab_])
                for m in range(2):
                    a_, a_b = acc[m][cpar]
                    _mm_group(S, psZ[m][1], psZ[m][0][:, :qn], [(ones_f[:], a_[:, :qn])], [ones_f_b, a_b] + ([acc_hi[cpar]] if m == 1 else []))
                for m in range(2):
                    S.op("act", lambda h, m=m: h.activation(out=rz[m][0][:, :qn], in_=psZ[m][0][:, :qn], func=AF.Ln), reads=[psZ[m][1]], writes=[rz[m][1]])
                    S.op("act", lambda h, m=m: h.activation(out=rz[m][0][:, :qn], in_=rz[m][0][:, :qn], func=AF.Exp, scale=-1.0), reads=[rz[m][1]], writes=[rz[m][1]])
                    S.op("dve", lambda h, m=m: h.tensor_tensor(out=oo[m][0][:, :qn], in0=psO[m][0][:, :qn], in1=rz[m][0][:, :qn], op=ALU.mult),
                         reads=[psO[m][1], rz[m][1]], writes=[oo[m][1]])
                S.op("dve", lambda h: h.scalar_tensor_tensor(out=of[:, :qn], in0=oo[1][0][:, :qn], scalar=lsc[:, 2:3], in1=oo[0][0][:, :qn],
                                                              op0=ALU.mult, op1=ALU.add), reads=[oo[0][1], oo[1][1], lsc_b], writes=[of_b])
                S.op("pool", lambda h: h.tensor_tensor(out=sqf[:, :qn], in0=of[:, :qn], in1=of[:, :qn], op=ALU.mult), reads=[of_b], writes=[sqf_b])
                ps, ps_b = psZ[0]
                _mm_group(S, ps_b, ps[:, :qn], [(ones_f[:], sqf[:, :qn])], [ones_f_b, sqf_b])
                S.op("dve", lambda h: h.tensor_scalar(out=rs[:, :qn], in0=ps[:, :qn], scalar1=1.0 / 128, scalar2=EPS, op0=ALU.mult, op1=ALU.add),
                     reads=[ps_b], writes=[rs_b])
                S.op("act", lambda h: h.activation(out=rs[:, :qn], in_=rs[:, :qn], func=AF.Ln), reads=[rs_b], writes=[rs_b])
                S.op("act", lambda h: h.activation(out=rs[:, :qn], in_=rs[:, :qn], func=AF.Exp, scale=-0.5), reads=[rs_b], writes=[rs_b])
                S.op("dve", lambda h: h.tensor_tensor(out=of[:, :qn], in0=of[:, :qn], in1=rs[:, :qn], op=ALU.mult), reads=[of_b, rs_b], writes=[of_b])
                o, o_b = ost[nch % 2]
                nch += 1
                S.op("act", lambda h, o=o: h.activation(out=o[:, :qn], in_=of[:, :qn], func=AF.Copy, scale=gsc[:, 0:1]), reads=[of_b, gsc_b], writes=[o_b])
                S.dma("sp", R["YdT"][0][hd * 128:(hd + 1) * 128, q0:q0 + qn], o[:, :qn], R["YdT"][1], [o_b])
        S.barrier()


def phase_swa(K, l, ctx_out):
    nc, S, I, R = K.nc, K.S, K.I, K.R
    ones_b, ones_b_b = K.ones_b
    with contextlib.ExitStack() as st:
        es_, es_b = K.sb("esink", [128, 16], F32, st)
        S.dma("sp", es_[:], _bcast_rows(I["swa_sink"][l], 16), es_b)
        S.op("act", lambda h: h.activation(out=es_[:], in_=es_[:], func=AF.Exp), reads=[es_b], writes=[es_b])
        mlo, mlo_b = K.sb("mlo", [128, 128], BF16, st)
        mup, mup_b = K.sb("mup", [128, 128], BF16, st)
        S.dma("pool", mlo[:], I["trilo"][:, :], mlo_b)
        S.dma("pool", mup[:], I["triup"][:, :], mup_b)
        QC = [[K.sb("sQ%d%d" % (c, i), [128, T], BF16, st) for i in range(2)] for c in range(2)]
        KT = [[K.sb("sKT%d%d" % (i, hf), [128, T], BF16, st) for hf in range(2)] for i in range(2)]
        for i in range(2):
            for hf in range(2):
                S.op("dve", lambda h, i=i, hf=hf: h.memset(KT[i][hf][0][(1 - hf) * 64:(2 - hf) * 64, :], 0.0), writes=[KT[i][hf][1]])
        VV = [K.sb("sV%d" % i, [128, T // 128, 64], BF16, st) for i in range(2)]
        pt = [K.sb("sP%d" % i, [128, 512], BF16, st) for i in range(2)]
        psS = [K.pst("spsS%d" % i, [128, 512], F32, st) for i in range(2)]
        psO = [K.pst("spsO%d" % i, [128, 512], F32, st) for i in range(2)]
        psZ = [K.pst("spsZ%d" % i, [128, 512], F32, st) for i in range(2)]
        zz, zz_b = K.sb("szz", [64, 512], F32, st)
        ost = [K.sb("sost%d" % i, [64, 512], BF16, st) for i in range(2)]
        nblk = 0
        npt = 0
        for kh in range(4):
            kz = KT[kh % 2]
            v, v_b = VV[kh % 2]
            for half in range(2):
                S.dma("sp", kz[half][0][half * 64:(half + 1) * 64, :], R["KsT"][0][kh * 64:(kh + 1) * 64, :], kz[half][1], [R["KsT"][1]])
            S.dma("sp", v[:], R["Vs"][0][:, kh * 64:(kh + 1) * 64].rearrange("(t p) e -> p t e", p=128), v_b, [R["Vs"][1]])
            qc = []
            for c in range(2):
                qq, qq_b = QC[c][kh % 2]
                S.dma("sp", qq[:], R["QsT"][0][kh * 256 + c * 128: kh * 256 + (c + 1) * 128, :], qq_b, [R["QsT"][1]])
                qc.append((qq, qq_b))
            nqb = T // 128 if ctx_out else L // 128
            dbgl = K.cfg.get("swa_dbg", 9)
            if dbgl < 9:
                nqb = 2 if kh == 0 else 0
            steps = []
            for qb in range(nqb):
                if qb < 32:
                    kts = [(kk, mk) for kk, mk in ((qb - 1, "lo"), (qb, None), (qb + 1, "up")) if 0 <= kk < 32] + [(32, None), (33, None)]
                else:
                    kts = [(32, None), (33, None)]
                for i, (kt, mk) in enumerate(kts):
                    steps.append((qb, kt, mk, i == 0, i == len(kts) - 1))

            def stage_a(si):
                qb, kt, mk, first, last = steps[si]
                ps, ps_b = psS[si % 2]
                p, p_b = pt[si % 2]
                for g in range(4):
                    qq, qq_b = qc[g // 2]
                    kk, kk_b = kz[g % 2]
                    _mm1(S, ps_b, ps[:, g * 128:(g + 1) * 128], kk[:, kt * 128:(kt + 1) * 128], qq[:, qb * 128:(qb + 1) * 128],
                         [kk_b, qq_b], True, True)
                S.op("act", lambda h: h.activation(out=p[:], in_=ps[:], func=AF.Exp, scale=0.125), reads=[ps_b], writes=[p_b])
                if mk is not None:
                    mt, mt_b = (mlo, mlo_b) if mk == "lo" else (mup, mup_b)
                    for g in range(4):
                        S.op("pool", lambda h, g=g, mt=mt: h.tensor_tensor(out=p[:, g * 128:(g + 1) * 128], in0=p[:, g * 128:(g + 1) * 128], in1=mt[:], op=ALU.mult),
                             reads=[p_b, mt_b], writes=[p_b])

            if steps:
                stage_a(0)
            for si, (qb, kt, mk, first, last) in enumerate(steps):
                if si + 1 < len(steps):
                    stage_a(si + 1)
                po, po_b = psO[qb % 2]
                pz, pz_b = psZ[qb % 2]
                p, p_b = pt[si % 2]
                _mm1(S, po_b, po[0:64, :], v[:, kt, :], p[:], [v_b, p_b], first, last)
                _mm1(S, pz_b, pz[0:64, :], ones_b[:, 0:64], p[:], [ones_b_b, p_b], first, last)
                if not last:
                    continue
                for g in range(4):
                    S.op("dve", lambda h, g=g: h.tensor_scalar(out=zz[:, g * 128:(g + 1) * 128], in0=pz[0:64, g * 128:(g + 1) * 128],
                                                               scalar1=es_[0:64, kh * 4 + g:kh * 4 + g + 1], scalar2=None, op0=ALU.add),
                         reads=[pz_b, es_b], writes=[zz_b])
                S.op("dve", lambda h: h.reciprocal(out=zz[:], in_=zz[:]), reads=[zz_b], writes=[zz_b])
                o, o_b = ost[qb % 2]
                S.op("dve", lambda h, o=o: h.tensor_tensor(out=o[:], in0=po[0:64, :], in1=zz[:], op=ALU.mult),
                     reads=[po_b, zz_b], writes=[o_b])
                S.dma("sp", R["YsT"][0][kh * 256:(kh + 1) * 256, qb * 128:(qb + 1) * 128].rearrange("(g d) q -> d g q", g=4),
                      o[:].rearrange("p (g q) -> p g q", g=4), R["YsT"][1], [o_b])
        S.barrier()


def phase_merge(K, l):
    nc, S, I, R = K.nc, K.S, K.I, K.R
    modT, modT_b = K.modT
    with contextlib.ExitStack() as st:
        wb, wb_b = K.sb("wbr", [128, 24, D], BF16, st)
        wo, wo_b = K.sb("wout", [128, 8, D], BF16, st)
        for b in range(3):
            S.dma("pool", wb[:, b * 8:(b + 1) * 8, :], I["w_branch"][l, b].rearrange("(k p) n -> p k n", p=128), wb_b)
        S.dma("pool", wo[:], I["w_out"][l].rearrange("(k p) n -> p k n", p=128), wo_b)
        yb = [K.sb("my%d" % b, [128, 8, 512], BF16, st) for b in range(3)]
        gt, gt_b = K.sb("mgt", [128, 24, 512], BF16, st)
        mg, mg_b = K.sb("mmg", [128, 8, 512], F32, st)
        mgb, mgb_b = K.sb("mmgb", [128, 8, 512], BF16, st)
        tmp = [K.sb("mtmp%d" % i, [128, 512], F32, st) for i in range(2)]
        xx, xx_b = K.sb("mx", [128, 8, 512], F32, st)
        pss = [K.pst("mps%d" % i, [128, 512], F32, st) for i in range(4)]
        npz = 0
        ntmp = 0
        XTv = R["XT"][0].rearrange("(k p) t -> p k t", p=128)
        for (t0, tn) in TCH:
            r = 0 if t0 < L else 1
            for b, nm in enumerate(("YdT", "YhT", "YsT")):
                S.dma("sp", yb[b][0][:, :, :tn], R[nm][0].rearrange("(k p) t -> p k t", p=128)[:, :, t0:t0 + tn], yb[b][1], [R[nm][1]])
            S.dma("sp", gt[:, :, :tn], R["GT"][0].rearrange("(k p) t -> p k t", p=128)[:, :, t0:t0 + tn], gt_b, [R["GT"][1]])
            S.dma("sp", xx[:, :, :tn], XTv[:, :, t0:t0 + tn], xx_b, [R["XT"][1]])
            for m in range(8):
                for b in range(3):
                    ps, ps_b = pss[npz % 4]
                    npz += 1
                    _mm_group(S, ps_b, ps[:, :tn], [(wb[:, b * 8 + k, m * 128:(m + 1) * 128], yb[b][0][:, k, :tn]) for k in range(8)],
                              [wb_b, yb[b][1]])
                    if b == 0:
                        S.op("dve", lambda h, m=m, ps=ps: h.tensor_tensor(out=mg[:, m, :tn], in0=ps[:, :tn], in1=gt[:, m, :tn], op=ALU.mult),
                             reads=[ps_b, gt_b], writes=[mg_b])
                    else:
                        tp, tp_b = tmp[ntmp % 2]
                        ntmp += 1
                        S.op("dve", lambda h, m=m, b=b, ps=ps, tp=tp: h.tensor_tensor(out=tp[:, :tn], in0=ps[:, :tn], in1=gt[:, b * 8 + m, :tn], op=ALU.mult),
                             reads=[ps_b, gt_b], writes=[tp_b])
                        S.op("pool", lambda h, m=m, tp=tp: h.tensor_tensor(out=mg[:, m, :tn], in0=mg[:, m, :tn], in1=tp[:, :tn], op=ALU.add),
                             reads=[tp_b, mg_b], writes=[mg_b])
                S.op("act", lambda h, m=m: h.activation(out=mgb[:, m, :tn], in_=mg[:, m, :tn], func=AF.Copy), reads=[mg_b], writes=[mgb_b])
            for m in range(8):
                ps, ps_b = pss[npz % 4]
                npz += 1
                _mm_group(S, ps_b, ps[:, :tn], [(wo[:, k, m * 128:(m + 1) * 128], mgb[:, k, :tn]) for k in range(8)], [wo_b, mgb_b])
                S.op("dve", lambda h, m=m, ps=ps: h.scalar_tensor_tensor(out=xx[:, m, :tn], in0=ps[:, :tn], scalar=modT[:, 16 + m, r:r + 1],
                                                                         in1=xx[:, m, :tn], op0=ALU.mult, op1=ALU.add),
                     reads=[ps_b, modT_b, xx_b], writes=[xx_b])
            S.dma("sp", XTv[:, :, t0:t0 + tn], xx[:, :, :tn], R["XT"][1], [xx_b])
        S.barrier()


PI = math.pi


def _hy_filter(K, l, Lx, embT_ap, negt_ap, Cm, Sm, wf_ap, KFs):
    nc, S, I, R = K.nc, K.S, K.I, K.R
    ones_b, ones_b_b = K.ones_b
    nlt = Lx // 128
    nft = (Lx + 128) // 128
    nch = max(1, Lx // 512)
    cw = min(512, Lx)
    with contextlib.ExitStack() as st:
        w1, w1_b = K.sb("fw1", [33, 64], F32, st)
        w2, w2_b = K.sb("fw2", [64, 64], F32, st)
        w3, w3_b = K.sb("fw3", [64, 2048], F32, st)
        b1, b1_b = K.sb("fb1", [64, 1], F32, st)
        b2, b2_b = K.sb("fb2", [64, 1], F32, st)
        fr, fr_b = K.sb("ffr", [64, 1], F32, st)
        emb, emb_b = K.sb("femb", [33, Lx], F32, st)
        ngt, ngt_b = K.sb("fngt", [128, nlt], F32, st)
        dl, dl_b = K.sb("fdl", [128, D], F32, st)
        wf, wf_b = K.sb("fwf", [128, nft], F32, st)
        z1, z1_b = K.sb("fz1", [64, Lx], F32, st)
        z2, z2_b = K.sb("fz2", [64, Lx], F32, st)
        rn, rn_b = K.sb("frn", [128, D], F32, st)
        dec, dec_b = K.sb("fdec", [128, D], F32, st)
        hd = [K.sb("fhd%d" % i, [128, D], F32, st) for i in range(2)]
        ab, ab_b = K.sb("fab", [128, 512], BF16, st)
        gsel, gsel_b = K.sb("fgsel", [64, 512], F32, st)
        AT, AT_b = K.sb("fAT", [128, nlt, D], BF16, st)
        for t_, src in ((w1, I["hy_w1"][l]), (w2, I["hy_w2"][l]), (w3, I["hy_w3"][l]), (b1, I["hy_b1"][l]), (b2, I["hy_b2"][l]),
                        (fr, I["hy_fr"][l]), (emb, embT_ap), (ngt, negt_ap), (wf, wf_ap)):
            pass
        S.dma("sp", w1[:], I["hy_w1"][l], w1_b)
        S.dma("sp", w2[:], I["hy_w2"][l], w2_b)
        S.dma("sp", w3[:], I["hy_w3"][l], w3_b)
        S.dma("sp", b1[:], I["hy_b1"][l], b1_b)
        S.dma("sp", b2[:], I["hy_b2"][l], b2_b)
        S.dma("sp", fr[:], I["hy_fr"][l], fr_b)
        S.dma("sp", emb[:], embT_ap, emb_b)
        S.dma("sp", ngt[:], negt_ap, ngt_b)
        S.dma("sp", wf[:], wf_ap, wf_b)
        S.dma("sp", dl[:], _bcast_rows(I["deltas"][0], D), dl_b)
        psm = [K.pst("fps%d" % i, [128, 512], F32, st) for i in range(4)]
        psN = [K.pst("fpsN%d" % i, [128, 512], F32, st) for i in range(2)]
        for (wm, wm_b, bb, bb_b, src, src_b, dst, dst_b) in ((w1, w1_b, b1, b1_b, emb, emb_b, z1, z1_b), (w2, w2_b, b2, b2_b, z1, z1_b, z2, z2_b)):
            for ci in range(nch):
                ps, ps_b = psm[ci % 4]
                sl = slice(ci * cw, (ci + 1) * cw)
                _mm_group(S, ps_b, ps[0:64, :cw], [(wm[:], src[:, sl])], [wm_b, src_b])
                S.op("dve", lambda h, ps=ps, sl=sl, bb=bb, dst=dst: h.tensor_scalar(out=dst[:, sl], in0=ps[0:64, :cw], scalar1=bb[:, 0:1], scalar2=fr[:, 0:1],
                                                                                 op0=ALU.add, op1=ALU.mult), reads=[ps_b, bb_b, fr_b], writes=[dst_b])
                for _ in range(2):
                    for cop, sh in ((ALU.is_gt, -2.0 * PI), (ALU.is_lt, 2.0 * PI)):
                        thr = PI if cop == ALU.is_gt else -PI
                        S.op("dve", lambda h, sl=sl, dst=dst, cop=cop, thr=thr: h.tensor_scalar(out=gsel[:, :cw], in0=dst[:, sl], scalar1=thr, scalar2=None, op0=cop),
                             reads=[dst_b], writes=[gsel_b])
                        S.op("dve", lambda h, sl=sl, dst=dst, sh=sh: h.scalar_tensor_tensor(out=dst[:, sl], in0=gsel[:, :cw], scalar=sh, in1=dst[:, sl],
                                                                                          op0=ALU.mult, op1=ALU.add), reads=[gsel_b, dst_b], writes=[dst_b])
                S.op("act", lambda h, sl=sl, dst=dst: h.activation(out=dst[:, sl], in_=dst[:, sl], func=AF.Sin), reads=[dst_b], writes=[dst_b])

        def htile(lt, cb):
            S.op("act", lambda h: h.activation(out=dec[:], in_=dl[:], func=AF.Exp, scale=ngt[:, lt:lt + 1]), reads=[dl_b, ngt_b], writes=[dec_b])
            for j in range(4):
                ps, ps_b = psm[j]
                _mm_group(S, ps_b, ps[:], [(z2[:, lt * 128:(lt + 1) * 128], w3[:, j * 512:(j + 1) * 512])], [z2_b, w3_b])
            cb(lt)

        for lt in range(nlt):
            def cb1(lt):
                for j in range(4):
                    ps, ps_b = psm[j]
                    half = j % 2
                    tf, tf_b = hd[j // 2]
                    S.op("dve", lambda h, ps=ps, half=half, tf=tf: h.tensor_tensor(out=tf[:, half * 512:(half + 1) * 512], in0=ps[:],
                                                                                   in1=dec[:, half * 512:(half + 1) * 512], op=ALU.mult),
                         reads=[ps_b, dec_b], writes=[tf_b])
                    S.op("act", lambda h, half=half, tf=tf: h.activation(out=ab[:], in_=tf[:, half * 512:(half + 1) * 512], func=AF.Abs),
                         reads=[tf_b], writes=[ab_b])
                    first = (lt == 0 and j < 2)
                    last = (lt == nlt - 1 and j >= 2)
                    _mm1(S, psN[half][1], psN[half][0][:], ones_b[:], ab[:], [ones_b_b, ab_b], first, last)
            htile(lt, cb1)
        for half in range(2):
            S.op("dve", lambda h, half=half: h.tensor_scalar(out=rn[:, half * 512:(half + 1) * 512], in0=psN[half][0][:], scalar1=EPS, scalar2=None, op0=ALU.add),
                 reads=[psN[half][1]], writes=[rn_b])
        S.op("dve", lambda h: h.reciprocal(out=rn[:], in_=rn[:]), reads=[rn_b], writes=[rn_b])
        for plane, op2 in (("C", ALU.add), ("S", ALU.subtract)):
            for lt in range(nlt):
                def cb2(lt):
                    for j in range(4):
                        ps, ps_b = psm[j]
                        half, dr = j % 2, j // 2
                        S.op("dve", lambda h, ps=ps, half=half, dr=dr: h.tensor_tensor(out=hd[dr][0][:, half * 512:(half + 1) * 512], in0=ps[:],
                                                                                      in1=dec[:, half * 512:(half + 1) * 512], op=ALU.mult),
                             reads=[ps_b, dec_b], writes=[hd[dr][1]])
                    if lt == 0:
                        S.op("dve", lambda h: h.memset(hd[1][0][0:1, :], 0.0), writes=[hd[1][1]])
                    S.op("pool", lambda h: h.tensor_tensor(out=hd[0][0][:], in0=hd[0][0][:], in1=hd[1][0][:], op=op2), reads=[hd[0][1], hd[1][1]], writes=[hd[0][1]])
                    S.op("pool", lambda h: h.tensor_tensor(out=AT[:, lt, :], in0=hd[0][0][:], in1=rn[:], op=ALU.mult), reads=[hd[0][1], rn_b], writes=[AT_b])
                htile(lt, cb2)
            pi = 0 if plane == "C" else 1

            def evac(ft, pc, ps_, pi=pi):
                src = pc if pi == 0 else ps_
                for hh in range(2):
                    o, o_b = hd[hh]
                    S.op("dve", lambda h, hh=hh, o=o: h.tensor_scalar(out=o[:, 0:512], in0=src[hh][0][:], scalar1=wf[:, ft:ft + 1], scalar2=None, op0=ALU.mult),
                         reads=[src[hh][1], wf_b], writes=[o_b])
                    S.dma("pool", KFs[0][pi, ft * 128:(ft + 1) * 128, hh * 512:(hh + 1) * 512], o[:, 0:512], KFs[1], [o_b])
            _fwd_dft(K, st, Cm, Sm, nlt, nft, AT, AT_b, 0, (plane,), evac, psm)
        S.barrier()


def _fwd_dft(K, st, Cm, Sm, ntt, nft, rhs, rhs_b, tt0, planes, evac, pspool):
    S = K.S
    with contextlib.ExitStack() as s2:
        blk = {p: [K.sb("dblk%s%d" % (p, i), [128, ntt, 128], BF16, s2) for i in range(2)] for p in planes}
        for ft in range(nft):
            pss = {}
            for pi, p in enumerate(("C", "S")):
                if p not in planes:
                    pss[p] = None
                    continue
                M = Cm if p == "C" else Sm
                b, b_b = blk[p][ft % 2]
                S.dma("sp", b[:], M.rearrange("(t p) f -> p t f", p=128)[:, 0:ntt, ft * 128:(ft + 1) * 128], b_b)
                pss[p] = [pspool[pi * 2 + hh] for hh in range(2)]
                for hh in range(2):
                    ps, ps_b = pss[p][hh]
                    _mm_group(S, ps_b, ps[:], [(b[:, t, :], rhs[:, tt0 + t, hh * 512:(hh + 1) * 512]) for t in range(ntt)], [b_b, rhs_b])
            evac(ft, pss["C"], pss["S"])


def phase_hyena(K, l, ctx_out):
    nc, S, I, R = K.nc, K.S, K.I, K.R
    ident_b, ident_b_b = K.ident_b
    _hy_filter(K, l, L, I["embT"][:, :], I["negt"][:, :], I["dftC"], I["dftS"], I["wf"][:, :], R["KF"])
    if ctx_out:
        _hy_filter(K, l, LC, I["embTc"][:, :], I["negtc"][:, :], I["dftCc"], I["dftSc"], I["wfc"][:, :], R["KFc"])
    segs = [(0, L)] + ([(L, LC)] if ctx_out else [])
    TT = T if ctx_out else L
    with contextlib.ExitStack() as st:
        utok, utok_b = K.sb("hutok", [128, T // 128, D], BF16, st)
        with contextlib.ExitStack() as s2:
            cw, cw_b = K.sb("hcw", [128, 24, 3], F32, s2)
            cb, cb_b = K.sb("hcb", [128, 24], F32, s2)
            S.dma("sp", cw[:], I["cwT"][l], cw_b)
            S.dma("sp", cb[:], I["cbT"][l], cb_b)
            xin = [K.sb("hxin%d" % i, [128, T], BF16, s2) for i in range(3)]
            yc = [K.sb("hyc%d" % i, [128, T], F32, s2) for i in range(3)]
            x0b, x0b_b = K.sb("hx0b", [128, T], BF16, s2)
            ub, ub_b = K.sb("hub", [128, T], BF16, s2)
            ptr = [K.pst("hptr%d" % i, [128, 1024], BF16, s2) for i in range(2)]
            ntr = 0
            for c in range(8):
                for s_ in range(3):
                    j = s_ * 8 + c
                    xi, xi_b = xin[s_]
                    y, y_b = yc[s_]
                    S.dma("sp", xi[:, :TT], R["HyT"][0][j * 128:(j + 1) * 128, 0:TT], xi_b, [R["HyT"][1]])
                    for (a0, n) in segs:
                        S.op("dve", lambda h, j=j, xi=xi, y=y: h.tensor_scalar(out=y[:, a0:a0 + n], in0=xi[:, a0:a0 + n], scalar1=cw[:, j, 1:2], scalar2=cb[:, j:j + 1],
                                                                            op0=ALU.mult, op1=ALU.add), reads=[xi_b, cw_b, cb_b], writes=[y_b])
                        S.op("dve", lambda h, j=j, xi=xi, y=y: h.scalar_tensor_tensor(out=y[:, a0 + 1:a0 + n], in0=xi[:, a0:a0 + n - 1], scalar=cw[:, j, 0:1],
                                                                                   in1=y[:, a0 + 1:a0 + n], op0=ALU.mult, op1=ALU.add),
                             reads=[xi_b, cw_b, y_b], writes=[y_b])
                        S.op("dve", lambda h, j=j, xi=xi, y=y: h.scalar_tensor_tensor(out=y[:, a0:a0 + n - 1], in0=xi[:, a0 + 1:a0 + n], scalar=cw[:, j, 2:3],
                                                                                   in1=y[:, a0:a0 + n - 1], op0=ALU.mult, op1=ALU.add),
                             reads=[xi_b, cw_b, y_b], writes=[y_b])
                S.op("act", lambda h: h.activation(out=x0b[:, :TT], in_=yc[0][0][:, :TT], func=AF.Copy), reads=[yc[0][1]], writes=[x0b_b])
                S.op("pool", lambda h: h.tensor_tensor(out=ub[:, :TT], in0=yc[1][0][:, :TT], in1=yc[2][0][:, :TT], op=ALU.mult),
                     reads=[yc[1][1], yc[2][1]], writes=[ub_b])
                S.dma("sp", R["X0T"][0][c * 128:(c + 1) * 128, 0:TT], x0b[:, :TT], R["X0T"][1], [x0b_b])
                S.dma("sp", R["UT"][0][c * 128:(c + 1) * 128, 0:TT], ub[:, :TT], R["UT"][1], [ub_b])
                for t8 in range(0, TT // 128, 8):
                    n8 = min(8, TT // 128 - t8)
                    pt_, pt_b = ptr[ntr % 2]
                    ntr += 1

                    def trf(h, t8=t8, n8=n8, pt_=pt_):
                        ins = None
                        for q in range(n8):
                            ins = h.transpose(pt_[:, q * 128:(q + 1) * 128], ub[:, (t8 + q) * 128:(t8 + q + 1) * 128], ident_b[:])
                        return ins
                    S.op("pe", trf, reads=[ub_b, ident_b_b], writes=[pt_b])
                    S.op("act", lambda h, t8=t8, n8=n8, pt_=pt_, c=c: h.activation(out=utok[:, t8:t8 + n8, c * 128:(c + 1) * 128],
                                                                                   in_=pt_[:, :n8 * 128].rearrange("p (q f) -> p q f", f=128), func=AF.Copy),
                         reads=[pt_b], writes=[utok_b])
            S.barrier()
        for si, (a0, n) in enumerate(segs):
            Cm, Sm = (I["dftC"], I["dftS"]) if si == 0 else (I["dftCc"], I["dftSc"])
            KFs = R["KF"] if si == 0 else R["KFc"]
            YFs = R["YF"] if si == 0 else R["YFc"]
            ntt = n // 128
            nft = (n + 128) // 128
            with contextlib.ExitStack() as s2:
                kf = [K.sb("hkf%d" % i, [128, 2, D], F32, s2) for i in range(2)]
                tq = [K.sb("htq%d" % i, [128, 512], F32, s2) for i in range(4)]
                yo = [K.sb("hyo%d" % i, [128, 2, D], BF16, s2) for i in range(2)]
                pspool = [K.pst("hps%d" % i, [128, 512], F32, s2) for i in range(4)]

                def evac(ft, pc, ps_, KFs=KFs, YFs=YFs):
                    k_, k_b = kf[ft % 2]
                    o, o_b = yo[ft % 2]
                    S.dma("sp", k_[:], KFs[0][:, ft * 128:(ft + 1) * 128, :].rearrange("a p c -> p a c"), k_b, [KFs[1]])
                    for hh in range(2):
                        cs = slice(hh * 512, (hh + 1) * 512)
                        ur, ur_b = pc[hh]
                        ui, ui_b = ps_[hh]
                        S.op("dve", lambda h: h.tensor_tensor(out=tq[0][0][:], in0=ur[:], in1=k_[:, 0, cs], op=ALU.mult), reads=[ur_b, k_b], writes=[tq[0][1]])
                        S.op("dve", lambda h: h.tensor_tensor(out=tq[1][0][:], in0=ui[:], in1=k_[:, 1, cs], op=ALU.mult), reads=[ui_b, k_b], writes=[tq[1][1]])
                        S.op("pool", lambda h: h.tensor_tensor(out=o[:, 0, cs], in0=tq[0][0][:], in1=tq[1][0][:], op=ALU.subtract),
                             reads=[tq[0][1], tq[1][1]], writes=[o_b])
                        S.op("dve", lambda h: h.tensor_tensor(out=tq[2][0][:], in0=ur[:], in1=k_[:, 1, cs], op=ALU.mult), reads=[ur_b, k_b], writes=[tq[2][1]])
                        S.op("dve", lambda h: h.tensor_tensor(out=tq[3][0][:], in0=ui[:], in1=k_[:, 0, cs], op=ALU.mult), reads=[ui_b, k_b], writes=[tq[3][1]])
                        S.op("pool", lambda h: h.tensor_tensor(out=o[:, 1, cs], in0=tq[2][0][:], in1=tq[3][0][:], op=ALU.add),
                             reads=[tq[2][1], tq[3][1]], writes=[o_b])
                    S.dma("pool", YFs[0][:, ft * 128:(ft + 1) * 128, :].rearrange("a p c -> p a c"), o[:], YFs[1], [o_b])
                _fwd_dft(K, s2, Cm, Sm, ntt, nft, utok, utok_b, a0 // 128, ("C", "S"), evac, pspool)
                S.barrier()
    for si, (a0, n) in enumerate(segs):
        Cm, Sm = (I["dftC"], I["dftS"]) if si == 0 else (I["dftCc"], I["dftSc"])
        YFs = R["YF"] if si == 0 else R["YFc"]
        nft = (n + 128) // 128
        with contextlib.ExitStack() as s2:
            hb, hb_b = K.sb("ihb", [128, 8], F32, s2)
            S.dma("sp", hb[:], I["hbT"][l], hb_b)
            Yh = [K.sb("iY%d" % i, [128, nft, 512], BF16, s2) for i in range(2)]
            Cr = [K.sb("iC%d" % i, [128, nft, 256], BF16, s2) for i in range(2)]
            Sr = [K.sb("iS%d" % i, [128, nft, 256], BF16, s2) for i in range(2)]
            x0c = [K.sb("ix0%d" % i, [128, 4, 256], BF16, s2) for i in range(2)]
            uc = [K.sb("iu%d" % i, [128, 4, 256], BF16, s2) for i in range(2)]
            tmp, tmp_b = K.sb("itmp", [128, 256], F32, s2)
            og = [K.sb("iog%d" % i, [128, 4, 256], BF16, s2) for i in range(2)]
            psI = [K.pst("ips%d" % i, [128, 512], F32, s2) for i in range(2)]
            nk = 0
            npi = 0
            for half in range(2):
                for pl in range(2):
                    S.dma("sp", Yh[pl][0][:], YFs[0][pl, 0:nft * 128, half * 512:(half + 1) * 512].rearrange("(f p) c -> p f c", p=128), Yh[pl][1], [YFs[1]])
                for t0 in range(0, n, 256):
                    cr, cr_b = Cr[nk % 2]
                    sr, sr_b = Sr[nk % 2]
                    xx, xx_b = x0c[nk % 2]
                    uu, uu_b = uc[nk % 2]
                    oo, oo_b = og[nk % 2]
                    nk += 1
                    S.dma("sp", cr[:], Cm.rearrange("(f p) t -> p f t", p=128)[:, 0:nft, t0:t0 + 256], cr_b)
                    S.dma("sp", sr[:], Sm.rearrange("(f p) t -> p f t", p=128)[:, 0:nft, t0:t0 + 256], sr_b)
                    rows = slice(half * 512, (half + 1) * 512)
                    S.dma("sp", xx[:], R["X0T"][0][rows, a0 + t0:a0 + t0 + 256].rearrange("(c p) t -> p c t", p=128), xx_b, [R["X0T"][1]])
                    S.dma("sp", uu[:], R["UT"][0][rows, a0 + t0:a0 + t0 + 256].rearrange("(c p) t -> p c t", p=128), uu_b, [R["UT"][1]])
                    for ci in range(4):
                        ps, ps_b = psI[npi % 2]
                        npi += 1
                        pairs = [(Yh[0][0][:, f, ci * 128:(ci + 1) * 128], cr[:, f, :]) for f in range(nft)] + \
                                [(Yh[1][0][:, f, ci * 128:(ci + 1) * 128], sr[:, f, :]) for f in range(nft)]
                        _mm_group(S, ps_b, ps[:, 0:256], pairs, [Yh[0][1], Yh[1][1], cr_b, sr_b])
                        cidx = half * 4 + ci
                        S.op("dve", lambda h, ci=ci, cidx=cidx, ps=ps, uu=uu: h.scalar_tensor_tensor(out=tmp[:], in0=uu[:, ci, :], scalar=hb[:, cidx:cidx + 1],
                                                                                                    in1=ps[:, 0:256], op0=ALU.mult, op1=ALU.add),
                             reads=[uu_b, hb_b, ps_b], writes=[tmp_b])
                        S.op("pool", lambda h, ci=ci, oo=oo, xx=xx: h.tensor_tensor(out=oo[:, ci, :], in0=tmp[:], in1=xx[:, ci, :], op=ALU.mult),
                             reads=[tmp_b, xx_b], writes=[oo_b])
                    S.dma("pool", R["YhT"][0][rows, a0 + t0:a0 + t0 + 256].rearrange("(c p) t -> p c t", p=128), oo[:], R["YhT"][1], [oo_b])
            S.barrier()
    if not ctx_out:
        pass


def _bc_mid(ap2d, n):
    a = ap2d.ap
    return bass.AP(ap2d.tensor, ap2d.offset, [list(a[0]), [0, n], list(a[1])])


def phase_moe(K, l, ctx_out):
    nc, S, I, R = K.nc, K.S, K.I, K.R
    modT, modT_b = K.modT
    A2, A2_b = K.A2
    ones_f, ones_f_b = K.ones_f
    ones_b, ones_b_b = K.ones_b
    ident_f, ident_f_b = K.ident_f
    ident_b, ident_b_b = K.ident_b
    debug = K.cfg.get("debug", ())
    XTv = R["XT"][0].rearrange("(k p) t -> p k t", p=128)
    chunks = TCH if ctx_out else TCH[:8]
    ntt = 34 if ctx_out else 32
    groups = [(0, 32, CAP)] + ([(32, 34, CAPC)] if ctx_out else [])
    with contextlib.ExitStack() as st:
        lg, lg_b = K.sb("lg", [128, 34, NE], F32, st)
        aff, aff_b = K.sb("aff", [128, 34, NE], F32, st)
        psel, psel_b = K.sb("psel", [128, 34, NE], F32, st)
        wr, wr_b = K.sb("wr", [128, 8, NE], F32, st)
        S.dma("sp", wr[:], I["router_w"][l].rearrange("(k p) e -> p k e", p=128), wr_b)
        sH = contextlib.ExitStack()
        H2, H2_b = K.sb("H2", [128, 34, D], BF16, sH)
        if ctx_out is False:
            S.op("dve", lambda h: h.memset(lg[:, 32:34, :], 0.0), writes=[lg_b])
        with contextlib.ExitStack() as s2:
            xs = [K.sb("qx%d" % i, [128, 8, 512], F32, s2) for i in range(2)]
            sq, sq_b = K.sb("qsq", [128, 8, 512], F32, s2)
            rstd, rstd_b = K.sb("qrstd", [128, 512], F32, s2)
            tmp, tmp_b = K.sb("qtmp", [128, 512], F32, s2)
            h2f, h2f_b = K.sb("qh2f", [128, 8, 512], F32, s2)
            hTc, hTc_b = K.sb("qhTc", [128, 8, 512], BF16, s2)
            ps, ps_b = K.pst("qps", [128, 512], F32, s2)
            psl, psl_b = K.pst("qpsl", [128, 64], F32, s2)
            pstr = [K.pst("qpst%d" % i, [128, 1024], BF16, s2) for i in range(2)]
            ntr = 0
            for ci, (t0, tn) in enumerate(chunks):
                r = 0 if t0 < L else 1
                x, x_b = xs[ci % 2]
                S.dma("sp", x[:, :, :tn], XTv[:, :, t0:t0 + tn], x_b, [R["XT"][1]])
                if "XT1" in debug:
                    S.dma("sp", R["XT1"][0].rearrange("(k p) t -> p k t", p=128)[:, :, t0:t0 + tn], x[:, :, :tn], R["XT1"][1], [x_b])
                S.op("act", lambda h: h.activation(out=sq[:, :, :tn], in_=x[:, :, :tn], func=AF.Square), reads=[x_b], writes=[sq_b])
                _mm_group(S, ps_b, ps[:, :tn], [(ones_f[:], sq[:, k, :tn]) for k in range(8)], [ones_f_b, sq_b])
                S.op("dve", lambda h: h.tensor_scalar(out=rstd[:, :tn], in0=ps[:, :tn], scalar1=1.0 / D, scalar2=EPS,
                                                      op0=ALU.mult, op1=ALU.add), reads=[ps_b], writes=[rstd_b])
                S.op("act", lambda h: h.activation(out=rstd[:, :tn], in_=rstd[:, :tn], func=AF.Sqrt), reads=[rstd_b], writes=[rstd_b])
                S.op("dve", lambda h: h.reciprocal(out=rstd[:, :tn], in_=rstd[:, :tn]), reads=[rstd_b], writes=[rstd_b])
                for k in range(8):
                    S.op("dve", lambda h, k=k: h.tensor_tensor(out=tmp[:, :tn], in0=x[:, k, :tn], in1=rstd[:, :tn], op=ALU.mult),
                         reads=[x_b, rstd_b], writes=[tmp_b])
                    S.op("act", lambda h, k=k: h.activation(out=h2f[:, k, :tn], in_=tmp[:, :tn], func=AF.Identity,
                                                            scale=A2[:, k, r:r + 1], bias=modT[:, 24 + k, r:r + 1]),
                         reads=[tmp_b, A2_b, modT_b], writes=[h2f_b])
                    S.op("pool", lambda h, k=k: h.tensor_copy(out=hTc[:, k, :tn], in_=h2f[:, k, :tn]), reads=[h2f_b], writes=[hTc_b])
                nj = tn // 128
                for j in range(nj):
                    tt = t0 // 128 + j
                    _mm_group(S, psl_b, psl[:, j * 16:(j + 1) * 16],
                              [(h2f[:, k, j * 128:(j + 1) * 128], wr[:, k, :]) for k in range(8)], [h2f_b, wr_b])
                    pt_, pt_b = pstr[ntr % 2]
                    ntr += 1

                    def trf(h, j=j, pt_=pt_):
                        ins = None
                        for k in range(8):
                            ins = h.transpose(pt_[:, k * 128:(k + 1) * 128], hTc[:, k, j * 128:(j + 1) * 128], ident_b[:])
                        return ins
                    S.op("pe", trf, reads=[hTc_b, ident_b_b], writes=[pt_b])
                    S.op("act", lambda h, tt=tt, pt_=pt_: h.activation(out=H2[:, tt, :], in_=pt_[:], func=AF.Copy), reads=[pt_b], writes=[H2_b])
                S.op("dve", lambda h: h.tensor_copy(out=lg[:, t0 // 128:t0 // 128 + nj, :].rearrange("p t e -> p (t e)"),
                                                    in_=psl[:, :nj * 16]), reads=[psl_b], writes=[lg_b])
            S.barrier()
        if "LG" in debug:
            S.dma("sp", R["LG"][0], lg[:], R["LG"][1], [lg_b])
            S.dma("sp", R["H2d"][0], H2[:], R["H2d"][1], [H2_b])
        with contextlib.ExitStack() as s2:
            se, se_b = K.sb("rse", [128, 34], F32, s2)
            lo, lo_b = K.sb("rlo", [128, NE], F32, s2)
            mid, mid_b = K.sb("rmid", [128, NE], F32, s2)
            cmpt, cmp_b = K.sb("rcmp", [128, 32, NE], F32, s2)
            cntp, cntp_b = K.sb("rcntp", [128, NE], F32, s2)
            ge, ge_b = K.sb("rge", [128, NE], F32, s2)
            mk, mk_b = K.sb("rmk", [128, 34, NE], F32, s2)
            mkb, mkb_b = K.sb("rmkb", [128, 34, NE], BF16, s2)
            tot, tot_b = K.sb("rtot", [128, 34, NE], F32, s2)
            base, base_b = K.sb("rbase", [128, 34, NE], F32, s2)
            posT, posT_b = K.sb("posT", [16, T], F32, s2)
            affT, affT_b = K.sb("affT", [16, T], F32, s2)
            us, us_b = K.sb("rus", [128, 128], BF16, s2)
            S.dma("pool", us[:], I["ustrict"][:, :], us_b)
            psc, psc_b = K.pst("rpsc", [128, 512], F32, s2)
            psw, psw_b = K.pst("rpsw", [128, 512], F32, s2)
            pst_, pst_b = K.pst("rpst", [128, 512], F32, s2)
            S.op("act", lambda h: h.activation(out=aff[:].rearrange("p t e -> p (t e)"), in_=lg[:].rearrange("p t e -> p (t e)"), func=AF.Exp),
                 reads=[lg_b], writes=[aff_b])
            S.op("dve", lambda h: h.reduce_sum(out=se[:], in_=aff[:], axis=AX.X), reads=[aff_b], writes=[se_b])
            S.op("dve", lambda h: h.reciprocal(out=se[:], in_=se[:]), reads=[se_b], writes=[se_b])
            for tt in range(34):
                S.op("dve", lambda h, tt=tt: h.tensor_scalar(out=aff[:, tt, :], in0=aff[:, tt, :], scalar1=se[:, tt:tt + 1], scalar2=None, op0=ALU.mult),
                     reads=[aff_b, se_b], writes=[aff_b])
            S.op("dve", lambda h: h.memset(mk[:], 0.0), writes=[mk_b])
            S.op("dve", lambda h: h.memset(base[:], 0.0), writes=[base_b])
            for (ta, tb, cap) in groups:
                nt = tb - ta
                S.op("dve", lambda h: h.memset(lo[:], 0.0), writes=[lo_b])
                for it in range(30):
                    w = 0.5 ** (it + 1)
                    S.op("dve", lambda h: h.tensor_scalar(out=mid[:], in0=lo[:], scalar1=w, scalar2=None, op0=ALU.add), reads=[lo_b], writes=[mid_b])
                    S.op("dve", lambda h: h.tensor_tensor(out=cmpt[:, :nt, :], in0=aff[:, ta:tb, :], in1=_bc_mid(mid[:], nt), op=ALU.is_ge),
                         reads=[aff_b, mid_b], writes=[cmp_b])
                    S.op("dve", lambda h: h.reduce_sum(out=cntp[:], in_=cmpt[:, :nt, :].rearrange("p t e -> p e t"), axis=AX.X),
                         reads=[cmp_b], writes=[cntp_b])
                    _mm_group(S, psc_b, psc[:, 0:NE], [(ones_f[:], cntp[:])], [ones_f_b, cntp_b])
                    S.op("dve", lambda h: h.tensor_scalar(out=ge[:], in0=psc[:, 0:NE], scalar1=cap - 0.5, scalar2=None, op0=ALU.is_ge),
                         reads=[psc_b], writes=[ge_b])
                    S.op("dve", lambda h: h.scalar_tensor_tensor(out=lo[:], in0=ge[:], scalar=w, in1=lo[:], op0=ALU.mult, op1=ALU.add),
                         reads=[ge_b, lo_b], writes=[lo_b])
                S.op("dve", lambda h: h.tensor_tensor(out=mk[:, ta:tb, :], in0=aff[:, ta:tb, :], in1=_bc_mid(lo[:], nt), op=ALU.is_ge),
                     reads=[aff_b, lo_b], writes=[mk_b])
                S.op("dve", lambda h: h.tensor_copy(out=mkb[:, ta:tb, :], in_=mk[:, ta:tb, :]), reads=[mk_b], writes=[mkb_b])
                mflat = mkb[:, ta:tb, :].rearrange("p t e -> p (t e)")
                _mm_group(S, psw_b, psw[:, :nt * NE], [(us[:], mflat)], [us_b, mkb_b])
                _mm_group(S, pst_b, pst_[:, :nt * NE], [(ones_b[:], mflat)], [ones_b_b, mkb_b])
                S.op("dve", lambda h: h.tensor_copy(out=tot[:, ta:tb, :].rearrange("p t e -> p (t e)"), in_=pst_[:, :nt * NE]),
                     reads=[pst_b], writes=[tot_b])
                for t in range(ta + 1, tb):
                    S.op("dve", lambda h, t=t: h.tensor_tensor(out=base[:, t, :], in0=base[:, t - 1, :], in1=tot[:, t - 1, :], op=ALU.add),
                         reads=[base_b, tot_b], writes=[base_b])
                S.op("dve", lambda h: h.tensor_tensor(out=psel[:, ta:tb, :].rearrange("p t e -> p (t e)"), in0=psw[:, :nt * NE],
                                                      in1=base[:, ta:tb, :].rearrange("p t e -> p (t e)"), op=ALU.add),
                     reads=[psw_b, base_b], writes=[psel_b])
                S.op("dve", lambda h: h.scalar_tensor_tensor(out=psel[:, ta:tb, :], in0=psel[:, ta:tb, :], scalar=1.0, in1=mk[:, ta:tb, :],
                                                             op0=ALU.add, op1=ALU.mult), reads=[psel_b, mk_b], writes=[psel_b])
                S.op("dve", lambda h: h.tensor_scalar(out=psel[:, ta:tb, :], in0=psel[:, ta:tb, :], scalar1=-1.0, scalar2=None, op0=ALU.add),
                     reads=[psel_b], writes=[psel_b])
            S.op("dve", lambda h: h.tensor_tensor(out=aff[:], in0=aff[:], in1=mk[:], op=ALU.mult), reads=[aff_b, mk_b], writes=[aff_b])
            for src, src_b, dstT, dstT_b in ((psel, psel_b, posT, posT_b), (aff, aff_b, affT, affT_b)):
                for t4 in range(0, ntt, 4):
                    n4 = min(4, ntt - t4)

                    def trf(h, t4=t4, n4=n4, src=src):
                        ins = None
                        for j in range(n4):
                            ins = h.transpose(psc[0:16, j * 128:(j + 1) * 128], src[:, t4 + j, :], ident_f[:])
                        return ins
                    S.op("pe", trf, reads=[src_b, ident_f_b], writes=[psc_b])
                    S.op("act", lambda h, t4=t4, n4=n4, dstT=dstT: h.activation(out=dstT[:, t4 * 128:(t4 + n4) * 128], in_=psc[0:16, :n4 * 128], func=AF.Copy),
                         reads=[psc_b], writes=[dstT_b])
            S.dma("sp", R["PT"][0][0, :, 0:ntt * 128], posT[:, 0:ntt * 128], R["PT"][1], [posT_b])
            S.dma("sp", R["PT"][0][1, :, 0:ntt * 128], affT[:, 0:ntt * 128], R["PT"][1], [affT_b])
            S.barrier()
        NS = CAP + CAPC
        with contextlib.ExitStack() as s3:
            w1, w1_b = K.sb("ew1", [128, 8, D], BF16, s3)
            w3, w3_b = K.sb("ew3", [128, 8, D], BF16, s3)
            w2, w2_b = K.sb("ew2", [128, 8, D], BF16, s3)
            Se, Se_b = K.sb("eS", [128, 32, 512], BF16, s3)
            Sc, Sc_b = K.sb("eSc", [128, 2, CAPC], BF16, s3)
            io, io_b = K.sb("eio", [128, 512], F32, s3)
            S.dma("sp", io[:], I["iota512"][:, :], io_b)
            xg, xg_b = K.sb("exg", [128, 8, NS], BF16, s3)
            gT, gT_b = K.sb("egT", [128, 8, NS], BF16, s3)
            sil, sil_b = K.sb("esil", [128, NS], F32, s3)
            yst = [K.sb("eyst%d" % i, [128, 512], BF16, s3) for i in range(2)]
            psG = [K.pst("epsG%d" % i, [128, 512], F32, s3) for i in range(2)]
            psA, psA_b = K.pst("epsA", [128, 512], F32, s3)
            psB, psB_b = K.pst("epsB", [128, 512], F32, s3)
            psC, psC_b = K.pst("epsC", [128, 512], F32, s3)
            psY = [K.pst("epsY%d" % i, [128, 512], F32, s3) for i in range(2)]
            ng = 0
            ny = 0
            for e in range(NE):
                S.dma("pool", w1[:], I["moe_w1"][l, e].rearrange("(k p) n -> p k n", p=128), w1_b)
                S.dma("pool", w3[:], I["moe_w3"][l, e].rearrange("(k p) n -> p k n", p=128), w3_b)
                S.dma("pool", w2[:], I["moe_w2"][l, e].rearrange("(k p) n -> p k n", p=128), w2_b)
                for tt in range(32):
                    S.op("dve", lambda h, tt=tt: h.tensor_scalar(out=Se[:, tt, :], in0=io[:], scalar1=psel[:, tt, e:e + 1], scalar2=None, op0=ALU.is_equal),
                         reads=[io_b, psel_b], writes=[Se_b])
                if ctx_out:
                    for tt in range(32, 34):
                        S.op("dve", lambda h, tt=tt: h.tensor_scalar(out=Sc[:, tt - 32, :], in0=io[:, 0:CAPC], scalar1=psel[:, tt, e:e + 1], scalar2=None,
                                                                     op0=ALU.is_equal), reads=[io_b, psel_b], writes=[Sc_b])
                for dk in range(8):
                    pg, pg_b = psG[ng % 2]
                    ng += 1
                    _mm_group(S, pg_b, pg[:], [(H2[:, tt, dk * 128:(dk + 1) * 128], Se[:, tt, :]) for tt in range(32)], [H2_b, Se_b])
                    S.op("act", lambda h, dk=dk, pg=pg: h.activation(out=xg[:, dk, 0:CAP], in_=pg[:], func=AF.Copy), reads=[pg_b], writes=[xg_b])
                    if ctx_out:
                        _mm_group(S, psC_b, psC[:, 0:CAPC], [(H2[:, tt, dk * 128:(dk + 1) * 128], Sc[:, tt - 32, :]) for tt in (32, 33)], [H2_b, Sc_b])
                        S.op("act", lambda h, dk=dk: h.activation(out=xg[:, dk, CAP:NS], in_=psC[:, 0:CAPC], func=AF.Copy), reads=[psC_b], writes=[xg_b])
                for m in range(8):
                    _mm_group(S, psA_b, psA[:], [(w1[:, k, m * 128:(m + 1) * 128], xg[:, k, 0:CAP]) for k in range(8)], [w1_b, xg_b])
                    _mm_group(S, psB_b, psB[:], [(w3[:, k, m * 128:(m + 1) * 128], xg[:, k, 0:CAP]) for k in range(8)], [w3_b, xg_b])
                    S.op("act", lambda h: h.activation(out=sil[:, 0:CAP], in_=psA[:], func=AF.Silu), reads=[psA_b], writes=[sil_b])
                    S.op("dve", lambda h, m=m: h.tensor_tensor(out=gT[:, m, 0:CAP], in0=psB[:], in1=sil[:, 0:CAP], op=ALU.mult),
                         reads=[psB_b, sil_b], writes=[gT_b])
                    if ctx_out:
                        _mm_group(S, psC_b, psC[:, 0:CAPC], [(w1[:, k, m * 128:(m + 1) * 128], xg[:, k, CAP:NS]) for k in range(8)], [w1_b, xg_b])
                        S.op("act", lambda h: h.activation(out=sil[:, CAP:NS], in_=psC[:, 0:CAPC], func=AF.Silu), reads=[psC_b], writes=[sil_b])
                        _mm_group(S, psC_b, psC[:, 0:CAPC], [(w3[:, k, m * 128:(m + 1) * 128], xg[:, k, CAP:NS]) for k in range(8)], [w3_b, xg_b])
                        S.op("dve", lambda h, m=m: h.tensor_tensor(out=gT[:, m, CAP:NS], in0=psC[:, 0:CAPC], in1=sil[:, CAP:NS], op=ALU.mult),
                             reads=[psC_b, sil_b], writes=[gT_b])
                for c in range(5 if ctx_out else 4):
                    rows = 128 if c < 4 else CAPC
                    for dh in range(2):
                        py, py_b = psY[ny % 2]
                        ys, ys_b = yst[ny % 2]
                        ny += 1
                        _mm_group(S, py_b, py[0:rows, :], [(gT[:, f, c * 128:c * 128 + rows], w2[:, f, dh * 512:(dh + 1) * 512]) for f in range(8)],
                                  [gT_b, w2_b])
                        S.op("act", lambda h, py=py, ys=ys, rows=rows: h.activation(out=ys[0:rows, :], in_=py[0:rows, :], func=AF.Copy),
                             reads=[py_b], writes=[ys_b])
                        S.dma("sp", R["YE"][0][e, c * 128:c * 128 + rows, dh * 512:(dh + 1) * 512], ys[0:rows, :], R["YE"][1], [ys_b])
            S.barrier()
        sH.close()
        with contextlib.ExitStack() as s4:
            YEs, YEs_b = K.sb("cYE", [128, NE, 5, 512], BF16, s4)
            ST, ST_b = K.sb("cST", [128, NE, 4, 512], BF16, s4)
            abc = [K.sb("cabc%d" % i, [128, 512], F32, s4) for i in range(2)]
            xh, xh_b = K.sb("cxh", [128, 4, 512], F32, s4)
            selm, selm_b = K.sb("cselm", [16, NE, 128], F32, s4)
            sidx, sidx_b = K.sb("csidx", [128, 5], F32, s4)
            posT, posT_b = K.sb("cposT", [16, T], F32, s4)
            affT, affT_b = K.sb("caffT", [16, T], F32, s4)
            S.dma("sp", posT[:, 0:ntt * 128], R["PT"][0][0, :, 0:ntt * 128], posT_b, [R["PT"][1]])
            S.dma("sp", affT[:, 0:ntt * 128], R["PT"][0][1, :, 0:ntt * 128], affT_b, [R["PT"][1]])
            S.dma("sp", selm[:], I["selm"].rearrange("k (e m) -> k e m", e=NE), selm_b)
            S.dma("sp", sidx[:], I["slotidx"][:, :], sidx_b)
            psP = [K.pst("cpsP%d" % i, [128, 512], F32, s4) for i in range(2)]
            psQ = [K.pst("cpsQ%d" % i, [128, 512], F32, s4) for i in range(2)]
            psO = [K.pst("cpsO%d" % i, [128, 512], F32, s4) for i in range(2)]
            nb_ = 0
            no = 0
            for dh in range(2):
                for e in range(NE):
                    S.dma("sp", YEs[:, e, 0:4, :], R["YE"][0][e, 0:CAP, dh * 512:(dh + 1) * 512].rearrange("(c p) d -> p c d", p=128), YEs_b, [R["YE"][1]])
                    if ctx_out:
                        S.dma("sp", YEs[0:CAPC, e, 4, :], R["YE"][0][e, CAP:NS, dh * 512:(dh + 1) * 512], YEs_b, [R["YE"][1]])
                for (t0, tn) in chunks:
                    r = 0 if t0 < L else 1
                    lat = t0 < L
                    for e in range(NE):
                        pp, pp_b = psP[nb_ % 2]
                        pq, pq_b = psQ[nb_ % 2]
                        ab, ab_b = abc[nb_ % 2]
                        nb_ += 1
                        _mm_group(S, pp_b, pp[:, :tn], [(selm[:, e, :], posT[:, t0:t0 + tn])], [selm_b, posT_b])
                        _mm_group(S, pq_b, pq[:, :tn], [(selm[:, e, :], affT[:, t0:t0 + tn])], [selm_b, affT_b])
                        S.op("act", lambda h, ab=ab, pq=pq: h.activation(out=ab[:, :tn], in_=pq[:, :tn], func=AF.Copy), reads=[pq_b], writes=[ab_b])
                        if lat:
                            for c in range(4):
                                S.op("dve", lambda h, c=c, pp=pp, ab=ab: h.scalar_tensor_tensor(out=ST[:, e, c, :tn], in0=pp[:, :tn], scalar=sidx[:, c:c + 1],
                                                                                               in1=ab[:, :tn], op0=ALU.is_equal, op1=ALU.mult),
                                     reads=[pp_b, ab_b, sidx_b], writes=[ST_b])
                        else:
                            S.op("dve", lambda h, pp=pp, ab=ab: h.scalar_tensor_tensor(out=ST[0:CAPC, e, 0, :tn], in0=pp[0:CAPC, :tn], scalar=sidx[0:CAPC, 4:5],
                                                                                      in1=ab[0:CAPC, :tn], op0=ALU.is_equal, op1=ALU.mult),
                                 reads=[pp_b, ab_b, sidx_b], writes=[ST_b])
                    S.dma("sp", xh[:, :, :tn], XTv[:, dh * 4:(dh + 1) * 4, t0:t0 + tn], xh_b, [R["XT"][1]])
                    for m in range(4):
                        po, po_b = psO[no % 2]
                        no += 1
                        if lat:
                            pairs = [(YEs[:, e, c, m * 128:(m + 1) * 128], ST[:, e, c, :tn]) for e in range(NE) for c in range(4)]
                        else:
                            pairs = [(YEs[0:CAPC, e, 4, m * 128:(m + 1) * 128], ST[0:CAPC, e, 0, :tn]) for e in range(NE)]
                        _mm_group(S, po_b, po[:, :tn], pairs, [YEs_b, ST_b])
                        S.op("dve", lambda h, m=m, po=po: h.scalar_tensor_tensor(out=xh[:, m, :tn], in0=po[:, :tn], scalar=modT[:, 40 + dh * 4 + m, r:r + 1],
                                                                                 in1=xh[:, m, :tn], op0=ALU.mult, op1=ALU.add),
                             reads=[po_b, modT_b, xh_b], writes=[xh_b])
                    S.dma("sp", XTv[:, dh * 4:(dh + 1) * 4, t0:t0 + tn], xh[:, :, :tn], R["XT"][1], [xh_b])
            S.barrier()


def phase_final(K):
    nc, S, I, R = K.nc, K.S, K.I, K.R
    ones_f, ones_f_b = K.ones_f
    XTv = R["XT"][0].rearrange("(k p) t -> p k t", p=128)
    with contextlib.ExitStack() as st:
        fg, fg_b = K.sb("fg", [128, 8], F32, st)
        S.dma("sp", fg[:], I["fgT"][:, :], fg_b)
        xs = [K.sb("fx%d" % i, [128, 8, 512], F32, st) for i in range(2)]
        sq, sq_b = K.sb("fsq", [128, 8, 512], F32, st)
        rstd, rstd_b = K.sb("frstd", [128, 512], F32, st)
        ps, ps_b = K.pst("ps_fin", [128, 512], F32, st)
        for ci in range(8):
            t0 = ci * 512
            x, x_b = xs[ci % 2]
            S.dma("sp", x[:], XTv[:, :, t0:t0 + 512], x_b, [R["XT"][1]])
            S.op("act", lambda h: h.activation(out=sq[:], in_=x[:], func=AF.Square), reads=[x_b], writes=[sq_b])
            _mm_group(S, ps_b, ps[:], [(ones_f[:], sq[:, k, :]) for k in range(8)], [ones_f_b, sq_b])
            S.op("dve", lambda h: h.tensor_scalar(out=rstd[:], in0=ps[:], scalar1=1.0 / D, scalar2=EPS, op0=ALU.mult, op1=ALU.add),
                 reads=[ps_b], writes=[rstd_b])
            S.op("act", lambda h: h.activation(out=rstd[:], in_=rstd[:], func=AF.Sqrt), reads=[rstd_b], writes=[rstd_b])
            S.op("dve", lambda h: h.reciprocal(out=rstd[:], in_=rstd[:]), reads=[rstd_b], writes=[rstd_b])
            for k in range(8):
                S.op("dve", lambda h, k=k: h.scalar_tensor_tensor(out=sq[:, k, :], in0=x[:, k, :], scalar=fg[:, k:k + 1], in1=rstd[:],
                                                                  op0=ALU.mult, op1=ALU.mult), reads=[x_b, rstd_b, fg_b], writes=[sq_b])
            S.dma("sp", K.outT.rearrange("(k p) t -> p k t", p=128)[:, :, t0:t0 + 512], sq[:], K.outT_b, [sq_b])
        S.barrier()


def rope_tables():
    rows = L // 64
    row = np.repeat(np.arange(rows, dtype=np.float32), 64)
    col = np.tile(np.arange(64, dtype=np.float32), rows)
    inv = (10000.0 ** (-np.arange(16, dtype=np.float32) / 16)).astype(np.float32)
    C = np.zeros((64, L), np.float32)
    Sg = np.zeros((64, L), np.float32)
    for j in range(64):
        a, hf, f = j // 32, (j % 32) // 16, j % 16
        pos = row if a == 0 else col
        ang = (pos * inv[f]).astype(np.float32)
        C[j] = np.cos(ang)
        Sg[j] = np.sin(ang) * (-1.0 if hf == 0 else 1.0)
    return np.concatenate([C, C], 0), np.concatenate([Sg, Sg], 0)


_HC = {}


def hyena_consts():
    if _HC:
        return _HC
    f32 = np.float32

    def emb_of(Lx):
        t = np.linspace(0.0, 1.0, Lx, dtype=f32)[:, None]
        w = (2.0 * math.pi * np.arange(Lx, dtype=f32)[:, None] / Lx).astype(f32)
        f = np.linspace(1e-4, 15, 16, dtype=f32)[None, :]
        e = np.concatenate([t, np.cos(f * w), -np.sin(f * w)], axis=-1).astype(f32)
        return np.ascontiguousarray(e.T), t[:, 0]
    eT, t = emb_of(L)
    eTc, tc = emb_of(LC)
    _HC["embT"], _HC["embTc"] = eT, eTc
    _HC["negt"] = np.ascontiguousarray((-t).reshape(L // 128, 128).T)
    _HC["negtc"] = np.ascontiguousarray((-tc).reshape(LC // 128, 128).T)
    _HC["deltas"] = np.abs(np.linspace(math.log(1e-2) / 0.3, math.log(1e-2) / 1.5, D, dtype=f32)).reshape(1, D).astype(f32)

    def dft(Lx, npad):
        N = 2 * Lx
        a = np.arange(Lx + 1, dtype=np.int64)
        ph = (a[:, None] * a[None, :]) % N
        ang = ph.astype(np.float64) * (2.0 * math.pi / N)
        C = np.zeros((npad, npad), f32)
        S_ = np.zeros((npad, npad), f32)
        C[:Lx + 1, :Lx + 1] = np.cos(ang)
        S_[:Lx + 1, :Lx + 1] = np.sin(ang)
        wfv = np.zeros(npad, f32)
        wfv[:Lx + 1] = 2.0 / N
        wfv[0] = 1.0 / N
        wfv[Lx] = 1.0 / N
        return C.astype(ml_dtypes.bfloat16), S_.astype(ml_dtypes.bfloat16), np.ascontiguousarray(wfv.reshape(npad // 128, 128).T)
    _HC["dftC"], _HC["dftS"], _HC["wf"] = dft(L, NF)
    _HC["dftCc"], _HC["dftSc"], _HC["wfc"] = dft(LC, NFC)
    return _HC


def host_inputs(inputs, b, nl):
    x, c, ctx, c_ctx = inputs["x"], inputs["c"], inputs["ctx"], inputs["c_ctx"]
    m = {}
    m["xT"] = np.ascontiguousarray(np.concatenate([x[b], ctx[b]], 0).T)
    cc = np.stack([c[b], c_ctx], 0)
    m["ccT"] = np.ascontiguousarray(cc.reshape(2, 8, 128).transpose(2, 1, 0))
    m["w_mod"] = np.ascontiguousarray(inputs["w_mod"][:nl])
    m["b_modT"] = np.ascontiguousarray(inputs["b_mod"][:nl].reshape(nl, 48, 128).transpose(0, 2, 1))
    m["g1T"] = np.ascontiguousarray(inputs["norm1_g"][:nl].reshape(nl, 8, 128).transpose(0, 2, 1))
    m["g2T"] = np.ascontiguousarray(inputs["norm2_g"][:nl].reshape(nl, 8, 128).transpose(0, 2, 1))
    m["w_in"] = np.ascontiguousarray(inputs["w_in"][:nl])
    rc, rs = rope_tables()
    m["ropeC"], m["ropeS"] = rc, rs
    m["ident"] = np.eye(128, dtype=np.float32)
    m["diff_lambda"] = np.ascontiguousarray(inputs["diff_lambda"][:nl].reshape(nl, 256))
    m["sublnT"] = np.ascontiguousarray(inputs["diff_subln_g"][:nl].reshape(nl, 128, 1))
    m["swa_sink"] = np.ascontiguousarray(inputs["swa_sink"][:nl])
    kk, qq = np.meshgrid(np.arange(128), np.arange(128), indexing="ij")
    m["trilo"] = (kk >= qq).astype(np.float32)
    m["triup"] = (kk <= qq).astype(np.float32)
    m["w_branch"] = np.ascontiguousarray(inputs["w_branch"][:nl])
    m["w_out"] = np.ascontiguousarray(inputs["w_out"][:nl])
    m["fgT"] = np.ascontiguousarray(inputs["final_g"].reshape(8, 128).T)
    if "hy_ff_w1" in inputs:
        cwv = inputs["hy_conv_w"][:nl]
        m["cwT"] = np.ascontiguousarray(cwv.reshape(nl, 3, 24, 128).transpose(0, 3, 2, 1))
        m["cbT"] = np.ascontiguousarray(inputs["hy_conv_b"][:nl].reshape(nl, 24, 128).transpose(0, 2, 1))
        m["hbT"] = np.ascontiguousarray(inputs["hy_bias"][:nl].reshape(nl, 8, 128).transpose(0, 2, 1))
        m["hy_w1"] = np.ascontiguousarray(inputs["hy_ff_w1"][:nl])
        m["hy_w2"] = np.ascontiguousarray(inputs["hy_ff_w2"][:nl])
        m["hy_w3"] = np.ascontiguousarray(inputs["hy_ff_w3"][:nl])
        m["hy_b1"] = np.ascontiguousarray(inputs["hy_ff_b1"][:nl].reshape(nl, 64, 1))
        m["hy_b2"] = np.ascontiguousarray(inputs["hy_ff_b2"][:nl].reshape(nl, 64, 1))
        m["hy_fr"] = np.ascontiguousarray(inputs["hy_sin_freq"][:nl].reshape(nl, 64, 1))
        m.update(hyena_consts())
    if "moe_w1" in inputs:
        m["router_w"] = np.ascontiguousarray(inputs["router_w"][:nl])
        for k in ("moe_w1", "moe_w3", "moe_w2"):
            m[k] = np.ascontiguousarray(inputs[k][:nl])
        sel = np.zeros((16, 16, 128), np.float32)
        for e in range(16):
            sel[e, e, :] = 1.0
        m["selm"] = sel.reshape(16, 16 * 128)
        si = np.zeros((128, 5), np.float32)
        for c in range(4):
            si[:, c] = np.arange(128) + 128 * c
        si[:, 4] = np.arange(128)
        m["slotidx"] = si
        m["iota512"] = np.tile(np.arange(512, dtype=np.float32)[None, :], (128, 1))
        m["ustrict"] = (kk < qq).astype(np.float32)
    return m


def kernel(**inputs):
    inputs = {k: np.asarray(v) for k, v in inputs.items()}
    nb = inputs["x"].shape[0]
    nc = build(dict(nl=DEPTH, phases=("diff", "swa", "hy", "merge", "moe")))
    in_maps = [host_inputs(inputs, b, DEPTH) for b in range(nb)]
    res = run_bass_kernel_spmd(nc, in_maps, core_ids=list(range(nb)))
    out = np.stack([np.asarray(res.results[b]["outT"]).T for b in range(nb)], 0)
    return np.ascontiguousarray(out.astype(np.float32))
```
